# Optimizing a Trainium2 kernel written in Bass

```python
import math
import jax, jax.numpy as jnp
from jax import lax
import numpy as np

D_MODEL = 1024
BATCH = 2
SEQ = 16384
DEPTH = 1

CHUNK = 64
RET_HEADS = 8
RET_QK_DIM = 64
RET_V_DIM = 128
DSA_HEADS = 8
DSA_HEAD_DIM = 64
DSA_KV_LATENT = 128
IDX_HEADS = 8
IDX_DIM = 64
IDX_TOPK = 256
Q_BLOCK = 128
N_BUCKETS = 32
MAX_DISTANCE = 128
N_GROUPS = 4
EXPERTS_PER_GROUP = 4
N_EXPERTS = N_GROUPS * EXPERTS_PER_GROUP
EXPERT_FF = 256
EXPERT_TOPK = 2
MOE_BLOCK = 4096
ROPE_BASE = 10000.0
NORM_EPS = 1e-6
GN_EPS = 1e-5
IN_SPLITS = (RET_HEADS * RET_QK_DIM, RET_HEADS * RET_QK_DIM, RET_HEADS * RET_V_DIM, RET_HEADS * RET_V_DIM,
             DSA_HEADS * DSA_HEAD_DIM, DSA_KV_LATENT, IDX_HEADS * IDX_DIM, IDX_DIM, IDX_HEADS)
D_IN = sum(IN_SPLITS)

kernel_name = "chunk_causal_retention_dsa_hmoe_block"


def _split_cols(z, sizes):
    offs, acc = [], 0
    for s in sizes[:-1]:
        acc += s
        offs.append(acc)
    return jnp.split(z, offs, axis=-1)


def rmsnorm(x, g):
    xf = x.astype(jnp.float32)
    y = xf * lax.rsqrt(jnp.mean(xf * xf, axis=-1, keepdims=True) + NORM_EPS)
    return (y * g.astype(jnp.float32)).astype(x.dtype)


def modulate(xn, shift, scale):
    return xn * (1.0 + scale[:, None, :]) + shift[:, None, :]


def rope(x, pos):
    d = x.shape[-1]
    freqs = ROPE_BASE ** (-jnp.arange(0, d, 2, dtype=jnp.float32) / d)
    ang = pos.astype(jnp.float32)[:, None] * freqs[None, :]
    cos = jnp.cos(ang)[None, :, None, :]
    sin = jnp.sin(ang)[None, :, None, :]
    x1, x2 = jnp.split(x.astype(jnp.float32), 2, axis=-1)
    return jnp.concatenate([x1 * cos - x2 * sin, x2 * cos + x1 * sin], axis=-1).astype(x.dtype)


def rel_bucket(rel):
    nb = N_BUCKETS // 2
    ret = (rel > 0).astype(jnp.int32) * nb
    n = jnp.abs(rel)
    max_exact = nb // 2
    nf = jnp.maximum(n, 1).astype(jnp.float32)
    large = max_exact + (jnp.log(nf / max_exact) / math.log(MAX_DISTANCE / max_exact)
                         * (nb - max_exact)).astype(jnp.int32)
    large = jnp.minimum(large, nb - 1)
    return ret + jnp.where(n < max_exact, n, large)


def retention_branch(q, k, v, g, gn_gain):
    B, S, H, dk = q.shape
    dv = v.shape[-1]
    N = S // CHUNK
    dt = q.dtype
    log_gamma = jnp.log(1.0 - 2.0 ** (-5.0 - jnp.arange(H, dtype=jnp.float32)))
    idx = jnp.arange(CHUNK, dtype=jnp.float32)
    intra_decay = jnp.exp(log_gamma[:, None, None] * jnp.abs(idx[:, None] - idx[None, :])).astype(dt)
    k_decay = jnp.exp(log_gamma[:, None] * (CHUNK - 1 - idx)[None, :]).astype(dt)
    q_decay = jnp.exp(log_gamma[:, None] * (idx + 1)[None, :]).astype(dt)
    chunk_decay = jnp.exp(log_gamma * CHUNK)
    qc = q.reshape(B, N, CHUNK, H, dk)
    kc = k.reshape(B, N, CHUNK, H, dk)
    vc = v.reshape(B, N, CHUNK, H, dv)
    scores = jnp.einsum('bnihd,bnjhd->bhnij', qc, kc) * intra_decay[None, :, None]
    o_intra = jnp.einsum('bhnij,bnjhe->bnihe', scores, vc)
    kv = jnp.einsum('bnjhd,hj,bnjhe->nbhde', kc, k_decay, vc)
    cd = chunk_decay.astype(kv.dtype)[None, :, None, None]

    def step(state, kv_n):
        return state * cd + kv_n, state

    _, prev = lax.scan(step, jnp.zeros((B, H, dk, dv), kv.dtype), kv)
    o_inter = jnp.einsum('bnihd,hi,nbhde->bnihe', qc, q_decay, prev)
    o = (o_intra + o_inter).reshape(B, S, H, dv).astype(jnp.float32)
    mu = jnp.mean(o, axis=-1, keepdims=True)
    var = jnp.mean(jnp.square(o - mu), axis=-1, keepdims=True)
    o = ((o - mu) * lax.rsqrt(var + GN_EPS)).reshape(B, S, H * dv) * gn_gain.astype(jnp.float32)
    return (jax.nn.silu(g.astype(jnp.float32)) * o).astype(dt)


def dsa_branch(q, k, v, qi, kidx, wi, rel_bias):
    B, S, H, dh = q.shape
    topk = min(IDX_TOPK, S // 4)
    nblk = S // Q_BLOCK
    key_chunk = jnp.arange(S, dtype=jnp.int32) // CHUNK
    idx_scale = (IDX_DIM ** -0.5) * (IDX_HEADS ** -0.5)
    kidx_f = kidx.astype(jnp.float32)

    def to_blocks(a):
        return a.reshape((B, nblk, Q_BLOCK) + a.shape[2:]).swapaxes(0, 1)

    def one_block(args):
        qb, qib, wb, start = args
        qpos = start + jnp.arange(Q_BLOCK, dtype=jnp.int32)
        qchunk = qpos // CHUNK
        s = jnp.einsum('bqhd,bkd->bqhk', qib.astype(jnp.float32), kidx_f)
        score = jnp.einsum('bqhk,bqh->bqk', jax.nn.relu(s), wb.astype(jnp.float32)) * idx_scale
        admissible = key_chunk[None, :] <= qchunk[:, None]
        score = jnp.where(admissible[None], score, -jnp.inf)
        _, sel = lax.top_k(score, topk)
        valid = key_chunk[sel] <= qchunk[None, :, None]
        kg = jax.vmap(lambda a, i: a[i])(k, sel)
        vg = jax.vmap(lambda a, i: a[i])(v, sel)
        logits = jnp.einsum('bqhd,bqkd->bqhk', qb, kg).astype(jnp.float32) * (dh ** -0.5)
        bias = rel_bias[rel_bucket(sel - qpos[None, :, None])].astype(jnp.float32)
        logits = logits + jnp.transpose(bias, (0, 1, 3, 2))
        logits = jnp.where(valid[:, :, None, :], logits, -jnp.inf)
        p = jax.nn.softmax(logits, axis=-1).astype(vg.dtype)
        return jnp.einsum('bqhk,bqkd->bqhd', p, vg)

    starts = jnp.arange(nblk, dtype=jnp.int32) * Q_BLOCK
    out = lax.map(one_block, (to_blocks(q), to_blocks(qi), to_blocks(wi), starts))
    return out.swapaxes(0, 1).reshape(B, S, H * dh)


def hier_moe(u, w_gr, b_gr, w_er, b_er, w1, w3, w2):
    B, S, D = u.shape
    T = B * S
    t = u.reshape(T, D)
    gl = (t @ w_gr + b_gr).astype(jnp.float32)
    pg = jax.nn.softmax(gl, axis=-1)
    gsel = jnp.argmax(gl, axis=-1)
    gp = jnp.take_along_axis(pg, gsel[:, None], axis=1)[:, 0]
    el = (t @ w_er + b_er).astype(jnp.float32).reshape(T, N_GROUPS, EXPERTS_PER_GROUP)
    el_sel = jnp.take_along_axis(el, gsel[:, None, None], axis=1)[:, 0]
    vals, ids = lax.top_k(el_sel, EXPERT_TOPK)
    wts = jax.nn.softmax(vals, axis=-1) * gp[:, None]
    eid = gsel[:, None].astype(jnp.int32) * EXPERTS_PER_GROUP + ids
    gates = jnp.einsum('tke,tk->te', jax.nn.one_hot(eid, N_EXPERTS, dtype=jnp.float32), wts).astype(u.dtype)
    blk = math.gcd(T, MOE_BLOCK)
    nb = T // blk

    def expert_block(args):
        xb, gb = args
        a = jnp.einsum('td,edf->tef', xb, w1)
        b = jnp.einsum('td,edf->tef', xb, w3)
        hid = jax.nn.silu(a) * b * gb[:, :, None]
        return jnp.einsum('tef,efd->td', hid, w2)

    y = lax.map(expert_block, (t.reshape(nb, blk, D), gates.reshape(nb, blk, N_EXPERTS)))
    return y.reshape(B, S, D)


def setup_inputs(seed: int = 0) -> dict:
    key = jax.random.key(seed)
    ks = jax.random.split(key, 24)
    f = jnp.float32
    L, D = DEPTH, D_MODEL
    RV = RET_HEADS * RET_V_DIM
    DV = DSA_HEADS * DSA_HEAD_DIM

    def nrm(k, shape, scale):
        return jax.random.normal(k, shape, f) * scale

    def gain(k, shape):
        return 1.0 + 0.05 * jax.random.normal(k, shape, f)

    return {
        "x": nrm(ks[0], (BATCH, SEQ, D), 1.0),
        "c": nrm(ks[1], (BATCH, D), 1.0),
        "w_ada": nrm(ks[2], (L, D, 6 * D), 0.5 * D ** -0.5),
        "b_ada": nrm(ks[3], (L, 6 * D), 0.01),
        "norm_mix_g": gain(ks[4], (L, D)),
        "w_in": nrm(ks[5], (L, D, D_IN), D ** -0.5),
        "ret_gn_g": gain(ks[6], (L, RV)),
        "dsa_kv_norm_g": gain(ks[7], (L, DSA_KV_LATENT)),
        "w_dsa_kv_up": nrm(ks[8], (L, DSA_KV_LATENT, 2 * DSA_HEAD_DIM), DSA_KV_LATENT ** -0.5),
        "rel_bias": nrm(ks[9], (N_BUCKETS, DSA_HEADS), 0.5),
        "w_ret_out": nrm(ks[10], (L, RV, D), RV ** -0.5),
        "w_dsa_out": nrm(ks[11], (L, DV, D), DV ** -0.5),
        "w_gate": nrm(ks[12], (L, D, 2 * D), D ** -0.5),
        "b_gate": nrm(ks[13], (L, 2 * D), 0.01),
        "w_o": nrm(ks[14], (L, D, D), D ** -0.5),
        "norm_ffn_g": gain(ks[15], (L, D)),
        "w_group_router": nrm(ks[16], (L, D, N_GROUPS), D ** -0.5),
        "b_group_router": nrm(ks[17], (L, N_GROUPS), 0.01),
        "w_expert_router": nrm(ks[18], (L, D, N_EXPERTS), D ** -0.5),
        "b_expert_router": nrm(ks[19], (L, N_EXPERTS), 0.01),
        "w_exp_gate": nrm(ks[20], (L, N_EXPERTS, D, EXPERT_FF), D ** -0.5),
        "w_exp_up": nrm(ks[21], (L, N_EXPERTS, D, EXPERT_FF), D ** -0.5),
        "w_exp_down": nrm(ks[22], (L, N_EXPERTS, EXPERT_FF, D), EXPERT_FF ** -0.5),
        "norm_final_g": gain(ks[23], (D,)),
    }


def reference(x, c, w_ada, b_ada, norm_mix_g, w_in, ret_gn_g, dsa_kv_norm_g, w_dsa_kv_up, rel_bias,
              w_ret_out, w_dsa_out, w_gate, b_gate, w_o, norm_ffn_g, w_group_router, b_group_router,
              w_expert_router, b_expert_router, w_exp_gate, w_exp_up, w_exp_down, norm_final_g):
    B, S, D = x.shape
    pos = jnp.arange(S, dtype=jnp.int32)
    h = x
    for l in range(DEPTH):
        mod = jax.nn.silu(c) @ w_ada[l] + b_ada[l]
        sh1, sc1, g1, sh2, sc2, g2 = jnp.split(mod, 6, axis=-1)
        u = modulate(rmsnorm(h, norm_mix_g[l]), sh1, sc1)
        z = u @ w_in[l]
        rq, rk, rv, rg, dq, dkv, iq, ik, iw = _split_cols(z, IN_SPLITS)
        rq = rope(rq.reshape(B, S, RET_HEADS, RET_QK_DIM), pos)
        rk = rope(rk.reshape(B, S, RET_HEADS, RET_QK_DIM), pos) * (RET_QK_DIM ** -0.5)
        y_ret = retention_branch(rq, rk, rv.reshape(B, S, RET_HEADS, RET_V_DIM), rg, ret_gn_g[l]) @ w_ret_out[l]
        lat = rmsnorm(dkv, dsa_kv_norm_g[l]) @ w_dsa_kv_up[l]
        dk, dv = jnp.split(lat, 2, axis=-1)
        y_dsa = dsa_branch(dq.reshape(B, S, DSA_HEADS, DSA_HEAD_DIM), dk, dv,
                           iq.reshape(B, S, IDX_HEADS, IDX_DIM), ik, iw, rel_bias) @ w_dsa_out[l]
        ga, gb = jnp.split(jax.nn.sigmoid(u @ w_gate[l] + b_gate[l]), 2, axis=-1)
        h = h + g1[:, None, :] * ((ga * y_ret + gb * y_dsa) @ w_o[l])
        u2 = modulate(rmsnorm(h, norm_ffn_g[l]), sh2, sc2)
        h = h + g2[:, None, :] * hier_moe(u2, w_group_router[l], b_group_router[l], w_expert_router[l],
                                         b_expert_router[l], w_exp_gate[l], w_exp_up[l], w_exp_down[l])
    return rmsnorm(h, norm_final_g)
```

```python
import contextlib
import math
import numpy as np
import concourse.bass as bass
import concourse.mybir as mybir
from concourse.bass_utils import run_bass_kernel_spmd

F32 = mybir.dt.float32
BF16 = mybir.dt.bfloat16
AF = mybir.ActivationFunctionType
ALU = mybir.AluOpType
AX = mybir.AxisListType

D = 1024
H = 8
NEG = -1.0e30
NBIS = 11
GAM = [1.0 - 2.0 ** (-5.0 - h) for h in range(H)]


class Sched:
    ENGS = ("pe", "act", "dve", "pool", "sp")
    ROT = 3000

    def __init__(self, nc, stack):
        self.nc = nc
        self.stack = stack
        self.ops = {e: [] for e in self.ENGS}
        self.last_write = {}
        self.readers = {}
        self.nsem = 0
        self.cur = {}
        self.dma = {}
        self.seen = {e: {} for e in self.ENGS}
        self.pe_sems = set()
        self.latest = {}

    def _newsem(self, name):
        self.nsem += 1
        return self.stack.enter_context(self.nc.semaphore(f"s{self.nsem}_{name}"))

    def _tok_compute(self, eng):
        c = self.cur.get(eng)
        if c is None or c[1] >= self.ROT:
            c = [self._newsem(eng), 0]
            self.cur[eng] = c
            if eng == "pe":
                self.pe_sems.add(id(c[0]))
        c[1] += 1
        return (c[0], c[1])

    def _tok_dma(self, key):
        c = self.dma.get(key)
        if c is None or c[1] >= 16 * 1500:
            c = [self._newsem("d"), 0]
            self.dma[key] = c
        c[1] += 16
        return (c[0], c[1])

    def op(self, eng, fn, reads=(), writes=(), dma_key=None, extra=()):
        deps = {}

        def add(tok):
            if tok is None:
                return
            s, v = tok
            k = id(s)
            if k not in deps or deps[k][1] < v:
                deps[k] = (s, v)

        for r in reads:
            add(self.last_write.get(r))
        for w in writes:
            add(self.last_write.get(w))
            for t in self.readers.get(w, ()):
                add(t)
        for t in extra:
            add(t)
        if fn is None:
            tok = None
        elif dma_key is not None:
            tok = self._tok_dma(dma_key)
        else:
            tok = self._tok_compute(eng)
        waits = []
        seen = self.seen[eng]
        for k, (s, v) in deps.items():
            if eng == "pe" and dma_key is None and k in self.pe_sems:
                continue
            if seen.get(k, 0) >= v:
                continue
            seen[k] = v
            waits.append((s, v))
        if tok is not None:
            self.latest[id(tok[0])] = tok
            for r in reads:
                lst = self.readers.setdefault(r, [])
                lst.append(tok)
                if len(lst) > 16:
                    best = {}
                    for (s, v) in lst:
                        if id(s) not in best or best[id(s)][1] < v:
                            best[id(s)] = (s, v)
                    self.readers[r] = list(best.values())
            for w in writes:
                self.last_write[w] = tok
                self.readers[w] = []
        self.ops[eng].append((fn, waits, tok, dma_key is not None))
        return tok

    def barrier(self):
        toks = list(self.latest.values())
        for e in self.ENGS:
            self.op(e, None, extra=toks)

    def emit(self):
        nc = self.nc
        ops = self.ops
        self.ops = {e: [] for e in self.ENGS}
        with nc.Block() as block:
            def run(engobj, name):
                for fn, waits, tok, is_dma in ops[name]:
                    for s, v in waits:
                        engobj.wait_ge(s, v)
                    if fn is not None:
                        ins = fn(engobj)
                        ins.then_inc(tok[0], 16 if is_dma else 1)

            @block.tensor
            def _(e):
                run(e, "pe")

            @block.scalar
            def _(e):
                run(e, "act")

            @block.vector
            def _(e):
                run(e, "dve")

            @block.gpsimd
            def _(e):
                run(e, "pool")

            @block.sync
            def _(e):
                run(e, "sp")


C_RQ, C_RK, C_RV, C_RG, C_DQ, C_DKV, C_IQ, C_IK, C_IW = 0, 512, 1024, 2048, 3072, 3584, 3712, 4224, 4288
D_IN = 4296


def build(NG, dbg=False):
    NT = 4 * NG
    SB = NT * 128
    NO = NG * 128
    nc = bass.Bass("TRN2", target_bir_lowering=False)

    def din(name, shape, dt=F32):
        return nc.dram_tensor(name, list(shape), dt, kind="ExternalInput").ap()

    def dscr(name, shape, dt):
        if dbg:
            return nc.dram_tensor(name, list(shape), dt, kind="ExternalOutput").ap()
        return nc.dram_tensor(name, list(shape), dt).ap()

    xb = din("xb", [SB, D]); xo = din("xo", [NO, D]); ccol = din("ccol", [128, 8])
    w_ada = din("w_ada", [D, 6 * D]); b_adaT = din("b_adaT", [128, 48]); b_g12 = din("b_g12", [128, 2 * D])
    gmixT = din("gmixT", [128, 8]); gffnT = din("gffnT", [128, 8])
    gfin_rep = din("gfin_rep", [128, D]); gn_rep = din("gn_rep", [128, D]); kvg_rep = din("kvg_rep", [128, 128])
    w_in = din("w_in", [D, D_IN]); w_kv = din("w_kv", [128, 128])
    w_ret_out = din("w_ret_out", [D, D]); w_dsa_out = din("w_dsa_out", [512, D])
    w_gate = din("w_gate", [D, 2 * D]); b_gate = din("b_gate", [1, 2 * D]); w_o = din("w_o", [D, D])
    w_rt = din("w_rt", [D, 20]); b_rt_rep = din("b_rt_rep", [128, 20])
    w1 = din("w1", [16, D, 256]); w3 = din("w3", [16, D, 256]); w2 = din("w2", [16, 256, D])
    rope_all = din("rope_all", [SB, 64]); rope_own = din("rope_own", [NO, 64])
    ident = din("ident", [128, 128]); DTd = din("DT", [128, H, 128]); qdec = din("qdec", [128, H])
    wtile = din("wtile", [128, H]); seld = din("sel", [64, 4]); maddd = din("madd", [128, 512])
    biasTd = din("biasT", [5, 128, H, 128]); c15 = din("c15_rep", [128, H]); seldend = din("selden", [65, 64])
    out = nc.dram_tensor("out", [NO, D], F32, kind="ExternalOutput").ap()

    uT_s = dscr("uT_s", [NG, 128, 8, 128], BF16)
    roT_s = dscr("roT_s", [NG, 128, 8, 128], BF16)
    dqT_s = dscr("dqT_s", [NG, 64, H, 128], BF16)
    iqT_s = dscr("iqT_s", [NG, 64, H, 128], BF16)
    iw_s = dscr("iw_s", [NG, 128, H], F32)
    ydT_s = dscr("ydT_s", [NG, 64, H, 128], BF16)
    h1_s = dscr("h1_s", [NG, 128, D], F32)
    if dbg:
        kT_dbg = dscr("kT_dbg", [128, SB], BF16)
        dv_dbg = dscr("dv_dbg", [128, NT, 65], BF16)
        thr_dbg = dscr("thr_dbg", [NG, 128, 8], F32)
        sc_dbg = dscr("sc_dbg", [NG, 128, SB], BF16)
        mk_dbg = dscr("mk_dbg", [NG, 128, SB], BF16)

    with contextlib.ExitStack() as top:
        S = Sched(nc, top)

        def sbt(st, n, sh, dt):
            return st.enter_context(nc.sbuf_tensor("sb_" + n, list(sh), dt))

        def pst(st, n, sh, dt):
            return st.enter_context(nc.psum_tensor("ps_" + n, list(sh), dt))

        V = lambda fn, r, w: S.op("dve", fn, reads=r, writes=w)
        A = lambda fn, r, w: S.op("act", fn, reads=r, writes=w)
        P = lambda fn, r, w: S.op("pe", fn, reads=r, writes=w)
        G = lambda fn, r, w: S.op("pool", fn, reads=r, writes=w)
        DM = lambda fn, r, w, k: S.op("sp", fn, reads=r, writes=w, dma_key=k)
        DG = lambda fn, r, w, k: S.op("pool", fn, reads=r, writes=w, dma_key=k)

        idb = sbt(top, "idb", [128, 128], BF16)
        epsc = sbt(top, "epsc", [128, 2], F32)
        G1T = sbt(top, "G1T", [128, 8], F32); sh1T = sbt(top, "sh1T", [128, 8], F32)
        G2T = sbt(top, "G2T", [128, 8], F32); sh2T = sbt(top, "sh2T", [128, 8], F32)
        g1rep = sbt(top, "g1rep", [128, D], BF16); g2rep = sbt(top, "g2rep", [128, D], BF16)
        DG(lambda e: e.dma_start(out=idb[:], in_=ident), [], ["idb"], "c0")
        V(lambda e: e.memset(epsc[:, 0:1], 1e-6), [], ["epsc"])
        V(lambda e: e.memset(epsc[:, 1:2], 1e-5), [], ["epsc"])

        def norm_T(x_ap, xres, junk, xs, pT, uT, stat, GT, shT, tag, ures=None, offload=False):
            ures = ures or (tag + "uT")
            A(lambda e: e.activation(out=junk, in_=x_ap, func=AF.Square, scale=1.0 / 32.0, accum_out=stat[:, 0:1]),
              [xres], [tag + "junk", tag + "st"])
            A(lambda e: e.activation(out=stat[:, 1:2], in_=stat[:, 0:1], func=AF.Ln, bias=epsc[:, 0:1]),
              [tag + "st", "epsc"], [tag + "st"])
            A(lambda e: e.activation(out=stat[:, 2:3], in_=stat[:, 1:2], func=AF.Exp, scale=-0.5),
              [tag + "st"], [tag + "st"])
            if offload:
                G(lambda e: e.tensor_scalar(out=xs, in0=x_ap, scalar1=stat[:, 2:3], scalar2=None, op0=ALU.mult),
                  [xres, tag + "st"], [tag + "xs"])
            else:
                V(lambda e: e.tensor_scalar(out=xs, in0=x_ap, scalar1=stat[:, 2:3], scalar2=None, op0=ALU.mult),
                  [xres, tag + "st"], [tag + "xs"])
            for k in range(8):
                P(lambda e, k=k: e.transpose(out=pT[:, k, :], in_=xs[:, k * 128:(k + 1) * 128], identity=idb[:]),
                  [tag + "xs", "idb"], ["pT"])
            for k in range(8):
                if offload:
                    A(lambda e, k=k: e.activation(out=uT[:, k, :], in_=pT[:, k, :], func=AF.Identity, scale=GT[:, k:k + 1], bias=shT[:, k:k + 1]),
                      ["pT", "mod"], [ures])
                else:
                    V(lambda e, k=k: e.tensor_scalar(out=uT[:, k, :], in0=pT[:, k, :], scalar1=GT[:, k:k + 1],
                                                     scalar2=shT[:, k:k + 1], op0=ALU.mult, op1=ALU.add),
                      ["pT", "mod"], [ures])

        def rope(src, cosb, sinb, dst, ta, tb, rres, wres, tag):
            s1, s2 = src[:, :, 0:32], src[:, :, 32:64]
            V(lambda e: e.tensor_tensor(out=ta, in0=s1, in1=cosb, op=ALU.mult), rres, [tag + "ta"])
            V(lambda e: e.tensor_tensor(out=tb, in0=s2, in1=sinb, op=ALU.mult), rres, [tag + "tb"])
            V(lambda e: e.tensor_tensor(out=dst[:, :, 0:32], in0=ta, in1=tb, op=ALU.subtract), [tag + "ta", tag + "tb"], wres)
            V(lambda e: e.tensor_tensor(out=ta, in0=s2, in1=cosb, op=ALU.mult), rres, [tag + "ta"])
            V(lambda e: e.tensor_tensor(out=tb, in0=s1, in1=sinb, op=ALU.mult), rres, [tag + "tb"])
            V(lambda e: e.tensor_tensor(out=dst[:, :, 32:64], in0=ta, in1=tb, op=ALU.add), [tag + "ta", tag + "tb"], wres)

        with contextlib.ExitStack() as ph:
            cs = sbt(ph, "cs", [128, 8], F32); csb = sbt(ph, "csb", [128, 8], BF16)
            csbc = sbt(ph, "csbc", [128, 8, 128], BF16)
            wa = [sbt(ph, f"wa{i}", [128, 8, D], BF16) for i in range(2)]
            badT = sbt(ph, "badT", [128, 48], F32); bg12 = sbt(ph, "bg12", [128, 2 * D], F32)
            gmx = sbt(ph, "gmx", [128, 8], F32); gff = sbt(ph, "gff", [128, 8], F32)
            modT = sbt(ph, "modT", [128, 32], F32)
            psm = pst(ph, "psm", [128, 32], F32)
            pg = pst(ph, "pg", [128, D], F32)
            DM(lambda e: e.dma_start(out=cs[:], in_=ccol), [], ["cs"], "c1_cs")
            DM(lambda e: e.dma_start(out=badT[:], in_=b_adaT), [], ["badT"], "c1_badT")
            DM(lambda e: e.dma_start(out=bg12[:], in_=b_g12), [], ["bg12"], "c1_bg12")
            DM(lambda e: e.dma_start(out=gmx[:], in_=gmixT), [], ["gmx"], "c1_gmx")
            DM(lambda e: e.dma_start(out=gff[:], in_=gffnT), [], ["gff"], "c1_gff")
            A(lambda e: e.activation(out=csb[:], in_=cs[:], func=AF.Silu), ["cs"], ["csb"])
            V(lambda e: e.tensor_copy(out=csbc[:], in_=csb[:].unsqueeze(2).broadcast_to([128, 8, 128])), ["csb"], ["csbc"])
            fm_i = 0
            for p in range(6):
                sl = p % 2
                for k in range(8):
                    DG(lambda e, p=p, k=k, sl=sl: e.dma_start(out=wa[sl][:, k, :], in_=w_ada[k * 128:(k + 1) * 128, p * D:(p + 1) * D]),
                       [], [f"wa{sl}"], f"wa{sl}")
                if p in (2, 5):
                    for hf in range(2):
                        for k in range(8):
                            P(lambda e, k=k, hf=hf, sl=sl: e.matmul(pg[:, hf * 512:(hf + 1) * 512], lhsT=csbc[:, k, :],
                                                                  rhs=wa[sl][:, k, hf * 512:(hf + 1) * 512], start=(k == 0), stop=(k == 7)),
                              ["csbc", f"wa{sl}"], ["pg"])
                    dst = g1rep if p == 2 else g2rep
                    off = 0 if p == 2 else D
                    V(lambda e, dst=dst, off=off: e.tensor_tensor(out=dst[:], in0=pg[:], in1=bg12[:, off:off + D], op=ALU.add),
                      ["pg", "bg12"], ["mod"])
                else:
                    for f in range(8):
                        for k in range(8):
                            P(lambda e, k=k, f=f, sl=sl, c=fm_i * 8 + f: e.matmul(psm[:, c:c + 1], lhsT=wa[sl][:, k, f * 128:(f + 1) * 128],
                                                                                 rhs=csb[:, k:k + 1], start=(k == 0), stop=(k == 7)),
                              ["csb", f"wa{sl}"], ["psm"])
                    fm_i += 1
            for i, p in enumerate((0, 1, 3, 4)):
                V(lambda e, i=i, p=p: e.tensor_tensor(out=modT[:, i * 8:(i + 1) * 8], in0=psm[:, i * 8:(i + 1) * 8],
                                                      in1=badT[:, p * 8:(p + 1) * 8], op=ALU.add), ["psm", "badT"], ["modT"])
            V(lambda e: e.tensor_copy(out=sh1T[:], in_=modT[:, 0:8]), ["modT"], ["mod"])
            V(lambda e: e.tensor_copy(out=sh2T[:], in_=modT[:, 16:24]), ["modT"], ["mod"])
            V(lambda e: e.scalar_tensor_tensor(out=G1T[:], in0=modT[:, 8:16], scalar=1.0, in1=gmx[:], op0=ALU.add, op1=ALU.mult),
              ["modT", "gmx"], ["mod"])
            V(lambda e: e.scalar_tensor_tensor(out=G2T[:], in0=modT[:, 24:32], scalar=1.0, in1=gff[:], op0=ALU.add, op1=ALU.mult),
              ["modT", "gff"], ["mod"])
            S.barrier()
            S.emit()

        kvs = contextlib.ExitStack()
        kT = sbt(kvs, "kT", [128, SB], BF16)
        dv1 = sbt(kvs, "dv1", [128, NT, 65], BF16)

        with contextlib.ExitStack() as ph:
            w_in_sb = sbt(ph, "w_in_sb", [128, 8, D_IN], BF16)
            wkv_sb = sbt(ph, "wkv_sb", [128, 192], BF16)
            kvg = sbt(ph, "kvg", [128, 128], F32); gnr = sbt(ph, "gnr", [128, D], F32)
            DTs = sbt(ph, "DTs", [128, H, 128], BF16); qdc = sbt(ph, "qdc", [128, H], F32)
            wtl = sbt(ph, "wtl", [128, H], F32); sels = sbt(ph, "sels", [64, 4], F32)
            xt = [sbt(ph, f"xt{i}", [128, D], F32) for i in range(2)]
            rp = [sbt(ph, f"rp{i}", [128, 64], F32) for i in range(2)]
            junk = sbt(ph, "junk", [128, D], BF16)
            xs = sbt(ph, "xs", [128, D], BF16)
            uT = [sbt(ph, f"uT{i}", [128, 8, 128], BF16) for i in range(2)]
            stat = sbt(ph, "stat", [128, 8], F32)
            ta = sbt(ph, "ta", [128, H, 32], F32); tb = sbt(ph, "tb", [128, H, 32], F32)
            krp = sbt(ph, "krp", [128, H, 64], F32); kw = sbt(ph, "kw", [128, H, 64], BF16)
            vb = sbt(ph, "vb", [128, D], BF16)
            Sst = sbt(ph, "Sst", [64, H, 128], F32); T0 = sbt(ph, "T0", [64, H, 128], F32)
            T0b = sbt(ph, "T0b", [64, H, 128], BF16)
            T0t = sbt(ph, "T0t", [64, H, 128], F32)
            latn = sbt(ph, "latn", [128, 128], BF16); latnT = sbt(ph, "latnT", [128, 128], BF16)
            qr = sbt(ph, "qr", [128, H, 64], BF16); kro = sbt(ph, "kro", [128, H, 64], BF16)
            qT = sbt(ph, "qT", [64, H, 128], BF16); kTo = sbt(ph, "kTo", [64, H, 128], BF16)
            scm = sbt(ph, "scm", [128, H, 128], BF16)
            sg = sbt(ph, "sg", [128, D], BF16)
            oo = sbt(ph, "oo", [128, H, 128], F32); o2 = sbt(ph, "o2", [128, H, 128], F32)
            gst = sbt(ph, "gst", [128, 40], F32)
            ro = sbt(ph, "ro", [128, D], BF16); roT = sbt(ph, "roT", [128, 8, 128], BF16)
            dqb = sbt(ph, "dqb", [128, 512], BF16); iqb = sbt(ph, "iqb", [128, 512], BF16)
            dqT = sbt(ph, "dqT", [64, H, 128], BF16); iqT = dqT
            iwb = sbt(ph, "iwb", [128, H], F32)
            pT = pst(ph, "pT", [128, 8, 128], BF16)
            zA = pst(ph, "zA", [128, D], F32); zB = pst(ph, "zB", [128, D], F32)
            misc = pst(ph, "misc", [128, 512], F32)
            kvp = pst(ph, "kvp", [128, H, 128], F32)
            pTs = misc[:, 448:512].bitcast(BF16)

            for k in range(8):
                DG(lambda e, k=k: e.dma_start(out=w_in_sb[:, k, :], in_=w_in[k * 128:(k + 1) * 128, :]), [], ["w_in_sb"], "w_in")
            V(lambda e: e.memset(wkv_sb[:, 0:64], 0.0), [], ["wkv0"])
            DG(lambda e: e.dma_start(out=wkv_sb[:, 64:192], in_=w_kv), [], ["wkv1"], "c2_wkv")
            DG(lambda e: e.dma_start(out=DTs[:], in_=DTd), [], ["DTs"], "c2_DT")
            DM(lambda e: e.dma_start(out=kvg[:], in_=kvg_rep), [], ["kvg"], "c3_kvg")
            DM(lambda e: e.dma_start(out=gnr[:], in_=gn_rep), [], ["gnr"], "c3_gnr")
            DM(lambda e: e.dma_start(out=qdc[:], in_=qdec), [], ["qdc"], "c3_qdc")
            DM(lambda e: e.dma_start(out=wtl[:], in_=wtile), [], ["wtl"], "c3_wtl")
            DM(lambda e: e.dma_start(out=sels[:], in_=seld), [], ["sels"], "c3_sels")
            V(lambda e: e.memset(Sst[:], 0.0), [], ["Sst"])
            V(lambda e: e.memset(dv1[:, :, 64:65], 1.0), [], ["dv1one"])

            def kside_s1(pos, j):
                sl = pos % 2
                DM(lambda e: e.dma_start(out=xt[sl][:], in_=xb[j * 128:(j + 1) * 128, :]), [], [f"xt{sl}"], f"xt{sl}")
                DM(lambda e: e.dma_start(out=rp[sl][:], in_=rope_all[j * 128:(j + 1) * 128, :]), [], [f"rp{sl}"], f"rp{sl}")
                norm_T(xt[sl][:], f"xt{sl}", junk[:], xs[:], pT, uT[sl], stat, G1T, sh1T, "k", ures=f"kuT{sl}", offload=True)

            def kside(pos, j):
                sl = pos % 2
                s = j % 4
                ures = [f"kuT{sl}", "w_in_sb"]
                for (dst, c0, n, res) in ((zA[:, 0:512], C_RK, 512, ("zA", 0)), (zB[:, 0:512], C_RV, 512, ("zB", 0)),
                                          (zB[:, 512:1024], C_RV + 512, 512, ("zB", 1)), (misc[:, 0:128], C_DKV, 128, "m_dkv")):
                    for k in range(8):
                        P(lambda e, k=k, dst=dst, c0=c0, n=n: e.matmul(dst, lhsT=uT[sl][:, k, :], rhs=w_in_sb[:, k, c0:c0 + n],
                                                                      start=(k == 0), stop=(k == 7)), ures, [res])
                for k in range(8):
                    P(lambda e, k=k: e.matmul(misc[0:64, 128:256], lhsT=w_in_sb[:, k, C_IK:C_IK + 64], rhs=uT[sl][:, k, :],
                                              start=(k == 0), stop=(k == 7)), ures, ["m_ik"])
                cosb = rp[sl][:, 0:32].unsqueeze(1).broadcast_to([128, H, 32])
                sinb = rp[sl][:, 32:64].unsqueeze(1).broadcast_to([128, H, 32])
                rope(zA[:, 0:512].rearrange("p (h d) -> p h d", h=H), cosb, sinb, krp, ta[:], tb[:], [("zA", 0), f"rp{sl}"], ["krp"], "r")
                V(lambda e: e.tensor_tensor(out=kw[:], in0=krp[:], in1=wtl[:].unsqueeze(2).broadcast_to([128, H, 64]), op=ALU.mult),
                  ["krp", "wtl"], ["kw"])
                A(lambda e: e.activation(out=vb[:], in_=zB[:], func=AF.Copy), [("zB", 0), ("zB", 1)], ["vb"])
                if s == 0:
                    G(lambda e: e.tensor_scalar(out=T0[:], in0=Sst[:], scalar1=sels[:, 0:1], scalar2=None, op0=ALU.mult),
                      ["Sst", "sels"], ["T0"])
                else:
                    G(lambda e: e.tensor_scalar(out=T0t[:], in0=Sst[:], scalar1=sels[:, s:s + 1], scalar2=None, op0=ALU.mult),
                      ["Sst", "sels"], ["T0t"])
                    G(lambda e: e.tensor_tensor(out=T0[:], in0=T0[:], in1=T0t[:], op=ALU.add), ["T0", "T0t"], ["T0"])
                for h in range(H):
                    P(lambda e, h=h: e.matmul(kvp[0:64, h, :], lhsT=kw[:, h, :], rhs=vb[:, h * 128:(h + 1) * 128], start=True, stop=True),
                      ["kw", "vb"], [("kvp", h // 4)])
                for h in range(H):
                    V(lambda e, h=h: e.scalar_tensor_tensor(out=Sst[:, h, :], in0=Sst[:, h, :], scalar=float(GAM[h] ** 128),
                                                           in1=kvp[0:64, h, :], op0=ALU.mult, op1=ALU.add),
                      ["Sst", ("kvp", h // 4)], ["Sst"])
                A(lambda e: e.activation(out=junk[:, 0:128], in_=misc[:, 0:128], func=AF.Square, scale=1.0 / math.sqrt(128.0),
                                         accum_out=stat[:, 4:5]), ["m_dkv"], ["kjunk", "st2"])
                A(lambda e: e.activation(out=stat[:, 5:6], in_=stat[:, 4:5], func=AF.Ln, bias=epsc[:, 0:1]), ["st2", "epsc"], ["st2"])
                A(lambda e: e.activation(out=stat[:, 6:7], in_=stat[:, 5:6], func=AF.Exp, scale=-0.5), ["st2"], ["st2"])
                V(lambda e: e.scalar_tensor_tensor(out=latn[:], in0=misc[:, 0:128], scalar=stat[:, 6:7], in1=kvg[:], op0=ALU.mult, op1=ALU.mult),
                  ["m_dkv", "st2", "kvg"], ["latn"])
                P(lambda e: e.transpose(out=pTs, in_=latn[:], identity=idb[:]), ["latn", "idb"], ["pTs"])
                A(lambda e: e.activation(out=latnT[:], in_=pTs, func=AF.Copy), ["pTs"], ["latnT"])
                P(lambda e: e.matmul(misc[:, 256:384], lhsT=wkv_sb[:, 0:128], rhs=latnT[:], start=True, stop=True),
                  ["latnT", "wkv0", "wkv1"], ["m_dk"])
                P(lambda e: e.matmul(misc[:, 384:448], lhsT=latnT[:], rhs=wkv_sb[:, 128:192], start=True, stop=True),
                  ["latnT", "wkv1"], ["m_dv"])
                A(lambda e: e.activation(out=kT[0:64, j * 128:(j + 1) * 128], in_=misc[0:64, 128:256], func=AF.Copy), ["m_ik"], [("kT", j)])
                A(lambda e: e.activation(out=kT[64:128, j * 128:(j + 1) * 128], in_=misc[64:128, 256:384], func=AF.Copy), ["m_dk"], [("kT", j)])
                V(lambda e: e.tensor_copy(out=dv1[:, j, 0:64], in_=misc[:, 384:448]), ["m_dv"], [("dv1", j)])

            def ownside_s1(pos, g):
                sl = pos % 2
                DM(lambda e: e.dma_start(out=xt[sl][:], in_=xo[g * 128:(g + 1) * 128, :]), [], [f"xt{sl}"], f"xt{sl}")
                DM(lambda e: e.dma_start(out=rp[sl][:], in_=rope_own[g * 128:(g + 1) * 128, :]), [], [f"rp{sl}"], f"rp{sl}")
                norm_T(xt[sl][:], f"xt{sl}", junk[:], xs[:], pT, uT[sl], stat, G1T, sh1T, "k", ures=f"kuT{sl}", offload=True)
                DM(lambda e: e.dma_start(out=uT_s[g], in_=uT[sl][:]), [f"kuT{sl}"], [("uT_s", g)], "st_u")

            def ownside(pos, g):
                sl = pos % 2
                ures = [f"kuT{sl}", "w_in_sb"]
                u = uT[sl]

                def proj(dst, c0, n, res):
                    for k in range(8):
                        P(lambda e, k=k: e.matmul(dst, lhsT=u[:, k, :], rhs=w_in_sb[:, k, c0:c0 + n], start=(k == 0), stop=(k == 7)), ures, [res])

                cosb = rp[sl][:, 0:32].unsqueeze(1).broadcast_to([128, H, 32])
                sinb = rp[sl][:, 32:64].unsqueeze(1).broadcast_to([128, H, 32])
                proj(zA[:, 0:512], C_RQ, 512, ("zA", 0))
                proj(zA[:, 512:1024], C_RK, 512, ("zA", 1))
                rope(zA[:, 0:512].rearrange("p (h d) -> p h d", h=H), cosb, sinb, qr, ta[:], tb[:], [("zA", 0), f"rp{sl}"], ["qr"], "r")
                rope(zA[:, 512:1024].rearrange("p (h d) -> p h d", h=H), cosb, sinb, kro, ta[:], tb[:], [("zA", 1), f"rp{sl}"], ["kro"], "r")
                pT64 = pT[0:64, :, :]
                for h in range(H):
                    P(lambda e, h=h: e.transpose(out=pT64[:, h, :], in_=qr[:, h, :], identity=idb[:]), ["qr", "idb"], ["pT"])
                V(lambda e: e.tensor_copy(out=qT[:], in_=pT64), ["pT"], ["qT"])
                for h in range(H):
                    P(lambda e, h=h: e.transpose(out=pT64[:, h, :], in_=kro[:, h, :], identity=idb[:]), ["kro", "idb"], ["pT"])
                A(lambda e: e.activation(out=kTo[:], in_=pT64, func=AF.Copy), ["pT"], ["kTo"])
                proj(zB[:, 0:512], C_RV, 512, ("zB", 0))
                proj(zB[:, 512:1024], C_RV + 512, 512, ("zB", 1))
                A(lambda e: e.activation(out=vb[:], in_=zB[:], func=AF.Copy), [("zB", 0), ("zB", 1)], ["vb"])
                zA3 = zA[:].rearrange("p (h d) -> p h d", h=H)
                for h in range(H):
                    P(lambda e, h=h: e.matmul(zA3[:, h, :], lhsT=kTo[:, h, :], rhs=qT[:, h, :], start=True, stop=True),
                      ["kTo", "qT"], [("zA", h // 4)])
                V(lambda e: e.tensor_tensor(out=scm[:], in0=zA3, in1=DTs[:], op=ALU.mult), [("zA", 0), ("zA", 1), "DTs"], ["scm"])
                zB3 = zB[:].rearrange("p (h d) -> p h d", h=H)
                for h in range(H):
                    P(lambda e, h=h: e.matmul(zB3[:, h, :], lhsT=scm[:, h, :], rhs=vb[:, h * 128:(h + 1) * 128], start=True, stop=True),
                      ["scm", "vb"], [("zB", h // 4)])
                V(lambda e: e.tensor_copy(out=T0b[:], in_=T0[:]), ["T0"], ["T0b"])
                for h in range(H):
                    P(lambda e, h=h: e.matmul(kvp[:, h, :], lhsT=qT[:, h, :], rhs=T0b[:, h, :], start=True, stop=True),
                      ["qT", "T0b"], [("kvp", h // 4)])
                V(lambda e: e.tensor_tensor(out=oo[:], in0=kvp[:], in1=qdc[:].unsqueeze(2).broadcast_to([128, H, 128]), op=ALU.mult),
                  [("kvp", 0), ("kvp", 1), "qdc"], ["oo"])
                V(lambda e: e.tensor_tensor(out=oo[:], in0=oo[:], in1=zB3, op=ALU.add), ["oo", ("zB", 0), ("zB", 1)], ["oo"])
                V(lambda e: e.tensor_reduce(out=gst[:, 0:8], in_=oo[:], axis=AX.X, op=ALU.add), ["oo"], ["gst"])
                V(lambda e: e.tensor_tensor(out=o2[:], in0=oo[:], in1=oo[:], op=ALU.mult), ["oo"], ["o2"])
                V(lambda e: e.tensor_reduce(out=gst[:, 8:16], in_=o2[:], axis=AX.X, op=ALU.add), ["o2"], ["gst"])
                V(lambda e: e.tensor_scalar(out=gst[:, 16:24], in0=gst[:, 0:8], scalar1=1.0 / 128.0, scalar2=None, op0=ALU.mult), ["gst"], ["gst"])
                V(lambda e: e.tensor_tensor(out=gst[:, 24:32], in0=gst[:, 16:24], in1=gst[:, 16:24], op=ALU.mult), ["gst"], ["gst"])
                V(lambda e: e.scalar_tensor_tensor(out=gst[:, 24:32], in0=gst[:, 8:16], scalar=1.0 / 128.0, in1=gst[:, 24:32],
                                                   op0=ALU.mult, op1=ALU.subtract), ["gst"], ["gst"])
                A(lambda e: e.activation(out=gst[:, 32:40], in_=gst[:, 24:32], func=AF.Ln, bias=epsc[:, 1:2]), ["gst", "epsc"], ["gst2"])
                A(lambda e: e.activation(out=gst[:, 32:40], in_=gst[:, 32:40], func=AF.Exp, scale=-0.5), ["gst2"], ["gst2"])
                for h in range(H):
                    V(lambda e, h=h: e.tensor_scalar(out=o2[:, h, :], in0=oo[:, h, :], scalar1=gst[:, 16 + h:17 + h], scalar2=gst[:, 32 + h:33 + h],
                                                     op0=ALU.subtract, op1=ALU.mult), ["oo", "gst", "gst2"], ["o2"])
                proj(zA[:, 0:512], C_RG, 512, ("zA", 0))
                proj(zA[:, 512:1024], C_RG + 512, 512, ("zA", 1))
                A(lambda e: e.activation(out=sg[:], in_=zA[:], func=AF.Silu), [("zA", 0), ("zA", 1)], ["sg"])
                o2f = o2[:].rearrange("p h d -> p (h d)")
                V(lambda e: e.tensor_tensor(out=o2f, in0=o2f, in1=gnr[:], op=ALU.mult), ["o2", "gnr"], ["o2"])
                V(lambda e: e.tensor_tensor(out=ro[:], in0=o2f, in1=sg[:], op=ALU.mult), ["o2", "sg"], ["ro"])
                for k in range(8):
                    P(lambda e, k=k: e.transpose(out=pT[:, k, :], in_=ro[:, k * 128:(k + 1) * 128], identity=idb[:]), ["ro", "idb"], ["pT"])
                A(lambda e: e.activation(out=roT[:], in_=pT[:], func=AF.Copy), ["pT"], ["roT"])
                DM(lambda e: e.dma_start(out=roT_s[g], in_=roT[:]), ["roT"], [("roT_s", g)], "st_r")
                proj(zB[:, 0:512], C_DQ, 512, ("zB", 0))
                proj(zB[:, 512:1024], C_IQ, 512, ("zB", 1))
                proj(misc[:, 0:8], C_IW, 8, "m_dkv")
                V(lambda e: e.tensor_copy(out=dqb[:], in_=zB[:, 0:512]), [("zB", 0)], ["dqb"])
                A(lambda e: e.activation(out=iqb[:], in_=zB[:, 512:1024], func=AF.Copy), [("zB", 1)], ["iqb"])
                V(lambda e: e.tensor_copy(out=iwb[:], in_=misc[:, 0:8]), ["m_dkv"], ["iwb"])
                DM(lambda e: e.dma_start(out=iw_s[g], in_=iwb[:]), ["iwb"], [("iw_s", g)], "st_w")
                for h in range(H):
                    P(lambda e, h=h: e.transpose(out=pT64[:, h, :], in_=dqb[:, h * 64:(h + 1) * 64], identity=idb[:]), ["dqb", "idb"], ["pT"])
                V(lambda e: e.tensor_copy(out=dqT[:], in_=pT64), ["pT"], ["dqT"])
                DM(lambda e: e.dma_start(out=dqT_s[g], in_=dqT[:]), ["dqT"], [("dqT_s", g)], "st_q")
                for h in range(H):
                    P(lambda e, h=h: e.transpose(out=pT64[:, h, :], in_=iqb[:, h * 64:(h + 1) * 64], identity=idb[:]), ["iqb", "idb"], ["pT"])
                A(lambda e: e.activation(out=iqT[:], in_=pT64, func=AF.Copy), ["pT"], ["dqT"])
                DM(lambda e: e.dma_start(out=iqT_s[g], in_=iqT[:]), ["dqT"], [("iqT_s", g)], "st_i")

            items = []
            for g in range(NG):
                for s4 in range(4):
                    items.append(("k", 4 * g + s4))
                items.append(("o", g))

            def stage1(pos):
                kind, idx = items[pos]
                (kside_s1 if kind == "k" else ownside_s1)(pos, idx)

            stage1(0)
            for pos in range(len(items)):
                if pos + 1 < len(items):
                    stage1(pos + 1)
                kind, idx = items[pos]
                (kside if kind == "k" else ownside)(pos, idx)
            if dbg:
                DM(lambda e: e.dma_start(out=kT_dbg, in_=kT[:]), [("kT", j) for j in range(NT)], ["kT_dbg"], "dbg")
                DM(lambda e: e.dma_start(out=dv_dbg, in_=dv1[:]), [("dv1", j) for j in range(NT)] + ["dv1one"], ["dv_dbg"], "dbg")
            S.barrier()
            S.emit()

        PHASES = build.phases
        if PHASES >= 2:
          with contextlib.ExitStack() as ph:
            score2 = [sbt(ph, f"score{i}", [128, SB], BF16) for i in range(2)]
            msk = sbt(ph, "msk", [128, SB], BF16)
            mskT = [sbt(ph, f"mskT{i}", [128, 8, 128], BF16) for i in range(2)]
            iq_sb2 = [sbt(ph, f"iq_sb{i}", [128, H, 128], BF16) for i in range(2)]
            dq_sb2 = [sbt(ph, f"dq_sb{i}", [128, H, 128], BF16) for i in range(2)]
            iw_sb2 = [sbt(ph, f"iw_sb{i}", [128, H], F32) for i in range(2)]
            dg2 = [sbt(ph, f"dg{i}", [128, H, 128], BF16) for i in range(2)]
            maddb = sbt(ph, "maddb", [128, 512], BF16)
            rh = [sbt(ph, f"rh{i}", [128, 512], BF16) for i in range(4)]
            madd = sbt(ph, "madd", [128, 512], F32)
            bT = sbt(ph, "bT", [128, 5, H, 128], BF16)
            c15s = sbt(ph, "c15s", [128, H], F32)
            selden = sbt(ph, "selden", [65, 64], F32)
            bst = sbt(ph, "bst", [128, 8], F32)
            steps = sbt(ph, "steps", [128, NBIS], F32)
            halves = sbt(ph, "halves", [128, NBIS], F32)
            tmpl = sbt(ph, "tmpl", [128, 512], F32)
            lg = sbt(ph, "lg", [128, H, 128], F32)
            pe_ = [sbt(ph, f"pe{i}", [128, H, 128], BF16) for i in range(2)]
            pm = [sbt(ph, f"pm{i}", [128, H, 128], BF16) for i in range(3)]
            OT = sbt(ph, "OT", [65, H * 128], F32)
            ydT = sbt(ph, "ydT", [64, H, 128], BF16)
            PP = [pst(ph, f"PP{i}", [128, 1024], F32) for i in range(4)]
            ps_s = [PP[k // 2][:, (k % 2) * 512:(k % 2 + 1) * 512] for k in range(4)]
            ps_s_res = [(f"PP{k // 2}", k % 2) for k in range(4)]
            ps_sc = [PP[2][:, 0:512], PP[2][:, 512:1024]]
            ps_sc_res = [("PP2", 0), ("PP2", 1)]
            ps_qk = [PP[0], PP[1]]
            ps_o = PP[2]
            ps_mT = PP[3][:, 0:512].bitcast(BF16).rearrange("p (a b) -> p a b", a=8)

            DM(lambda e: e.dma_start(out=madd[:], in_=maddd), [], ["madd"], "c4_madd")
            DG(lambda e: e.dma_start(out=bT[:], in_=biasTd.rearrange("s p h q -> p s h q")), [], ["bT"], "c4_bT")
            DM(lambda e: e.dma_start(out=c15s[:], in_=c15), [], ["c15s"], "c4_c15s")
            DM(lambda e: e.dma_start(out=selden[:], in_=seldend), [], ["selden"], "c4_selden")
            for s5 in range(5):
                V(lambda e, s5=s5: e.tensor_tensor(out=bT[:, s5, :, :], in0=bT[:, s5, :, :],
                                                   in1=c15s[:].unsqueeze(2).broadcast_to([128, H, 128]), op=ALU.subtract),
                  ["bT", "c15s"], ["bT"])
            for i in range(NBIS):
                V(lambda e, i=i: e.memset(halves[:, i:i + 1], 2.0 ** (-(i + 1))), [], ["halves"])
            for i in range(2):
                G(lambda e, i=i: e.memset(iq_sb2[i][64:128, :, :], 0.0), [], [f"iq_sb{i}"])
                G(lambda e, i=i: e.memset(dq_sb2[i][0:64, :, :], 0.0), [], [f"dq_sb{i}"])
            V(lambda e: e.tensor_copy(out=maddb[:], in_=madd[:]), ["madd"], ["maddb"])

            def load_q(g):
                sl = g % 2
                DM(lambda e: e.dma_start(out=iq_sb2[sl][0:64, :, :], in_=iqT_s[g]), [("iqT_s", g)], [f"iq_sb{sl}"], f"ld_i{sl}")
                DM(lambda e: e.dma_start(out=dq_sb2[sl][64:128, :, :], in_=dqT_s[g]), [("dqT_s", g)], [f"dq_sb{sl}"], f"ld_q{sl}")
                DM(lambda e: e.dma_start(out=iw_sb2[sl][:], in_=iw_s[g]), [("iw_s", g)], [f"iw_sb{sl}"], f"ld_w{sl}")
                for h in range(H):
                    V(lambda e, h=h: e.tensor_scalar(out=dg2[sl][:, h, :], in0=idb[:], scalar1=iw_sb2[sl][:, h:h + 1], scalar2=None, op0=ALU.mult),
                      ["idb", f"iw_sb{sl}"], [f"dg{sl}"])

            def indexer(g):
                sl = g % 2
                iq_sb = iq_sb2[sl]; dg = dg2[sl]; score = score2[sl]
                iqres = f"iq_sb{sl}"; dgres = f"dg{sl}"
                M = 8 * (g + 1)

                def smm(i):
                    c, h = i // 8, i % 8
                    kres = [("kT", 4 * c + t) for t in range(4)]
                    P(lambda e: e.matmul(ps_s[i % 4], lhsT=iq_sb[:, h, :], rhs=kT[:, c * 512:(c + 1) * 512], start=True, stop=True),
                      [iqres] + kres, [ps_s_res[i % 4]])

                def rl_dmm(i):
                    c, h = i // 8, i % 8
                    r4 = i % 4
                    A(lambda e: e.activation(out=rh[r4][:], in_=ps_s[r4], func=AF.Relu), [ps_s_res[r4]], [f"rh{r4}"])
                    last = (c == g)
                    P(lambda e: e.matmul(ps_sc[c % 2], lhsT=dg[:, h, :], rhs=rh[r4][:], start=(h == 0), stop=(h == H - 1 and not last)),
                      [dgres, f"rh{r4}"], [ps_sc_res[c % 2]])
                    if h == H - 1:
                        if last:
                            P(lambda e: e.matmul(ps_sc[c % 2], lhsT=idb[:], rhs=maddb[:], start=False, stop=True), ["idb", "maddb"], [ps_sc_res[c % 2]])
                        A(lambda e: e.activation(out=score[:, c * 512:(c + 1) * 512], in_=ps_sc[c % 2], func=AF.Copy), [ps_sc_res[c % 2]], [(f"score{sl}", c)])

                smm(0)
                smm(1)
                for i_ in range(M):
                    if i_ + 2 < M:
                        smm(i_ + 2)
                    rl_dmm(i_)

            def bisect(g):
                sl = g % 2
                score = score2[sl]
                nk = (g + 1) * 512
                sres = [(f"score{sl}", c) for c in range(g + 1)]
                V(lambda e: e.scalar_tensor_tensor(out=tmpl[:], in0=madd[:], scalar=-2.0, in1=score[:, nk - 512:nk], op0=ALU.mult, op1=ALU.add),
                  [(f"score{sl}", g), "madd"], ["tmpl"])
                V(lambda e: e.tensor_reduce(out=bst[:, 2:3], in_=tmpl[:], axis=AX.X, op=ALU.min), ["tmpl"], ["bst2"])
                V(lambda e: e.tensor_reduce(out=bst[:, 0:1], in_=score[:, 0:nk], axis=AX.X, op=ALU.max), sres, ["bst0"])
                if g > 0:
                    V(lambda e: e.tensor_reduce(out=bst[:, 1:2], in_=score[:, 0:nk - 512], axis=AX.X, op=ALU.min), sres, ["bst1"])
                    V(lambda e: e.tensor_tensor(out=bst[:, 3:4], in0=bst[:, 1:2], in1=bst[:, 2:3], op=ALU.min), ["bst1", "bst2"], ["bt"])
                else:
                    V(lambda e: e.tensor_copy(out=bst[:, 3:4], in_=bst[:, 2:3]), ["bst2"], ["bt"])
                V(lambda e: e.tensor_tensor(out=bst[:, 4:5], in0=bst[:, 0:1], in1=bst[:, 3:4], op=ALU.subtract), ["bst0", "bt"], ["bR"])
                V(lambda e: e.tensor_scalar(out=steps[:], in0=halves[:], scalar1=bst[:, 4:5], scalar2=None, op0=ALU.mult), ["halves", "bR"], ["steps"])
                for i in range(NBIS):
                    V(lambda e, i=i: e.tensor_tensor(out=bst[:, 5:6], in0=bst[:, 3:4], in1=steps[:, i:i + 1], op=ALU.add), ["bt", "steps"], ["bcand"])
                    V(lambda e: e.tensor_scalar(out=msk[:, 0:nk], in0=score[:, 0:nk], scalar1=bst[:, 5:6], scalar2=0.0, op0=ALU.is_ge, op1=ALU.add,
                                                accum_out=bst[:, 6:7]), sres + ["bcand"], ["msk", "bcnt"])
                    V(lambda e, i=i: e.tensor_scalar(out=bst[:, 7:8], in0=bst[:, 6:7], scalar1=255.5, scalar2=steps[:, i:i + 1], op0=ALU.is_ge, op1=ALU.mult),
                      ["bcnt", "steps"], ["binc"])
                    V(lambda e: e.tensor_tensor(out=bst[:, 3:4], in0=bst[:, 3:4], in1=bst[:, 7:8], op=ALU.add), ["bt", "binc"], ["bt"])
                V(lambda e: e.tensor_scalar(out=msk[:, 0:nk], in0=score[:, 0:nk], scalar1=bst[:, 3:4], scalar2=0.0, op0=ALU.is_ge, op1=ALU.add,
                                            accum_out=bst[:, 6:7]), sres + ["bt"], ["msk", "bcnt"])
                if dbg:
                    DM(lambda e: e.dma_start(out=thr_dbg[g], in_=bst[:, 0:8]), ["bt", "bcnt", "bR", "bcand", "bst0", "bst1", "bst2", "binc"], [("thr_dbg", g)], "dbg2")
                    DM(lambda e: e.dma_start(out=sc_dbg[g, :, 0:nk], in_=score[:, 0:nk]), sres, [("sc_dbg", g)], "dbg2")
                    DM(lambda e: e.dma_start(out=mk_dbg[g, :, 0:nk], in_=msk[:, 0:nk]), ["msk"], [("mk_dbg", g)], "dbg2")

            def attention(g):
                sl = g % 2
                dq_sb = dq_sb2[sl]
                dqres = f"dq_sb{sl}"
                ntile = (g + 1) * 4

                def mask_blk(j0):
                    nj = min(8, ntile - j0)
                    for jj in range(nj):
                        j = j0 + jj
                        P(lambda e, j=j, jj=jj: e.transpose(out=ps_mT[:, jj, :], in_=msk[:, j * 128:(j + 1) * 128], identity=idb[:]),
                          ["msk", "idb"], [("PP3", 0)])
                    mb = (j0 // 8) % 2
                    if mb == 0:
                        A(lambda e, mb=mb, nj=nj: e.activation(out=mskT[mb][:, 0:nj, :], in_=ps_mT[:, 0:nj, :], func=AF.Copy), [("PP3", 0)], [f"mskT{mb}"])
                    else:
                        V(lambda e, mb=mb, nj=nj: e.tensor_copy(out=mskT[mb][:, 0:nj, :], in_=ps_mT[:, 0:nj, :]), [("PP3", 0)], [f"mskT{mb}"])
                def att_qk(j):
                    qb = j % 2
                    for hf in range(2):
                        P(lambda e, hf=hf: e.matmul(ps_qk[qb][:, hf * 512:(hf + 1) * 512], lhsT=kT[:, j * 128:(j + 1) * 128],
                                                    rhs=dq_sb[:, 4 * hf:4 * hf + 4, :], start=True, stop=True),
                          [("kT", j), dqres], [(f"PP{qb}", hf)])

                def att_sm(j):
                    b = j % 2
                    if j % 8 == 0:
                        mask_blk(j)
                    mb = (j // 8) % 2
                    slot = j - (4 * g - 1)
                    qk3 = ps_qk[b][:].rearrange("p (h q) -> p h q", h=H)
                    qres = [(f"PP{b}", 0), (f"PP{b}", 1)]
                    if slot >= 0:
                        V(lambda e: e.scalar_tensor_tensor(out=lg[:], in0=qk3, scalar=0.125, in1=bT[:, slot, :, :], op0=ALU.mult, op1=ALU.add),
                          qres + ["bT"], ["lg"])
                        A(lambda e: e.activation(out=pe_[b][:], in_=lg[:], func=AF.Exp), ["lg"], [f"pe{b}"])
                    else:
                        A(lambda e: e.activation(out=pe_[b][:], in_=qk3, func=AF.Exp, scale=0.125), qres, [f"pe{b}"])
                    b3 = j % 3
                    V(lambda e: e.tensor_tensor(out=pm[b3][:], in0=pe_[b][:], in1=mskT[mb][:, j % 8, :].unsqueeze(1).broadcast_to([128, H, 128]), op=ALU.mult),
                      [f"pe{b}", f"mskT{mb}"], [f"pm{b3}"])

                def att_pv(j):
                    b = j % 3
                    for hf in range(2):
                        P(lambda e, hf=hf: e.matmul(ps_o[0:65, hf * 512:(hf + 1) * 512], lhsT=dv1[:, j, :], rhs=pm[b][:, 4 * hf:4 * hf + 4, :],
                                                    start=(j == 0), stop=(j == ntile - 1)),
                          [("dv1", j), "dv1one", f"pm{b}"], [("PP2", hf)])

                att_qk(0)
                for j_ in range(ntile):
                    if j_ + 1 < ntile:
                        att_qk(j_ + 1)
                    att_sm(j_)
                    if j_ >= 1:
                        att_pv(j_ - 1)
                att_pv(ntile - 1)
                A(lambda e: e.activation(out=OT[:], in_=ps_o[0:65, :], func=AF.Copy), [("PP2", 0), ("PP2", 1)], ["OT"])
                for hf in range(2):
                    P(lambda e, hf=hf: e.matmul(PP[3][0:64, hf * 512:(hf + 1) * 512], lhsT=selden[:], rhs=OT[:, hf * 512:(hf + 1) * 512], start=True, stop=True),
                      ["selden", "OT"], [("PP3", hf)])
                rden = lg[0:64, :, :].rearrange("p h q -> p (h q)")
                V(lambda e: e.reciprocal(out=rden, in_=PP[3][0:64, :]), [("PP3", 0), ("PP3", 1)], ["lg"])
                V(lambda e: e.tensor_tensor(out=ydT[:].rearrange("p h q -> p (h q)"), in0=OT[0:64, :], in1=rden, op=ALU.mult), ["OT", "lg"], ["ydT"])
                DM(lambda e: e.dma_start(out=ydT_s[g], in_=ydT[:]), ["ydT"], [("ydT_s", g)], "st_y")

            load_q(0)
            indexer(0)
            for g_ in range(NG):
                if g_ + 1 < NG:
                    load_q(g_ + 1)
                    indexer(g_ + 1)
                bisect(g_)
                attention(g_)
            S.barrier()
            S.emit()

        kvs.close()
        if PHASES >= 3:
          with contextlib.ExitStack() as ph:
            u2T = sbt(ph, "u2T", [128, NG, 8, 128], BF16)
            gates = sbt(ph, "gates", [128, NG, 16], F32)
            with contextlib.ExitStack() as pc:
                wg_sb = sbt(pc, "wg_sb", [128, 8, 2 * D], BF16)
                wro_sb = sbt(pc, "wro_sb", [128, 8, D], BF16)
                wdo_sb = sbt(pc, "wdo_sb", [64, H, D], BF16)
                wo_sb = sbt(pc, "wo_sb", [128, 8, D], BF16)
                wrt_sb = sbt(pc, "wrt_sb", [128, 8, 20], BF16)
                bg_sb = sbt(pc, "bg_sb", [1, 2 * D], BF16)
                ones1 = sbt(pc, "ones1", [1, 128], BF16)
                brt = sbt(pc, "brt", [128, 20], F32)
                xt = [sbt(pc, f"cxt{i}", [128, D], F32) for i in range(2)]
                uTc = [sbt(pc, f"cuT{i}", [128, 8, 128], BF16) for i in range(2)]
                roTc = [sbt(pc, f"croT{i}", [128, 8, 128], BF16) for i in range(2)]
                ydTc = [sbt(pc, f"cydT{i}", [64, H, 128], BF16) for i in range(2)]
                gab = sbt(pc, "gab", [128, 2 * D], BF16)
                t1 = sbt(pc, "t1", [128, D], F32); t2 = sbt(pc, "t2", [128, D], F32)
                mm = sbt(pc, "mm", [128, D], BF16); mT = sbt(pc, "mT", [128, 8, 128], BF16)
                h1 = sbt(pc, "h1", [128, D], F32)
                junk = sbt(pc, "cjunk", [128, D], BF16); xs = sbt(pc, "cxs", [128, D], BF16)
                stat = sbt(pc, "cstat", [128, 8], F32)
                rl = sbt(pc, "rl", [128, 20], F32)
                rs = sbt(pc, "rs", [128, 48], F32)
                u2t = sbt(pc, "u2t", [128, 8, 128], BF16)
                pT = pst(pc, "cpT", [128, 8, 128], BF16)
                pgA = pst(pc, "pgA", [128, 2 * D], F32)
                pyr = pst(pc, "pyr", [128, D], F32)
                prt = pst(pc, "prt", [128, 32], F32)

                for k in range(8):
                    DG(lambda e, k=k: e.dma_start(out=wg_sb[:, k, :], in_=w_gate[k * 128:(k + 1) * 128, :]), [], ["wg_sb"], "wC_wg_sb")
                    DG(lambda e, k=k: e.dma_start(out=wro_sb[:, k, :], in_=w_ret_out[k * 128:(k + 1) * 128, :]), [], ["wro_sb"], "wC_wro_sb")
                    DG(lambda e, k=k: e.dma_start(out=wo_sb[:, k, :], in_=w_o[k * 128:(k + 1) * 128, :]), [], ["wo_sb"], "wC_wo_sb")
                    DG(lambda e, k=k: e.dma_start(out=wdo_sb[:, k, :], in_=w_dsa_out[k * 64:(k + 1) * 64, :]), [], ["wdo_sb"], "wC_wdo_sb")
                DG(lambda e: e.dma_start(out=wrt_sb[:], in_=w_rt.rearrange("(k p) n -> p k n", p=128)), [], ["wrt_sb"], "wC_wrt_sb")
                DG(lambda e: e.dma_start(out=bg_sb[:], in_=b_gate), [], ["bg_sb"], "wC_bg_sb")
                DM(lambda e: e.dma_start(out=brt[:], in_=b_rt_rep), [], ["brt"], "c5")
                V(lambda e: e.memset(ones1[:], 1.0), [], ["ones1"])
                for k in range(8):
                    G(lambda e, k=k: e.tensor_tensor(out=wo_sb[:, k, :], in0=wo_sb[:, k, :], in1=g1rep[:], op=ALU.mult), ["wo_sb", "mod"], ["wo_sb"])

                def merge_tile(g):
                    sl = g % 2
                    DM(lambda e: e.dma_start(out=xt[sl][:], in_=xo[g * 128:(g + 1) * 128, :]), [], [f"cxt{sl}"], f"cxt{sl}")
                    DM(lambda e: e.dma_start(out=uTc[sl][:], in_=uT_s[g]), [("uT_s", g)], [f"cuT{sl}"], f"cuT{sl}")
                    DM(lambda e: e.dma_start(out=roTc[sl][:], in_=roT_s[g]), [("roT_s", g)], [f"croT{sl}"], f"croT{sl}")
                    DM(lambda e: e.dma_start(out=ydTc[sl][:], in_=ydT_s[g]), [("ydT_s", g)], [f"cydT{sl}"], f"cydT{sl}")
                    for q4 in range(4):
                        cs_ = slice(q4 * 512, (q4 + 1) * 512)
                        for k in range(8):
                            P(lambda e, k=k, cs_=cs_: e.matmul(pgA[:, cs_], lhsT=uTc[sl][:, k, :], rhs=wg_sb[:, k, cs_], start=(k == 0), stop=False),
                              [f"cuT{sl}", "wg_sb"], [("pgA", q4)])
                        P(lambda e, cs_=cs_: e.matmul(pgA[:, cs_], lhsT=ones1[:], rhs=bg_sb[:, cs_], start=False, stop=True),
                          ["ones1", "bg_sb"], [("pgA", q4)])
                    A(lambda e: e.activation(out=gab[:], in_=pgA[:], func=AF.Sigmoid), [("pgA", i) for i in range(4)], ["gab"])
                    for hf in range(2):
                        cs_ = slice(hf * 512, (hf + 1) * 512)
                        for k in range(8):
                            P(lambda e, k=k, cs_=cs_: e.matmul(pyr[:, cs_], lhsT=roTc[sl][:, k, :], rhs=wro_sb[:, k, cs_], start=(k == 0), stop=(k == 7)),
                              [f"croT{sl}", "wro_sb"], [("pyr", hf)])
                    V(lambda e: e.tensor_tensor(out=t1[:], in0=pyr[:], in1=gab[:, 0:D], op=ALU.mult), [("pyr", 0), ("pyr", 1), "gab"], ["t1"])
                    for hf in range(2):
                        cs_ = slice(hf * 512, (hf + 1) * 512)
                        for h in range(H):
                            P(lambda e, h=h, cs_=cs_: e.matmul(pyr[:, cs_], lhsT=ydTc[sl][:, h, :], rhs=wdo_sb[:, h, cs_], start=(h == 0), stop=(h == H - 1)),
                              [f"cydT{sl}", "wdo_sb"], [("pyr", hf)])
                    V(lambda e: e.tensor_tensor(out=t2[:], in0=pyr[:], in1=gab[:, D:2 * D], op=ALU.mult), [("pyr", 0), ("pyr", 1), "gab"], ["t2"])
                    V(lambda e: e.tensor_tensor(out=mm[:], in0=t1[:], in1=t2[:], op=ALU.add), ["t1", "t2"], ["mm"])
                    for k in range(8):
                        P(lambda e, k=k: e.transpose(out=pT[:, k, :], in_=mm[:, k * 128:(k + 1) * 128], identity=idb[:]), ["mm", "idb"], ["pT"])
                    A(lambda e: e.activation(out=mT[:], in_=pT[:], func=AF.Copy), ["pT"], ["mT"])
                    for hf in range(2):
                        cs_ = slice(hf * 512, (hf + 1) * 512)
                        for k in range(8):
                            P(lambda e, k=k, cs_=cs_: e.matmul(pyr[:, cs_], lhsT=mT[:, k, :], rhs=wo_sb[:, k, cs_], start=(k == 0), stop=(k == 7)),
                              ["mT", "wo_sb"], [("pyr", hf)])
                    V(lambda e: e.tensor_tensor(out=h1[:], in0=pyr[:], in1=xt[sl][:], op=ALU.add), [("pyr", 0), ("pyr", 1), f"cxt{sl}"], ["h1"])
                    DM(lambda e: e.dma_start(out=h1_s[g], in_=h1[:]), ["h1"], [("h1_s", g)], "st_h")
                    norm_T(h1[:], "h1", junk[:], xs[:], pT, u2t, stat, G2T, sh2T, "c")
                    G(lambda e: e.tensor_copy(out=u2T[:, g, :, :], in_=u2t[:]), ["cuT"], [("u2T", g)])
                    for k in range(8):
                        P(lambda e, k=k: e.matmul(prt[:, 0:20], lhsT=u2t[:, k, :], rhs=wrt_sb[:, k, :], start=(k == 0), stop=(k == 7)),
                          ["cuT", "wrt_sb"], ["prt"])
                    V(lambda e: e.tensor_tensor(out=rl[:], in0=prt[:, 0:20], in1=brt[:], op=ALU.add), ["prt", "brt"], ["rl"])
                    R = lambda a, b_: rs[:, a:b_]
                    V(lambda e: e.tensor_reduce(out=R(0, 1), in_=rl[:, 0:4], axis=AX.X, op=ALU.max), ["rl"], ["rs"])
                    V(lambda e: e.tensor_scalar(out=R(1, 5), in0=rl[:, 0:4], scalar1=R(0, 1), scalar2=None, op0=ALU.is_ge), ["rl", "rs"], ["rs"])
                    V(lambda e: e.tensor_scalar(out=R(5, 6), in0=R(0, 1), scalar1=-1.0, scalar2=None, op0=ALU.mult), ["rs"], ["rs"])
                    A(lambda e: e.activation(out=R(6, 10), in_=rl[:, 0:4], func=AF.Exp, bias=R(5, 6), accum_out=R(10, 11)), ["rl", "rs"], ["rs2"])
                    V(lambda e: e.reciprocal(out=R(11, 12), in_=R(10, 11)), ["rs2"], ["rs3"])
                    V(lambda e: e.tensor_scalar(out=R(12, 16), in0=rl[:, 4:8], scalar1=R(1, 2), scalar2=None, op0=ALU.mult), ["rl", "rs"], ["rs4"])
                    for gi in range(1, 4):
                        V(lambda e, gi=gi: e.scalar_tensor_tensor(out=R(12, 16), in0=rl[:, 4 + 4 * gi:8 + 4 * gi], scalar=R(1 + gi, 2 + gi), in1=R(12, 16),
                                                                  op0=ALU.mult, op1=ALU.add), ["rl", "rs", "rs4"], ["rs4"])
                    V(lambda e: e.tensor_reduce(out=R(16, 17), in_=R(12, 16), axis=AX.X, op=ALU.max), ["rs4"], ["rs5"])
                    V(lambda e: e.tensor_scalar(out=R(17, 21), in0=R(12, 16), scalar1=R(16, 17), scalar2=None, op0=ALU.is_ge), ["rs4", "rs5"], ["rs6"])
                    V(lambda e: e.scalar_tensor_tensor(out=R(21, 25), in0=R(17, 21), scalar=NEG, in1=R(12, 16), op0=ALU.mult, op1=ALU.add),
                      ["rs6", "rs4"], ["rs7"])
                    V(lambda e: e.tensor_reduce(out=R(25, 26), in_=R(21, 25), axis=AX.X, op=ALU.max), ["rs7"], ["rs8"])
                    V(lambda e: e.tensor_scalar(out=R(26, 30), in0=R(21, 25), scalar1=R(25, 26), scalar2=None, op0=ALU.is_ge), ["rs7", "rs8"], ["rs9"])
                    V(lambda e: e.tensor_tensor(out=R(30, 31), in0=R(16, 17), in1=R(25, 26), op=ALU.subtract), ["rs5", "rs8"], ["rs10"])
                    A(lambda e: e.activation(out=R(31, 32), in_=R(30, 31), func=AF.Sigmoid), ["rs10"], ["rs11"])
                    V(lambda e: e.tensor_scalar(out=R(32, 33), in0=R(31, 32), scalar1=-1.0, scalar2=1.0, op0=ALU.mult, op1=ALU.add), ["rs11"], ["rs12"])
                    V(lambda e: e.tensor_scalar(out=R(33, 37), in0=R(17, 21), scalar1=R(31, 32), scalar2=None, op0=ALU.mult), ["rs6", "rs11"], ["rs13"])
                    V(lambda e: e.scalar_tensor_tensor(out=R(33, 37), in0=R(26, 30), scalar=R(32, 33), in1=R(33, 37), op0=ALU.mult, op1=ALU.add),
                      ["rs9", "rs12", "rs13"], ["rs13"])
                    V(lambda e: e.tensor_scalar(out=R(33, 37), in0=R(33, 37), scalar1=R(11, 12), scalar2=None, op0=ALU.mult), ["rs13", "rs3"], ["rs13"])
                    for gi in range(4):
                        V(lambda e, gi=gi: e.tensor_scalar(out=gates[:, g, 4 * gi:4 * gi + 4], in0=R(33, 37), scalar1=R(1 + gi, 2 + gi), scalar2=None, op0=ALU.mult),
                          ["rs13", "rs"], [("gates", g)])

                for g_ in range(NG):
                    merge_tile(g_)
                S.barrier()
                S.emit()

            with contextlib.ExitStack() as pd:
                NH = 2 if NG >= 2 else 1
                TPH = NG // NH
                hacc = sbt(pd, "hacc", [128, TPH, D], F32)
                w13 = [sbt(pd, f"w13_{i}", [128, 8, 512], BF16) for i in range(2)]
                w2s = [sbt(pd, f"w2s_{i}", [128, 2, D], BF16) for i in range(2)]
                gfin = sbt(pd, "gfin", [128, D], F32)
                sa = [sbt(pd, f"sa{i}", [128, 256], F32) for i in range(2)]
                hid = [sbt(pd, f"hid{i}", [128, 256], BF16) for i in range(2)]
                hidT = [sbt(pd, f"hidT{i}", [128, 2, 128], BF16) for i in range(2)]
                fjunk = sbt(pd, "fjunk", [128, D], BF16)
                fstat = sbt(pd, "fstat", [128, 4], F32)
                ot = [sbt(pd, f"ot{i}", [128, D], F32) for i in range(2)]
                pab = [pst(pd, f"pab{i}", [128, 512], F32) for i in range(2)]
                phT = [pst(pd, f"phT{i}", [128, 2, 128], BF16) for i in range(2)]
                py = [pst(pd, f"py{i}", [128, D], F32) for i in range(2)]
                DM(lambda e: e.dma_start(out=gfin[:], in_=gfin_rep), [], ["gfin"], "c6")

                def load_expert(ex, ws):
                    for k in range(8):
                        DG(lambda e, k=k: e.dma_start(out=w13[ws][:, k, 0:256], in_=w1[ex, k * 128:(k + 1) * 128, :]), [], [f"w13_{ws}"], f"w13_{ws}")
                        DG(lambda e, k=k: e.dma_start(out=w13[ws][:, k, 256:512], in_=w3[ex, k * 128:(k + 1) * 128, :]), [], [f"w13_{ws}"], f"w13_{ws}")
                    for f in range(2):
                        DG(lambda e, f=f: e.dma_start(out=w2s[ws][:, f, :], in_=w2[ex, f * 128:(f + 1) * 128, :]), [], [f"w2s_{ws}"], f"w2s_{ws}")
                        G(lambda e, f=f: e.tensor_tensor(out=w2s[ws][:, f, :], in0=w2s[ws][:, f, :], in1=g2rep[:], op=ALU.mult),
                          [f"w2s_{ws}", "mod"], [f"w2s_{ws}"])

                def u_ab(u, g, ex, ws):
                    b = u % 2
                    for k in range(8):
                        P(lambda e, k=k: e.matmul(pab[b][:], lhsT=u2T[:, g, k, :], rhs=w13[ws][:, k, :], start=(k == 0), stop=(k == 7)),
                          [("u2T", g), f"w13_{ws}"], [f"pab{b}"])

                def u_act_tr(u, g, ex, ws):
                    b = u % 2
                    A(lambda e: e.activation(out=sa[b][:], in_=pab[b][:, 0:256], func=AF.Silu), [f"pab{b}"], [f"sa{b}"])
                    V(lambda e: e.scalar_tensor_tensor(out=hid[b][:], in0=pab[b][:, 256:512], scalar=gates[:, g, ex:ex + 1], in1=sa[b][:],
                                                       op0=ALU.mult, op1=ALU.mult), [f"pab{b}", ("gates", g), f"sa{b}"], [f"hid{b}"])
                    for f in range(2):
                        P(lambda e, f=f: e.transpose(out=phT[b][:, f, :], in_=hid[b][:, f * 128:(f + 1) * 128], identity=idb[:]), [f"hid{b}", "idb"], [f"phT{b}"])
                    A(lambda e: e.activation(out=hidT[b][:], in_=phT[b][:], func=AF.Copy), [f"phT{b}"], [f"hidT{b}"])

                def u_y(u, g, ex, ws, tl):
                    b = u % 2
                    for hf in range(2):
                        cs_ = slice(hf * 512, (hf + 1) * 512)
                        for f in range(2):
                            P(lambda e, f=f, cs_=cs_: e.matmul(py[b][:, cs_], lhsT=hidT[b][:, f, :], rhs=w2s[ws][:, f, cs_], start=(f == 0), stop=(f == 1)),
                              [f"hidT{b}", f"w2s_{ws}"], [(f"py{b}", hf)])
                    V(lambda e: e.tensor_tensor(out=hacc[:, tl, :], in0=hacc[:, tl, :], in1=py[b][:], op=ALU.add),
                      [("hacc", tl), (f"py{b}", 0), (f"py{b}", 1)], [("hacc", tl)])

                for hh in range(NH):
                    for tl in range(TPH):
                        g = hh * TPH + tl
                        DM(lambda e, tl=tl, g=g: e.dma_start(out=hacc[:, tl, :], in_=h1_s[g]), [("h1_s", g)], [("hacc", tl)], f"ld_h{tl}")
                    units = []
                    for ex in range(16):
                        for tl in range(TPH):
                            units.append((hh * TPH + tl, ex, (hh * 16 + ex) % 2, tl))
                    NU = len(units)
                    load_expert(0, (hh * 16) % 2)
                    loaded = {0}
                    g0_, ex0_, ws0_, tl0_ = units[0]
                    u_ab(0, g0_, ex0_, ws0_)
                    for u in range(NU):
                        g, ex, ws, tl = units[u]
                        deferred = False
                        if u + 1 < NU:
                            g2_, ex2_, ws2_, tl2_ = units[u + 1]
                            if ex2_ in loaded:
                                u_ab(u + 1, g2_, ex2_, ws2_)
                            else:
                                deferred = True
                        u_act_tr(u, g, ex, ws)
                        if u >= 1:
                            g1_, ex1_, ws1_, tl1_ = units[u - 1]
                            u_y(u - 1, g1_, ex1_, ws1_, tl1_)
                        if tl == 0 and ex + 1 < 16:
                            load_expert(ex + 1, (hh * 16 + ex + 1) % 2)
                            loaded.add(ex + 1)
                        if deferred:
                            u_ab(u + 1, g2_, ex2_, ws2_)
                    g1_, ex1_, ws1_, tl1_ = units[NU - 1]
                    u_y(NU - 1, g1_, ex1_, ws1_, tl1_)
                    for tl in range(TPH):
                        g = hh * TPH + tl
                        b = tl % 2
                        A(lambda e, tl=tl: e.activation(out=fjunk[:], in_=hacc[:, tl, :], func=AF.Square, scale=1.0 / 32.0, accum_out=fstat[:, 0:1]),
                          [("hacc", tl)], ["fjunk", "fstat"])
                        A(lambda e: e.activation(out=fstat[:, 1:2], in_=fstat[:, 0:1], func=AF.Ln, bias=epsc[:, 0:1]), ["fstat", "epsc"], ["fstat"])
                        A(lambda e: e.activation(out=fstat[:, 2:3], in_=fstat[:, 1:2], func=AF.Exp, scale=-0.5), ["fstat"], ["fstat"])
                        V(lambda e, tl=tl, b=b: e.scalar_tensor_tensor(out=ot[b][:], in0=hacc[:, tl, :], scalar=fstat[:, 2:3], in1=gfin[:], op0=ALU.mult, op1=ALU.mult),
                          [("hacc", tl), "fstat", "gfin"], [f"ot{b}"])
                        DM(lambda e, g=g, b=b: e.dma_start(out=out[g * 128:(g + 1) * 128, :], in_=ot[b][:]), [f"ot{b}"], [("out", g)], f"st_o{b}")
                S.op("sp", None, reads=[("out", g) for g in range(NG)])
                S.barrier()
                S.emit()
    return nc


build.phases = 3


def _rel_bucket(rel):
    nb = 16
    ret = (rel > 0).astype(np.int64) * nb
    n = np.abs(rel)
    max_exact = nb // 2
    nf = np.maximum(n, 1).astype(np.float32)
    large = max_exact + (np.log(nf / max_exact) / math.log(128 / max_exact) * (nb - max_exact)).astype(np.int32)
    large = np.minimum(large, nb - 1)
    return ret + np.where(n < max_exact, n, large)


def make_inputs(NG, inp):
    NT = 4 * NG
    SB = NT * 128
    f = np.float32
    x = np.asarray(inp["x"], f)
    assert x.shape[1] == SB
    pos = np.arange(SB, dtype=np.float64)
    freqs = 10000.0 ** (-np.arange(0, 64, 2, dtype=np.float64) / 64)
    ang = (pos[:, None].astype(np.float32) * freqs[None, :].astype(np.float32)).astype(np.float32)
    rope_all = np.concatenate([np.cos(ang), np.sin(ang)], axis=1).astype(f)
    gam = np.array(GAM, dtype=np.float64)
    i_ = np.arange(128)
    ci = i_ // 64
    DT = np.zeros((128, H, 128), f)
    for h in range(H):
        same = ci[:, None] == ci[None, :]
        dm = np.where(same, gam[h] ** np.abs(i_[:, None] - i_[None, :]),
                      np.where((ci[:, None] == 1) & (ci[None, :] == 0), gam[h] ** (i_[:, None] - i_[None, :]).clip(0), 0.0))
        DT[:, h, :] = (dm.T * 0.125).astype(f)
    qdec = (gam[None, :] ** (i_[:, None] + 1)).astype(f)
    wtile = (gam[None, :] ** (127 - i_[:, None]) * 0.125).astype(f)
    ident = np.eye(128, dtype=f)
    selden = np.zeros((65, 64), f); selden[64, :] = 1.0
    rb = np.asarray(inp["rel_bias"], f)

    def rep(v, n=128):
        return np.ascontiguousarray(np.broadcast_to(np.asarray(v, f).reshape(1, -1), (n, np.asarray(v).size)))

    def fm(v):
        return np.ascontiguousarray(np.asarray(v, f).reshape(-1, 128).T)

    b_ada = np.asarray(inp["b_ada"], f)[0]
    common = dict(
        w_ada=np.ascontiguousarray(inp["w_ada"][0], dtype=f), b_adaT=fm(b_ada),
        b_g12=np.ascontiguousarray(np.concatenate([rep(b_ada[2 * D:3 * D]), rep(b_ada[5 * D:6 * D])], axis=1)),
        gmixT=fm(inp["norm_mix_g"][0]), gffnT=fm(inp["norm_ffn_g"][0]),
        gfin_rep=rep(inp["norm_final_g"]), gn_rep=rep(inp["ret_gn_g"][0]), kvg_rep=rep(inp["dsa_kv_norm_g"][0]),
        w_in=np.ascontiguousarray(inp["w_in"][0], dtype=f), w_kv=np.ascontiguousarray(inp["w_dsa_kv_up"][0], dtype=f),
        w_ret_out=np.ascontiguousarray(inp["w_ret_out"][0], dtype=f), w_dsa_out=np.ascontiguousarray(inp["w_dsa_out"][0], dtype=f),
        w_gate=np.ascontiguousarray(inp["w_gate"][0], dtype=f), b_gate=np.ascontiguousarray(inp["b_gate"], dtype=f).reshape(1, -1),
        w_o=np.ascontiguousarray(inp["w_o"][0], dtype=f),
        w_rt=np.ascontiguousarray(np.concatenate([inp["w_group_router"][0], inp["w_expert_router"][0]], axis=1), dtype=f),
        b_rt_rep=rep(np.concatenate([inp["b_group_router"][0], inp["b_expert_router"][0]])),
        w1=np.ascontiguousarray(inp["w_exp_gate"][0], dtype=f), w3=np.ascontiguousarray(inp["w_exp_up"][0], dtype=f),
        w2=np.ascontiguousarray(inp["w_exp_down"][0], dtype=f),
        rope_all=rope_all, ident=ident, DT=DT, qdec=qdec, wtile=wtile, selden=selden, c15_rep=rep(rb[15]),
    )
    maps = []
    for c in range(8):
        b, r = c // 4, c % 4
        own = np.array([4 * g + r for g in range(NG)])
        rows = (own[:, None] * 128 + np.arange(128)[None, :]).reshape(-1)
        sel = np.zeros((64, 4), f); sel[:, r] = 1.0
        madd = np.zeros((128, 512), f)
        kt = np.arange(512) // 128
        kc = (np.arange(512) % 128) // 64
        qc = np.arange(128) // 64
        inadm = (kt[None, :] > r) | ((kt[None, :] == r) & (kc[None, :] > qc[:, None]))
        madd[inadm] = NEG
        biasT = np.zeros((5, 128, H, 128), f)
        for s5 in range(5):
            rel = (s5 - 1 - r) * 128 + np.arange(128)[:, None] - np.arange(128)[None, :]
            bk = _rel_bucket(rel)
            biasT[s5] = np.transpose(rb[bk], (0, 2, 1))
        m = dict(common)
        m.update(xb=np.ascontiguousarray(x[b]), xo=np.ascontiguousarray(x[b][rows]),
                 ccol=fm(inp["c"][b]), rope_own=np.ascontiguousarray(rope_all[rows]),
                 sel=sel, madd=madd, biasT=biasT)
        maps.append(m)
    return maps


_NC_CACHE = {}


def kernel(**inputs):
    NG = 32
    if NG not in _NC_CACHE:
        _NC_CACHE[NG] = build(NG)
    nc = _NC_CACHE[NG]
    maps = make_inputs(NG, inputs)
    res = run_bass_kernel_spmd(nc, maps, core_ids=list(range(8)))
    B, SEQ = inputs["x"].shape[0], inputs["x"].shape[1]
    outp = np.zeros((B, SEQ, D), np.float32)
    for c in range(8):
        b, r = c // 4, c % 4
        o = np.asarray(res.results[c]["out"]).reshape(NG, 128, D)
        for g in range(NG):
            t = 4 * g + r
            outp[b, t * 128:(t + 1) * 128, :] = o[g]
    return outp
```

```python
import contextlib
import math
import numpy as np
import concourse.bass as bass
import concourse.mybir as mybir
from concourse.bass_utils import run_bass_kernel_spmd

F32 = mybir.dt.float32
BF16 = mybir.dt.bfloat16
AF = mybir.ActivationFunctionType
ALU = mybir.AluOpType
AX = mybir.AxisListType

D = 1024
H = 8
NEG = -1.0e30
NBIS = 11
GAM = [1.0 - 2.0 ** (-5.0 - h) for h in range(H)]


class Sched:
    ENGS = ("pe", "act", "dve", "pool", "sp")
    ROT = 3000

    def __init__(self, nc, stack):
        self.nc = nc
        self.stack = stack
        self.ops = {e: [] for e in self.ENGS}
        self.last_write = {}
        self.readers = {}
        self.nsem = 0
        self.cur = {}
        self.dma = {}
        self.seen = {e: {} for e in self.ENGS}
        self.pe_sems = set()
        self.latest = {}

    def _newsem(self, name):
        self.nsem += 1
        return self.stack.enter_context(self.nc.semaphore(f"s{self.nsem}_{name}"))

    def _tok_compute(self, eng):
        c = self.cur.get(eng)
        if c is None or c[1] >= self.ROT:
            c = [self._newsem(eng), 0]
            self.cur[eng] = c
            if eng == "pe":
                self.pe_sems.add(id(c[0]))
        c[1] += 1
        return (c[0], c[1])

    def _tok_dma(self, key):
        c = self.dma.get(key)
        if c is None or c[1] >= 16 * 1500:
            c = [self._newsem("d"), 0]
            self.dma[key] = c
        c[1] += 16
        return (c[0], c[1])

    def op(self, eng, fn, reads=(), writes=(), dma_key=None, extra=()):
        deps = {}

        def add(tok):
            if tok is None:
                return
            s, v = tok
            k = id(s)
            if k not in deps or deps[k][1] < v:
                deps[k] = (s, v)

        for r in reads:
            add(self.last_write.get(r))
        for w in writes:
            add(self.last_write.get(w))
            for t in self.readers.get(w, ()):
                add(t)
        for t in extra:
            add(t)
        if fn is None:
            tok = None
        elif dma_key is not None:
            tok = self._tok_dma(dma_key)
        else:
            tok = self._tok_compute(eng)
        waits = []
        seen = self.seen[eng]
        for k, (s, v) in deps.items():
            if eng == "pe" and dma_key is None and k in self.pe_sems:
                continue
            if seen.get(k, 0) >= v:
                continue
            seen[k] = v
            waits.append((s, v))
        if tok is not None:
            self.latest[id(tok[0])] = tok
            for r in reads:
                lst = self.readers.setdefault(r, [])
                lst.append(tok)
                if len(lst) > 16:
                    best = {}
                    for (s, v) in lst:
                        if id(s) not in best or best[id(s)][1] < v:
                            best[id(s)] = (s, v)
                    self.readers[r] = list(best.values())
            for w in writes:
                self.last_write[w] = tok
                self.readers[w] = []
        self.ops[eng].append((fn, waits, tok, dma_key is not None))
        return tok

    def barrier(self):
        toks = list(self.latest.values())
        for e in self.ENGS:
            self.op(e, None, extra=toks)

    def emit(self):
        nc = self.nc
        ops = self.ops
        self.ops = {e: [] for e in self.ENGS}
        with nc.Block() as block:
            def run(engobj, name):
                for fn, waits, tok, is_dma in ops[name]:
                    for s, v in waits:
                        engobj.wait_ge(s, v)
                    if fn is not None:
                        ins = fn(engobj)
                        ins.then_inc(tok[0], 16 if is_dma else 1)

            @block.tensor
            def _(e):
                run(e, "pe")

            @block.scalar
            def _(e):
                run(e, "act")

            @block.vector
            def _(e):
                run(e, "dve")

            @block.gpsimd
            def _(e):
                run(e, "pool")

            @block.sync
            def _(e):
                run(e, "sp")


C_RQ, C_RK, C_RV, C_RG, C_DQ, C_DKV, C_IQ, C_IK, C_IW = 0, 512, 1024, 2048, 3072, 3584, 3712, 4224, 4288
D_IN = 4296


def build(NG, dbg=False):
    NT = 4 * NG
    SB = NT * 128
    NO = NG * 128
    nc = bass.Bass("TRN2", target_bir_lowering=False)

    def din(name, shape, dt=F32):
        return nc.dram_tensor(name, list(shape), dt, kind="ExternalInput").ap()

    def dscr(name, shape, dt):
        if dbg:
            return nc.dram_tensor(name, list(shape), dt, kind="ExternalOutput").ap()
        return nc.dram_tensor(name, list(shape), dt).ap()

    xb = din("xb", [SB, D]); xo = din("xo", [NO, D]); ccol = din("ccol", [128, 8])
    w_ada = din("w_ada", [D, 6 * D]); b_adaT = din("b_adaT", [128, 48]); b_g12 = din("b_g12", [128, 2 * D])
    gmixT = din("gmixT", [128, 8]); gffnT = din("gffnT", [128, 8])
    gfin_rep = din("gfin_rep", [128, D]); gn_rep = din("gn_rep", [128, D]); kvg_rep = din("kvg_rep", [128, 128])
    w_in = din("w_in", [D, D_IN]); w_kv = din("w_kv", [128, 128])
    w_ret_out = din("w_ret_out", [D, D]); w_dsa_out = din("w_dsa_out", [512, D])
    w_gate = din("w_gate", [D, 2 * D]); b_gate = din("b_gate", [1, 2 * D]); w_o = din("w_o", [D, D])
    w_rt = din("w_rt", [D, 20]); b_rt_rep = din("b_rt_rep", [128, 20])
    w1 = din("w1", [16, D, 256]); w3 = din("w3", [16, D, 256]); w2 = din("w2", [16, 256, D])
    rope_all = din("rope_all", [SB, 64]); rope_own = din("rope_own", [NO, 64])
    ident = din("ident", [128, 128]); DTd = din("DT", [128, H, 128]); qdec = din("qdec", [128, H])
    wtile = din("wtile", [128, H]); seld = din("sel", [64, 4]); maddd = din("madd", [128, 512])
    biasTd = din("biasT", [5, 128, H, 128]); c15 = din("c15_rep", [128, H]); seldend = din("selden", [65, 64])
    out = nc.dram_tensor("out", [NO, D], F32, kind="ExternalOutput").ap()

    uT_s = dscr("uT_s", [NG, 128, 8, 128], BF16)
    roT_s = dscr("roT_s", [NG, 128, 8, 128], BF16)
    dqT_s = dscr("dqT_s", [NG, 64, H, 128], BF16)
    iqT_s = dscr("iqT_s", [NG, 64, H, 128], BF16)
    iw_s = dscr("iw_s", [NG, 128, H], F32)
    ydT_s = dscr("ydT_s", [NG, 64, H, 128], BF16)
    h1_s = dscr("h1_s", [NG, 128, D], F32)
    if dbg:
        kT_dbg = dscr("kT_dbg", [128, SB], BF16)
        dv_dbg = dscr("dv_dbg", [128, NT, 65], BF16)
        thr_dbg = dscr("thr_dbg", [NG, 128, 8], F32)
        sc_dbg = dscr("sc_dbg", [NG, 128, SB], BF16)
        mk_dbg = dscr("mk_dbg", [NG, 128, SB], BF16)

    with contextlib.ExitStack() as top:
        S = Sched(nc, top)

        def sbt(st, n, sh, dt):
            return st.enter_context(nc.sbuf_tensor("sb_" + n, list(sh), dt))

        def pst(st, n, sh, dt):
            return st.enter_context(nc.psum_tensor("ps_" + n, list(sh), dt))

        V = lambda fn, r, w: S.op("dve", fn, reads=r, writes=w)
        A = lambda fn, r, w: S.op("act", fn, reads=r, writes=w)
        P = lambda fn, r, w: S.op("pe", fn, reads=r, writes=w)
        G = lambda fn, r, w: S.op("pool", fn, reads=r, writes=w)
        DM = lambda fn, r, w, k: S.op("sp", fn, reads=r, writes=w, dma_key=k)
        DG = lambda fn, r, w, k: S.op("pool", fn, reads=r, writes=w, dma_key=k)

        idb = sbt(top, "idb", [128, 128], BF16)
        epsc = sbt(top, "epsc", [128, 2], F32)
        G1T = sbt(top, "G1T", [128, 8], F32); sh1T = sbt(top, "sh1T", [128, 8], F32)
        G2T = sbt(top, "G2T", [128, 8], F32); sh2T = sbt(top, "sh2T", [128, 8], F32)
        g1rep = sbt(top, "g1rep", [128, D], BF16); g2rep = sbt(top, "g2rep", [128, D], BF16)
        DG(lambda e: e.dma_start(out=idb[:], in_=ident), [], ["idb"], "c0")
        V(lambda e: e.memset(epsc[:, 0:1], 1e-6), [], ["epsc"])
        V(lambda e: e.memset(epsc[:, 1:2], 1e-5), [], ["epsc"])

        def norm_T(x_ap, xres, junk, xs, pT, uT, stat, GT, shT, tag, ures=None):
            ures = ures or (tag + "uT")
            A(lambda e: e.activation(out=junk, in_=x_ap, func=AF.Square, scale=1.0 / 32.0, accum_out=stat[:, 0:1]),
              [xres], [tag + "junk", tag + "st"])
            A(lambda e: e.activation(out=stat[:, 1:2], in_=stat[:, 0:1], func=AF.Ln, bias=epsc[:, 0:1]),
              [tag + "st", "epsc"], [tag + "st"])
            A(lambda e: e.activation(out=stat[:, 2:3], in_=stat[:, 1:2], func=AF.Exp, scale=-0.5),
              [tag + "st"], [tag + "st"])
            V(lambda e: e.tensor_scalar(out=xs, in0=x_ap, scalar1=stat[:, 2:3], scalar2=None, op0=ALU.mult),
              [xres, tag + "st"], [tag + "xs"])
            for k in range(8):
                P(lambda e, k=k: e.transpose(out=pT[:, k, :], in_=xs[:, k * 128:(k + 1) * 128], identity=idb[:]),
                  [tag + "xs", "idb"], ["pT"])
            for k in range(8):
                V(lambda e, k=k: e.tensor_scalar(out=uT[:, k, :], in0=pT[:, k, :], scalar1=GT[:, k:k + 1],
                                                 scalar2=shT[:, k:k + 1], op0=ALU.mult, op1=ALU.add),
                  ["pT", "mod"], [ures])

        def rope(src, cosb, sinb, dst, ta, tb, rres, wres, tag):
            s1, s2 = src[:, :, 0:32], src[:, :, 32:64]
            V(lambda e: e.tensor_tensor(out=ta, in0=s1, in1=cosb, op=ALU.mult), rres, [tag + "ta"])
            V(lambda e: e.tensor_tensor(out=tb, in0=s2, in1=sinb, op=ALU.mult), rres, [tag + "tb"])
            V(lambda e: e.tensor_tensor(out=dst[:, :, 0:32], in0=ta, in1=tb, op=ALU.subtract), [tag + "ta", tag + "tb"], wres)
            V(lambda e: e.tensor_tensor(out=ta, in0=s2, in1=cosb, op=ALU.mult), rres, [tag + "ta"])
            V(lambda e: e.tensor_tensor(out=tb, in0=s1, in1=sinb, op=ALU.mult), rres, [tag + "tb"])
            V(lambda e: e.tensor_tensor(out=dst[:, :, 32:64], in0=ta, in1=tb, op=ALU.add), [tag + "ta", tag + "tb"], wres)

        with contextlib.ExitStack() as ph:
            cs = sbt(ph, "cs", [128, 8], F32); csb = sbt(ph, "csb", [128, 8], BF16)
            csbc = sbt(ph, "csbc", [128, 8, 128], BF16)
            wa = [sbt(ph, f"wa{i}", [128, 8, D], BF16) for i in range(2)]
            badT = sbt(ph, "badT", [128, 48], F32); bg12 = sbt(ph, "bg12", [128, 2 * D], F32)
            gmx = sbt(ph, "gmx", [128, 8], F32); gff = sbt(ph, "gff", [128, 8], F32)
            modT = sbt(ph, "modT", [128, 32], F32)
            psm = pst(ph, "psm", [128, 32], F32)
            pg = pst(ph, "pg", [128, D], F32)
            DM(lambda e: e.dma_start(out=cs[:], in_=ccol), [], ["cs"], "c1_cs")
            DM(lambda e: e.dma_start(out=badT[:], in_=b_adaT), [], ["badT"], "c1_badT")
            DM(lambda e: e.dma_start(out=bg12[:], in_=b_g12), [], ["bg12"], "c1_bg12")
            DM(lambda e: e.dma_start(out=gmx[:], in_=gmixT), [], ["gmx"], "c1_gmx")
            DM(lambda e: e.dma_start(out=gff[:], in_=gffnT), [], ["gff"], "c1_gff")
            A(lambda e: e.activation(out=csb[:], in_=cs[:], func=AF.Silu), ["cs"], ["csb"])
            V(lambda e: e.tensor_copy(out=csbc[:], in_=csb[:].unsqueeze(2).broadcast_to([128, 8, 128])), ["csb"], ["csbc"])
            fm_i = 0
            for p in range(6):
                sl = p % 2
                for k in range(8):
                    DG(lambda e, p=p, k=k, sl=sl: e.dma_start(out=wa[sl][:, k, :], in_=w_ada[k * 128:(k + 1) * 128, p * D:(p + 1) * D]),
                       [], [f"wa{sl}"], f"wa{sl}")
                if p in (2, 5):
                    for hf in range(2):
                        for k in range(8):
                            P(lambda e, k=k, hf=hf, sl=sl: e.matmul(pg[:, hf * 512:(hf + 1) * 512], lhsT=csbc[:, k, :],
                                                                  rhs=wa[sl][:, k, hf * 512:(hf + 1) * 512], start=(k == 0), stop=(k == 7)),
                              ["csbc", f"wa{sl}"], ["pg"])
                    dst = g1rep if p == 2 else g2rep
                    off = 0 if p == 2 else D
                    V(lambda e, dst=dst, off=off: e.tensor_tensor(out=dst[:], in0=pg[:], in1=bg12[:, off:off + D], op=ALU.add),
                      ["pg", "bg12"], ["mod"])
                else:
                    for f in range(8):
                        for k in range(8):
                            P(lambda e, k=k, f=f, sl=sl, c=fm_i * 8 + f: e.matmul(psm[:, c:c + 1], lhsT=wa[sl][:, k, f * 128:(f + 1) * 128],
                                                                                 rhs=csb[:, k:k + 1], start=(k == 0), stop=(k == 7)),
                              ["csb", f"wa{sl}"], ["psm"])
                    fm_i += 1
            for i, p in enumerate((0, 1, 3, 4)):
                V(lambda e, i=i, p=p: e.tensor_tensor(out=modT[:, i * 8:(i + 1) * 8], in0=psm[:, i * 8:(i + 1) * 8],
                                                      in1=badT[:, p * 8:(p + 1) * 8], op=ALU.add), ["psm", "badT"], ["modT"])
            V(lambda e: e.tensor_copy(out=sh1T[:], in_=modT[:, 0:8]), ["modT"], ["mod"])
            V(lambda e: e.tensor_copy(out=sh2T[:], in_=modT[:, 16:24]), ["modT"], ["mod"])
            V(lambda e: e.scalar_tensor_tensor(out=G1T[:], in0=modT[:, 8:16], scalar=1.0, in1=gmx[:], op0=ALU.add, op1=ALU.mult),
              ["modT", "gmx"], ["mod"])
            V(lambda e: e.scalar_tensor_tensor(out=G2T[:], in0=modT[:, 24:32], scalar=1.0, in1=gff[:], op0=ALU.add, op1=ALU.mult),
              ["modT", "gff"], ["mod"])
            S.barrier()
            S.emit()

        kvs = contextlib.ExitStack()
        kT = sbt(kvs, "kT", [128, SB], BF16)
        dv1 = sbt(kvs, "dv1", [128, NT, 65], BF16)

        with contextlib.ExitStack() as ph:
            w_in_sb = sbt(ph, "w_in_sb", [128, 8, D_IN], BF16)
            wkv_sb = sbt(ph, "wkv_sb", [128, 192], BF16)
            kvg = sbt(ph, "kvg", [128, 128], F32); gnr = sbt(ph, "gnr", [128, D], F32)
            DTs = sbt(ph, "DTs", [128, H, 128], BF16); qdc = sbt(ph, "qdc", [128, H], F32)
            wtl = sbt(ph, "wtl", [128, H], F32); sels = sbt(ph, "sels", [64, 4], F32)
            xt = [sbt(ph, f"xt{i}", [128, D], F32) for i in range(2)]
            rp = [sbt(ph, f"rp{i}", [128, 64], F32) for i in range(2)]
            junk = sbt(ph, "junk", [128, D], BF16)
            xs = sbt(ph, "xs", [128, D], BF16)
            uT = [sbt(ph, f"uT{i}", [128, 8, 128], BF16) for i in range(2)]
            stat = sbt(ph, "stat", [128, 8], F32)
            ta = sbt(ph, "ta", [128, H, 32], F32); tb = sbt(ph, "tb", [128, H, 32], F32)
            krp = sbt(ph, "krp", [128, H, 64], F32); kw = sbt(ph, "kw", [128, H, 64], BF16)
            vb = sbt(ph, "vb", [128, D], BF16)
            Sst = sbt(ph, "Sst", [64, H, 128], F32); T0 = sbt(ph, "T0", [64, H, 128], F32)
            T0b = sbt(ph, "T0b", [64, H, 128], BF16)
            latn = sbt(ph, "latn", [128, 128], BF16); latnT = sbt(ph, "latnT", [128, 128], BF16)
            qr = sbt(ph, "qr", [128, H, 64], BF16); kro = sbt(ph, "kro", [128, H, 64], BF16)
            qT = sbt(ph, "qT", [64, H, 128], BF16); kTo = sbt(ph, "kTo", [64, H, 128], BF16)
            scm = sbt(ph, "scm", [128, H, 128], BF16)
            sg = sbt(ph, "sg", [128, D], BF16)
            oo = sbt(ph, "oo", [128, H, 128], F32); o2 = sbt(ph, "o2", [128, H, 128], F32)
            gst = sbt(ph, "gst", [128, 40], F32)
            ro = sbt(ph, "ro", [128, D], BF16); roT = sbt(ph, "roT", [128, 8, 128], BF16)
            dqb = sbt(ph, "dqb", [128, 512], BF16); iqb = sbt(ph, "iqb", [128, 512], BF16)
            dqT = sbt(ph, "dqT", [64, H, 128], BF16); iqT = dqT
            iwb = sbt(ph, "iwb", [128, H], F32)
            pT = pst(ph, "pT", [128, 8, 128], BF16)
            zA = pst(ph, "zA", [128, D], F32); zB = pst(ph, "zB", [128, D], F32)
            misc = pst(ph, "misc", [128, 512], F32)
            kvp = pst(ph, "kvp", [128, H, 128], F32)
            pTs = misc[:, 448:512].bitcast(BF16)

            for k in range(8):
                DG(lambda e, k=k: e.dma_start(out=w_in_sb[:, k, :], in_=w_in[k * 128:(k + 1) * 128, :]), [], ["w_in_sb"], "w_in")
            V(lambda e: e.memset(wkv_sb[:, 0:64], 0.0), [], ["wkv0"])
            DG(lambda e: e.dma_start(out=wkv_sb[:, 64:192], in_=w_kv), [], ["wkv1"], "c2_wkv")
            DG(lambda e: e.dma_start(out=DTs[:], in_=DTd), [], ["DTs"], "c2_DT")
            DM(lambda e: e.dma_start(out=kvg[:], in_=kvg_rep), [], ["kvg"], "c3_kvg")
            DM(lambda e: e.dma_start(out=gnr[:], in_=gn_rep), [], ["gnr"], "c3_gnr")
            DM(lambda e: e.dma_start(out=qdc[:], in_=qdec), [], ["qdc"], "c3_qdc")
            DM(lambda e: e.dma_start(out=wtl[:], in_=wtile), [], ["wtl"], "c3_wtl")
            DM(lambda e: e.dma_start(out=sels[:], in_=seld), [], ["sels"], "c3_sels")
            V(lambda e: e.memset(Sst[:], 0.0), [], ["Sst"])
            V(lambda e: e.memset(dv1[:, :, 64:65], 1.0), [], ["dv1one"])

            def kside_s1(pos, j):
                sl = pos % 2
                DM(lambda e: e.dma_start(out=xt[sl][:], in_=xb[j * 128:(j + 1) * 128, :]), [], [f"xt{sl}"], f"xt{sl}")
                DM(lambda e: e.dma_start(out=rp[sl][:], in_=rope_all[j * 128:(j + 1) * 128, :]), [], [f"rp{sl}"], f"rp{sl}")
                norm_T(xt[sl][:], f"xt{sl}", junk[:], xs[:], pT, uT[sl], stat, G1T, sh1T, "k", ures=f"kuT{sl}")

            def kside(pos, j):
                sl = pos % 2
                s = j % 4
                ures = [f"kuT{sl}", "w_in_sb"]
                for (dst, c0, n, res) in ((zA[:, 0:512], C_RK, 512, ("zA", 0)), (zB[:, 0:512], C_RV, 512, ("zB", 0)),
                                          (zB[:, 512:1024], C_RV + 512, 512, ("zB", 1)), (misc[:, 0:128], C_DKV, 128, "m_dkv")):
                    for k in range(8):
                        P(lambda e, k=k, dst=dst, c0=c0, n=n: e.matmul(dst, lhsT=uT[sl][:, k, :], rhs=w_in_sb[:, k, c0:c0 + n],
                                                                      start=(k == 0), stop=(k == 7)), ures, [res])
                for k in range(8):
                    P(lambda e, k=k: e.matmul(misc[0:64, 128:256], lhsT=w_in_sb[:, k, C_IK:C_IK + 64], rhs=uT[sl][:, k, :],
                                              start=(k == 0), stop=(k == 7)), ures, ["m_ik"])
                cosb = rp[sl][:, 0:32].unsqueeze(1).broadcast_to([128, H, 32])
                sinb = rp[sl][:, 32:64].unsqueeze(1).broadcast_to([128, H, 32])
                rope(zA[:, 0:512].rearrange("p (h d) -> p h d", h=H), cosb, sinb, krp, ta[:], tb[:], [("zA", 0), f"rp{sl}"], ["krp"], "r")
                V(lambda e: e.tensor_tensor(out=kw[:], in0=krp[:], in1=wtl[:].unsqueeze(2).broadcast_to([128, H, 64]), op=ALU.mult),
                  ["krp", "wtl"], ["kw"])
                A(lambda e: e.activation(out=vb[:], in_=zB[:], func=AF.Copy), [("zB", 0), ("zB", 1)], ["vb"])
                if s == 0:
                    V(lambda e: e.tensor_scalar(out=T0[:], in0=Sst[:], scalar1=sels[:, 0:1], scalar2=None, op0=ALU.mult),
                      ["Sst", "sels"], ["T0"])
                else:
                    V(lambda e: e.scalar_tensor_tensor(out=T0[:], in0=Sst[:], scalar=sels[:, s:s + 1], in1=T0[:], op0=ALU.mult, op1=ALU.add),
                      ["Sst", "sels", "T0"], ["T0"])
                for h in range(H):
                    P(lambda e, h=h: e.matmul(kvp[0:64, h, :], lhsT=kw[:, h, :], rhs=vb[:, h * 128:(h + 1) * 128], start=True, stop=True),
                      ["kw", "vb"], [("kvp", h // 4)])
                for h in range(H):
                    V(lambda e, h=h: e.scalar_tensor_tensor(out=Sst[:, h, :], in0=Sst[:, h, :], scalar=float(GAM[h] ** 128),
                                                           in1=kvp[0:64, h, :], op0=ALU.mult, op1=ALU.add),
                      ["Sst", ("kvp", h // 4)], ["Sst"])
                A(lambda e: e.activation(out=junk[:, 0:128], in_=misc[:, 0:128], func=AF.Square, scale=1.0 / math.sqrt(128.0),
                                         accum_out=stat[:, 4:5]), ["m_dkv"], ["kjunk", "st2"])
                A(lambda e: e.activation(out=stat[:, 5:6], in_=stat[:, 4:5], func=AF.Ln, bias=epsc[:, 0:1]), ["st2", "epsc"], ["st2"])
                A(lambda e: e.activation(out=stat[:, 6:7], in_=stat[:, 5:6], func=AF.Exp, scale=-0.5), ["st2"], ["st2"])
                V(lambda e: e.scalar_tensor_tensor(out=latn[:], in0=misc[:, 0:128], scalar=stat[:, 6:7], in1=kvg[:], op0=ALU.mult, op1=ALU.mult),
                  ["m_dkv", "st2", "kvg"], ["latn"])
                P(lambda e: e.transpose(out=pTs, in_=latn[:], identity=idb[:]), ["latn", "idb"], ["pTs"])
                A(lambda e: e.activation(out=latnT[:], in_=pTs, func=AF.Copy), ["pTs"], ["latnT"])
                P(lambda e: e.matmul(misc[:, 256:384], lhsT=wkv_sb[:, 0:128], rhs=latnT[:], start=True, stop=True),
                  ["latnT", "wkv0", "wkv1"], ["m_dk"])
                P(lambda e: e.matmul(misc[:, 384:448], lhsT=latnT[:], rhs=wkv_sb[:, 128:192], start=True, stop=True),
                  ["latnT", "wkv1"], ["m_dv"])
                A(lambda e: e.activation(out=kT[0:64, j * 128:(j + 1) * 128], in_=misc[0:64, 128:256], func=AF.Copy), ["m_ik"], [("kT", j)])
                A(lambda e: e.activation(out=kT[64:128, j * 128:(j + 1) * 128], in_=misc[64:128, 256:384], func=AF.Copy), ["m_dk"], [("kT", j)])
                V(lambda e: e.tensor_copy(out=dv1[:, j, 0:64], in_=misc[:, 384:448]), ["m_dv"], [("dv1", j)])

            def ownside_s1(pos, g):
                sl = pos % 2
                DM(lambda e: e.dma_start(out=xt[sl][:], in_=xo[g * 128:(g + 1) * 128, :]), [], [f"xt{sl}"], f"xt{sl}")
                DM(lambda e: e.dma_start(out=rp[sl][:], in_=rope_own[g * 128:(g + 1) * 128, :]), [], [f"rp{sl}"], f"rp{sl}")
                norm_T(xt[sl][:], f"xt{sl}", junk[:], xs[:], pT, uT[sl], stat, G1T, sh1T, "k", ures=f"kuT{sl}")
                DM(lambda e: e.dma_start(out=uT_s[g], in_=uT[sl][:]), [f"kuT{sl}"], [("uT_s", g)], "st_u")

            def ownside(pos, g):
                sl = pos % 2
                ures = [f"kuT{sl}", "w_in_sb"]
                u = uT[sl]

                def proj(dst, c0, n, res):
                    for k in range(8):
                        P(lambda e, k=k: e.matmul(dst, lhsT=u[:, k, :], rhs=w_in_sb[:, k, c0:c0 + n], start=(k == 0), stop=(k == 7)), ures, [res])

                cosb = rp[sl][:, 0:32].unsqueeze(1).broadcast_to([128, H, 32])
                sinb = rp[sl][:, 32:64].unsqueeze(1).broadcast_to([128, H, 32])
                proj(zA[:, 0:512], C_RQ, 512, ("zA", 0))
                proj(zA[:, 512:1024], C_RK, 512, ("zA", 1))
                rope(zA[:, 0:512].rearrange("p (h d) -> p h d", h=H), cosb, sinb, qr, ta[:], tb[:], [("zA", 0), f"rp{sl}"], ["qr"], "r")
                rope(zA[:, 512:1024].rearrange("p (h d) -> p h d", h=H), cosb, sinb, kro, ta[:], tb[:], [("zA", 1), f"rp{sl}"], ["kro"], "r")
                pT64 = pT[0:64, :, :]
                for h in range(H):
                    P(lambda e, h=h: e.transpose(out=pT64[:, h, :], in_=qr[:, h, :], identity=idb[:]), ["qr", "idb"], ["pT"])
                V(lambda e: e.tensor_copy(out=qT[:], in_=pT64), ["pT"], ["qT"])
                for h in range(H):
                    P(lambda e, h=h: e.transpose(out=pT64[:, h, :], in_=kro[:, h, :], identity=idb[:]), ["kro", "idb"], ["pT"])
                A(lambda e: e.activation(out=kTo[:], in_=pT64, func=AF.Copy), ["pT"], ["kTo"])
                proj(zB[:, 0:512], C_RV, 512, ("zB", 0))
                proj(zB[:, 512:1024], C_RV + 512, 512, ("zB", 1))
                A(lambda e: e.activation(out=vb[:], in_=zB[:], func=AF.Copy), [("zB", 0), ("zB", 1)], ["vb"])
                zA3 = zA[:].rearrange("p (h d) -> p h d", h=H)
                for h in range(H):
                    P(lambda e, h=h: e.matmul(zA3[:, h, :], lhsT=kTo[:, h, :], rhs=qT[:, h, :], start=True, stop=True),
                      ["kTo", "qT"], [("zA", h // 4)])
                V(lambda e: e.tensor_tensor(out=scm[:], in0=zA3, in1=DTs[:], op=ALU.mult), [("zA", 0), ("zA", 1), "DTs"], ["scm"])
                zB3 = zB[:].rearrange("p (h d) -> p h d", h=H)
                for h in range(H):
                    P(lambda e, h=h: e.matmul(zB3[:, h, :], lhsT=scm[:, h, :], rhs=vb[:, h * 128:(h + 1) * 128], start=True, stop=True),
                      ["scm", "vb"], [("zB", h // 4)])
                V(lambda e: e.tensor_copy(out=T0b[:], in_=T0[:]), ["T0"], ["T0b"])
                for h in range(H):
                    P(lambda e, h=h: e.matmul(kvp[:, h, :], lhsT=qT[:, h, :], rhs=T0b[:, h, :], start=True, stop=True),
                      ["qT", "T0b"], [("kvp", h // 4)])
                V(lambda e: e.tensor_tensor(out=oo[:], in0=kvp[:], in1=qdc[:].unsqueeze(2).broadcast_to([128, H, 128]), op=ALU.mult),
                  [("kvp", 0), ("kvp", 1), "qdc"], ["oo"])
                V(lambda e: e.tensor_tensor(out=oo[:], in0=oo[:], in1=zB3, op=ALU.add), ["oo", ("zB", 0), ("zB", 1)], ["oo"])
                V(lambda e: e.tensor_reduce(out=gst[:, 0:8], in_=oo[:], axis=AX.X, op=ALU.add), ["oo"], ["gst"])
                V(lambda e: e.tensor_tensor(out=o2[:], in0=oo[:], in1=oo[:], op=ALU.mult), ["oo"], ["o2"])
                V(lambda e: e.tensor_reduce(out=gst[:, 8:16], in_=o2[:], axis=AX.X, op=ALU.add), ["o2"], ["gst"])
                V(lambda e: e.tensor_scalar(out=gst[:, 16:24], in0=gst[:, 0:8], scalar1=1.0 / 128.0, scalar2=None, op0=ALU.mult), ["gst"], ["gst"])
                V(lambda e: e.tensor_tensor(out=gst[:, 24:32], in0=gst[:, 16:24], in1=gst[:, 16:24], op=ALU.mult), ["gst"], ["gst"])
                V(lambda e: e.scalar_tensor_tensor(out=gst[:, 24:32], in0=gst[:, 8:16], scalar=1.0 / 128.0, in1=gst[:, 24:32],
                                                   op0=ALU.mult, op1=ALU.subtract), ["gst"], ["gst"])
                A(lambda e: e.activation(out=gst[:, 32:40], in_=gst[:, 24:32], func=AF.Ln, bias=epsc[:, 1:2]), ["gst", "epsc"], ["gst2"])
                A(lambda e: e.activation(out=gst[:, 32:40], in_=gst[:, 32:40], func=AF.Exp, scale=-0.5), ["gst2"], ["gst2"])
                for h in range(H):
                    V(lambda e, h=h: e.tensor_scalar(out=o2[:, h, :], in0=oo[:, h, :], scalar1=gst[:, 16 + h:17 + h], scalar2=gst[:, 32 + h:33 + h],
                                                     op0=ALU.subtract, op1=ALU.mult), ["oo", "gst", "gst2"], ["o2"])
                proj(zA[:, 0:512], C_RG, 512, ("zA", 0))
                proj(zA[:, 512:1024], C_RG + 512, 512, ("zA", 1))
                A(lambda e: e.activation(out=sg[:], in_=zA[:], func=AF.Silu), [("zA", 0), ("zA", 1)], ["sg"])
                o2f = o2[:].rearrange("p h d -> p (h d)")
                V(lambda e: e.tensor_tensor(out=o2f, in0=o2f, in1=gnr[:], op=ALU.mult), ["o2", "gnr"], ["o2"])
                V(lambda e: e.tensor_tensor(out=ro[:], in0=o2f, in1=sg[:], op=ALU.mult), ["o2", "sg"], ["ro"])
                for k in range(8):
                    P(lambda e, k=k: e.transpose(out=pT[:, k, :], in_=ro[:, k * 128:(k + 1) * 128], identity=idb[:]), ["ro", "idb"], ["pT"])
                A(lambda e: e.activation(out=roT[:], in_=pT[:], func=AF.Copy), ["pT"], ["roT"])
                DM(lambda e: e.dma_start(out=roT_s[g], in_=roT[:]), ["roT"], [("roT_s", g)], "st_r")
                proj(zB[:, 0:512], C_DQ, 512, ("zB", 0))
                proj(zB[:, 512:1024], C_IQ, 512, ("zB", 1))
                proj(misc[:, 0:8], C_IW, 8, "m_dkv")
                V(lambda e: e.tensor_copy(out=dqb[:], in_=zB[:, 0:512]), [("zB", 0)], ["dqb"])
                A(lambda e: e.activation(out=iqb[:], in_=zB[:, 512:1024], func=AF.Copy), [("zB", 1)], ["iqb"])
                V(lambda e: e.tensor_copy(out=iwb[:], in_=misc[:, 0:8]), ["m_dkv"], ["iwb"])
                DM(lambda e: e.dma_start(out=iw_s[g], in_=iwb[:]), ["iwb"], [("iw_s", g)], "st_w")
                for h in range(H):
                    P(lambda e, h=h: e.transpose(out=pT64[:, h, :], in_=dqb[:, h * 64:(h + 1) * 64], identity=idb[:]), ["dqb", "idb"], ["pT"])
                V(lambda e: e.tensor_copy(out=dqT[:], in_=pT64), ["pT"], ["dqT"])
                DM(lambda e: e.dma_start(out=dqT_s[g], in_=dqT[:]), ["dqT"], [("dqT_s", g)], "st_q")
                for h in range(H):
                    P(lambda e, h=h: e.transpose(out=pT64[:, h, :], in_=iqb[:, h * 64:(h + 1) * 64], identity=idb[:]), ["iqb", "idb"], ["pT"])
                A(lambda e: e.activation(out=iqT[:], in_=pT64, func=AF.Copy), ["pT"], ["dqT"])
                DM(lambda e: e.dma_start(out=iqT_s[g], in_=iqT[:]), ["dqT"], [("iqT_s", g)], "st_i")

            items = []
            for g in range(NG):
                for s4 in range(4):
                    items.append(("k", 4 * g + s4))
                items.append(("o", g))

            def stage1(pos):
                kind, idx = items[pos]
                (kside_s1 if kind == "k" else ownside_s1)(pos, idx)

            stage1(0)
            for pos in range(len(items)):
                if pos + 1 < len(items):
                    stage1(pos + 1)
                kind, idx = items[pos]
                (kside if kind == "k" else ownside)(pos, idx)
            if dbg:
                DM(lambda e: e.dma_start(out=kT_dbg, in_=kT[:]), [("kT", j) for j in range(NT)], ["kT_dbg"], "dbg")
                DM(lambda e: e.dma_start(out=dv_dbg, in_=dv1[:]), [("dv1", j) for j in range(NT)] + ["dv1one"], ["dv_dbg"], "dbg")
            S.barrier()
            S.emit()

        PHASES = build.phases
        if PHASES >= 2:
          with contextlib.ExitStack() as ph:
            score2 = [sbt(ph, f"score{i}", [128, SB], BF16) for i in range(2)]
            msk = sbt(ph, "msk", [128, SB], BF16)
            mskT = [sbt(ph, f"mskT{i}", [128, 8, 128], BF16) for i in range(2)]
            iq_sb2 = [sbt(ph, f"iq_sb{i}", [128, H, 128], BF16) for i in range(2)]
            dq_sb2 = [sbt(ph, f"dq_sb{i}", [128, H, 128], BF16) for i in range(2)]
            iw_sb2 = [sbt(ph, f"iw_sb{i}", [128, H], F32) for i in range(2)]
            dg2 = [sbt(ph, f"dg{i}", [128, H, 128], BF16) for i in range(2)]
            maddb = sbt(ph, "maddb", [128, 512], BF16)
            rh = [sbt(ph, f"rh{i}", [128, 512], BF16) for i in range(4)]
            madd = sbt(ph, "madd", [128, 512], F32)
            bT = sbt(ph, "bT", [128, 5, H, 128], BF16)
            c15s = sbt(ph, "c15s", [128, H], F32)
            selden = sbt(ph, "selden", [65, 64], F32)
            bst = sbt(ph, "bst", [128, 8], F32)
            steps = sbt(ph, "steps", [128, NBIS], F32)
            halves = sbt(ph, "halves", [128, NBIS], F32)
            tmpl = sbt(ph, "tmpl", [128, 512], F32)
            lg = sbt(ph, "lg", [128, H, 128], F32)
            pe_ = [sbt(ph, f"pe{i}", [128, H, 128], BF16) for i in range(2)]
            pm = [sbt(ph, f"pm{i}", [128, H, 128], BF16) for i in range(3)]
            OT = sbt(ph, "OT", [65, H * 128], F32)
            ydT = sbt(ph, "ydT", [64, H, 128], BF16)
            PP = [pst(ph, f"PP{i}", [128, 1024], F32) for i in range(4)]
            ps_s = [PP[k // 2][:, (k % 2) * 512:(k % 2 + 1) * 512] for k in range(4)]
            ps_s_res = [(f"PP{k // 2}", k % 2) for k in range(4)]
            ps_sc = [PP[2][:, 0:512], PP[2][:, 512:1024]]
            ps_sc_res = [("PP2", 0), ("PP2", 1)]
            ps_qk = [PP[0], PP[1]]
            ps_o = PP[2]
            ps_mT = PP[3][:, 0:512].bitcast(BF16).rearrange("p (a b) -> p a b", a=8)

            DM(lambda e: e.dma_start(out=madd[:], in_=maddd), [], ["madd"], "c4_madd")
            DG(lambda e: e.dma_start(out=bT[:], in_=biasTd.rearrange("s p h q -> p s h q")), [], ["bT"], "c4_bT")
            DM(lambda e: e.dma_start(out=c15s[:], in_=c15), [], ["c15s"], "c4_c15s")
            DM(lambda e: e.dma_start(out=selden[:], in_=seldend), [], ["selden"], "c4_selden")
            for s5 in range(5):
                V(lambda e, s5=s5: e.tensor_tensor(out=bT[:, s5, :, :], in0=bT[:, s5, :, :],
                                                   in1=c15s[:].unsqueeze(2).broadcast_to([128, H, 128]), op=ALU.subtract),
                  ["bT", "c15s"], ["bT"])
            for i in range(NBIS):
                V(lambda e, i=i: e.memset(halves[:, i:i + 1], 2.0 ** (-(i + 1))), [], ["halves"])
            for i in range(2):
                G(lambda e, i=i: e.memset(iq_sb2[i][64:128, :, :], 0.0), [], [f"iq_sb{i}"])
                G(lambda e, i=i: e.memset(dq_sb2[i][0:64, :, :], 0.0), [], [f"dq_sb{i}"])
            V(lambda e: e.tensor_copy(out=maddb[:], in_=madd[:]), ["madd"], ["maddb"])

            def load_q(g):
                sl = g % 2
                DM(lambda e: e.dma_start(out=iq_sb2[sl][0:64, :, :], in_=iqT_s[g]), [("iqT_s", g)], [f"iq_sb{sl}"], f"ld_i{sl}")
                DM(lambda e: e.dma_start(out=dq_sb2[sl][64:128, :, :], in_=dqT_s[g]), [("dqT_s", g)], [f"dq_sb{sl}"], f"ld_q{sl}")
                DM(lambda e: e.dma_start(out=iw_sb2[sl][:], in_=iw_s[g]), [("iw_s", g)], [f"iw_sb{sl}"], f"ld_w{sl}")
                for h in range(H):
                    V(lambda e, h=h: e.tensor_scalar(out=dg2[sl][:, h, :], in0=idb[:], scalar1=iw_sb2[sl][:, h:h + 1], scalar2=None, op0=ALU.mult),
                      ["idb", f"iw_sb{sl}"], [f"dg{sl}"])

            def indexer(g):
                sl = g % 2
                iq_sb = iq_sb2[sl]; dg = dg2[sl]; score = score2[sl]
                iqres = f"iq_sb{sl}"; dgres = f"dg{sl}"
                M = 8 * (g + 1)

                def smm(i):
                    c, h = i // 8, i % 8
                    kres = [("kT", 4 * c + t) for t in range(4)]
                    P(lambda e: e.matmul(ps_s[i % 4], lhsT=iq_sb[:, h, :], rhs=kT[:, c * 512:(c + 1) * 512], start=True, stop=True),
                      [iqres] + kres, [ps_s_res[i % 4]])

                def rl_dmm(i):
                    c, h = i // 8, i % 8
                    r4 = i % 4
                    A(lambda e: e.activation(out=rh[r4][:], in_=ps_s[r4], func=AF.Relu), [ps_s_res[r4]], [f"rh{r4}"])
                    last = (c == g)
                    P(lambda e: e.matmul(ps_sc[c % 2], lhsT=dg[:, h, :], rhs=rh[r4][:], start=(h == 0), stop=(h == H - 1 and not last)),
                      [dgres, f"rh{r4}"], [ps_sc_res[c % 2]])
                    if h == H - 1:
                        if last:
                            P(lambda e: e.matmul(ps_sc[c % 2], lhsT=idb[:], rhs=maddb[:], start=False, stop=True), ["idb", "maddb"], [ps_sc_res[c % 2]])
                        A(lambda e: e.activation(out=score[:, c * 512:(c + 1) * 512], in_=ps_sc[c % 2], func=AF.Copy), [ps_sc_res[c % 2]], [(f"score{sl}", c)])

                smm(0)
                smm(1)
                for i_ in range(M):
                    if i_ + 2 < M:
                        smm(i_ + 2)
                    rl_dmm(i_)

            def bisect(g):
                sl = g % 2
                score = score2[sl]
                nk = (g + 1) * 512
                sres = [(f"score{sl}", c) for c in range(g + 1)]
                V(lambda e: e.scalar_tensor_tensor(out=tmpl[:], in0=madd[:], scalar=-2.0, in1=score[:, nk - 512:nk], op0=ALU.mult, op1=ALU.add),
                  [(f"score{sl}", g), "madd"], ["tmpl"])
                V(lambda e: e.tensor_reduce(out=bst[:, 2:3], in_=tmpl[:], axis=AX.X, op=ALU.min), ["tmpl"], ["bst2"])
                V(lambda e: e.tensor_reduce(out=bst[:, 0:1], in_=score[:, 0:nk], axis=AX.X, op=ALU.max), sres, ["bst0"])
                if g > 0:
                    V(lambda e: e.tensor_reduce(out=bst[:, 1:2], in_=score[:, 0:nk - 512], axis=AX.X, op=ALU.min), sres, ["bst1"])
                    V(lambda e: e.tensor_tensor(out=bst[:, 3:4], in0=bst[:, 1:2], in1=bst[:, 2:3], op=ALU.min), ["bst1", "bst2"], ["bt"])
                else:
                    V(lambda e: e.tensor_copy(out=bst[:, 3:4], in_=bst[:, 2:3]), ["bst2"], ["bt"])
                V(lambda e: e.tensor_tensor(out=bst[:, 4:5], in0=bst[:, 0:1], in1=bst[:, 3:4], op=ALU.subtract), ["bst0", "bt"], ["bR"])
                V(lambda e: e.tensor_scalar(out=steps[:], in0=halves[:], scalar1=bst[:, 4:5], scalar2=None, op0=ALU.mult), ["halves", "bR"], ["steps"])
                for i in range(NBIS):
                    V(lambda e, i=i: e.tensor_tensor(out=bst[:, 5:6], in0=bst[:, 3:4], in1=steps[:, i:i + 1], op=ALU.add), ["bt", "steps"], ["bcand"])
                    V(lambda e: e.tensor_scalar(out=msk[:, 0:nk], in0=score[:, 0:nk], scalar1=bst[:, 5:6], scalar2=0.0, op0=ALU.is_ge, op1=ALU.add,
                                                accum_out=bst[:, 6:7]), sres + ["bcand"], ["msk", "bcnt"])
                    V(lambda e, i=i: e.tensor_scalar(out=bst[:, 7:8], in0=bst[:, 6:7], scalar1=255.5, scalar2=steps[:, i:i + 1], op0=ALU.is_ge, op1=ALU.mult),
                      ["bcnt", "steps"], ["binc"])
                    V(lambda e: e.tensor_tensor(out=bst[:, 3:4], in0=bst[:, 3:4], in1=bst[:, 7:8], op=ALU.add), ["bt", "binc"], ["bt"])
                V(lambda e: e.tensor_scalar(out=msk[:, 0:nk], in0=score[:, 0:nk], scalar1=bst[:, 3:4], scalar2=0.0, op0=ALU.is_ge, op1=ALU.add,
                                            accum_out=bst[:, 6:7]), sres + ["bt"], ["msk", "bcnt"])
                if dbg:
                    DM(lambda e: e.dma_start(out=thr_dbg[g], in_=bst[:, 0:8]), ["bt", "bcnt", "bR", "bcand", "bst0", "bst1", "bst2", "binc"], [("thr_dbg", g)], "dbg2")
                    DM(lambda e: e.dma_start(out=sc_dbg[g, :, 0:nk], in_=score[:, 0:nk]), sres, [("sc_dbg", g)], "dbg2")
                    DM(lambda e: e.dma_start(out=mk_dbg[g, :, 0:nk], in_=msk[:, 0:nk]), ["msk"], [("mk_dbg", g)], "dbg2")

            def attention(g):
                sl = g % 2
                dq_sb = dq_sb2[sl]
                dqres = f"dq_sb{sl}"
                ntile = (g + 1) * 4

                def mask_blk(j0):
                    nj = min(8, ntile - j0)
                    for jj in range(nj):
                        j = j0 + jj
                        P(lambda e, j=j, jj=jj: e.transpose(out=ps_mT[:, jj, :], in_=msk[:, j * 128:(j + 1) * 128], identity=idb[:]),
                          ["msk", "idb"], [("PP3", 0)])
                    mb = (j0 // 8) % 2
                    if mb == 0:
                        A(lambda e, mb=mb, nj=nj: e.activation(out=mskT[mb][:, 0:nj, :], in_=ps_mT[:, 0:nj, :], func=AF.Copy), [("PP3", 0)], [f"mskT{mb}"])
                    else:
                        V(lambda e, mb=mb, nj=nj: e.tensor_copy(out=mskT[mb][:, 0:nj, :], in_=ps_mT[:, 0:nj, :]), [("PP3", 0)], [f"mskT{mb}"])
                def att_qk(j):
                    qb = j % 2
                    for hf in range(2):
                        P(lambda e, hf=hf: e.matmul(ps_qk[qb][:, hf * 512:(hf + 1) * 512], lhsT=kT[:, j * 128:(j + 1) * 128],
                                                    rhs=dq_sb[:, 4 * hf:4 * hf + 4, :], start=True, stop=True),
                          [("kT", j), dqres], [(f"PP{qb}", hf)])

                def att_sm(j):
                    b = j % 2
                    if j % 8 == 0:
                        mask_blk(j)
                    mb = (j // 8) % 2
                    slot = j - (4 * g - 1)
                    qk3 = ps_qk[b][:].rearrange("p (h q) -> p h q", h=H)
                    qres = [(f"PP{b}", 0), (f"PP{b}", 1)]
                    if slot >= 0:
                        V(lambda e: e.scalar_tensor_tensor(out=lg[:], in0=qk3, scalar=0.125, in1=bT[:, slot, :, :], op0=ALU.mult, op1=ALU.add),
                          qres + ["bT"], ["lg"])
                        A(lambda e: e.activation(out=pe_[b][:], in_=lg[:], func=AF.Exp), ["lg"], [f"pe{b}"])
                    else:
                        A(lambda e: e.activation(out=pe_[b][:], in_=qk3, func=AF.Exp, scale=0.125), qres, [f"pe{b}"])
                    b3 = j % 3
                    V(lambda e: e.tensor_tensor(out=pm[b3][:], in0=pe_[b][:], in1=mskT[mb][:, j % 8, :].unsqueeze(1).broadcast_to([128, H, 128]), op=ALU.mult),
                      [f"pe{b}", f"mskT{mb}"], [f"pm{b3}"])

                def att_pv(j):
                    b = j % 3
                    for hf in range(2):
                        P(lambda e, hf=hf: e.matmul(ps_o[0:65, hf * 512:(hf + 1) * 512], lhsT=dv1[:, j, :], rhs=pm[b][:, 4 * hf:4 * hf + 4, :],
                                                    start=(j == 0), stop=(j == ntile - 1)),
                          [("dv1", j), "dv1one", f"pm{b}"], [("PP2", hf)])

                att_qk(0)
                for j_ in range(ntile):
                    if j_ + 1 < ntile:
                        att_qk(j_ + 1)
                    att_sm(j_)
                    if j_ >= 1:
                        att_pv(j_ - 1)
                att_pv(ntile - 1)
                A(lambda e: e.activation(out=OT[:], in_=ps_o[0:65, :], func=AF.Copy), [("PP2", 0), ("PP2", 1)], ["OT"])
                for hf in range(2):
                    P(lambda e, hf=hf: e.matmul(PP[3][0:64, hf * 512:(hf + 1) * 512], lhsT=selden[:], rhs=OT[:, hf * 512:(hf + 1) * 512], start=True, stop=True),
                      ["selden", "OT"], [("PP3", hf)])
                rden = lg[0:64, :, :].rearrange("p h q -> p (h q)")
                V(lambda e: e.reciprocal(out=rden, in_=PP[3][0:64, :]), [("PP3", 0), ("PP3", 1)], ["lg"])
                V(lambda e: e.tensor_tensor(out=ydT[:].rearrange("p h q -> p (h q)"), in0=OT[0:64, :], in1=rden, op=ALU.mult), ["OT", "lg"], ["ydT"])
                DM(lambda e: e.dma_start(out=ydT_s[g], in_=ydT[:]), ["ydT"], [("ydT_s", g)], "st_y")

            load_q(0)
            indexer(0)
            for g_ in range(NG):
                if g_ + 1 < NG:
                    load_q(g_ + 1)
                    indexer(g_ + 1)
                bisect(g_)
                attention(g_)
            S.barrier()
            S.emit()

        kvs.close()
        if PHASES >= 3:
          with contextlib.ExitStack() as ph:
            u2T = sbt(ph, "u2T", [128, NG, 8, 128], BF16)
            gates = sbt(ph, "gates", [128, NG, 16], F32)
            with contextlib.ExitStack() as pc:
                wg_sb = sbt(pc, "wg_sb", [128, 8, 2 * D], BF16)
                wro_sb = sbt(pc, "wro_sb", [128, 8, D], BF16)
                wdo_sb = sbt(pc, "wdo_sb", [64, H, D], BF16)
                wo_sb = sbt(pc, "wo_sb", [128, 8, D], BF16)
                wrt_sb = sbt(pc, "wrt_sb", [128, 8, 20], BF16)
                bg_sb = sbt(pc, "bg_sb", [1, 2 * D], BF16)
                ones1 = sbt(pc, "ones1", [1, 128], BF16)
                brt = sbt(pc, "brt", [128, 20], F32)
                xt = [sbt(pc, f"cxt{i}", [128, D], F32) for i in range(2)]
                uTc = [sbt(pc, f"cuT{i}", [128, 8, 128], BF16) for i in range(2)]
                roTc = [sbt(pc, f"croT{i}", [128, 8, 128], BF16) for i in range(2)]
                ydTc = [sbt(pc, f"cydT{i}", [64, H, 128], BF16) for i in range(2)]
                gab = sbt(pc, "gab", [128, 2 * D], BF16)
                t1 = sbt(pc, "t1", [128, D], F32); t2 = sbt(pc, "t2", [128, D], F32)
                mm = sbt(pc, "mm", [128, D], BF16); mT = sbt(pc, "mT", [128, 8, 128], BF16)
                h1b = [sbt(pc, f"h1_{i}", [128, D], F32) for i in range(2)]
                junk = sbt(pc, "cjunk", [128, D], BF16); xs = sbt(pc, "cxs", [128, D], BF16)
                stat = sbt(pc, "cstat", [128, 8], F32)
                rl = sbt(pc, "rl", [128, 20], F32)
                rs = sbt(pc, "rs", [128, 48], F32)
                u2t = sbt(pc, "u2t", [128, 8, 128], BF16)
                pT = pst(pc, "cpT", [128, 8, 128], BF16)
                pgA = pst(pc, "pgA", [128, 2 * D], F32)
                pyr = pst(pc, "pyr", [128, D], F32)
                prt = pst(pc, "prt", [128, 32], F32)

                for k in range(8):
                    DG(lambda e, k=k: e.dma_start(out=wg_sb[:, k, :], in_=w_gate[k * 128:(k + 1) * 128, :]), [], ["wg_sb"], "wC_wg_sb")
                    DG(lambda e, k=k: e.dma_start(out=wro_sb[:, k, :], in_=w_ret_out[k * 128:(k + 1) * 128, :]), [], ["wro_sb"], "wC_wro_sb")
                    DG(lambda e, k=k: e.dma_start(out=wo_sb[:, k, :], in_=w_o[k * 128:(k + 1) * 128, :]), [], ["wo_sb"], "wC_wo_sb")
                    DG(lambda e, k=k: e.dma_start(out=wdo_sb[:, k, :], in_=w_dsa_out[k * 64:(k + 1) * 64, :]), [], ["wdo_sb"], "wC_wdo_sb")
                DG(lambda e: e.dma_start(out=wrt_sb[:], in_=w_rt.rearrange("(k p) n -> p k n", p=128)), [], ["wrt_sb"], "wC_wrt_sb")
                DG(lambda e: e.dma_start(out=bg_sb[:], in_=b_gate), [], ["bg_sb"], "wC_bg_sb")
                DM(lambda e: e.dma_start(out=brt[:], in_=b_rt_rep), [], ["brt"], "c5")
                V(lambda e: e.memset(ones1[:], 1.0), [], ["ones1"])
                for k in range(8):
                    G(lambda e, k=k: e.tensor_tensor(out=wo_sb[:, k, :], in0=wo_sb[:, k, :], in1=g1rep[:], op=ALU.mult), ["wo_sb", "mod"], ["wo_sb"])

                def merge_tile(g):
                    sl = g % 2
                    DM(lambda e: e.dma_start(out=xt[sl][:], in_=xo[g * 128:(g + 1) * 128, :]), [], [f"cxt{sl}"], f"cxt{sl}")
                    DM(lambda e: e.dma_start(out=uTc[sl][:], in_=uT_s[g]), [("uT_s", g)], [f"cuT{sl}"], f"cuT{sl}")
                    DM(lambda e: e.dma_start(out=roTc[sl][:], in_=roT_s[g]), [("roT_s", g)], [f"croT{sl}"], f"croT{sl}")
                    DM(lambda e: e.dma_start(out=ydTc[sl][:], in_=ydT_s[g]), [("ydT_s", g)], [f"cydT{sl}"], f"cydT{sl}")
                    for q4 in range(4):
                        cs_ = slice(q4 * 512, (q4 + 1) * 512)
                        for k in range(8):
                            P(lambda e, k=k, cs_=cs_: e.matmul(pgA[:, cs_], lhsT=uTc[sl][:, k, :], rhs=wg_sb[:, k, cs_], start=(k == 0), stop=False),
                              [f"cuT{sl}", "wg_sb"], [("pgA", q4)])
                        P(lambda e, cs_=cs_: e.matmul(pgA[:, cs_], lhsT=ones1[:], rhs=bg_sb[:, cs_], start=False, stop=True),
                          ["ones1", "bg_sb"], [("pgA", q4)])
                    A(lambda e: e.activation(out=gab[:], in_=pgA[:], func=AF.Sigmoid), [("pgA", i) for i in range(4)], ["gab"])
                    for hf in range(2):
                        cs_ = slice(hf * 512, (hf + 1) * 512)
                        for k in range(8):
                            P(lambda e, k=k, cs_=cs_: e.matmul(pyr[:, cs_], lhsT=roTc[sl][:, k, :], rhs=wro_sb[:, k, cs_], start=(k == 0), stop=(k == 7)),
                              [f"croT{sl}", "wro_sb"], [("pyr", hf)])
                    V(lambda e: e.tensor_tensor(out=t1[:], in0=pyr[:], in1=gab[:, 0:D], op=ALU.mult), [("pyr", 0), ("pyr", 1), "gab"], ["t1"])
                    for hf in range(2):
                        cs_ = slice(hf * 512, (hf + 1) * 512)
                        for h in range(H):
                            P(lambda e, h=h, cs_=cs_: e.matmul(pyr[:, cs_], lhsT=ydTc[sl][:, h, :], rhs=wdo_sb[:, h, cs_], start=(h == 0), stop=(h == H - 1)),
                              [f"cydT{sl}", "wdo_sb"], [("pyr", hf)])
                    V(lambda e: e.tensor_tensor(out=t2[:], in0=pyr[:], in1=gab[:, D:2 * D], op=ALU.mult), [("pyr", 0), ("pyr", 1), "gab"], ["t2"])
                    V(lambda e: e.tensor_tensor(out=mm[:], in0=t1[:], in1=t2[:], op=ALU.add), ["t1", "t2"], ["mm"])
                    for k in range(8):
                        P(lambda e, k=k: e.transpose(out=pT[:, k, :], in_=mm[:, k * 128:(k + 1) * 128], identity=idb[:]), ["mm", "idb"], ["pT"])
                    A(lambda e: e.activation(out=mT[:], in_=pT[:], func=AF.Copy), ["pT"], ["mT"])
                    for hf in range(2):
                        cs_ = slice(hf * 512, (hf + 1) * 512)
                        for k in range(8):
                            P(lambda e, k=k, cs_=cs_: e.matmul(pyr[:, cs_], lhsT=mT[:, k, :], rhs=wo_sb[:, k, cs_], start=(k == 0), stop=(k == 7)),
                              ["mT", "wo_sb"], [("pyr", hf)])
                    h1 = h1b[sl]
                    V(lambda e: e.tensor_tensor(out=h1[:], in0=pyr[:], in1=xt[sl][:], op=ALU.add), [("pyr", 0), ("pyr", 1), f"cxt{sl}"], [f"h1_{sl}"])
                    DM(lambda e: e.dma_start(out=h1_s[g], in_=h1[:]), [f"h1_{sl}"], [("h1_s", g)], f"st_h{sl}")

                def route_tile(g):
                    sl = g % 2
                    h1 = h1b[sl]
                    norm_T(h1[:], f"h1_{sl}", junk[:], xs[:], pT, u2t, stat, G2T, sh2T, "c")
                    G(lambda e: e.tensor_copy(out=u2T[:, g, :, :], in_=u2t[:]), ["cuT"], [("u2T", g)])
                    for k in range(8):
                        P(lambda e, k=k: e.matmul(prt[:, 0:20], lhsT=u2t[:, k, :], rhs=wrt_sb[:, k, :], start=(k == 0), stop=(k == 7)),
                          ["cuT", "wrt_sb"], ["prt"])
                    V(lambda e: e.tensor_tensor(out=rl[:], in0=prt[:, 0:20], in1=brt[:], op=ALU.add), ["prt", "brt"], ["rl"])
                    R = lambda a, b_: rs[:, a:b_]
                    V(lambda e: e.tensor_reduce(out=R(0, 1), in_=rl[:, 0:4], axis=AX.X, op=ALU.max), ["rl"], ["rs"])
                    V(lambda e: e.tensor_scalar(out=R(1, 5), in0=rl[:, 0:4], scalar1=R(0, 1), scalar2=None, op0=ALU.is_ge), ["rl", "rs"], ["rs"])
                    V(lambda e: e.tensor_scalar(out=R(5, 6), in0=R(0, 1), scalar1=-1.0, scalar2=None, op0=ALU.mult), ["rs"], ["rs"])
                    A(lambda e: e.activation(out=R(6, 10), in_=rl[:, 0:4], func=AF.Exp, bias=R(5, 6), accum_out=R(10, 11)), ["rl", "rs"], ["rs2"])
                    V(lambda e: e.reciprocal(out=R(11, 12), in_=R(10, 11)), ["rs2"], ["rs3"])
                    V(lambda e: e.tensor_scalar(out=R(12, 16), in0=rl[:, 4:8], scalar1=R(1, 2), scalar2=None, op0=ALU.mult), ["rl", "rs"], ["rs4"])
                    for gi in range(1, 4):
                        V(lambda e, gi=gi: e.scalar_tensor_tensor(out=R(12, 16), in0=rl[:, 4 + 4 * gi:8 + 4 * gi], scalar=R(1 + gi, 2 + gi), in1=R(12, 16),
                                                                  op0=ALU.mult, op1=ALU.add), ["rl", "rs", "rs4"], ["rs4"])
                    V(lambda e: e.tensor_reduce(out=R(16, 17), in_=R(12, 16), axis=AX.X, op=ALU.max), ["rs4"], ["rs5"])
                    V(lambda e: e.tensor_scalar(out=R(17, 21), in0=R(12, 16), scalar1=R(16, 17), scalar2=None, op0=ALU.is_ge), ["rs4", "rs5"], ["rs6"])
                    V(lambda e: e.scalar_tensor_tensor(out=R(21, 25), in0=R(17, 21), scalar=NEG, in1=R(12, 16), op0=ALU.mult, op1=ALU.add),
                      ["rs6", "rs4"], ["rs7"])
                    V(lambda e: e.tensor_reduce(out=R(25, 26), in_=R(21, 25), axis=AX.X, op=ALU.max), ["rs7"], ["rs8"])
                    V(lambda e: e.tensor_scalar(out=R(26, 30), in0=R(21, 25), scalar1=R(25, 26), scalar2=None, op0=ALU.is_ge), ["rs7", "rs8"], ["rs9"])
                    V(lambda e: e.tensor_tensor(out=R(30, 31), in0=R(16, 17), in1=R(25, 26), op=ALU.subtract), ["rs5", "rs8"], ["rs10"])
                    A(lambda e: e.activation(out=R(31, 32), in_=R(30, 31), func=AF.Sigmoid), ["rs10"], ["rs11"])
                    V(lambda e: e.tensor_scalar(out=R(32, 33), in0=R(31, 32), scalar1=-1.0, scalar2=1.0, op0=ALU.mult, op1=ALU.add), ["rs11"], ["rs12"])
                    V(lambda e: e.tensor_scalar(out=R(33, 37), in0=R(17, 21), scalar1=R(31, 32), scalar2=None, op0=ALU.mult), ["rs6", "rs11"], ["rs13"])
                    V(lambda e: e.scalar_tensor_tensor(out=R(33, 37), in0=R(26, 30), scalar=R(32, 33), in1=R(33, 37), op0=ALU.mult, op1=ALU.add),
                      ["rs9", "rs12", "rs13"], ["rs13"])
                    V(lambda e: e.tensor_scalar(out=R(33, 37), in0=R(33, 37), scalar1=R(11, 12), scalar2=None, op0=ALU.mult), ["rs13", "rs3"], ["rs13"])
                    for gi in range(4):
                        V(lambda e, gi=gi: e.tensor_scalar(out=gates[:, g, 4 * gi:4 * gi + 4], in0=R(33, 37), scalar1=R(1 + gi, 2 + gi), scalar2=None, op0=ALU.mult),
                          ["rs13", "rs"], [("gates", g)])

                merge_tile(0)
                for g_ in range(NG):
                    if g_ + 1 < NG:
                        merge_tile(g_ + 1)
                    route_tile(g_)
                S.barrier()
                S.emit()

            with contextlib.ExitStack() as pd:
                NH = 2 if NG >= 2 else 1
                TPH = NG // NH
                hacc = sbt(pd, "hacc", [128, TPH, D], F32)
                w13 = [sbt(pd, f"w13_{i}", [128, 8, 512], BF16) for i in range(2)]
                w2s = [sbt(pd, f"w2s_{i}", [128, 2, D], BF16) for i in range(2)]
                gfin = sbt(pd, "gfin", [128, D], F32)
                sa = [sbt(pd, f"sa{i}", [128, 256], F32) for i in range(2)]
                hid = [sbt(pd, f"hid{i}", [128, 256], BF16) for i in range(2)]
                hidT = [sbt(pd, f"hidT{i}", [128, 2, 128], BF16) for i in range(2)]
                fjunk = sbt(pd, "fjunk", [128, D], BF16)
                fstat = sbt(pd, "fstat", [128, 4], F32)
                ot = [sbt(pd, f"ot{i}", [128, D], F32) for i in range(2)]
                pab = [pst(pd, f"pab{i}", [128, 512], F32) for i in range(2)]
                phT = [pst(pd, f"phT{i}", [128, 2, 128], BF16) for i in range(2)]
                py = [pst(pd, f"py{i}", [128, D], F32) for i in range(2)]
                DM(lambda e: e.dma_start(out=gfin[:], in_=gfin_rep), [], ["gfin"], "c6")

                def load_expert(ex, ws):
                    for k in range(8):
                        DG(lambda e, k=k: e.dma_start(out=w13[ws][:, k, 0:256], in_=w1[ex, k * 128:(k + 1) * 128, :]), [], [f"w13_{ws}"], f"w13_{ws}")
                        DG(lambda e, k=k: e.dma_start(out=w13[ws][:, k, 256:512], in_=w3[ex, k * 128:(k + 1) * 128, :]), [], [f"w13_{ws}"], f"w13_{ws}")
                    for f in range(2):
                        DG(lambda e, f=f: e.dma_start(out=w2s[ws][:, f, :], in_=w2[ex, f * 128:(f + 1) * 128, :]), [], [f"w2s_{ws}"], f"w2s_{ws}")
                        G(lambda e, f=f: e.tensor_tensor(out=w2s[ws][:, f, :], in0=w2s[ws][:, f, :], in1=g2rep[:], op=ALU.mult),
                          [f"w2s_{ws}", "mod"], [f"w2s_{ws}"])

                def u_ab(u, g, ex, ws):
                    b = u % 2
                    for k in range(8):
                        P(lambda e, k=k: e.matmul(pab[b][:], lhsT=u2T[:, g, k, :], rhs=w13[ws][:, k, :], start=(k == 0), stop=(k == 7)),
                          [("u2T", g), f"w13_{ws}"], [f"pab{b}"])

                def u_act_tr(u, g, ex, ws):
                    b = u % 2
                    A(lambda e: e.activation(out=sa[b][:], in_=pab[b][:, 0:256], func=AF.Silu), [f"pab{b}"], [f"sa{b}"])
                    V(lambda e: e.scalar_tensor_tensor(out=hid[b][:], in0=pab[b][:, 256:512], scalar=gates[:, g, ex:ex + 1], in1=sa[b][:],
                                                       op0=ALU.mult, op1=ALU.mult), [f"pab{b}", ("gates", g), f"sa{b}"], [f"hid{b}"])
                    for f in range(2):
                        P(lambda e, f=f: e.transpose(out=phT[b][:, f, :], in_=hid[b][:, f * 128:(f + 1) * 128], identity=idb[:]), [f"hid{b}", "idb"], [f"phT{b}"])
                    A(lambda e: e.activation(out=hidT[b][:], in_=phT[b][:], func=AF.Copy), [f"phT{b}"], [f"hidT{b}"])

                def u_y(u, g, ex, ws, tl):
                    b = u % 2
                    for hf in range(2):
                        cs_ = slice(hf * 512, (hf + 1) * 512)
                        for f in range(2):
                            P(lambda e, f=f, cs_=cs_: e.matmul(py[b][:, cs_], lhsT=hidT[b][:, f, :], rhs=w2s[ws][:, f, cs_], start=(f == 0), stop=(f == 1)),
                              [f"hidT{b}", f"w2s_{ws}"], [(f"py{b}", hf)])
                    V(lambda e: e.tensor_tensor(out=hacc[:, tl, :], in0=hacc[:, tl, :], in1=py[b][:], op=ALU.add),
                      [("hacc", tl), (f"py{b}", 0), (f"py{b}", 1)], [("hacc", tl)])

                for hh in range(NH):
                    for tl in range(TPH):
                        g = hh * TPH + tl
                        DM(lambda e, tl=tl, g=g: e.dma_start(out=hacc[:, tl, :], in_=h1_s[g]), [("h1_s", g)], [("hacc", tl)], f"ld_h{tl}")
                    units = []
                    for ex in range(16):
                        for tl in range(TPH):
                            units.append((hh * TPH + tl, ex, (hh * 16 + ex) % 2, tl))
                    NU = len(units)
                    load_expert(0, (hh * 16) % 2)
                    loaded = {0}
                    g0_, ex0_, ws0_, tl0_ = units[0]
                    u_ab(0, g0_, ex0_, ws0_)
                    for u in range(NU):
                        g, ex, ws, tl = units[u]
                        deferred = False
                        if u + 1 < NU:
                            g2_, ex2_, ws2_, tl2_ = units[u + 1]
                            if ex2_ in loaded:
                                u_ab(u + 1, g2_, ex2_, ws2_)
                            else:
                                deferred = True
                        u_act_tr(u, g, ex, ws)
                        if u >= 1:
                            g1_, ex1_, ws1_, tl1_ = units[u - 1]
                            u_y(u - 1, g1_, ex1_, ws1_, tl1_)
                        if tl == 0 and ex + 1 < 16:
                            load_expert(ex + 1, (hh * 16 + ex + 1) % 2)
                            loaded.add(ex + 1)
                        if deferred:
                            u_ab(u + 1, g2_, ex2_, ws2_)
                    g1_, ex1_, ws1_, tl1_ = units[NU - 1]
                    u_y(NU - 1, g1_, ex1_, ws1_, tl1_)
                    for tl in range(TPH):
                        g = hh * TPH + tl
                        b = tl % 2
                        A(lambda e, tl=tl: e.activation(out=fjunk[:], in_=hacc[:, tl, :], func=AF.Square, scale=1.0 / 32.0, accum_out=fstat[:, 0:1]),
                          [("hacc", tl)], ["fjunk", "fstat"])
                        A(lambda e: e.activation(out=fstat[:, 1:2], in_=fstat[:, 0:1], func=AF.Ln, bias=epsc[:, 0:1]), ["fstat", "epsc"], ["fstat"])
                        A(lambda e: e.activation(out=fstat[:, 2:3], in_=fstat[:, 1:2], func=AF.Exp, scale=-0.5), ["fstat"], ["fstat"])
                        V(lambda e, tl=tl, b=b: e.scalar_tensor_tensor(out=ot[b][:], in0=hacc[:, tl, :], scalar=fstat[:, 2:3], in1=gfin[:], op0=ALU.mult, op1=ALU.mult),
                          [("hacc", tl), "fstat", "gfin"], [f"ot{b}"])
                        DM(lambda e, g=g, b=b: e.dma_start(out=out[g * 128:(g + 1) * 128, :], in_=ot[b][:]), [f"ot{b}"], [("out", g)], f"st_o{b}")
                S.op("sp", None, reads=[("out", g) for g in range(NG)])
                S.barrier()
                S.emit()
    return nc


build.phases = 3


def _rel_bucket(rel):
    nb = 16
    ret = (rel > 0).astype(np.int64) * nb
    n = np.abs(rel)
    max_exact = nb // 2
    nf = np.maximum(n, 1).astype(np.float32)
    large = max_exact + (np.log(nf / max_exact) / math.log(128 / max_exact) * (nb - max_exact)).astype(np.int32)
    large = np.minimum(large, nb - 1)
    return ret + np.where(n < max_exact, n, large)


def make_inputs(NG, inp):
    NT = 4 * NG
    SB = NT * 128
    f = np.float32
    x = np.asarray(inp["x"], f)
    assert x.shape[1] == SB
    pos = np.arange(SB, dtype=np.float64)
    freqs = 10000.0 ** (-np.arange(0, 64, 2, dtype=np.float64) / 64)
    ang = (pos[:, None].astype(np.float32) * freqs[None, :].astype(np.float32)).astype(np.float32)
    rope_all = np.concatenate([np.cos(ang), np.sin(ang)], axis=1).astype(f)
    gam = np.array(GAM, dtype=np.float64)
    i_ = np.arange(128)
    ci = i_ // 64
    DT = np.zeros((128, H, 128), f)
    for h in range(H):
        same = ci[:, None] == ci[None, :]
        dm = np.where(same, gam[h] ** np.abs(i_[:, None] - i_[None, :]),
                      np.where((ci[:, None] == 1) & (ci[None, :] == 0), gam[h] ** (i_[:, None] - i_[None, :]).clip(0), 0.0))
        DT[:, h, :] = (dm.T * 0.125).astype(f)
    qdec = (gam[None, :] ** (i_[:, None] + 1)).astype(f)
    wtile = (gam[None, :] ** (127 - i_[:, None]) * 0.125).astype(f)
    ident = np.eye(128, dtype=f)
    selden = np.zeros((65, 64), f); selden[64, :] = 1.0
    rb = np.asarray(inp["rel_bias"], f)

    def rep(v, n=128):
        return np.ascontiguousarray(np.broadcast_to(np.asarray(v, f).reshape(1, -1), (n, np.asarray(v).size)))

    def fm(v):
        return np.ascontiguousarray(np.asarray(v, f).reshape(-1, 128).T)

    b_ada = np.asarray(inp["b_ada"], f)[0]
    common = dict(
        w_ada=np.ascontiguousarray(inp["w_ada"][0], dtype=f), b_adaT=fm(b_ada),
        b_g12=np.ascontiguousarray(np.concatenate([rep(b_ada[2 * D:3 * D]), rep(b_ada[5 * D:6 * D])], axis=1)),
        gmixT=fm(inp["norm_mix_g"][0]), gffnT=fm(inp["norm_ffn_g"][0]),
        gfin_rep=rep(inp["norm_final_g"]), gn_rep=rep(inp["ret_gn_g"][0]), kvg_rep=rep(inp["dsa_kv_norm_g"][0]),
        w_in=np.ascontiguousarray(inp["w_in"][0], dtype=f), w_kv=np.ascontiguousarray(inp["w_dsa_kv_up"][0], dtype=f),
        w_ret_out=np.ascontiguousarray(inp["w_ret_out"][0], dtype=f), w_dsa_out=np.ascontiguousarray(inp["w_dsa_out"][0], dtype=f),
        w_gate=np.ascontiguousarray(inp["w_gate"][0], dtype=f), b_gate=np.ascontiguousarray(inp["b_gate"], dtype=f).reshape(1, -1),
        w_o=np.ascontiguousarray(inp["w_o"][0], dtype=f),
        w_rt=np.ascontiguousarray(np.concatenate([inp["w_group_router"][0], inp["w_expert_router"][0]], axis=1), dtype=f),
        b_rt_rep=rep(np.concatenate([inp["b_group_router"][0], inp["b_expert_router"][0]])),
        w1=np.ascontiguousarray(inp["w_exp_gate"][0], dtype=f), w3=np.ascontiguousarray(inp["w_exp_up"][0], dtype=f),
        w2=np.ascontiguousarray(inp["w_exp_down"][0], dtype=f),
        rope_all=rope_all, ident=ident, DT=DT, qdec=qdec, wtile=wtile, selden=selden, c15_rep=rep(rb[15]),
    )
    maps = []
    for c in range(8):
        b, r = c // 4, c % 4
        own = np.array([4 * g + r for g in range(NG)])
        rows = (own[:, None] * 128 + np.arange(128)[None, :]).reshape(-1)
        sel = np.zeros((64, 4), f); sel[:, r] = 1.0
        madd = np.zeros((128, 512), f)
        kt = np.arange(512) // 128
        kc = (np.arange(512) % 128) // 64
        qc = np.arange(128) // 64
        inadm = (kt[None, :] > r) | ((kt[None, :] == r) & (kc[None, :] > qc[:, None]))
        madd[inadm] = NEG
        biasT = np.zeros((5, 128, H, 128), f)
        for s5 in range(5):
            rel = (s5 - 1 - r) * 128 + np.arange(128)[:, None] - np.arange(128)[None, :]
            bk = _rel_bucket(rel)
            biasT[s5] = np.transpose(rb[bk], (0, 2, 1))
        m = dict(common)
        m.update(xb=np.ascontiguousarray(x[b]), xo=np.ascontiguousarray(x[b][rows]),
                 ccol=fm(inp["c"][b]), rope_own=np.ascontiguousarray(rope_all[rows]),
                 sel=sel, madd=madd, biasT=biasT)
        maps.append(m)
    return maps


_NC_CACHE = {}


def kernel(**inputs):
    NG = 32
    if NG not in _NC_CACHE:
        _NC_CACHE[NG] = build(NG)
    nc = _NC_CACHE[NG]
    maps = make_inputs(NG, inputs)
    res = run_bass_kernel_spmd(nc, maps, core_ids=list(range(8)))
    B, SEQ = inputs["x"].shape[0], inputs["x"].shape[1]
    outp = np.zeros((B, SEQ, D), np.float32)
    for c in range(8):
        b, r = c // 4, c % 4
        o = np.asarray(res.results[c]["out"]).reshape(NG, 128, D)
        for g in range(NG):
            t = 4 * g + r
            outp[b, t * 128:(t + 1) * 128, :] = o[g]
    return outp
```

```python
import contextlib
import math
import numpy as np
import concourse.bass as bass
import concourse.mybir as mybir
from concourse.bass_utils import run_bass_kernel_spmd

F32 = mybir.dt.float32
BF16 = mybir.dt.bfloat16
AF = mybir.ActivationFunctionType
ALU = mybir.AluOpType
AX = mybir.AxisListType

D = 1024
H = 8
NEG = -1.0e30
NBIS = 10
GAM = [1.0 - 2.0 ** (-5.0 - h) for h in range(H)]


class Sched:
    ENGS = ("pe", "act", "dve", "pool", "sp")
    ROT = 3000

    def __init__(self, nc, stack):
        self.nc = nc
        self.stack = stack
        self.ops = {e: [] for e in self.ENGS}
        self.last_write = {}
        self.readers = {}
        self.nsem = 0
        self.cur = {}
        self.dma = {}
        self.seen = {e: {} for e in self.ENGS}
        self.pe_sems = set()
        self.latest = {}

    def _newsem(self, name):
        self.nsem += 1
        return self.stack.enter_context(self.nc.semaphore(f"s{self.nsem}_{name}"))

    def _tok_compute(self, eng):
        c = self.cur.get(eng)
        if c is None or c[1] >= self.ROT:
            c = [self._newsem(eng), 0]
            self.cur[eng] = c
            if eng == "pe":
                self.pe_sems.add(id(c[0]))
        c[1] += 1
        return (c[0], c[1])

    def _tok_dma(self, key):
        c = self.dma.get(key)
        if c is None or c[1] >= 16 * 1500:
            c = [self._newsem("d"), 0]
            self.dma[key] = c
        c[1] += 16
        return (c[0], c[1])

    def op(self, eng, fn, reads=(), writes=(), dma_key=None, extra=()):
        deps = {}

        def add(tok):
            if tok is None:
                return
            s, v = tok
            k = id(s)
            if k not in deps or deps[k][1] < v:
                deps[k] = (s, v)

        for r in reads:
            add(self.last_write.get(r))
        for w in writes:
            add(self.last_write.get(w))
            for t in self.readers.get(w, ()):
                add(t)
        for t in extra:
            add(t)
        if fn is None:
            tok = None
        elif dma_key is not None:
            tok = self._tok_dma(dma_key)
        else:
            tok = self._tok_compute(eng)
        waits = []
        seen = self.seen[eng]
        for k, (s, v) in deps.items():
            if eng == "pe" and dma_key is None and k in self.pe_sems:
                continue
            if seen.get(k, 0) >= v:
                continue
            seen[k] = v
            waits.append((s, v))
        if tok is not None:
            self.latest[id(tok[0])] = tok
            for r in reads:
                lst = self.readers.setdefault(r, [])
                lst.append(tok)
                if len(lst) > 16:
                    best = {}
                    for (s, v) in lst:
                        if id(s) not in best or best[id(s)][1] < v:
                            best[id(s)] = (s, v)
                    self.readers[r] = list(best.values())
            for w in writes:
                self.last_write[w] = tok
                self.readers[w] = []
        self.ops[eng].append((fn, waits, tok, dma_key is not None))
        return tok

    def barrier(self):
        toks = list(self.latest.values())
        for e in self.ENGS:
            self.op(e, None, extra=toks)

    def emit(self):
        nc = self.nc
        ops = self.ops
        self.ops = {e: [] for e in self.ENGS}
        with nc.Block() as block:
            def run(engobj, name):
                for fn, waits, tok, is_dma in ops[name]:
                    for s, v in waits:
                        engobj.wait_ge(s, v)
                    if fn is not None:
                        ins = fn(engobj)
                        ins.then_inc(tok[0], 16 if is_dma else 1)

            @block.tensor
            def _(e):
                run(e, "pe")

            @block.scalar
            def _(e):
                run(e, "act")

            @block.vector
            def _(e):
                run(e, "dve")

            @block.gpsimd
            def _(e):
                run(e, "pool")

            @block.sync
            def _(e):
                run(e, "sp")


C_RQ, C_RK, C_RV, C_RG, C_DQ, C_DKV, C_IQ, C_IK, C_IW = 0, 512, 1024, 2048, 3072, 3584, 3712, 4224, 4288
D_IN = 4296


def build(NG, dbg=False):
    NT = 4 * NG
    SB = NT * 128
    NO = NG * 128
    nc = bass.Bass("TRN2", target_bir_lowering=False)

    def din(name, shape, dt=F32):
        return nc.dram_tensor(name, list(shape), dt, kind="ExternalInput").ap()

    def dscr(name, shape, dt):
        if dbg:
            return nc.dram_tensor(name, list(shape), dt, kind="ExternalOutput").ap()
        return nc.dram_tensor(name, list(shape), dt).ap()

    xb = din("xb", [SB, D]); xo = din("xo", [NO, D]); ccol = din("ccol", [128, 8])
    w_ada = din("w_ada", [D, 6 * D]); b_adaT = din("b_adaT", [128, 48]); b_g12 = din("b_g12", [128, 2 * D])
    gmixT = din("gmixT", [128, 8]); gffnT = din("gffnT", [128, 8])
    gfin_rep = din("gfin_rep", [128, D]); gn_rep = din("gn_rep", [128, D]); kvg_rep = din("kvg_rep", [128, 128])
    w_in = din("w_in", [D, D_IN]); w_kv = din("w_kv", [128, 128])
    w_ret_out = din("w_ret_out", [D, D]); w_dsa_out = din("w_dsa_out", [512, D])
    w_gate = din("w_gate", [D, 2 * D]); b_gate = din("b_gate", [1, 2 * D]); w_o = din("w_o", [D, D])
    w_rt = din("w_rt", [D, 20]); b_rt_rep = din("b_rt_rep", [128, 20])
    w1 = din("w1", [16, D, 256]); w3 = din("w3", [16, D, 256]); w2 = din("w2", [16, 256, D])
    rope_all = din("rope_all", [SB, 64]); rope_own = din("rope_own", [NO, 64])
    ident = din("ident", [128, 128]); DTd = din("DT", [128, H, 128]); qdec = din("qdec", [128, H])
    wtile = din("wtile", [128, H]); seld = din("sel", [64, 4]); maddd = din("madd", [128, 512])
    biasTd = din("biasT", [5, 128, H, 128]); c15 = din("c15_rep", [128, H]); seldend = din("selden", [65, 64])
    out = nc.dram_tensor("out", [NO, D], F32, kind="ExternalOutput").ap()

    uT_s = dscr("uT_s", [NG, 128, 8, 128], BF16)
    roT_s = dscr("roT_s", [NG, 128, 8, 128], BF16)
    dqT_s = dscr("dqT_s", [NG, 64, H, 128], BF16)
    iqT_s = dscr("iqT_s", [NG, 64, H, 128], BF16)
    iw_s = dscr("iw_s", [NG, 128, H], F32)
    ydT_s = dscr("ydT_s", [NG, 64, H, 128], BF16)
    h1_s = dscr("h1_s", [NG, 128, D], F32)
    if dbg:
        kT_dbg = dscr("kT_dbg", [128, SB], BF16)
        dv_dbg = dscr("dv_dbg", [128, NT, 65], BF16)
        thr_dbg = dscr("thr_dbg", [NG, 128, 8], F32)
        sc_dbg = dscr("sc_dbg", [NG, 128, SB], BF16)
        mk_dbg = dscr("mk_dbg", [NG, 128, SB], BF16)

    with contextlib.ExitStack() as top:
        S = Sched(nc, top)

        def sbt(st, n, sh, dt):
            return st.enter_context(nc.sbuf_tensor("sb_" + n, list(sh), dt))

        def pst(st, n, sh, dt):
            return st.enter_context(nc.psum_tensor("ps_" + n, list(sh), dt))

        V = lambda fn, r, w: S.op("dve", fn, reads=r, writes=w)
        A = lambda fn, r, w: S.op("act", fn, reads=r, writes=w)
        P = lambda fn, r, w: S.op("pe", fn, reads=r, writes=w)
        G = lambda fn, r, w: S.op("pool", fn, reads=r, writes=w)
        DM = lambda fn, r, w, k: S.op("sp", fn, reads=r, writes=w, dma_key=k)
        DG = lambda fn, r, w, k: S.op("pool", fn, reads=r, writes=w, dma_key=k)

        idb = sbt(top, "idb", [128, 128], BF16)
        epsc = sbt(top, "epsc", [128, 2], F32)
        G1T = sbt(top, "G1T", [128, 8], F32); sh1T = sbt(top, "sh1T", [128, 8], F32)
        G2T = sbt(top, "G2T", [128, 8], F32); sh2T = sbt(top, "sh2T", [128, 8], F32)
        g1rep = sbt(top, "g1rep", [128, D], BF16); g2rep = sbt(top, "g2rep", [128, D], BF16)
        DG(lambda e: e.dma_start(out=idb[:], in_=ident), [], ["idb"], "c0")
        V(lambda e: e.memset(epsc[:, 0:1], 1e-6), [], ["epsc"])
        V(lambda e: e.memset(epsc[:, 1:2], 1e-5), [], ["epsc"])

        def norm_T(x_ap, xres, junk, xs, pT, uT, stat, GT, shT, tag, ures=None):
            ures = ures or (tag + "uT")
            A(lambda e: e.activation(out=junk, in_=x_ap, func=AF.Square, scale=1.0 / 32.0, accum_out=stat[:, 0:1]),
              [xres], [tag + "junk", tag + "st"])
            A(lambda e: e.activation(out=stat[:, 1:2], in_=stat[:, 0:1], func=AF.Ln, bias=epsc[:, 0:1]),
              [tag + "st", "epsc"], [tag + "st"])
            A(lambda e: e.activation(out=stat[:, 2:3], in_=stat[:, 1:2], func=AF.Exp, scale=-0.5),
              [tag + "st"], [tag + "st"])
            V(lambda e: e.tensor_scalar(out=xs, in0=x_ap, scalar1=stat[:, 2:3], scalar2=None, op0=ALU.mult),
              [xres, tag + "st"], [tag + "xs"])
            for k in range(8):
                P(lambda e, k=k: e.transpose(out=pT[:, k, :], in_=xs[:, k * 128:(k + 1) * 128], identity=idb[:]),
                  [tag + "xs", "idb"], ["pT"])
            for k in range(8):
                V(lambda e, k=k: e.tensor_scalar(out=uT[:, k, :], in0=pT[:, k, :], scalar1=GT[:, k:k + 1],
                                                 scalar2=shT[:, k:k + 1], op0=ALU.mult, op1=ALU.add),
                  ["pT", "mod"], [ures])

        def rope(src, cosb, sinb, dst, ta, tb, rres, wres, tag):
            s1, s2 = src[:, :, 0:32], src[:, :, 32:64]
            V(lambda e: e.tensor_tensor(out=ta, in0=s1, in1=cosb, op=ALU.mult), rres, [tag + "ta"])
            V(lambda e: e.tensor_tensor(out=tb, in0=s2, in1=sinb, op=ALU.mult), rres, [tag + "tb"])
            V(lambda e: e.tensor_tensor(out=dst[:, :, 0:32], in0=ta, in1=tb, op=ALU.subtract), [tag + "ta", tag + "tb"], wres)
            V(lambda e: e.tensor_tensor(out=ta, in0=s2, in1=cosb, op=ALU.mult), rres, [tag + "ta"])
            V(lambda e: e.tensor_tensor(out=tb, in0=s1, in1=sinb, op=ALU.mult), rres, [tag + "tb"])
            V(lambda e: e.tensor_tensor(out=dst[:, :, 32:64], in0=ta, in1=tb, op=ALU.add), [tag + "ta", tag + "tb"], wres)

        with contextlib.ExitStack() as ph:
            cs = sbt(ph, "cs", [128, 8], F32); csb = sbt(ph, "csb", [128, 8], BF16)
            csbc = sbt(ph, "csbc", [128, 8, 128], BF16)
            wa = [sbt(ph, f"wa{i}", [128, 8, D], BF16) for i in range(2)]
            badT = sbt(ph, "badT", [128, 48], F32); bg12 = sbt(ph, "bg12", [128, 2 * D], F32)
            gmx = sbt(ph, "gmx", [128, 8], F32); gff = sbt(ph, "gff", [128, 8], F32)
            modT = sbt(ph, "modT", [128, 32], F32)
            psm = pst(ph, "psm", [128, 32], F32)
            pg = pst(ph, "pg", [128, D], F32)
            DM(lambda e: e.dma_start(out=cs[:], in_=ccol), [], ["cs"], "c1_cs")
            DM(lambda e: e.dma_start(out=badT[:], in_=b_adaT), [], ["badT"], "c1_badT")
            DM(lambda e: e.dma_start(out=bg12[:], in_=b_g12), [], ["bg12"], "c1_bg12")
            DM(lambda e: e.dma_start(out=gmx[:], in_=gmixT), [], ["gmx"], "c1_gmx")
            DM(lambda e: e.dma_start(out=gff[:], in_=gffnT), [], ["gff"], "c1_gff")
            A(lambda e: e.activation(out=csb[:], in_=cs[:], func=AF.Silu), ["cs"], ["csb"])
            V(lambda e: e.tensor_copy(out=csbc[:], in_=csb[:].unsqueeze(2).broadcast_to([128, 8, 128])), ["csb"], ["csbc"])
            fm_i = 0
            for p in range(6):
                sl = p % 2
                for k in range(8):
                    DG(lambda e, p=p, k=k, sl=sl: e.dma_start(out=wa[sl][:, k, :], in_=w_ada[k * 128:(k + 1) * 128, p * D:(p + 1) * D]),
                       [], [f"wa{sl}"], f"wa{sl}")
                if p in (2, 5):
                    for hf in range(2):
                        for k in range(8):
                            P(lambda e, k=k, hf=hf, sl=sl: e.matmul(pg[:, hf * 512:(hf + 1) * 512], lhsT=csbc[:, k, :],
                                                                  rhs=wa[sl][:, k, hf * 512:(hf + 1) * 512], start=(k == 0), stop=(k == 7)),
                              ["csbc", f"wa{sl}"], ["pg"])
                    dst = g1rep if p == 2 else g2rep
                    off = 0 if p == 2 else D
                    V(lambda e, dst=dst, off=off: e.tensor_tensor(out=dst[:], in0=pg[:], in1=bg12[:, off:off + D], op=ALU.add),
                      ["pg", "bg12"], ["mod"])
                else:
                    for f in range(8):
                        for k in range(8):
                            P(lambda e, k=k, f=f, sl=sl, c=fm_i * 8 + f: e.matmul(psm[:, c:c + 1], lhsT=wa[sl][:, k, f * 128:(f + 1) * 128],
                                                                                 rhs=csb[:, k:k + 1], start=(k == 0), stop=(k == 7)),
                              ["csb", f"wa{sl}"], ["psm"])
                    fm_i += 1
            for i, p in enumerate((0, 1, 3, 4)):
                V(lambda e, i=i, p=p: e.tensor_tensor(out=modT[:, i * 8:(i + 1) * 8], in0=psm[:, i * 8:(i + 1) * 8],
                                                      in1=badT[:, p * 8:(p + 1) * 8], op=ALU.add), ["psm", "badT"], ["modT"])
            V(lambda e: e.tensor_copy(out=sh1T[:], in_=modT[:, 0:8]), ["modT"], ["mod"])
            V(lambda e: e.tensor_copy(out=sh2T[:], in_=modT[:, 16:24]), ["modT"], ["mod"])
            V(lambda e: e.scalar_tensor_tensor(out=G1T[:], in0=modT[:, 8:16], scalar=1.0, in1=gmx[:], op0=ALU.add, op1=ALU.mult),
              ["modT", "gmx"], ["mod"])
            V(lambda e: e.scalar_tensor_tensor(out=G2T[:], in0=modT[:, 24:32], scalar=1.0, in1=gff[:], op0=ALU.add, op1=ALU.mult),
              ["modT", "gff"], ["mod"])
            S.barrier()
            S.emit()

        kvs = contextlib.ExitStack()
        kT = sbt(kvs, "kT", [128, SB], BF16)
        dv1 = sbt(kvs, "dv1", [128, NT, 65], BF16)

        with contextlib.ExitStack() as ph:
            w_in_sb = sbt(ph, "w_in_sb", [128, 8, D_IN], BF16)
            wkv_sb = sbt(ph, "wkv_sb", [128, 192], BF16)
            kvg = sbt(ph, "kvg", [128, 128], F32); gnr = sbt(ph, "gnr", [128, D], F32)
            DTs = sbt(ph, "DTs", [128, H, 128], BF16); qdc = sbt(ph, "qdc", [128, H], F32)
            wtl = sbt(ph, "wtl", [128, H], F32); sels = sbt(ph, "sels", [64, 4], F32)
            xt = [sbt(ph, f"xt{i}", [128, D], F32) for i in range(2)]
            rp = [sbt(ph, f"rp{i}", [128, 64], F32) for i in range(2)]
            junk = sbt(ph, "junk", [128, D], BF16)
            xs = sbt(ph, "xs", [128, D], BF16)
            uT = [sbt(ph, f"uT{i}", [128, 8, 128], BF16) for i in range(2)]
            stat = sbt(ph, "stat", [128, 8], F32)
            ta = sbt(ph, "ta", [128, H, 32], F32); tb = sbt(ph, "tb", [128, H, 32], F32)
            krp = sbt(ph, "krp", [128, H, 64], F32); kw = sbt(ph, "kw", [128, H, 64], BF16)
            vb = sbt(ph, "vb", [128, D], BF16)
            Sst = sbt(ph, "Sst", [64, H, 128], F32); T0 = sbt(ph, "T0", [64, H, 128], F32)
            T0b = sbt(ph, "T0b", [64, H, 128], BF16)
            latn = sbt(ph, "latn", [128, 128], BF16); latnT = sbt(ph, "latnT", [128, 128], BF16)
            qr = sbt(ph, "qr", [128, H, 64], BF16); kro = sbt(ph, "kro", [128, H, 64], BF16)
            qT = sbt(ph, "qT", [64, H, 128], BF16); kTo = sbt(ph, "kTo", [64, H, 128], BF16)
            scm = sbt(ph, "scm", [128, H, 128], BF16)
            sg = sbt(ph, "sg", [128, D], BF16)
            oo = sbt(ph, "oo", [128, H, 128], F32); o2 = sbt(ph, "o2", [128, H, 128], F32)
            gst = sbt(ph, "gst", [128, 40], F32)
            ro = sbt(ph, "ro", [128, D], BF16); roT = sbt(ph, "roT", [128, 8, 128], BF16)
            dqb = sbt(ph, "dqb", [128, 512], BF16); iqb = sbt(ph, "iqb", [128, 512], BF16)
            dqT = sbt(ph, "dqT", [64, H, 128], BF16); iqT = dqT
            iwb = sbt(ph, "iwb", [128, H], F32)
            pT = pst(ph, "pT", [128, 8, 128], BF16)
            zA = pst(ph, "zA", [128, D], F32); zB = pst(ph, "zB", [128, D], F32)
            misc = pst(ph, "misc", [128, 512], F32)
            kvp = pst(ph, "kvp", [128, H, 128], F32)
            pTs = misc[:, 448:512].bitcast(BF16)

            for k in range(8):
                DG(lambda e, k=k: e.dma_start(out=w_in_sb[:, k, :], in_=w_in[k * 128:(k + 1) * 128, :]), [], ["w_in_sb"], "w_in")
            V(lambda e: e.memset(wkv_sb[:, 0:64], 0.0), [], ["wkv0"])
            DG(lambda e: e.dma_start(out=wkv_sb[:, 64:192], in_=w_kv), [], ["wkv1"], "c2_wkv")
            DG(lambda e: e.dma_start(out=DTs[:], in_=DTd), [], ["DTs"], "c2_DT")
            DM(lambda e: e.dma_start(out=kvg[:], in_=kvg_rep), [], ["kvg"], "c3_kvg")
            DM(lambda e: e.dma_start(out=gnr[:], in_=gn_rep), [], ["gnr"], "c3_gnr")
            DM(lambda e: e.dma_start(out=qdc[:], in_=qdec), [], ["qdc"], "c3_qdc")
            DM(lambda e: e.dma_start(out=wtl[:], in_=wtile), [], ["wtl"], "c3_wtl")
            DM(lambda e: e.dma_start(out=sels[:], in_=seld), [], ["sels"], "c3_sels")
            V(lambda e: e.memset(Sst[:], 0.0), [], ["Sst"])
            V(lambda e: e.memset(dv1[:, :, 64:65], 1.0), [], ["dv1one"])

            def kside_s1(pos, j):
                sl = pos % 2
                DM(lambda e: e.dma_start(out=xt[sl][:], in_=xb[j * 128:(j + 1) * 128, :]), [], [f"xt{sl}"], f"xt{sl}")
                DM(lambda e: e.dma_start(out=rp[sl][:], in_=rope_all[j * 128:(j + 1) * 128, :]), [], [f"rp{sl}"], f"rp{sl}")
                norm_T(xt[sl][:], f"xt{sl}", junk[:], xs[:], pT, uT[sl], stat, G1T, sh1T, "k", ures=f"kuT{sl}")

            def kside(pos, j):
                sl = pos % 2
                s = j % 4
                ures = [f"kuT{sl}", "w_in_sb"]
                for (dst, c0, n, res) in ((zA[:, 0:512], C_RK, 512, ("zA", 0)), (zB[:, 0:512], C_RV, 512, ("zB", 0)),
                                          (zB[:, 512:1024], C_RV + 512, 512, ("zB", 1)), (misc[:, 0:128], C_DKV, 128, "m_dkv")):
                    for k in range(8):
                        P(lambda e, k=k, dst=dst, c0=c0, n=n: e.matmul(dst, lhsT=uT[sl][:, k, :], rhs=w_in_sb[:, k, c0:c0 + n],
                                                                      start=(k == 0), stop=(k == 7)), ures, [res])
                for k in range(8):
                    P(lambda e, k=k: e.matmul(misc[0:64, 128:256], lhsT=w_in_sb[:, k, C_IK:C_IK + 64], rhs=uT[sl][:, k, :],
                                              start=(k == 0), stop=(k == 7)), ures, ["m_ik"])
                cosb = rp[sl][:, 0:32].unsqueeze(1).broadcast_to([128, H, 32])
                sinb = rp[sl][:, 32:64].unsqueeze(1).broadcast_to([128, H, 32])
                rope(zA[:, 0:512].rearrange("p (h d) -> p h d", h=H), cosb, sinb, krp, ta[:], tb[:], [("zA", 0), f"rp{sl}"], ["krp"], "r")
                V(lambda e: e.tensor_tensor(out=kw[:], in0=krp[:], in1=wtl[:].unsqueeze(2).broadcast_to([128, H, 64]), op=ALU.mult),
                  ["krp", "wtl"], ["kw"])
                A(lambda e: e.activation(out=vb[:], in_=zB[:], func=AF.Copy), [("zB", 0), ("zB", 1)], ["vb"])
                if s == 0:
                    V(lambda e: e.tensor_scalar(out=T0[:], in0=Sst[:], scalar1=sels[:, 0:1], scalar2=None, op0=ALU.mult),
                      ["Sst", "sels"], ["T0"])
                else:
                    V(lambda e: e.scalar_tensor_tensor(out=T0[:], in0=Sst[:], scalar=sels[:, s:s + 1], in1=T0[:], op0=ALU.mult, op1=ALU.add),
                      ["Sst", "sels", "T0"], ["T0"])
                for h in range(H):
                    P(lambda e, h=h: e.matmul(kvp[0:64, h, :], lhsT=kw[:, h, :], rhs=vb[:, h * 128:(h + 1) * 128], start=True, stop=True),
                      ["kw", "vb"], [("kvp", h // 4)])
                for h in range(H):
                    V(lambda e, h=h: e.scalar_tensor_tensor(out=Sst[:, h, :], in0=Sst[:, h, :], scalar=float(GAM[h] ** 128),
                                                           in1=kvp[0:64, h, :], op0=ALU.mult, op1=ALU.add),
                      ["Sst", ("kvp", h // 4)], ["Sst"])
                A(lambda e: e.activation(out=junk[:, 0:128], in_=misc[:, 0:128], func=AF.Square, scale=1.0 / math.sqrt(128.0),
                                         accum_out=stat[:, 4:5]), ["m_dkv"], ["kjunk", "st2"])
                A(lambda e: e.activation(out=stat[:, 5:6], in_=stat[:, 4:5], func=AF.Ln, bias=epsc[:, 0:1]), ["st2", "epsc"], ["st2"])
                A(lambda e: e.activation(out=stat[:, 6:7], in_=stat[:, 5:6], func=AF.Exp, scale=-0.5), ["st2"], ["st2"])
                V(lambda e: e.scalar_tensor_tensor(out=latn[:], in0=misc[:, 0:128], scalar=stat[:, 6:7], in1=kvg[:], op0=ALU.mult, op1=ALU.mult),
                  ["m_dkv", "st2", "kvg"], ["latn"])
                P(lambda e: e.transpose(out=pTs, in_=latn[:], identity=idb[:]), ["latn", "idb"], ["pTs"])
                A(lambda e: e.activation(out=latnT[:], in_=pTs, func=AF.Copy), ["pTs"], ["latnT"])
                P(lambda e: e.matmul(misc[:, 256:384], lhsT=wkv_sb[:, 0:128], rhs=latnT[:], start=True, stop=True),
                  ["latnT", "wkv0", "wkv1"], ["m_dk"])
                P(lambda e: e.matmul(misc[:, 384:448], lhsT=latnT[:], rhs=wkv_sb[:, 128:192], start=True, stop=True),
                  ["latnT", "wkv1"], ["m_dv"])
                A(lambda e: e.activation(out=kT[0:64, j * 128:(j + 1) * 128], in_=misc[0:64, 128:256], func=AF.Copy), ["m_ik"], [("kT", j)])
                A(lambda e: e.activation(out=kT[64:128, j * 128:(j + 1) * 128], in_=misc[64:128, 256:384], func=AF.Copy), ["m_dk"], [("kT", j)])
                V(lambda e: e.tensor_copy(out=dv1[:, j, 0:64], in_=misc[:, 384:448]), ["m_dv"], [("dv1", j)])

            def ownside_s1(pos, g):
                sl = pos % 2
                DM(lambda e: e.dma_start(out=xt[sl][:], in_=xo[g * 128:(g + 1) * 128, :]), [], [f"xt{sl}"], f"xt{sl}")
                DM(lambda e: e.dma_start(out=rp[sl][:], in_=rope_own[g * 128:(g + 1) * 128, :]), [], [f"rp{sl}"], f"rp{sl}")
                norm_T(xt[sl][:], f"xt{sl}", junk[:], xs[:], pT, uT[sl], stat, G1T, sh1T, "k", ures=f"kuT{sl}")
                DM(lambda e: e.dma_start(out=uT_s[g], in_=uT[sl][:]), [f"kuT{sl}"], [("uT_s", g)], "st_u")

            def ownside(pos, g):
                sl = pos % 2
                ures = [f"kuT{sl}", "w_in_sb"]
                u = uT[sl]

                def proj(dst, c0, n, res):
                    for k in range(8):
                        P(lambda e, k=k: e.matmul(dst, lhsT=u[:, k, :], rhs=w_in_sb[:, k, c0:c0 + n], start=(k == 0), stop=(k == 7)), ures, [res])

                cosb = rp[sl][:, 0:32].unsqueeze(1).broadcast_to([128, H, 32])
                sinb = rp[sl][:, 32:64].unsqueeze(1).broadcast_to([128, H, 32])
                proj(zA[:, 0:512], C_RQ, 512, ("zA", 0))
                proj(zA[:, 512:1024], C_RK, 512, ("zA", 1))
                rope(zA[:, 0:512].rearrange("p (h d) -> p h d", h=H), cosb, sinb, qr, ta[:], tb[:], [("zA", 0), f"rp{sl}"], ["qr"], "r")
                rope(zA[:, 512:1024].rearrange("p (h d) -> p h d", h=H), cosb, sinb, kro, ta[:], tb[:], [("zA", 1), f"rp{sl}"], ["kro"], "r")
                pT64 = pT[0:64, :, :]
                for h in range(H):
                    P(lambda e, h=h: e.transpose(out=pT64[:, h, :], in_=qr[:, h, :], identity=idb[:]), ["qr", "idb"], ["pT"])
                V(lambda e: e.tensor_copy(out=qT[:], in_=pT64), ["pT"], ["qT"])
                for h in range(H):
                    P(lambda e, h=h: e.transpose(out=pT64[:, h, :], in_=kro[:, h, :], identity=idb[:]), ["kro", "idb"], ["pT"])
                A(lambda e: e.activation(out=kTo[:], in_=pT64, func=AF.Copy), ["pT"], ["kTo"])
                proj(zB[:, 0:512], C_RV, 512, ("zB", 0))
                proj(zB[:, 512:1024], C_RV + 512, 512, ("zB", 1))
                A(lambda e: e.activation(out=vb[:], in_=zB[:], func=AF.Copy), [("zB", 0), ("zB", 1)], ["vb"])
                zA3 = zA[:].rearrange("p (h d) -> p h d", h=H)
                for h in range(H):
                    P(lambda e, h=h: e.matmul(zA3[:, h, :], lhsT=kTo[:, h, :], rhs=qT[:, h, :], start=True, stop=True),
                      ["kTo", "qT"], [("zA", h // 4)])
                V(lambda e: e.tensor_tensor(out=scm[:], in0=zA3, in1=DTs[:], op=ALU.mult), [("zA", 0), ("zA", 1), "DTs"], ["scm"])
                zB3 = zB[:].rearrange("p (h d) -> p h d", h=H)
                for h in range(H):
                    P(lambda e, h=h: e.matmul(zB3[:, h, :], lhsT=scm[:, h, :], rhs=vb[:, h * 128:(h + 1) * 128], start=True, stop=True),
                      ["scm", "vb"], [("zB", h // 4)])
                V(lambda e: e.tensor_copy(out=T0b[:], in_=T0[:]), ["T0"], ["T0b"])
                for h in range(H):
                    P(lambda e, h=h: e.matmul(kvp[:, h, :], lhsT=qT[:, h, :], rhs=T0b[:, h, :], start=True, stop=True),
                      ["qT", "T0b"], [("kvp", h // 4)])
                V(lambda e: e.tensor_tensor(out=oo[:], in0=kvp[:], in1=qdc[:].unsqueeze(2).broadcast_to([128, H, 128]), op=ALU.mult),
                  [("kvp", 0), ("kvp", 1), "qdc"], ["oo"])
                V(lambda e: e.tensor_tensor(out=oo[:], in0=oo[:], in1=zB3, op=ALU.add), ["oo", ("zB", 0), ("zB", 1)], ["oo"])
                V(lambda e: e.tensor_reduce(out=gst[:, 0:8], in_=oo[:], axis=AX.X, op=ALU.add), ["oo"], ["gst"])
                V(lambda e: e.tensor_tensor(out=o2[:], in0=oo[:], in1=oo[:], op=ALU.mult), ["oo"], ["o2"])
                V(lambda e: e.tensor_reduce(out=gst[:, 8:16], in_=o2[:], axis=AX.X, op=ALU.add), ["o2"], ["gst"])
                V(lambda e: e.tensor_scalar(out=gst[:, 16:24], in0=gst[:, 0:8], scalar1=1.0 / 128.0, scalar2=None, op0=ALU.mult), ["gst"], ["gst"])
                V(lambda e: e.tensor_tensor(out=gst[:, 24:32], in0=gst[:, 16:24], in1=gst[:, 16:24], op=ALU.mult), ["gst"], ["gst"])
                V(lambda e: e.scalar_tensor_tensor(out=gst[:, 24:32], in0=gst[:, 8:16], scalar=1.0 / 128.0, in1=gst[:, 24:32],
                                                   op0=ALU.mult, op1=ALU.subtract), ["gst"], ["gst"])
                A(lambda e: e.activation(out=gst[:, 32:40], in_=gst[:, 24:32], func=AF.Ln, bias=epsc[:, 1:2]), ["gst", "epsc"], ["gst2"])
                A(lambda e: e.activation(out=gst[:, 32:40], in_=gst[:, 32:40], func=AF.Exp, scale=-0.5), ["gst2"], ["gst2"])
                for h in range(H):
                    V(lambda e, h=h: e.tensor_scalar(out=o2[:, h, :], in0=oo[:, h, :], scalar1=gst[:, 16 + h:17 + h], scalar2=gst[:, 32 + h:33 + h],
                                                     op0=ALU.subtract, op1=ALU.mult), ["oo", "gst", "gst2"], ["o2"])
                proj(zA[:, 0:512], C_RG, 512, ("zA", 0))
                proj(zA[:, 512:1024], C_RG + 512, 512, ("zA", 1))
                A(lambda e: e.activation(out=sg[:], in_=zA[:], func=AF.Silu), [("zA", 0), ("zA", 1)], ["sg"])
                o2f = o2[:].rearrange("p h d -> p (h d)")
                V(lambda e: e.tensor_tensor(out=o2f, in0=o2f, in1=gnr[:], op=ALU.mult), ["o2", "gnr"], ["o2"])
                V(lambda e: e.tensor_tensor(out=ro[:], in0=o2f, in1=sg[:], op=ALU.mult), ["o2", "sg"], ["ro"])
                for k in range(8):
                    P(lambda e, k=k: e.transpose(out=pT[:, k, :], in_=ro[:, k * 128:(k + 1) * 128], identity=idb[:]), ["ro", "idb"], ["pT"])
                A(lambda e: e.activation(out=roT[:], in_=pT[:], func=AF.Copy), ["pT"], ["roT"])
                DM(lambda e: e.dma_start(out=roT_s[g], in_=roT[:]), ["roT"], [("roT_s", g)], "st_r")
                proj(zB[:, 0:512], C_DQ, 512, ("zB", 0))
                proj(zB[:, 512:1024], C_IQ, 512, ("zB", 1))
                proj(misc[:, 0:8], C_IW, 8, "m_dkv")
                V(lambda e: e.tensor_copy(out=dqb[:], in_=zB[:, 0:512]), [("zB", 0)], ["dqb"])
                A(lambda e: e.activation(out=iqb[:], in_=zB[:, 512:1024], func=AF.Copy), [("zB", 1)], ["iqb"])
                V(lambda e: e.tensor_copy(out=iwb[:], in_=misc[:, 0:8]), ["m_dkv"], ["iwb"])
                DM(lambda e: e.dma_start(out=iw_s[g], in_=iwb[:]), ["iwb"], [("iw_s", g)], "st_w")
                for h in range(H):
                    P(lambda e, h=h: e.transpose(out=pT64[:, h, :], in_=dqb[:, h * 64:(h + 1) * 64], identity=idb[:]), ["dqb", "idb"], ["pT"])
                V(lambda e: e.tensor_copy(out=dqT[:], in_=pT64), ["pT"], ["dqT"])
                DM(lambda e: e.dma_start(out=dqT_s[g], in_=dqT[:]), ["dqT"], [("dqT_s", g)], "st_q")
                for h in range(H):
                    P(lambda e, h=h: e.transpose(out=pT64[:, h, :], in_=iqb[:, h * 64:(h + 1) * 64], identity=idb[:]), ["iqb", "idb"], ["pT"])
                A(lambda e: e.activation(out=iqT[:], in_=pT64, func=AF.Copy), ["pT"], ["dqT"])
                DM(lambda e: e.dma_start(out=iqT_s[g], in_=iqT[:]), ["dqT"], [("iqT_s", g)], "st_i")

            items = []
            for g in range(NG):
                for s4 in range(4):
                    items.append(("k", 4 * g + s4))
                items.append(("o", g))

            def stage1(pos):
                kind, idx = items[pos]
                (kside_s1 if kind == "k" else ownside_s1)(pos, idx)

            stage1(0)
            for pos in range(len(items)):
                if pos + 1 < len(items):
                    stage1(pos + 1)
                kind, idx = items[pos]
                (kside if kind == "k" else ownside)(pos, idx)
            if dbg:
                DM(lambda e: e.dma_start(out=kT_dbg, in_=kT[:]), [("kT", j) for j in range(NT)], ["kT_dbg"], "dbg")
                DM(lambda e: e.dma_start(out=dv_dbg, in_=dv1[:]), [("dv1", j) for j in range(NT)] + ["dv1one"], ["dv_dbg"], "dbg")
            S.barrier()
            S.emit()

        PHASES = build.phases
        if PHASES >= 2:
          with contextlib.ExitStack() as ph:
            score2 = [sbt(ph, f"score{i}", [128, SB], BF16) for i in range(2)]
            msk = sbt(ph, "msk", [128, SB], BF16)
            mskT = [sbt(ph, f"mskT{i}", [128, 8, 128], BF16) for i in range(2)]
            iq_sb2 = [sbt(ph, f"iq_sb{i}", [128, H, 128], BF16) for i in range(2)]
            dq_sb2 = [sbt(ph, f"dq_sb{i}", [128, H, 128], BF16) for i in range(2)]
            iw_sb2 = [sbt(ph, f"iw_sb{i}", [128, H], F32) for i in range(2)]
            dg2 = [sbt(ph, f"dg{i}", [128, H, 128], BF16) for i in range(2)]
            maddb = sbt(ph, "maddb", [128, 512], BF16)
            rh = [sbt(ph, f"rh{i}", [128, 512], BF16) for i in range(4)]
            madd = sbt(ph, "madd", [128, 512], F32)
            bT = sbt(ph, "bT", [128, 5, H, 128], BF16)
            c15s = sbt(ph, "c15s", [128, H], F32)
            selden = sbt(ph, "selden", [65, 64], F32)
            bst = sbt(ph, "bst", [128, 8], F32)
            steps = sbt(ph, "steps", [128, NBIS], F32)
            pmx = [sbt(ph, f"pmx{i}", [128, 16], F32) for i in range(2)]
            pmn = [sbt(ph, f"pmn{i}", [128, 16], F32) for i in range(2)]
            bx = sbt(ph, "bx", [128, 2], F32)
            halves = sbt(ph, "halves", [128, NBIS], F32)
            tmpl = sbt(ph, "tmpl", [128, 512], F32)
            lg = sbt(ph, "lg", [128, H, 128], F32)
            pe_ = [sbt(ph, f"pe{i}", [128, H, 128], BF16) for i in range(2)]
            pm = [sbt(ph, f"pm{i}", [128, H, 128], BF16) for i in range(3)]
            OT = sbt(ph, "OT", [65, H * 128], F32)
            ydT = sbt(ph, "ydT", [64, H, 128], BF16)
            PP = [pst(ph, f"PP{i}", [128, 1024], F32) for i in range(4)]
            ps_s = [PP[k // 2][:, (k % 2) * 512:(k % 2 + 1) * 512] for k in range(4)]
            ps_s_res = [(f"PP{k // 2}", k % 2) for k in range(4)]
            ps_sc = [PP[2][:, 0:512], PP[2][:, 512:1024]]
            ps_sc_res = [("PP2", 0), ("PP2", 1)]
            ps_qk = [PP[0], PP[1]]
            ps_o = PP[2]
            ps_mT = PP[3][:, 0:512].bitcast(BF16).rearrange("p (a b) -> p a b", a=8)

            DM(lambda e: e.dma_start(out=madd[:], in_=maddd), [], ["madd"], "c4_madd")
            DG(lambda e: e.dma_start(out=bT[:], in_=biasTd.rearrange("s p h q -> p s h q")), [], ["bT"], "c4_bT")
            DM(lambda e: e.dma_start(out=c15s[:], in_=c15), [], ["c15s"], "c4_c15s")
            DM(lambda e: e.dma_start(out=selden[:], in_=seldend), [], ["selden"], "c4_selden")
            for s5 in range(5):
                V(lambda e, s5=s5: e.tensor_tensor(out=bT[:, s5, :, :], in0=bT[:, s5, :, :],
                                                   in1=c15s[:].unsqueeze(2).broadcast_to([128, H, 128]), op=ALU.subtract),
                  ["bT", "c15s"], ["bT"])
            for i in range(NBIS):
                V(lambda e, i=i: e.memset(halves[:, i:i + 1], 2.0 ** (-(i + 1))), [], ["halves"])
            for i in range(2):
                G(lambda e, i=i: e.memset(iq_sb2[i][64:128, :, :], 0.0), [], [f"iq_sb{i}"])
                G(lambda e, i=i: e.memset(dq_sb2[i][0:64, :, :], 0.0), [], [f"dq_sb{i}"])
            V(lambda e: e.tensor_copy(out=maddb[:], in_=madd[:]), ["madd"], ["maddb"])

            def load_q(g):
                sl = g % 2
                DM(lambda e: e.dma_start(out=iq_sb2[sl][0:64, :, :], in_=iqT_s[g]), [("iqT_s", g)], [f"iq_sb{sl}"], f"ld_i{sl}")
                DM(lambda e: e.dma_start(out=dq_sb2[sl][64:128, :, :], in_=dqT_s[g]), [("dqT_s", g)], [f"dq_sb{sl}"], f"ld_q{sl}")
                DM(lambda e: e.dma_start(out=iw_sb2[sl][:], in_=iw_s[g]), [("iw_s", g)], [f"iw_sb{sl}"], f"ld_w{sl}")
                for h in range(H):
                    V(lambda e, h=h: e.tensor_scalar(out=dg2[sl][:, h, :], in0=idb[:], scalar1=iw_sb2[sl][:, h:h + 1], scalar2=None, op0=ALU.mult),
                      ["idb", f"iw_sb{sl}"], [f"dg{sl}"])

            def indexer(g):
                sl = g % 2
                iq_sb = iq_sb2[sl]; dg = dg2[sl]; score = score2[sl]
                iqres = f"iq_sb{sl}"; dgres = f"dg{sl}"
                M = 8 * (g + 1)

                def smm(i):
                    c, h = i // 8, i % 8
                    kres = [("kT", 4 * c + t) for t in range(4)]
                    P(lambda e: e.matmul(ps_s[i % 4], lhsT=iq_sb[:, h, :], rhs=kT[:, c * 512:(c + 1) * 512], start=True, stop=True),
                      [iqres] + kres, [ps_s_res[i % 4]])

                def rl_dmm(i):
                    c, h = i // 8, i % 8
                    r4 = i % 4
                    A(lambda e: e.activation(out=rh[r4][:], in_=ps_s[r4], func=AF.Relu), [ps_s_res[r4]], [f"rh{r4}"])
                    last = (c == g)
                    P(lambda e: e.matmul(ps_sc[c % 2], lhsT=dg[:, h, :], rhs=rh[r4][:], start=(h == 0), stop=(h == H - 1 and not last)),
                      [dgres, f"rh{r4}"], [ps_sc_res[c % 2]])
                    if h == H - 1:
                        if last:
                            P(lambda e: e.matmul(ps_sc[c % 2], lhsT=idb[:], rhs=maddb[:], start=False, stop=True), ["idb", "maddb"], [ps_sc_res[c % 2]])
                        A(lambda e: e.activation(out=score[:, c * 512:(c + 1) * 512], in_=ps_sc[c % 2], func=AF.Copy), [ps_sc_res[c % 2]], [(f"score{sl}", c)])

                smm(0)
                smm(1)
                for i_ in range(M):
                    if i_ + 2 < M:
                        smm(i_ + 2)
                    rl_dmm(i_)

            def mm_ops(G):
                slg = G % 2
                sc = score2[slg]
                ops_ = []
                npc = (G + 3) // 4
                for p in range(npc):
                    a, b_ = p * 2048, min((p + 1) * 2048, G * 512)
                    rr = [(f"score{slg}", c) for c in range(4 * p, min(4 * p + 4, G))]
                    ops_.append(lambda p=p, a=a, b_=b_, rr=rr: V(lambda e: e.tensor_reduce(out=pmx[slg][:, p:p + 1], in_=sc[:, a:b_], axis=AX.X, op=ALU.max),
                                                                   rr, [(f"pmx{slg}", p)]))
                    ops_.append(lambda p=p, a=a, b_=b_, rr=rr: V(lambda e: e.tensor_reduce(out=pmn[slg][:, p:p + 1], in_=sc[:, a:b_], axis=AX.X, op=ALU.min),
                                                                   rr, [(f"pmn{slg}", p)]))
                return ops_

            def bisect(g):
                sl = g % 2
                score = score2[sl]
                nk = (g + 1) * 512
                sres = [(f"score{sl}", c) for c in range(g + 1)]
                V(lambda e: e.scalar_tensor_tensor(out=tmpl[:], in0=madd[:], scalar=-2.0, in1=score[:, nk - 512:nk], op0=ALU.mult, op1=ALU.add),
                  [(f"score{sl}", g), "madd"], ["tmpl"])
                V(lambda e: e.tensor_reduce(out=bst[:, 2:3], in_=tmpl[:], axis=AX.X, op=ALU.min), ["tmpl"], ["bst2"])
                V(lambda e: e.tensor_reduce(out=bst[:, 0:1], in_=score[:, nk - 512:nk], axis=AX.X, op=ALU.max), [(f"score{sl}", g)], ["bst0"])
                if g > 0:
                    npc = (g + 3) // 4
                    V(lambda e: e.tensor_reduce(out=bx[:, 0:1], in_=pmx[sl][:, 0:npc], axis=AX.X, op=ALU.max), [(f"pmx{sl}", p) for p in range(npc)], ["bx0"])
                    V(lambda e: e.tensor_tensor(out=bst[:, 0:1], in0=bst[:, 0:1], in1=bx[:, 0:1], op=ALU.max), ["bst0", "bx0"], ["bst0"])
                    V(lambda e: e.tensor_reduce(out=bst[:, 1:2], in_=pmn[sl][:, 0:npc], axis=AX.X, op=ALU.min), [(f"pmn{sl}", p) for p in range(npc)], ["bst1"])
                    V(lambda e: e.tensor_tensor(out=bst[:, 3:4], in0=bst[:, 1:2], in1=bst[:, 2:3], op=ALU.min), ["bst1", "bst2"], ["bt"])
                else:
                    V(lambda e: e.tensor_copy(out=bst[:, 3:4], in_=bst[:, 2:3]), ["bst2"], ["bt"])
                V(lambda e: e.tensor_tensor(out=bst[:, 4:5], in0=bst[:, 0:1], in1=bst[:, 3:4], op=ALU.subtract), ["bst0", "bt"], ["bR"])
                V(lambda e: e.tensor_scalar(out=steps[:], in0=halves[:], scalar1=bst[:, 4:5], scalar2=None, op0=ALU.mult), ["halves", "bR"], ["steps"])
                for i in range(NBIS):
                    V(lambda e, i=i: e.tensor_tensor(out=bst[:, 5:6], in0=bst[:, 3:4], in1=steps[:, i:i + 1], op=ALU.add), ["bt", "steps"], ["bcand"])
                    V(lambda e: e.tensor_scalar(out=msk[:, 0:nk], in0=score[:, 0:nk], scalar1=bst[:, 5:6], scalar2=0.0, op0=ALU.is_ge, op1=ALU.add,
                                                accum_out=bst[:, 6:7]), sres + ["bcand"], ["msk", "bcnt"])
                    V(lambda e, i=i: e.tensor_scalar(out=bst[:, 7:8], in0=bst[:, 6:7], scalar1=255.5, scalar2=steps[:, i:i + 1], op0=ALU.is_ge, op1=ALU.mult),
                      ["bcnt", "steps"], ["binc"])
                    V(lambda e: e.tensor_tensor(out=bst[:, 3:4], in0=bst[:, 3:4], in1=bst[:, 7:8], op=ALU.add), ["bt", "binc"], ["bt"])
                V(lambda e: e.tensor_scalar(out=msk[:, 0:nk], in0=score[:, 0:nk], scalar1=bst[:, 3:4], scalar2=0.0, op0=ALU.is_ge, op1=ALU.add,
                                            accum_out=bst[:, 6:7]), sres + ["bt"], ["msk", "bcnt"])
                if dbg:
                    DM(lambda e: e.dma_start(out=thr_dbg[g], in_=bst[:, 0:8]), ["bt", "bcnt", "bR", "bcand", "bst0", "bst1", "bst2", "binc"], [("thr_dbg", g)], "dbg2")
                    DM(lambda e: e.dma_start(out=sc_dbg[g, :, 0:nk], in_=score[:, 0:nk]), sres, [("sc_dbg", g)], "dbg2")
                    DM(lambda e: e.dma_start(out=mk_dbg[g, :, 0:nk], in_=msk[:, 0:nk]), ["msk"], [("mk_dbg", g)], "dbg2")

            def attention(g, pend):
                sl = g % 2
                dq_sb = dq_sb2[sl]
                dqres = f"dq_sb{sl}"
                ntile = (g + 1) * 4

                def mask_blk(j0):
                    nj = min(8, ntile - j0)
                    for jj in range(nj):
                        j = j0 + jj
                        P(lambda e, j=j, jj=jj: e.transpose(out=ps_mT[:, jj, :], in_=msk[:, j * 128:(j + 1) * 128], identity=idb[:]),
                          ["msk", "idb"], [("PP3", 0)])
                    mb = (j0 // 8) % 2
                    if mb == 0:
                        A(lambda e, mb=mb, nj=nj: e.activation(out=mskT[mb][:, 0:nj, :], in_=ps_mT[:, 0:nj, :], func=AF.Copy), [("PP3", 0)], [f"mskT{mb}"])
                    else:
                        V(lambda e, mb=mb, nj=nj: e.tensor_copy(out=mskT[mb][:, 0:nj, :], in_=ps_mT[:, 0:nj, :]), [("PP3", 0)], [f"mskT{mb}"])
                def att_qk(j):
                    qb = j % 2
                    for hf in range(2):
                        P(lambda e, hf=hf: e.matmul(ps_qk[qb][:, hf * 512:(hf + 1) * 512], lhsT=kT[:, j * 128:(j + 1) * 128],
                                                    rhs=dq_sb[:, 4 * hf:4 * hf + 4, :], start=True, stop=True),
                          [("kT", j), dqres], [(f"PP{qb}", hf)])

                def att_sm(j):
                    b = j % 2
                    if j % 8 == 0:
                        mask_blk(j)
                    mb = (j // 8) % 2
                    slot = j - (4 * g - 1)
                    qk3 = ps_qk[b][:].rearrange("p (h q) -> p h q", h=H)
                    qres = [(f"PP{b}", 0), (f"PP{b}", 1)]
                    if slot >= 0:
                        V(lambda e: e.scalar_tensor_tensor(out=lg[:], in0=qk3, scalar=0.125, in1=bT[:, slot, :, :], op0=ALU.mult, op1=ALU.add),
                          qres + ["bT"], ["lg"])
                        A(lambda e: e.activation(out=pe_[b][:], in_=lg[:], func=AF.Exp), ["lg"], [f"pe{b}"])
                    else:
                        A(lambda e: e.activation(out=pe_[b][:], in_=qk3, func=AF.Exp, scale=0.125), qres, [f"pe{b}"])
                    b3 = j % 3
                    V(lambda e: e.tensor_tensor(out=pm[b3][:], in0=pe_[b][:], in1=mskT[mb][:, j % 8, :].unsqueeze(1).broadcast_to([128, H, 128]), op=ALU.mult),
                      [f"pe{b}", f"mskT{mb}"], [f"pm{b3}"])

                def att_pv(j):
                    b = j % 3
                    for hf in range(2):
                        P(lambda e, hf=hf: e.matmul(ps_o[0:65, hf * 512:(hf + 1) * 512], lhsT=dv1[:, j, :], rhs=pm[b][:, 4 * hf:4 * hf + 4, :],
                                                    start=(j == 0), stop=(j == ntile - 1)),
                          [("dv1", j), "dv1one", f"pm{b}"], [("PP2", hf)])

                att_qk(0)
                for j_ in range(ntile):
                    if j_ + 1 < ntile:
                        att_qk(j_ + 1)
                    att_sm(j_)
                    if j_ % 4 == 1 and pend:
                        pend.pop(0)()
                    if j_ >= 1:
                        att_pv(j_ - 1)
                att_pv(ntile - 1)
                while pend:
                    pend.pop(0)()
                A(lambda e: e.activation(out=OT[:], in_=ps_o[0:65, :], func=AF.Copy), [("PP2", 0), ("PP2", 1)], ["OT"])
                for hf in range(2):
                    P(lambda e, hf=hf: e.matmul(PP[3][0:64, hf * 512:(hf + 1) * 512], lhsT=selden[:], rhs=OT[:, hf * 512:(hf + 1) * 512], start=True, stop=True),
                      ["selden", "OT"], [("PP3", hf)])
                rden = lg[0:64, :, :].rearrange("p h q -> p (h q)")
                V(lambda e: e.reciprocal(out=rden, in_=PP[3][0:64, :]), [("PP3", 0), ("PP3", 1)], ["lg"])
                V(lambda e: e.tensor_tensor(out=ydT[:].rearrange("p h q -> p (h q)"), in0=OT[0:64, :], in1=rden, op=ALU.mult), ["OT", "lg"], ["ydT"])
                DM(lambda e: e.dma_start(out=ydT_s[g], in_=ydT[:]), ["ydT"], [("ydT_s", g)], "st_y")

            load_q(0)
            indexer(0)
            for g_ in range(NG):
                pend_ = []
                if g_ + 1 < NG:
                    load_q(g_ + 1)
                    indexer(g_ + 1)
                    pend_ = mm_ops(g_ + 1)
                bisect(g_)
                attention(g_, pend_)
            S.barrier()
            S.emit()

        kvs.close()
        if PHASES >= 3:
          with contextlib.ExitStack() as ph:
            u2T = sbt(ph, "u2T", [128, NG, 8, 128], BF16)
            gates = sbt(ph, "gates", [128, NG, 16], F32)
            with contextlib.ExitStack() as pc:
                wg_sb = sbt(pc, "wg_sb", [128, 8, 2 * D], BF16)
                wro_sb = sbt(pc, "wro_sb", [128, 8, D], BF16)
                wdo_sb = sbt(pc, "wdo_sb", [64, H, D], BF16)
                wo_sb = sbt(pc, "wo_sb", [128, 8, D], BF16)
                wrt_sb = sbt(pc, "wrt_sb", [128, 8, 20], BF16)
                bg_sb = sbt(pc, "bg_sb", [1, 2 * D], BF16)
                ones1 = sbt(pc, "ones1", [1, 128], BF16)
                brt = sbt(pc, "brt", [128, 20], F32)
                xt = [sbt(pc, f"cxt{i}", [128, D], F32) for i in range(2)]
                uTc = [sbt(pc, f"cuT{i}", [128, 8, 128], BF16) for i in range(2)]
                roTc = [sbt(pc, f"croT{i}", [128, 8, 128], BF16) for i in range(2)]
                ydTc = [sbt(pc, f"cydT{i}", [64, H, 128], BF16) for i in range(2)]
                gab = sbt(pc, "gab", [128, 2 * D], BF16)
                t1 = sbt(pc, "t1", [128, D], F32); t2 = sbt(pc, "t2", [128, D], F32)
                mm = sbt(pc, "mm", [128, D], BF16); mT = sbt(pc, "mT", [128, 8, 128], BF16)
                h1b = [sbt(pc, f"h1_{i}", [128, D], F32) for i in range(2)]
                junk = sbt(pc, "cjunk", [128, D], BF16); xs = sbt(pc, "cxs", [128, D], BF16)
                stat = sbt(pc, "cstat", [128, 8], F32)
                rl = sbt(pc, "rl", [128, 20], F32)
                rs = sbt(pc, "rs", [128, 48], F32)
                u2t = sbt(pc, "u2t", [128, 8, 128], BF16)
                pT = pst(pc, "cpT", [128, 8, 128], BF16)
                pgA = pst(pc, "pgA", [128, 2 * D], F32)
                pyr = pst(pc, "pyr", [128, D], F32)
                prt = pst(pc, "prt", [128, 32], F32)

                for k in range(8):
                    DG(lambda e, k=k: e.dma_start(out=wg_sb[:, k, :], in_=w_gate[k * 128:(k + 1) * 128, :]), [], ["wg_sb"], "wC_wg_sb")
                    DG(lambda e, k=k: e.dma_start(out=wro_sb[:, k, :], in_=w_ret_out[k * 128:(k + 1) * 128, :]), [], ["wro_sb"], "wC_wro_sb")
                    DG(lambda e, k=k: e.dma_start(out=wo_sb[:, k, :], in_=w_o[k * 128:(k + 1) * 128, :]), [], ["wo_sb"], "wC_wo_sb")
                    DG(lambda e, k=k: e.dma_start(out=wdo_sb[:, k, :], in_=w_dsa_out[k * 64:(k + 1) * 64, :]), [], ["wdo_sb"], "wC_wdo_sb")
                DG(lambda e: e.dma_start(out=wrt_sb[:], in_=w_rt.rearrange("(k p) n -> p k n", p=128)), [], ["wrt_sb"], "wC_wrt_sb")
                DG(lambda e: e.dma_start(out=bg_sb[:], in_=b_gate), [], ["bg_sb"], "wC_bg_sb")
                DM(lambda e: e.dma_start(out=brt[:], in_=b_rt_rep), [], ["brt"], "c5")
                V(lambda e: e.memset(ones1[:], 1.0), [], ["ones1"])
                for k in range(8):
                    G(lambda e, k=k: e.tensor_tensor(out=wo_sb[:, k, :], in0=wo_sb[:, k, :], in1=g1rep[:], op=ALU.mult), ["wo_sb", "mod"], ["wo_sb"])

                def merge_tile(g):
                    sl = g % 2
                    DM(lambda e: e.dma_start(out=xt[sl][:], in_=xo[g * 128:(g + 1) * 128, :]), [], [f"cxt{sl}"], f"cxt{sl}")
                    DM(lambda e: e.dma_start(out=uTc[sl][:], in_=uT_s[g]), [("uT_s", g)], [f"cuT{sl}"], f"cuT{sl}")
                    DM(lambda e: e.dma_start(out=roTc[sl][:], in_=roT_s[g]), [("roT_s", g)], [f"croT{sl}"], f"croT{sl}")
                    DM(lambda e: e.dma_start(out=ydTc[sl][:], in_=ydT_s[g]), [("ydT_s", g)], [f"cydT{sl}"], f"cydT{sl}")
                    for q4 in range(4):
                        cs_ = slice(q4 * 512, (q4 + 1) * 512)
                        for k in range(8):
                            P(lambda e, k=k, cs_=cs_: e.matmul(pgA[:, cs_], lhsT=uTc[sl][:, k, :], rhs=wg_sb[:, k, cs_], start=(k == 0), stop=False),
                              [f"cuT{sl}", "wg_sb"], [("pgA", q4)])
                        P(lambda e, cs_=cs_: e.matmul(pgA[:, cs_], lhsT=ones1[:], rhs=bg_sb[:, cs_], start=False, stop=True),
                          ["ones1", "bg_sb"], [("pgA", q4)])
                    A(lambda e: e.activation(out=gab[:], in_=pgA[:], func=AF.Sigmoid), [("pgA", i) for i in range(4)], ["gab"])
                    for hf in range(2):
                        cs_ = slice(hf * 512, (hf + 1) * 512)
                        for k in range(8):
                            P(lambda e, k=k, cs_=cs_: e.matmul(pyr[:, cs_], lhsT=roTc[sl][:, k, :], rhs=wro_sb[:, k, cs_], start=(k == 0), stop=(k == 7)),
                              [f"croT{sl}", "wro_sb"], [("pyr", hf)])
                    V(lambda e: e.tensor_tensor(out=t1[:], in0=pyr[:], in1=gab[:, 0:D], op=ALU.mult), [("pyr", 0), ("pyr", 1), "gab"], ["t1"])
                    for hf in range(2):
                        cs_ = slice(hf * 512, (hf + 1) * 512)
                        for h in range(H):
                            P(lambda e, h=h, cs_=cs_: e.matmul(pyr[:, cs_], lhsT=ydTc[sl][:, h, :], rhs=wdo_sb[:, h, cs_], start=(h == 0), stop=(h == H - 1)),
                              [f"cydT{sl}", "wdo_sb"], [("pyr", hf)])
                    V(lambda e: e.tensor_tensor(out=t2[:], in0=pyr[:], in1=gab[:, D:2 * D], op=ALU.mult), [("pyr", 0), ("pyr", 1), "gab"], ["t2"])
                    V(lambda e: e.tensor_tensor(out=mm[:], in0=t1[:], in1=t2[:], op=ALU.add), ["t1", "t2"], ["mm"])
                    for k in range(8):
                        P(lambda e, k=k: e.transpose(out=pT[:, k, :], in_=mm[:, k * 128:(k + 1) * 128], identity=idb[:]), ["mm", "idb"], ["pT"])
                    A(lambda e: e.activation(out=mT[:], in_=pT[:], func=AF.Copy), ["pT"], ["mT"])
                    for hf in range(2):
                        cs_ = slice(hf * 512, (hf + 1) * 512)
                        for k in range(8):
                            P(lambda e, k=k, cs_=cs_: e.matmul(pyr[:, cs_], lhsT=mT[:, k, :], rhs=wo_sb[:, k, cs_], start=(k == 0), stop=(k == 7)),
                              ["mT", "wo_sb"], [("pyr", hf)])
                    h1 = h1b[sl]
                    V(lambda e: e.tensor_tensor(out=h1[:], in0=pyr[:], in1=xt[sl][:], op=ALU.add), [("pyr", 0), ("pyr", 1), f"cxt{sl}"], [f"h1_{sl}"])
                    DM(lambda e: e.dma_start(out=h1_s[g], in_=h1[:]), [f"h1_{sl}"], [("h1_s", g)], f"st_h{sl}")

                def route_tile(g):
                    sl = g % 2
                    h1 = h1b[sl]
                    norm_T(h1[:], f"h1_{sl}", junk[:], xs[:], pT, u2t, stat, G2T, sh2T, "c")
                    G(lambda e: e.tensor_copy(out=u2T[:, g, :, :], in_=u2t[:]), ["cuT"], [("u2T", g)])
                    for k in range(8):
                        P(lambda e, k=k: e.matmul(prt[:, 0:20], lhsT=u2t[:, k, :], rhs=wrt_sb[:, k, :], start=(k == 0), stop=(k == 7)),
                          ["cuT", "wrt_sb"], ["prt"])
                    V(lambda e: e.tensor_tensor(out=rl[:], in0=prt[:, 0:20], in1=brt[:], op=ALU.add), ["prt", "brt"], ["rl"])
                    R = lambda a, b_: rs[:, a:b_]
                    V(lambda e: e.tensor_reduce(out=R(0, 1), in_=rl[:, 0:4], axis=AX.X, op=ALU.max), ["rl"], ["rs"])
                    V(lambda e: e.tensor_scalar(out=R(1, 5), in0=rl[:, 0:4], scalar1=R(0, 1), scalar2=None, op0=ALU.is_ge), ["rl", "rs"], ["rs"])
                    V(lambda e: e.tensor_scalar(out=R(5, 6), in0=R(0, 1), scalar1=-1.0, scalar2=None, op0=ALU.mult), ["rs"], ["rs"])
                    A(lambda e: e.activation(out=R(6, 10), in_=rl[:, 0:4], func=AF.Exp, bias=R(5, 6), accum_out=R(10, 11)), ["rl", "rs"], ["rs2"])
                    V(lambda e: e.reciprocal(out=R(11, 12), in_=R(10, 11)), ["rs2"], ["rs3"])
                    V(lambda e: e.tensor_scalar(out=R(12, 16), in0=rl[:, 4:8], scalar1=R(1, 2), scalar2=None, op0=ALU.mult), ["rl", "rs"], ["rs4"])
                    for gi in range(1, 4):
                        V(lambda e, gi=gi: e.scalar_tensor_tensor(out=R(12, 16), in0=rl[:, 4 + 4 * gi:8 + 4 * gi], scalar=R(1 + gi, 2 + gi), in1=R(12, 16),
                                                                  op0=ALU.mult, op1=ALU.add), ["rl", "rs", "rs4"], ["rs4"])
                    V(lambda e: e.tensor_reduce(out=R(16, 17), in_=R(12, 16), axis=AX.X, op=ALU.max), ["rs4"], ["rs5"])
                    V(lambda e: e.tensor_scalar(out=R(17, 21), in0=R(12, 16), scalar1=R(16, 17), scalar2=None, op0=ALU.is_ge), ["rs4", "rs5"], ["rs6"])
                    V(lambda e: e.scalar_tensor_tensor(out=R(21, 25), in0=R(17, 21), scalar=NEG, in1=R(12, 16), op0=ALU.mult, op1=ALU.add),
                      ["rs6", "rs4"], ["rs7"])
                    V(lambda e: e.tensor_reduce(out=R(25, 26), in_=R(21, 25), axis=AX.X, op=ALU.max), ["rs7"], ["rs8"])
                    V(lambda e: e.tensor_scalar(out=R(26, 30), in0=R(21, 25), scalar1=R(25, 26), scalar2=None, op0=ALU.is_ge), ["rs7", "rs8"], ["rs9"])
                    V(lambda e: e.tensor_tensor(out=R(30, 31), in0=R(16, 17), in1=R(25, 26), op=ALU.subtract), ["rs5", "rs8"], ["rs10"])
                    A(lambda e: e.activation(out=R(31, 32), in_=R(30, 31), func=AF.Sigmoid), ["rs10"], ["rs11"])
                    V(lambda e: e.tensor_scalar(out=R(32, 33), in0=R(31, 32), scalar1=-1.0, scalar2=1.0, op0=ALU.mult, op1=ALU.add), ["rs11"], ["rs12"])
                    V(lambda e: e.tensor_scalar(out=R(33, 37), in0=R(17, 21), scalar1=R(31, 32), scalar2=None, op0=ALU.mult), ["rs6", "rs11"], ["rs13"])
                    V(lambda e: e.scalar_tensor_tensor(out=R(33, 37), in0=R(26, 30), scalar=R(32, 33), in1=R(33, 37), op0=ALU.mult, op1=ALU.add),
                      ["rs9", "rs12", "rs13"], ["rs13"])
                    V(lambda e: e.tensor_scalar(out=R(33, 37), in0=R(33, 37), scalar1=R(11, 12), scalar2=None, op0=ALU.mult), ["rs13", "rs3"], ["rs13"])
                    for gi in range(4):
                        V(lambda e, gi=gi: e.tensor_scalar(out=gates[:, g, 4 * gi:4 * gi + 4], in0=R(33, 37), scalar1=R(1 + gi, 2 + gi), scalar2=None, op0=ALU.mult),
                          ["rs13", "rs"], [("gates", g)])

                merge_tile(0)
                for g_ in range(NG):
                    if g_ + 1 < NG:
                        merge_tile(g_ + 1)
                    route_tile(g_)
                S.barrier()
                S.emit()

            with contextlib.ExitStack() as pd:
                NH = 2 if NG >= 2 else 1
                TPH = NG // NH
                hacc = sbt(pd, "hacc", [128, TPH, D], F32)
                w13 = [sbt(pd, f"w13_{i}", [128, 8, 512], BF16) for i in range(2)]
                w2s = [sbt(pd, f"w2s_{i}", [128, 2, D], BF16) for i in range(2)]
                gfin = sbt(pd, "gfin", [128, D], F32)
                sa = [sbt(pd, f"sa{i}", [128, 256], F32) for i in range(2)]
                hid = [sbt(pd, f"hid{i}", [128, 256], BF16) for i in range(2)]
                hidT = [sbt(pd, f"hidT{i}", [128, 2, 128], BF16) for i in range(2)]
                fjunk = sbt(pd, "fjunk", [128, D], BF16)
                fstat = sbt(pd, "fstat", [128, 4], F32)
                ot = [sbt(pd, f"ot{i}", [128, D], F32) for i in range(2)]
                pab = [pst(pd, f"pab{i}", [128, 512], F32) for i in range(2)]
                phT = [pst(pd, f"phT{i}", [128, 2, 128], BF16) for i in range(2)]
                py = [pst(pd, f"py{i}", [128, D], F32) for i in range(2)]
                DM(lambda e: e.dma_start(out=gfin[:], in_=gfin_rep), [], ["gfin"], "c6")

                def load_expert(ex, ws):
                    for k in range(8):
                        DG(lambda e, k=k: e.dma_start(out=w13[ws][:, k, 0:256], in_=w1[ex, k * 128:(k + 1) * 128, :]), [], [f"w13_{ws}"], f"w13_{ws}")
                        DG(lambda e, k=k: e.dma_start(out=w13[ws][:, k, 256:512], in_=w3[ex, k * 128:(k + 1) * 128, :]), [], [f"w13_{ws}"], f"w13_{ws}")
                    for f in range(2):
                        DG(lambda e, f=f: e.dma_start(out=w2s[ws][:, f, :], in_=w2[ex, f * 128:(f + 1) * 128, :]), [], [f"w2s_{ws}"], f"w2s_{ws}")
                        G(lambda e, f=f: e.tensor_tensor(out=w2s[ws][:, f, :], in0=w2s[ws][:, f, :], in1=g2rep[:], op=ALU.mult),
                          [f"w2s_{ws}", "mod"], [f"w2s_{ws}"])

                def u_ab(u, g, ex, ws):
                    b = u % 2
                    for k in range(8):
                        P(lambda e, k=k: e.matmul(pab[b][:], lhsT=u2T[:, g, k, :], rhs=w13[ws][:, k, :], start=(k == 0), stop=(k == 7)),
                          [("u2T", g), f"w13_{ws}"], [f"pab{b}"])

                def u_act_tr(u, g, ex, ws):
                    b = u % 2
                    A(lambda e: e.activation(out=sa[b][:], in_=pab[b][:, 0:256], func=AF.Silu), [f"pab{b}"], [f"sa{b}"])
                    V(lambda e: e.scalar_tensor_tensor(out=hid[b][:], in0=pab[b][:, 256:512], scalar=gates[:, g, ex:ex + 1], in1=sa[b][:],
                                                       op0=ALU.mult, op1=ALU.mult), [f"pab{b}", ("gates", g), f"sa{b}"], [f"hid{b}"])
                    for f in range(2):
                        P(lambda e, f=f: e.transpose(out=phT[b][:, f, :], in_=hid[b][:, f * 128:(f + 1) * 128], identity=idb[:]), [f"hid{b}", "idb"], [f"phT{b}"])
                    A(lambda e: e.activation(out=hidT[b][:], in_=phT[b][:], func=AF.Copy), [f"phT{b}"], [f"hidT{b}"])

                def u_y(u, g, ex, ws, tl):
                    b = u % 2
                    for hf in range(2):
                        cs_ = slice(hf * 512, (hf + 1) * 512)
                        for f in range(2):
                            P(lambda e, f=f, cs_=cs_: e.matmul(py[b][:, cs_], lhsT=hidT[b][:, f, :], rhs=w2s[ws][:, f, cs_], start=(f == 0), stop=(f == 1)),
                              [f"hidT{b}", f"w2s_{ws}"], [(f"py{b}", hf)])
                    V(lambda e: e.tensor_tensor(out=hacc[:, tl, :], in0=hacc[:, tl, :], in1=py[b][:], op=ALU.add),
                      [("hacc", tl), (f"py{b}", 0), (f"py{b}", 1)], [("hacc", tl)])

                for hh in range(NH):
                    for tl in range(TPH):
                        g = hh * TPH + tl
                        DM(lambda e, tl=tl, g=g: e.dma_start(out=hacc[:, tl, :], in_=h1_s[g]), [("h1_s", g)], [("hacc", tl)], f"ld_h{tl}")
                    units = []
                    for ex in range(16):
                        for tl in range(TPH):
                            units.append((hh * TPH + tl, ex, (hh * 16 + ex) % 2, tl))
                    NU = len(units)
                    load_expert(0, (hh * 16) % 2)
                    loaded = {0}
                    g0_, ex0_, ws0_, tl0_ = units[0]
                    u_ab(0, g0_, ex0_, ws0_)
                    for u in range(NU):
                        g, ex, ws, tl = units[u]
                        deferred = False
                        if u + 1 < NU:
                            g2_, ex2_, ws2_, tl2_ = units[u + 1]
                            if ex2_ in loaded:
                                u_ab(u + 1, g2_, ex2_, ws2_)
                            else:
                                deferred = True
                        u_act_tr(u, g, ex, ws)
                        if u >= 1:
                            g1_, ex1_, ws1_, tl1_ = units[u - 1]
                            u_y(u - 1, g1_, ex1_, ws1_, tl1_)
                        if tl == 0 and ex + 1 < 16:
                            load_expert(ex + 1, (hh * 16 + ex + 1) % 2)
                            loaded.add(ex + 1)
                        if deferred:
                            u_ab(u + 1, g2_, ex2_, ws2_)
                    g1_, ex1_, ws1_, tl1_ = units[NU - 1]
                    u_y(NU - 1, g1_, ex1_, ws1_, tl1_)
                    for tl in range(TPH):
                        g = hh * TPH + tl
                        b = tl % 2
                        A(lambda e, tl=tl: e.activation(out=fjunk[:], in_=hacc[:, tl, :], func=AF.Square, scale=1.0 / 32.0, accum_out=fstat[:, 0:1]),
                          [("hacc", tl)], ["fjunk", "fstat"])
                        A(lambda e: e.activation(out=fstat[:, 1:2], in_=fstat[:, 0:1], func=AF.Ln, bias=epsc[:, 0:1]), ["fstat", "epsc"], ["fstat"])
                        A(lambda e: e.activation(out=fstat[:, 2:3], in_=fstat[:, 1:2], func=AF.Exp, scale=-0.5), ["fstat"], ["fstat"])
                        V(lambda e, tl=tl, b=b: e.scalar_tensor_tensor(out=ot[b][:], in0=hacc[:, tl, :], scalar=fstat[:, 2:3], in1=gfin[:], op0=ALU.mult, op1=ALU.mult),
                          [("hacc", tl), "fstat", "gfin"], [f"ot{b}"])
                        DM(lambda e, g=g, b=b: e.dma_start(out=out[g * 128:(g + 1) * 128, :], in_=ot[b][:]), [f"ot{b}"], [("out", g)], f"st_o{b}")
                S.op("sp", None, reads=[("out", g) for g in range(NG)])
                S.barrier()
                S.emit()
    return nc


build.phases = 3


def _rel_bucket(rel):
    nb = 16
    ret = (rel > 0).astype(np.int64) * nb
    n = np.abs(rel)
    max_exact = nb // 2
    nf = np.maximum(n, 1).astype(np.float32)
    large = max_exact + (np.log(nf / max_exact) / math.log(128 / max_exact) * (nb - max_exact)).astype(np.int32)
    large = np.minimum(large, nb - 1)
    return ret + np.where(n < max_exact, n, large)


def make_inputs(NG, inp):
    NT = 4 * NG
    SB = NT * 128
    f = np.float32
    x = np.asarray(inp["x"], f)
    assert x.shape[1] == SB
    pos = np.arange(SB, dtype=np.float64)
    freqs = 10000.0 ** (-np.arange(0, 64, 2, dtype=np.float64) / 64)
    ang = (pos[:, None].astype(np.float32) * freqs[None, :].astype(np.float32)).astype(np.float32)
    rope_all = np.concatenate([np.cos(ang), np.sin(ang)], axis=1).astype(f)
    gam = np.array(GAM, dtype=np.float64)
    i_ = np.arange(128)
    ci = i_ // 64
    DT = np.zeros((128, H, 128), f)
    for h in range(H):
        same = ci[:, None] == ci[None, :]
        dm = np.where(same, gam[h] ** np.abs(i_[:, None] - i_[None, :]),
                      np.where((ci[:, None] == 1) & (ci[None, :] == 0), gam[h] ** (i_[:, None] - i_[None, :]).clip(0), 0.0))
        DT[:, h, :] = (dm.T * 0.125).astype(f)
    qdec = (gam[None, :] ** (i_[:, None] + 1)).astype(f)
    wtile = (gam[None, :] ** (127 - i_[:, None]) * 0.125).astype(f)
    ident = np.eye(128, dtype=f)
    selden = np.zeros((65, 64), f); selden[64, :] = 1.0
    rb = np.asarray(inp["rel_bias"], f)

    def rep(v, n=128):
        return np.ascontiguousarray(np.broadcast_to(np.asarray(v, f).reshape(1, -1), (n, np.asarray(v).size)))

    def fm(v):
        return np.ascontiguousarray(np.asarray(v, f).reshape(-1, 128).T)

    b_ada = np.asarray(inp["b_ada"], f)[0]
    common = dict(
        w_ada=np.ascontiguousarray(inp["w_ada"][0], dtype=f), b_adaT=fm(b_ada),
        b_g12=np.ascontiguousarray(np.concatenate([rep(b_ada[2 * D:3 * D]), rep(b_ada[5 * D:6 * D])], axis=1)),
        gmixT=fm(inp["norm_mix_g"][0]), gffnT=fm(inp["norm_ffn_g"][0]),
        gfin_rep=rep(inp["norm_final_g"]), gn_rep=rep(inp["ret_gn_g"][0]), kvg_rep=rep(inp["dsa_kv_norm_g"][0]),
        w_in=np.ascontiguousarray(inp["w_in"][0], dtype=f), w_kv=np.ascontiguousarray(inp["w_dsa_kv_up"][0], dtype=f),
        w_ret_out=np.ascontiguousarray(inp["w_ret_out"][0], dtype=f), w_dsa_out=np.ascontiguousarray(inp["w_dsa_out"][0], dtype=f),
        w_gate=np.ascontiguousarray(inp["w_gate"][0], dtype=f), b_gate=np.ascontiguousarray(inp["b_gate"], dtype=f).reshape(1, -1),
        w_o=np.ascontiguousarray(inp["w_o"][0], dtype=f),
        w_rt=np.ascontiguousarray(np.concatenate([inp["w_group_router"][0], inp["w_expert_router"][0]], axis=1), dtype=f),
        b_rt_rep=rep(np.concatenate([inp["b_group_router"][0], inp["b_expert_router"][0]])),
        w1=np.ascontiguousarray(inp["w_exp_gate"][0], dtype=f), w3=np.ascontiguousarray(inp["w_exp_up"][0], dtype=f),
        w2=np.ascontiguousarray(inp["w_exp_down"][0], dtype=f),
        rope_all=rope_all, ident=ident, DT=DT, qdec=qdec, wtile=wtile, selden=selden, c15_rep=rep(rb[15]),
    )
    maps = []
    for c in range(8):
        b, r = c // 4, c % 4
        own = np.array([4 * g + r for g in range(NG)])
        rows = (own[:, None] * 128 + np.arange(128)[None, :]).reshape(-1)
        sel = np.zeros((64, 4), f); sel[:, r] = 1.0
        madd = np.zeros((128, 512), f)
        kt = np.arange(512) // 128
        kc = (np.arange(512) % 128) // 64
        qc = np.arange(128) // 64
        inadm = (kt[None, :] > r) | ((kt[None, :] == r) & (kc[None, :] > qc[:, None]))
        madd[inadm] = NEG
        biasT = np.zeros((5, 128, H, 128), f)
        for s5 in range(5):
            rel = (s5 - 1 - r) * 128 + np.arange(128)[:, None] - np.arange(128)[None, :]
            bk = _rel_bucket(rel)
            biasT[s5] = np.transpose(rb[bk], (0, 2, 1))
        m = dict(common)
        m.update(xb=np.ascontiguousarray(x[b]), xo=np.ascontiguousarray(x[b][rows]),
                 ccol=fm(inp["c"][b]), rope_own=np.ascontiguousarray(rope_all[rows]),
                 sel=sel, madd=madd, biasT=biasT)
        maps.append(m)
    return maps


_NC_CACHE = {}


def kernel(**inputs):
    NG = 32
    if NG not in _NC_CACHE:
        _NC_CACHE[NG] = build(NG)
    nc = _NC_CACHE[NG]
    maps = make_inputs(NG, inputs)
    res = run_bass_kernel_spmd(nc, maps, core_ids=list(range(8)))
    B, SEQ = inputs["x"].shape[0], inputs["x"].shape[1]
    outp = np.zeros((B, SEQ, D), np.float32)
    for c in range(8):
        b, r = c // 4, c % 4
        o = np.asarray(res.results[c]["out"]).reshape(NG, 128, D)
        for g in range(NG):
            t = 4 * g + r
            outp[b, t * 128:(t + 1) * 128, :] = o[g]
    return outp
```

```python
import contextlib
import math
import numpy as np
import concourse.bass as bass
import concourse.mybir as mybir
from concourse.bass_utils import run_bass_kernel_spmd

F32 = mybir.dt.float32
BF16 = mybir.dt.bfloat16
AF = mybir.ActivationFunctionType
ALU = mybir.AluOpType
AX = mybir.AxisListType

D = 1024
H = 8
NEG = -1.0e30
NBIS = 10
GAM = [1.0 - 2.0 ** (-5.0 - h) for h in range(H)]


class Sched:
    ENGS = ("pe", "act", "dve", "pool", "sp")
    ROT = 3000

    def __init__(self, nc, stack):
        self.nc = nc
        self.stack = stack
        self.ops = {e: [] for e in self.ENGS}
        self.last_write = {}
        self.readers = {}
        self.nsem = 0
        self.cur = {}
        self.dma = {}
        self.seen = {e: {} for e in self.ENGS}
        self.pe_sems = set()
        self.latest = {}

    def _newsem(self, name):
        self.nsem += 1
        return self.stack.enter_context(self.nc.semaphore(f"s{self.nsem}_{name}"))

    def _tok_compute(self, eng):
        c = self.cur.get(eng)
        if c is None or c[1] >= self.ROT:
            c = [self._newsem(eng), 0]
            self.cur[eng] = c
            if eng == "pe":
                self.pe_sems.add(id(c[0]))
        c[1] += 1
        return (c[0], c[1])

    def _tok_dma(self, key):
        c = self.dma.get(key)
        if c is None or c[1] >= 16 * 1500:
            c = [self._newsem("d"), 0]
            self.dma[key] = c
        c[1] += 16
        return (c[0], c[1])

    def op(self, eng, fn, reads=(), writes=(), dma_key=None, extra=()):
        deps = {}

        def add(tok):
            if tok is None:
                return
            s, v = tok
            k = id(s)
            if k not in deps or deps[k][1] < v:
                deps[k] = (s, v)

        for r in reads:
            add(self.last_write.get(r))
        for w in writes:
            add(self.last_write.get(w))
            for t in self.readers.get(w, ()):
                add(t)
        for t in extra:
            add(t)
        if fn is None:
            tok = None
        elif dma_key is not None:
            tok = self._tok_dma(dma_key)
        else:
            tok = self._tok_compute(eng)
        waits = []
        seen = self.seen[eng]
        for k, (s, v) in deps.items():
            if eng == "pe" and dma_key is None and k in self.pe_sems:
                continue
            if seen.get(k, 0) >= v:
                continue
            seen[k] = v
            waits.append((s, v))
        if tok is not None:
            self.latest[id(tok[0])] = tok
            for r in reads:
                lst = self.readers.setdefault(r, [])
                lst.append(tok)
                if len(lst) > 16:
                    best = {}
                    for (s, v) in lst:
                        if id(s) not in best or best[id(s)][1] < v:
                            best[id(s)] = (s, v)
                    self.readers[r] = list(best.values())
            for w in writes:
                self.last_write[w] = tok
                self.readers[w] = []
        self.ops[eng].append((fn, waits, tok, dma_key is not None))
        return tok

    def barrier(self):
        toks = list(self.latest.values())
        for e in self.ENGS:
            self.op(e, None, extra=toks)

    def emit(self):
        nc = self.nc
        ops = self.ops
        self.ops = {e: [] for e in self.ENGS}
        with nc.Block() as block:
            def run(engobj, name):
                for fn, waits, tok, is_dma in ops[name]:
                    for s, v in waits:
                        engobj.wait_ge(s, v)
                    if fn is not None:
                        ins = fn(engobj)
                        ins.then_inc(tok[0], 16 if is_dma else 1)

            @block.tensor
            def _(e):
                run(e, "pe")

            @block.scalar
            def _(e):
                run(e, "act")

            @block.vector
            def _(e):
                run(e, "dve")

            @block.gpsimd
            def _(e):
                run(e, "pool")

            @block.sync
            def _(e):
                run(e, "sp")


C_RQ, C_RK, C_RV, C_RG, C_DQ, C_DKV, C_IQ, C_IK, C_IW = 0, 512, 1024, 2048, 3072, 3584, 3712, 4224, 4288
D_IN = 4296


def build(NG, dbg=False):
    NT = 4 * NG
    SB = NT * 128
    NO = NG * 128
    nc = bass.Bass("TRN2", target_bir_lowering=False)

    def din(name, shape, dt=F32):
        return nc.dram_tensor(name, list(shape), dt, kind="ExternalInput").ap()

    def dscr(name, shape, dt):
        if dbg:
            return nc.dram_tensor(name, list(shape), dt, kind="ExternalOutput").ap()
        return nc.dram_tensor(name, list(shape), dt).ap()

    xb = din("xb", [SB, D]); xo = din("xo", [NO, D]); ccol = din("ccol", [128, 8])
    w_ada = din("w_ada", [D, 6 * D]); b_adaT = din("b_adaT", [128, 48]); b_g12 = din("b_g12", [128, 2 * D])
    gmixT = din("gmixT", [128, 8]); gffnT = din("gffnT", [128, 8])
    gfin_rep = din("gfin_rep", [128, D]); gn_rep = din("gn_rep", [128, D]); kvg_rep = din("kvg_rep", [128, 128])
    w_in = din("w_in", [D, D_IN]); w_kv = din("w_kv", [128, 128])
    w_ret_out = din("w_ret_out", [D, D]); w_dsa_out = din("w_dsa_out", [512, D])
    w_gate = din("w_gate", [D, 2 * D]); b_gate = din("b_gate", [1, 2 * D]); w_o = din("w_o", [D, D])
    w_rt = din("w_rt", [D, 20]); b_rt_rep = din("b_rt_rep", [128, 20])
    w1 = din("w1", [16, D, 256]); w3 = din("w3", [16, D, 256]); w2 = din("w2", [16, 256, D])
    rope_all = din("rope_all", [SB, 64]); rope_own = din("rope_own", [NO, 64])
    ident = din("ident", [128, 128]); DTd = din("DT", [128, H, 128]); qdec = din("qdec", [128, H])
    wtile = din("wtile", [128, H]); seld = din("sel", [64, 4]); maddd = din("madd", [128, 512])
    biasTd = din("biasT", [5, 128, H, 128]); c15 = din("c15_rep", [128, H]); seldend = din("selden", [65, 64])
    out = nc.dram_tensor("out", [NO, D], F32, kind="ExternalOutput").ap()

    uT_s = dscr("uT_s", [NG, 128, 8, 128], BF16)
    roT_s = dscr("roT_s", [NG, 128, 8, 128], BF16)
    dqT_s = dscr("dqT_s", [NG, 64, H, 128], BF16)
    iqT_s = dscr("iqT_s", [NG, 64, H, 128], BF16)
    iw_s = dscr("iw_s", [NG, 128, H], F32)
    ydT_s = dscr("ydT_s", [NG, 64, H, 128], BF16)
    h1_s = dscr("h1_s", [NG, 128, D], F32)
    if dbg:
        kT_dbg = dscr("kT_dbg", [128, SB], BF16)
        dv_dbg = dscr("dv_dbg", [128, NT, 65], BF16)
        thr_dbg = dscr("thr_dbg", [NG, 128, 8], F32)
        sc_dbg = dscr("sc_dbg", [NG, 128, SB], BF16)
        mk_dbg = dscr("mk_dbg", [NG, 128, SB], BF16)

    with contextlib.ExitStack() as top:
        S = Sched(nc, top)

        def sbt(st, n, sh, dt):
            return st.enter_context(nc.sbuf_tensor("sb_" + n, list(sh), dt))

        def pst(st, n, sh, dt):
            return st.enter_context(nc.psum_tensor("ps_" + n, list(sh), dt))

        V = lambda fn, r, w: S.op("dve", fn, reads=r, writes=w)
        A = lambda fn, r, w: S.op("act", fn, reads=r, writes=w)
        P = lambda fn, r, w: S.op("pe", fn, reads=r, writes=w)
        G = lambda fn, r, w: S.op("pool", fn, reads=r, writes=w)
        DM = lambda fn, r, w, k: S.op("sp", fn, reads=r, writes=w, dma_key=k)
        DG = lambda fn, r, w, k: S.op("pool", fn, reads=r, writes=w, dma_key=k)

        idb = sbt(top, "idb", [128, 128], BF16)
        epsc = sbt(top, "epsc", [128, 2], F32)
        G1T = sbt(top, "G1T", [128, 8], F32); sh1T = sbt(top, "sh1T", [128, 8], F32)
        G2T = sbt(top, "G2T", [128, 8], F32); sh2T = sbt(top, "sh2T", [128, 8], F32)
        g1rep = sbt(top, "g1rep", [128, D], BF16); g2rep = sbt(top, "g2rep", [128, D], BF16)
        DG(lambda e: e.dma_start(out=idb[:], in_=ident), [], ["idb"], "c0")
        V(lambda e: e.memset(epsc[:, 0:1], 1e-6), [], ["epsc"])
        V(lambda e: e.memset(epsc[:, 1:2], 1e-5), [], ["epsc"])

        def norm_T(x_ap, xres, junk, xs, pT, uT, stat, GT, shT, tag, ures=None):
            ures = ures or (tag + "uT")
            A(lambda e: e.activation(out=junk, in_=x_ap, func=AF.Square, scale=1.0 / 32.0, accum_out=stat[:, 0:1]),
              [xres], [tag + "junk", tag + "st"])
            A(lambda e: e.activation(out=stat[:, 1:2], in_=stat[:, 0:1], func=AF.Ln, bias=epsc[:, 0:1]),
              [tag + "st", "epsc"], [tag + "st"])
            A(lambda e: e.activation(out=stat[:, 2:3], in_=stat[:, 1:2], func=AF.Exp, scale=-0.5),
              [tag + "st"], [tag + "st"])
            V(lambda e: e.tensor_scalar(out=xs, in0=x_ap, scalar1=stat[:, 2:3], scalar2=None, op0=ALU.mult),
              [xres, tag + "st"], [tag + "xs"])
            for k in range(8):
                P(lambda e, k=k: e.transpose(out=pT[:, k, :], in_=xs[:, k * 128:(k + 1) * 128], identity=idb[:]),
                  [tag + "xs", "idb"], ["pT"])
            for k in range(8):
                V(lambda e, k=k: e.tensor_scalar(out=uT[:, k, :], in0=pT[:, k, :], scalar1=GT[:, k:k + 1],
                                                 scalar2=shT[:, k:k + 1], op0=ALU.mult, op1=ALU.add),
                  ["pT", "mod"], [ures])

        def rope(src, cosb, sinb, dst, ta, tb, rres, wres, tag):
            s1, s2 = src[:, :, 0:32], src[:, :, 32:64]
            V(lambda e: e.tensor_tensor(out=ta, in0=s1, in1=cosb, op=ALU.mult), rres, [tag + "ta"])
            V(lambda e: e.tensor_tensor(out=tb, in0=s2, in1=sinb, op=ALU.mult), rres, [tag + "tb"])
            V(lambda e: e.tensor_tensor(out=dst[:, :, 0:32], in0=ta, in1=tb, op=ALU.subtract), [tag + "ta", tag + "tb"], wres)
            V(lambda e: e.tensor_tensor(out=ta, in0=s2, in1=cosb, op=ALU.mult), rres, [tag + "ta"])
            V(lambda e: e.tensor_tensor(out=tb, in0=s1, in1=sinb, op=ALU.mult), rres, [tag + "tb"])
            V(lambda e: e.tensor_tensor(out=dst[:, :, 32:64], in0=ta, in1=tb, op=ALU.add), [tag + "ta", tag + "tb"], wres)

        with contextlib.ExitStack() as ph:
            cs = sbt(ph, "cs", [128, 8], F32); csb = sbt(ph, "csb", [128, 8], BF16)
            csbc = sbt(ph, "csbc", [128, 8, 128], BF16)
            wa = [sbt(ph, f"wa{i}", [128, 8, D], BF16) for i in range(2)]
            badT = sbt(ph, "badT", [128, 48], F32); bg12 = sbt(ph, "bg12", [128, 2 * D], F32)
            gmx = sbt(ph, "gmx", [128, 8], F32); gff = sbt(ph, "gff", [128, 8], F32)
            modT = sbt(ph, "modT", [128, 32], F32)
            psm = pst(ph, "psm", [128, 32], F32)
            pg = pst(ph, "pg", [128, D], F32)
            DM(lambda e: e.dma_start(out=cs[:], in_=ccol), [], ["cs"], "c1_cs")
            DM(lambda e: e.dma_start(out=badT[:], in_=b_adaT), [], ["badT"], "c1_badT")
            DM(lambda e: e.dma_start(out=bg12[:], in_=b_g12), [], ["bg12"], "c1_bg12")
            DM(lambda e: e.dma_start(out=gmx[:], in_=gmixT), [], ["gmx"], "c1_gmx")
            DM(lambda e: e.dma_start(out=gff[:], in_=gffnT), [], ["gff"], "c1_gff")
            A(lambda e: e.activation(out=csb[:], in_=cs[:], func=AF.Silu), ["cs"], ["csb"])
            V(lambda e: e.tensor_copy(out=csbc[:], in_=csb[:].unsqueeze(2).broadcast_to([128, 8, 128])), ["csb"], ["csbc"])
            fm_i = 0
            for p in range(6):
                sl = p % 2
                for k in range(8):
                    DG(lambda e, p=p, k=k, sl=sl: e.dma_start(out=wa[sl][:, k, :], in_=w_ada[k * 128:(k + 1) * 128, p * D:(p + 1) * D]),
                       [], [f"wa{sl}"], f"wa{sl}")
                if p in (2, 5):
                    for hf in range(2):
                        for k in range(8):
                            P(lambda e, k=k, hf=hf, sl=sl: e.matmul(pg[:, hf * 512:(hf + 1) * 512], lhsT=csbc[:, k, :],
                                                                  rhs=wa[sl][:, k, hf * 512:(hf + 1) * 512], start=(k == 0), stop=(k == 7)),
                              ["csbc", f"wa{sl}"], ["pg"])
                    dst = g1rep if p == 2 else g2rep
                    off = 0 if p == 2 else D
                    V(lambda e, dst=dst, off=off: e.tensor_tensor(out=dst[:], in0=pg[:], in1=bg12[:, off:off + D], op=ALU.add),
                      ["pg", "bg12"], ["mod"])
                else:
                    for f in range(8):
                        for k in range(8):
                            P(lambda e, k=k, f=f, sl=sl, c=fm_i * 8 + f: e.matmul(psm[:, c:c + 1], lhsT=wa[sl][:, k, f * 128:(f + 1) * 128],
                                                                                 rhs=csb[:, k:k + 1], start=(k == 0), stop=(k == 7)),
                              ["csb", f"wa{sl}"], ["psm"])
                    fm_i += 1
            for i, p in enumerate((0, 1, 3, 4)):
                V(lambda e, i=i, p=p: e.tensor_tensor(out=modT[:, i * 8:(i + 1) * 8], in0=psm[:, i * 8:(i + 1) * 8],
                                                      in1=badT[:, p * 8:(p + 1) * 8], op=ALU.add), ["psm", "badT"], ["modT"])
            V(lambda e: e.tensor_copy(out=sh1T[:], in_=modT[:, 0:8]), ["modT"], ["mod"])
            V(lambda e: e.tensor_copy(out=sh2T[:], in_=modT[:, 16:24]), ["modT"], ["mod"])
            V(lambda e: e.scalar_tensor_tensor(out=G1T[:], in0=modT[:, 8:16], scalar=1.0, in1=gmx[:], op0=ALU.add, op1=ALU.mult),
              ["modT", "gmx"], ["mod"])
            V(lambda e: e.scalar_tensor_tensor(out=G2T[:], in0=modT[:, 24:32], scalar=1.0, in1=gff[:], op0=ALU.add, op1=ALU.mult),
              ["modT", "gff"], ["mod"])
            S.barrier()
            S.emit()

        kvs = contextlib.ExitStack()
        kT = sbt(kvs, "kT", [128, SB], BF16)
        dv1 = sbt(kvs, "dv1", [128, NT, 65], BF16)

        with contextlib.ExitStack() as ph:
            w_in_sb = sbt(ph, "w_in_sb", [128, 8, D_IN], BF16)
            wkv_sb = sbt(ph, "wkv_sb", [128, 192], BF16)
            kvg = sbt(ph, "kvg", [128, 128], F32); gnr = sbt(ph, "gnr", [128, D], F32)
            DTs = sbt(ph, "DTs", [128, H, 128], BF16); qdc = sbt(ph, "qdc", [128, H], F32)
            wtl = sbt(ph, "wtl", [128, H], F32); sels = sbt(ph, "sels", [64, 4], F32)
            xt = [sbt(ph, f"xt{i}", [128, D], F32) for i in range(2)]
            rp = [sbt(ph, f"rp{i}", [128, 64], F32) for i in range(2)]
            junk = sbt(ph, "junk", [128, D], BF16)
            xs = sbt(ph, "xs", [128, D], BF16)
            uT = [sbt(ph, f"uT{i}", [128, 8, 128], BF16) for i in range(2)]
            stat = sbt(ph, "stat", [128, 8], F32)
            ta = sbt(ph, "ta", [128, H, 32], F32); tb = sbt(ph, "tb", [128, H, 32], F32)
            krp = sbt(ph, "krp", [128, H, 64], F32); kw = sbt(ph, "kw", [128, H, 64], BF16)
            vb = sbt(ph, "vb", [128, D], BF16)
            Sst = sbt(ph, "Sst", [64, H, 128], F32); T0 = sbt(ph, "T0", [64, H, 128], F32)
            T0b = sbt(ph, "T0b", [64, H, 128], BF16)
            latn = sbt(ph, "latn", [128, 128], BF16); latnT = sbt(ph, "latnT", [128, 128], BF16)
            qr = sbt(ph, "qr", [128, H, 64], BF16); kro = sbt(ph, "kro", [128, H, 64], BF16)
            qT = sbt(ph, "qT", [64, H, 128], BF16); kTo = sbt(ph, "kTo", [64, H, 128], BF16)
            scm = sbt(ph, "scm", [128, H, 128], BF16)
            sg = sbt(ph, "sg", [128, D], BF16)
            oo = sbt(ph, "oo", [128, H, 128], F32); o2 = sbt(ph, "o2", [128, H, 128], F32)
            gst = sbt(ph, "gst", [128, 40], F32)
            ro = sbt(ph, "ro", [128, D], BF16); roT = sbt(ph, "roT", [128, 8, 128], BF16)
            dqb = sbt(ph, "dqb", [128, 512], BF16); iqb = sbt(ph, "iqb", [128, 512], BF16)
            dqT = sbt(ph, "dqT", [64, H, 128], BF16); iqT = dqT
            iwb = sbt(ph, "iwb", [128, H], F32)
            pT = pst(ph, "pT", [128, 8, 128], BF16)
            zA = pst(ph, "zA", [128, D], F32); zB = pst(ph, "zB", [128, D], F32)
            misc = pst(ph, "misc", [128, 512], F32)
            kvp = pst(ph, "kvp", [128, H, 128], F32)
            pTs = misc[:, 448:512].bitcast(BF16)

            for k in range(8):
                DG(lambda e, k=k: e.dma_start(out=w_in_sb[:, k, :], in_=w_in[k * 128:(k + 1) * 128, :]), [], ["w_in_sb"], "w_in")
            V(lambda e: e.memset(wkv_sb[:, 0:64], 0.0), [], ["wkv0"])
            DG(lambda e: e.dma_start(out=wkv_sb[:, 64:192], in_=w_kv), [], ["wkv1"], "c2_wkv")
            DG(lambda e: e.dma_start(out=DTs[:], in_=DTd), [], ["DTs"], "c2_DT")
            DM(lambda e: e.dma_start(out=kvg[:], in_=kvg_rep), [], ["kvg"], "c3_kvg")
            DM(lambda e: e.dma_start(out=gnr[:], in_=gn_rep), [], ["gnr"], "c3_gnr")
            DM(lambda e: e.dma_start(out=qdc[:], in_=qdec), [], ["qdc"], "c3_qdc")
            DM(lambda e: e.dma_start(out=wtl[:], in_=wtile), [], ["wtl"], "c3_wtl")
            DM(lambda e: e.dma_start(out=sels[:], in_=seld), [], ["sels"], "c3_sels")
            V(lambda e: e.memset(Sst[:], 0.0), [], ["Sst"])
            V(lambda e: e.memset(dv1[:, :, 64:65], 1.0), [], ["dv1one"])

            def kside_s1(pos, j):
                sl = pos % 2
                DM(lambda e: e.dma_start(out=xt[sl][:], in_=xb[j * 128:(j + 1) * 128, :]), [], [f"xt{sl}"], f"xt{sl}")
                DM(lambda e: e.dma_start(out=rp[sl][:], in_=rope_all[j * 128:(j + 1) * 128, :]), [], [f"rp{sl}"], f"rp{sl}")
                norm_T(xt[sl][:], f"xt{sl}", junk[:], xs[:], pT, uT[sl], stat, G1T, sh1T, "k", ures=f"kuT{sl}")

            def kside(pos, j):
                sl = pos % 2
                s = j % 4
                ures = [f"kuT{sl}", "w_in_sb"]
                for (dst, c0, n, res) in ((zA[:, 0:512], C_RK, 512, ("zA", 0)), (zB[:, 0:512], C_RV, 512, ("zB", 0)),
                                          (zB[:, 512:1024], C_RV + 512, 512, ("zB", 1)), (misc[:, 0:128], C_DKV, 128, "m_dkv")):
                    for k in range(8):
                        P(lambda e, k=k, dst=dst, c0=c0, n=n: e.matmul(dst, lhsT=uT[sl][:, k, :], rhs=w_in_sb[:, k, c0:c0 + n],
                                                                      start=(k == 0), stop=(k == 7)), ures, [res])
                for k in range(8):
                    P(lambda e, k=k: e.matmul(misc[0:64, 128:256], lhsT=w_in_sb[:, k, C_IK:C_IK + 64], rhs=uT[sl][:, k, :],
                                              start=(k == 0), stop=(k == 7)), ures, ["m_ik"])
                cosb = rp[sl][:, 0:32].unsqueeze(1).broadcast_to([128, H, 32])
                sinb = rp[sl][:, 32:64].unsqueeze(1).broadcast_to([128, H, 32])
                rope(zA[:, 0:512].rearrange("p (h d) -> p h d", h=H), cosb, sinb, krp, ta[:], tb[:], [("zA", 0), f"rp{sl}"], ["krp"], "r")
                V(lambda e: e.tensor_tensor(out=kw[:], in0=krp[:], in1=wtl[:].unsqueeze(2).broadcast_to([128, H, 64]), op=ALU.mult),
                  ["krp", "wtl"], ["kw"])
                A(lambda e: e.activation(out=vb[:], in_=zB[:], func=AF.Copy), [("zB", 0), ("zB", 1)], ["vb"])
                if s == 0:
                    V(lambda e: e.tensor_scalar(out=T0[:], in0=Sst[:], scalar1=sels[:, 0:1], scalar2=None, op0=ALU.mult),
                      ["Sst", "sels"], ["T0"])
                else:
                    V(lambda e: e.scalar_tensor_tensor(out=T0[:], in0=Sst[:], scalar=sels[:, s:s + 1], in1=T0[:], op0=ALU.mult, op1=ALU.add),
                      ["Sst", "sels", "T0"], ["T0"])
                for h in range(H):
                    P(lambda e, h=h: e.matmul(kvp[0:64, h, :], lhsT=kw[:, h, :], rhs=vb[:, h * 128:(h + 1) * 128], start=True, stop=True),
                      ["kw", "vb"], [("kvp", h // 4)])
                for h in range(H):
                    V(lambda e, h=h: e.scalar_tensor_tensor(out=Sst[:, h, :], in0=Sst[:, h, :], scalar=float(GAM[h] ** 128),
                                                           in1=kvp[0:64, h, :], op0=ALU.mult, op1=ALU.add),
                      ["Sst", ("kvp", h // 4)], ["Sst"])
                A(lambda e: e.activation(out=junk[:, 0:128], in_=misc[:, 0:128], func=AF.Square, scale=1.0 / math.sqrt(128.0),
                                         accum_out=stat[:, 4:5]), ["m_dkv"], ["kjunk", "st2"])
                A(lambda e: e.activation(out=stat[:, 5:6], in_=stat[:, 4:5], func=AF.Ln, bias=epsc[:, 0:1]), ["st2", "epsc"], ["st2"])
                A(lambda e: e.activation(out=stat[:, 6:7], in_=stat[:, 5:6], func=AF.Exp, scale=-0.5), ["st2"], ["st2"])
                V(lambda e: e.scalar_tensor_tensor(out=latn[:], in0=misc[:, 0:128], scalar=stat[:, 6:7], in1=kvg[:], op0=ALU.mult, op1=ALU.mult),
                  ["m_dkv", "st2", "kvg"], ["latn"])
                P(lambda e: e.transpose(out=pTs, in_=latn[:], identity=idb[:]), ["latn", "idb"], ["pTs"])
                A(lambda e: e.activation(out=latnT[:], in_=pTs, func=AF.Copy), ["pTs"], ["latnT"])
                P(lambda e: e.matmul(misc[:, 256:384], lhsT=wkv_sb[:, 0:128], rhs=latnT[:], start=True, stop=True),
                  ["latnT", "wkv0", "wkv1"], ["m_dk"])
                P(lambda e: e.matmul(misc[:, 384:448], lhsT=latnT[:], rhs=wkv_sb[:, 128:192], start=True, stop=True),
                  ["latnT", "wkv1"], ["m_dv"])
                A(lambda e: e.activation(out=kT[0:64, j * 128:(j + 1) * 128], in_=misc[0:64, 128:256], func=AF.Copy), ["m_ik"], [("kT", j)])
                A(lambda e: e.activation(out=kT[64:128, j * 128:(j + 1) * 128], in_=misc[64:128, 256:384], func=AF.Copy), ["m_dk"], [("kT", j)])
                V(lambda e: e.tensor_copy(out=dv1[:, j, 0:64], in_=misc[:, 384:448]), ["m_dv"], [("dv1", j)])

            def ownside_s1(pos, g):
                sl = pos % 2
                DM(lambda e: e.dma_start(out=xt[sl][:], in_=xo[g * 128:(g + 1) * 128, :]), [], [f"xt{sl}"], f"xt{sl}")
                DM(lambda e: e.dma_start(out=rp[sl][:], in_=rope_own[g * 128:(g + 1) * 128, :]), [], [f"rp{sl}"], f"rp{sl}")
                norm_T(xt[sl][:], f"xt{sl}", junk[:], xs[:], pT, uT[sl], stat, G1T, sh1T, "k", ures=f"kuT{sl}")
                DM(lambda e: e.dma_start(out=uT_s[g], in_=uT[sl][:]), [f"kuT{sl}"], [("uT_s", g)], "st_u")

            def ownside(pos, g):
                sl = pos % 2
                ures = [f"kuT{sl}", "w_in_sb"]
                u = uT[sl]

                def proj(dst, c0, n, res):
                    for k in range(8):
                        P(lambda e, k=k: e.matmul(dst, lhsT=u[:, k, :], rhs=w_in_sb[:, k, c0:c0 + n], start=(k == 0), stop=(k == 7)), ures, [res])

                cosb = rp[sl][:, 0:32].unsqueeze(1).broadcast_to([128, H, 32])
                sinb = rp[sl][:, 32:64].unsqueeze(1).broadcast_to([128, H, 32])
                proj(zA[:, 0:512], C_RQ, 512, ("zA", 0))
                proj(zA[:, 512:1024], C_RK, 512, ("zA", 1))
                proj(zB[:, 0:512], C_DQ, 512, ("zB", 0))
                proj(zB[:, 512:1024], C_IQ, 512, ("zB", 1))
                proj(misc[:, 0:8], C_IW, 8, "m_dkv")
                rope(zA[:, 0:512].rearrange("p (h d) -> p h d", h=H), cosb, sinb, qr, ta[:], tb[:], [("zA", 0), f"rp{sl}"], ["qr"], "r")
                rope(zA[:, 512:1024].rearrange("p (h d) -> p h d", h=H), cosb, sinb, kro, ta[:], tb[:], [("zA", 1), f"rp{sl}"], ["kro"], "r")
                pT64 = pT[0:64, :, :]
                for h in range(H):
                    P(lambda e, h=h: e.transpose(out=pT64[:, h, :], in_=qr[:, h, :], identity=idb[:]), ["qr", "idb"], ["pT"])
                V(lambda e: e.tensor_copy(out=qT[:], in_=pT64), ["pT"], ["qT"])
                for h in range(H):
                    P(lambda e, h=h: e.transpose(out=pT64[:, h, :], in_=kro[:, h, :], identity=idb[:]), ["kro", "idb"], ["pT"])
                A(lambda e: e.activation(out=kTo[:], in_=pT64, func=AF.Copy), ["pT"], ["kTo"])
                V(lambda e: e.tensor_copy(out=dqb[:], in_=zB[:, 0:512]), [("zB", 0)], ["dqb"])
                A(lambda e: e.activation(out=iqb[:], in_=zB[:, 512:1024], func=AF.Copy), [("zB", 1)], ["iqb"])
                V(lambda e: e.tensor_copy(out=iwb[:], in_=misc[:, 0:8]), ["m_dkv"], ["iwb"])
                DM(lambda e: e.dma_start(out=iw_s[g], in_=iwb[:]), ["iwb"], [("iw_s", g)], "st_w")
                for h in range(H):
                    P(lambda e, h=h: e.transpose(out=pT64[:, h, :], in_=dqb[:, h * 64:(h + 1) * 64], identity=idb[:]), ["dqb", "idb"], ["pT"])
                V(lambda e: e.tensor_copy(out=dqT[:], in_=pT64), ["pT"], ["dqT"])
                DM(lambda e: e.dma_start(out=dqT_s[g], in_=dqT[:]), ["dqT"], [("dqT_s", g)], "st_q")
                for h in range(H):
                    P(lambda e, h=h: e.transpose(out=pT64[:, h, :], in_=iqb[:, h * 64:(h + 1) * 64], identity=idb[:]), ["iqb", "idb"], ["pT"])
                A(lambda e: e.activation(out=iqT[:], in_=pT64, func=AF.Copy), ["pT"], ["dqT"])
                DM(lambda e: e.dma_start(out=iqT_s[g], in_=iqT[:]), ["dqT"], [("iqT_s", g)], "st_i")
                proj(zB[:, 0:512], C_RV, 512, ("zB", 0))
                proj(zB[:, 512:1024], C_RV + 512, 512, ("zB", 1))
                A(lambda e: e.activation(out=vb[:], in_=zB[:], func=AF.Copy), [("zB", 0), ("zB", 1)], ["vb"])
                zA3 = zA[:].rearrange("p (h d) -> p h d", h=H)
                for h in range(H):
                    P(lambda e, h=h: e.matmul(zA3[:, h, :], lhsT=kTo[:, h, :], rhs=qT[:, h, :], start=True, stop=True),
                      ["kTo", "qT"], [("zA", h // 4)])
                V(lambda e: e.tensor_tensor(out=scm[:], in0=zA3, in1=DTs[:], op=ALU.mult), [("zA", 0), ("zA", 1), "DTs"], ["scm"])
                zB3 = zB[:].rearrange("p (h d) -> p h d", h=H)
                for h in range(H):
                    P(lambda e, h=h: e.matmul(zB3[:, h, :], lhsT=scm[:, h, :], rhs=vb[:, h * 128:(h + 1) * 128], start=True, stop=True),
                      ["scm", "vb"], [("zB", h // 4)])
                V(lambda e: e.tensor_copy(out=T0b[:], in_=T0[:]), ["T0"], ["T0b"])
                for h in range(H):
                    P(lambda e, h=h: e.matmul(kvp[:, h, :], lhsT=qT[:, h, :], rhs=T0b[:, h, :], start=True, stop=True),
                      ["qT", "T0b"], [("kvp", h // 4)])
                V(lambda e: e.tensor_tensor(out=oo[:], in0=kvp[:], in1=qdc[:].unsqueeze(2).broadcast_to([128, H, 128]), op=ALU.mult),
                  [("kvp", 0), ("kvp", 1), "qdc"], ["oo"])
                V(lambda e: e.tensor_tensor(out=oo[:], in0=oo[:], in1=zB3, op=ALU.add), ["oo", ("zB", 0), ("zB", 1)], ["oo"])
                V(lambda e: e.tensor_reduce(out=gst[:, 0:8], in_=oo[:], axis=AX.X, op=ALU.add), ["oo"], ["gst"])
                V(lambda e: e.tensor_tensor(out=o2[:], in0=oo[:], in1=oo[:], op=ALU.mult), ["oo"], ["o2"])
                V(lambda e: e.tensor_reduce(out=gst[:, 8:16], in_=o2[:], axis=AX.X, op=ALU.add), ["o2"], ["gst"])
                V(lambda e: e.tensor_scalar(out=gst[:, 16:24], in0=gst[:, 0:8], scalar1=1.0 / 128.0, scalar2=None, op0=ALU.mult), ["gst"], ["gst"])
                V(lambda e: e.tensor_tensor(out=gst[:, 24:32], in0=gst[:, 16:24], in1=gst[:, 16:24], op=ALU.mult), ["gst"], ["gst"])
                V(lambda e: e.scalar_tensor_tensor(out=gst[:, 24:32], in0=gst[:, 8:16], scalar=1.0 / 128.0, in1=gst[:, 24:32],
                                                   op0=ALU.mult, op1=ALU.subtract), ["gst"], ["gst"])
                A(lambda e: e.activation(out=gst[:, 32:40], in_=gst[:, 24:32], func=AF.Ln, bias=epsc[:, 1:2]), ["gst", "epsc"], ["gst2"])
                A(lambda e: e.activation(out=gst[:, 32:40], in_=gst[:, 32:40], func=AF.Exp, scale=-0.5), ["gst2"], ["gst2"])
                for h in range(H):
                    V(lambda e, h=h: e.tensor_scalar(out=o2[:, h, :], in0=oo[:, h, :], scalar1=gst[:, 16 + h:17 + h], scalar2=gst[:, 32 + h:33 + h],
                                                     op0=ALU.subtract, op1=ALU.mult), ["oo", "gst", "gst2"], ["o2"])
                proj(zA[:, 0:512], C_RG, 512, ("zA", 0))
                proj(zA[:, 512:1024], C_RG + 512, 512, ("zA", 1))
                A(lambda e: e.activation(out=sg[:], in_=zA[:], func=AF.Silu), [("zA", 0), ("zA", 1)], ["sg"])
                o2f = o2[:].rearrange("p h d -> p (h d)")
                V(lambda e: e.tensor_tensor(out=o2f, in0=o2f, in1=gnr[:], op=ALU.mult), ["o2", "gnr"], ["o2"])
                V(lambda e: e.tensor_tensor(out=ro[:], in0=o2f, in1=sg[:], op=ALU.mult), ["o2", "sg"], ["ro"])
                for k in range(8):
                    P(lambda e, k=k: e.transpose(out=pT[:, k, :], in_=ro[:, k * 128:(k + 1) * 128], identity=idb[:]), ["ro", "idb"], ["pT"])
                A(lambda e: e.activation(out=roT[:], in_=pT[:], func=AF.Copy), ["pT"], ["roT"])
                DM(lambda e: e.dma_start(out=roT_s[g], in_=roT[:]), ["roT"], [("roT_s", g)], "st_r")

            items = []
            for g in range(NG):
                for s4 in range(4):
                    items.append(("k", 4 * g + s4))
                items.append(("o", g))

            def stage1(pos):
                kind, idx = items[pos]
                (kside_s1 if kind == "k" else ownside_s1)(pos, idx)

            stage1(0)
            for pos in range(len(items)):
                if pos + 1 < len(items):
                    stage1(pos + 1)
                kind, idx = items[pos]
                (kside if kind == "k" else ownside)(pos, idx)
            if dbg:
                DM(lambda e: e.dma_start(out=kT_dbg, in_=kT[:]), [("kT", j) for j in range(NT)], ["kT_dbg"], "dbg")
                DM(lambda e: e.dma_start(out=dv_dbg, in_=dv1[:]), [("dv1", j) for j in range(NT)] + ["dv1one"], ["dv_dbg"], "dbg")
            S.barrier()
            S.emit()

        PHASES = build.phases
        if PHASES >= 2:
          with contextlib.ExitStack() as ph:
            score2 = [sbt(ph, f"score{i}", [128, SB], BF16) for i in range(2)]
            msk = sbt(ph, "msk", [128, SB], BF16)
            mskT = [sbt(ph, f"mskT{i}", [128, 8, 128], BF16) for i in range(2)]
            iq_sb2 = [sbt(ph, f"iq_sb{i}", [128, H, 128], BF16) for i in range(2)]
            dq_sb2 = [sbt(ph, f"dq_sb{i}", [128, H, 128], BF16) for i in range(2)]
            iw_sb2 = [sbt(ph, f"iw_sb{i}", [128, H], F32) for i in range(2)]
            dg2 = [sbt(ph, f"dg{i}", [128, H, 128], BF16) for i in range(2)]
            maddb = sbt(ph, "maddb", [128, 512], BF16)
            rh = [sbt(ph, f"rh{i}", [128, 512], BF16) for i in range(4)]
            madd = sbt(ph, "madd", [128, 512], F32)
            bT = sbt(ph, "bT", [128, 5, H, 128], BF16)
            c15s = sbt(ph, "c15s", [128, H], F32)
            selden = sbt(ph, "selden", [65, 64], F32)
            bst = sbt(ph, "bst", [128, 8], F32)
            steps = sbt(ph, "steps", [128, NBIS], F32)
            pmx = [sbt(ph, f"pmx{i}", [128, 16], F32) for i in range(2)]
            pmn = [sbt(ph, f"pmn{i}", [128, 16], F32) for i in range(2)]
            bx = sbt(ph, "bx", [128, 2], F32)
            halves = sbt(ph, "halves", [128, NBIS], F32)
            tmpl = sbt(ph, "tmpl", [128, 512], F32)
            lg = sbt(ph, "lg", [128, H, 128], F32)
            pe_ = [sbt(ph, f"pe{i}", [128, H, 128], BF16) for i in range(2)]
            pm = [sbt(ph, f"pm{i}", [128, H, 128], BF16) for i in range(3)]
            OT = sbt(ph, "OT", [65, H * 128], F32)
            ydT = sbt(ph, "ydT", [64, H, 128], BF16)
            PP = [pst(ph, f"PP{i}", [128, 1024], F32) for i in range(4)]
            ps_s = [PP[k // 2][:, (k % 2) * 512:(k % 2 + 1) * 512] for k in range(4)]
            ps_s_res = [(f"PP{k // 2}", k % 2) for k in range(4)]
            ps_sc = [PP[2][:, 0:512], PP[2][:, 512:1024]]
            ps_sc_res = [("PP2", 0), ("PP2", 1)]
            ps_qk = [PP[0], PP[1]]
            ps_o = PP[2]
            ps_mT = PP[3][:, 0:512].bitcast(BF16).rearrange("p (a b) -> p a b", a=8)

            DM(lambda e: e.dma_start(out=madd[:], in_=maddd), [], ["madd"], "c4_madd")
            DG(lambda e: e.dma_start(out=bT[:], in_=biasTd.rearrange("s p h q -> p s h q")), [], ["bT"], "c4_bT")
            DM(lambda e: e.dma_start(out=c15s[:], in_=c15), [], ["c15s"], "c4_c15s")
            DM(lambda e: e.dma_start(out=selden[:], in_=seldend), [], ["selden"], "c4_selden")
            for s5 in range(5):
                V(lambda e, s5=s5: e.tensor_tensor(out=bT[:, s5, :, :], in0=bT[:, s5, :, :],
                                                   in1=c15s[:].unsqueeze(2).broadcast_to([128, H, 128]), op=ALU.subtract),
                  ["bT", "c15s"], ["bT"])
            for i in range(NBIS):
                V(lambda e, i=i: e.memset(halves[:, i:i + 1], 2.0 ** (-(i + 1))), [], ["halves"])
            for i in range(2):
                G(lambda e, i=i: e.memset(iq_sb2[i][64:128, :, :], 0.0), [], [f"iq_sb{i}"])
                G(lambda e, i=i: e.memset(dq_sb2[i][0:64, :, :], 0.0), [], [f"dq_sb{i}"])
            V(lambda e: e.tensor_copy(out=maddb[:], in_=madd[:]), ["madd"], ["maddb"])

            def load_q(g):
                sl = g % 2
                DM(lambda e: e.dma_start(out=iq_sb2[sl][0:64, :, :], in_=iqT_s[g]), [("iqT_s", g)], [f"iq_sb{sl}"], f"ld_i{sl}")
                DM(lambda e: e.dma_start(out=dq_sb2[sl][64:128, :, :], in_=dqT_s[g]), [("dqT_s", g)], [f"dq_sb{sl}"], f"ld_q{sl}")
                DM(lambda e: e.dma_start(out=iw_sb2[sl][:], in_=iw_s[g]), [("iw_s", g)], [f"iw_sb{sl}"], f"ld_w{sl}")
                for h in range(H):
                    V(lambda e, h=h: e.tensor_scalar(out=dg2[sl][:, h, :], in0=idb[:], scalar1=iw_sb2[sl][:, h:h + 1], scalar2=None, op0=ALU.mult),
                      ["idb", f"iw_sb{sl}"], [f"dg{sl}"])

            def indexer(g):
                sl = g % 2
                iq_sb = iq_sb2[sl]; dg = dg2[sl]; score = score2[sl]
                iqres = f"iq_sb{sl}"; dgres = f"dg{sl}"
                M = 8 * (g + 1)

                def smm(i):
                    c, h = i // 8, i % 8
                    kres = [("kT", 4 * c + t) for t in range(4)]
                    P(lambda e: e.matmul(ps_s[i % 4], lhsT=iq_sb[:, h, :], rhs=kT[:, c * 512:(c + 1) * 512], start=True, stop=True),
                      [iqres] + kres, [ps_s_res[i % 4]])

                def rl_dmm(i):
                    c, h = i // 8, i % 8
                    r4 = i % 4
                    A(lambda e: e.activation(out=rh[r4][:], in_=ps_s[r4], func=AF.Relu), [ps_s_res[r4]], [f"rh{r4}"])
                    last = (c == g)
                    P(lambda e: e.matmul(ps_sc[c % 2], lhsT=dg[:, h, :], rhs=rh[r4][:], start=(h == 0), stop=(h == H - 1 and not last)),
                      [dgres, f"rh{r4}"], [ps_sc_res[c % 2]])
                    if h == H - 1:
                        if last:
                            P(lambda e: e.matmul(ps_sc[c % 2], lhsT=idb[:], rhs=maddb[:], start=False, stop=True), ["idb", "maddb"], [ps_sc_res[c % 2]])
                        A(lambda e: e.activation(out=score[:, c * 512:(c + 1) * 512], in_=ps_sc[c % 2], func=AF.Copy), [ps_sc_res[c % 2]], [(f"score{sl}", c)])

                smm(0)
                smm(1)
                for i_ in range(M):
                    if i_ + 2 < M:
                        smm(i_ + 2)
                    rl_dmm(i_)

            def mm_ops(G):
                slg = G % 2
                sc = score2[slg]
                ops_ = []
                npc = (G + 3) // 4
                for p in range(npc):
                    a, b_ = p * 2048, min((p + 1) * 2048, G * 512)
                    rr = [(f"score{slg}", c) for c in range(4 * p, min(4 * p + 4, G))]
                    ops_.append(lambda p=p, a=a, b_=b_, rr=rr: V(lambda e: e.tensor_reduce(out=pmx[slg][:, p:p + 1], in_=sc[:, a:b_], axis=AX.X, op=ALU.max),
                                                                   rr, [(f"pmx{slg}", p)]))
                    ops_.append(lambda p=p, a=a, b_=b_, rr=rr: V(lambda e: e.tensor_reduce(out=pmn[slg][:, p:p + 1], in_=sc[:, a:b_], axis=AX.X, op=ALU.min),
                                                                   rr, [(f"pmn{slg}", p)]))
                return ops_

            def bisect(g):
                sl = g % 2
                score = score2[sl]
                nk = (g + 1) * 512
                sres = [(f"score{sl}", c) for c in range(g + 1)]
                V(lambda e: e.scalar_tensor_tensor(out=tmpl[:], in0=madd[:], scalar=-2.0, in1=score[:, nk - 512:nk], op0=ALU.mult, op1=ALU.add),
                  [(f"score{sl}", g), "madd"], ["tmpl"])
                V(lambda e: e.tensor_reduce(out=bst[:, 2:3], in_=tmpl[:], axis=AX.X, op=ALU.min), ["tmpl"], ["bst2"])
                V(lambda e: e.tensor_reduce(out=bst[:, 0:1], in_=score[:, nk - 512:nk], axis=AX.X, op=ALU.max), [(f"score{sl}", g)], ["bst0"])
                if g > 0:
                    npc = (g + 3) // 4
                    V(lambda e: e.tensor_reduce(out=bx[:, 0:1], in_=pmx[sl][:, 0:npc], axis=AX.X, op=ALU.max), [(f"pmx{sl}", p) for p in range(npc)], ["bx0"])
                    V(lambda e: e.tensor_tensor(out=bst[:, 0:1], in0=bst[:, 0:1], in1=bx[:, 0:1], op=ALU.max), ["bst0", "bx0"], ["bst0"])
                    V(lambda e: e.tensor_reduce(out=bst[:, 1:2], in_=pmn[sl][:, 0:npc], axis=AX.X, op=ALU.min), [(f"pmn{sl}", p) for p in range(npc)], ["bst1"])
                    V(lambda e: e.tensor_tensor(out=bst[:, 3:4], in0=bst[:, 1:2], in1=bst[:, 2:3], op=ALU.min), ["bst1", "bst2"], ["bt"])
                else:
                    V(lambda e: e.tensor_copy(out=bst[:, 3:4], in_=bst[:, 2:3]), ["bst2"], ["bt"])
                V(lambda e: e.tensor_tensor(out=bst[:, 4:5], in0=bst[:, 0:1], in1=bst[:, 3:4], op=ALU.subtract), ["bst0", "bt"], ["bR"])
                V(lambda e: e.tensor_scalar(out=steps[:], in0=halves[:], scalar1=bst[:, 4:5], scalar2=None, op0=ALU.mult), ["halves", "bR"], ["steps"])
                for i in range(NBIS):
                    V(lambda e, i=i: e.tensor_tensor(out=bst[:, 5:6], in0=bst[:, 3:4], in1=steps[:, i:i + 1], op=ALU.add), ["bt", "steps"], ["bcand"])
                    V(lambda e: e.tensor_scalar(out=msk[:, 0:nk], in0=score[:, 0:nk], scalar1=bst[:, 5:6], scalar2=0.0, op0=ALU.is_ge, op1=ALU.add,
                                                accum_out=bst[:, 6:7]), sres + ["bcand"], ["msk", "bcnt"])
                    V(lambda e, i=i: e.tensor_scalar(out=bst[:, 7:8], in0=bst[:, 6:7], scalar1=255.5, scalar2=steps[:, i:i + 1], op0=ALU.is_ge, op1=ALU.mult),
                      ["bcnt", "steps"], ["binc"])
                    V(lambda e: e.tensor_tensor(out=bst[:, 3:4], in0=bst[:, 3:4], in1=bst[:, 7:8], op=ALU.add), ["bt", "binc"], ["bt"])
                V(lambda e: e.tensor_scalar(out=msk[:, 0:nk], in0=score[:, 0:nk], scalar1=bst[:, 3:4], scalar2=0.0, op0=ALU.is_ge, op1=ALU.add,
                                            accum_out=bst[:, 6:7]), sres + ["bt"], ["msk", "bcnt"])
                if dbg:
                    DM(lambda e: e.dma_start(out=thr_dbg[g], in_=bst[:, 0:8]), ["bt", "bcnt", "bR", "bcand", "bst0", "bst1", "bst2", "binc"], [("thr_dbg", g)], "dbg2")
                    DM(lambda e: e.dma_start(out=sc_dbg[g, :, 0:nk], in_=score[:, 0:nk]), sres, [("sc_dbg", g)], "dbg2")
                    DM(lambda e: e.dma_start(out=mk_dbg[g, :, 0:nk], in_=msk[:, 0:nk]), ["msk"], [("mk_dbg", g)], "dbg2")

            def attention(g, pend):
                sl = g % 2
                dq_sb = dq_sb2[sl]
                dqres = f"dq_sb{sl}"
                ntile = (g + 1) * 4

                def mask_blk(j0):
                    nj = min(8, ntile - j0)
                    for jj in range(nj):
                        j = j0 + jj
                        P(lambda e, j=j, jj=jj: e.transpose(out=ps_mT[:, jj, :], in_=msk[:, j * 128:(j + 1) * 128], identity=idb[:]),
                          ["msk", "idb"], [("PP3", 0)])
                    mb = (j0 // 8) % 2
                    if mb == 0:
                        A(lambda e, mb=mb, nj=nj: e.activation(out=mskT[mb][:, 0:nj, :], in_=ps_mT[:, 0:nj, :], func=AF.Copy), [("PP3", 0)], [f"mskT{mb}"])
                    else:
                        V(lambda e, mb=mb, nj=nj: e.tensor_copy(out=mskT[mb][:, 0:nj, :], in_=ps_mT[:, 0:nj, :]), [("PP3", 0)], [f"mskT{mb}"])
                def att_qk(j):
                    qb = j % 2
                    for hf in range(2):
                        P(lambda e, hf=hf: e.matmul(ps_qk[qb][:, hf * 512:(hf + 1) * 512], lhsT=kT[:, j * 128:(j + 1) * 128],
                                                    rhs=dq_sb[:, 4 * hf:4 * hf + 4, :], start=True, stop=True),
                          [("kT", j), dqres], [(f"PP{qb}", hf)])

                def att_sm(j):
                    b = j % 2
                    if j % 8 == 0:
                        mask_blk(j)
                    mb = (j // 8) % 2
                    slot = j - (4 * g - 1)
                    qk3 = ps_qk[b][:].rearrange("p (h q) -> p h q", h=H)
                    qres = [(f"PP{b}", 0), (f"PP{b}", 1)]
                    if slot >= 0:
                        V(lambda e: e.scalar_tensor_tensor(out=lg[:], in0=qk3, scalar=0.125, in1=bT[:, slot, :, :], op0=ALU.mult, op1=ALU.add),
                          qres + ["bT"], ["lg"])
                        A(lambda e: e.activation(out=pe_[b][:], in_=lg[:], func=AF.Exp), ["lg"], [f"pe{b}"])
                    else:
                        A(lambda e: e.activation(out=pe_[b][:], in_=qk3, func=AF.Exp, scale=0.125), qres, [f"pe{b}"])
                    b3 = j % 3
                    V(lambda e: e.tensor_tensor(out=pm[b3][:], in0=pe_[b][:], in1=mskT[mb][:, j % 8, :].unsqueeze(1).broadcast_to([128, H, 128]), op=ALU.mult),
                      [f"pe{b}", f"mskT{mb}"], [f"pm{b3}"])

                def att_pv(j):
                    b = j % 3
                    for hf in range(2):
                        P(lambda e, hf=hf: e.matmul(ps_o[0:65, hf * 512:(hf + 1) * 512], lhsT=dv1[:, j, :], rhs=pm[b][:, 4 * hf:4 * hf + 4, :],
                                                    start=(j == 0), stop=(j == ntile - 1)),
                          [("dv1", j), "dv1one", f"pm{b}"], [("PP2", hf)])

                att_qk(0)
                for j_ in range(ntile):
                    if j_ + 1 < ntile:
                        att_qk(j_ + 1)
                    att_sm(j_)
                    if j_ % 4 == 1 and pend:
                        pend.pop(0)()
                    if j_ >= 1:
                        att_pv(j_ - 1)
                att_pv(ntile - 1)
                while pend:
                    pend.pop(0)()
                A(lambda e: e.activation(out=OT[:], in_=ps_o[0:65, :], func=AF.Copy), [("PP2", 0), ("PP2", 1)], ["OT"])
                for hf in range(2):
                    P(lambda e, hf=hf: e.matmul(PP[3][0:64, hf * 512:(hf + 1) * 512], lhsT=selden[:], rhs=OT[:, hf * 512:(hf + 1) * 512], start=True, stop=True),
                      ["selden", "OT"], [("PP3", hf)])
                rden = lg[0:64, :, :].rearrange("p h q -> p (h q)")
                V(lambda e: e.reciprocal(out=rden, in_=PP[3][0:64, :]), [("PP3", 0), ("PP3", 1)], ["lg"])
                V(lambda e: e.tensor_tensor(out=ydT[:].rearrange("p h q -> p (h q)"), in0=OT[0:64, :], in1=rden, op=ALU.mult), ["OT", "lg"], ["ydT"])
                DM(lambda e: e.dma_start(out=ydT_s[g], in_=ydT[:]), ["ydT"], [("ydT_s", g)], "st_y")

            load_q(0)
            indexer(0)
            for g_ in range(NG):
                pend_ = []
                if g_ + 1 < NG:
                    load_q(g_ + 1)
                    indexer(g_ + 1)
                    pend_ = mm_ops(g_ + 1)
                bisect(g_)
                attention(g_, pend_)
            S.barrier()
            S.emit()

        kvs.close()
        if PHASES >= 3:
          with contextlib.ExitStack() as ph:
            u2T = sbt(ph, "u2T", [128, NG, 8, 128], BF16)
            gates = sbt(ph, "gates", [128, NG, 16], F32)
            with contextlib.ExitStack() as pc:
                wg_sb = sbt(pc, "wg_sb", [128, 8, 2 * D], BF16)
                wro_sb = sbt(pc, "wro_sb", [128, 8, D], BF16)
                wdo_sb = sbt(pc, "wdo_sb", [64, H, D], BF16)
                wo_sb = sbt(pc, "wo_sb", [128, 8, D], BF16)
                wrt_sb = sbt(pc, "wrt_sb", [128, 8, 20], BF16)
                bg_sb = sbt(pc, "bg_sb", [1, 2 * D], BF16)
                ones1 = sbt(pc, "ones1", [1, 128], BF16)
                brt = sbt(pc, "brt", [128, 20], F32)
                xt = [sbt(pc, f"cxt{i}", [128, D], F32) for i in range(2)]
                uTc = [sbt(pc, f"cuT{i}", [128, 8, 128], BF16) for i in range(2)]
                roTc = [sbt(pc, f"croT{i}", [128, 8, 128], BF16) for i in range(2)]
                ydTc = [sbt(pc, f"cydT{i}", [64, H, 128], BF16) for i in range(2)]
                gab = sbt(pc, "gab", [128, 2 * D], BF16)
                t1 = sbt(pc, "t1", [128, D], F32); t2 = sbt(pc, "t2", [128, D], F32)
                mm = sbt(pc, "mm", [128, D], BF16); mT = sbt(pc, "mT", [128, 8, 128], BF16)
                h1b = [sbt(pc, f"h1_{i}", [128, D], F32) for i in range(2)]
                junk = sbt(pc, "cjunk", [128, D], BF16); xs = sbt(pc, "cxs", [128, D], BF16)
                stat = sbt(pc, "cstat", [128, 8], F32)
                rl = sbt(pc, "rl", [128, 20], F32)
                rs = sbt(pc, "rs", [128, 48], F32)
                u2t = sbt(pc, "u2t", [128, 8, 128], BF16)
                pT = pst(pc, "cpT", [128, 8, 128], BF16)
                pgA = pst(pc, "pgA", [128, 2 * D], F32)
                pyr = pst(pc, "pyr", [128, D], F32)
                prt = pst(pc, "prt", [128, 32], F32)

                for k in range(8):
                    DG(lambda e, k=k: e.dma_start(out=wg_sb[:, k, :], in_=w_gate[k * 128:(k + 1) * 128, :]), [], ["wg_sb"], "wC_wg_sb")
                    DG(lambda e, k=k: e.dma_start(out=wro_sb[:, k, :], in_=w_ret_out[k * 128:(k + 1) * 128, :]), [], ["wro_sb"], "wC_wro_sb")
                    DG(lambda e, k=k: e.dma_start(out=wo_sb[:, k, :], in_=w_o[k * 128:(k + 1) * 128, :]), [], ["wo_sb"], "wC_wo_sb")
                    DG(lambda e, k=k: e.dma_start(out=wdo_sb[:, k, :], in_=w_dsa_out[k * 64:(k + 1) * 64, :]), [], ["wdo_sb"], "wC_wdo_sb")
                DG(lambda e: e.dma_start(out=wrt_sb[:], in_=w_rt.rearrange("(k p) n -> p k n", p=128)), [], ["wrt_sb"], "wC_wrt_sb")
                DG(lambda e: e.dma_start(out=bg_sb[:], in_=b_gate), [], ["bg_sb"], "wC_bg_sb")
                DM(lambda e: e.dma_start(out=brt[:], in_=b_rt_rep), [], ["brt"], "c5")
                V(lambda e: e.memset(ones1[:], 1.0), [], ["ones1"])
                for k in range(8):
                    G(lambda e, k=k: e.tensor_tensor(out=wo_sb[:, k, :], in0=wo_sb[:, k, :], in1=g1rep[:], op=ALU.mult), ["wo_sb", "mod"], ["wo_sb"])

                def merge_tile(g):
                    sl = g % 2
                    DM(lambda e: e.dma_start(out=xt[sl][:], in_=xo[g * 128:(g + 1) * 128, :]), [], [f"cxt{sl}"], f"cxt{sl}")
                    DM(lambda e: e.dma_start(out=uTc[sl][:], in_=uT_s[g]), [("uT_s", g)], [f"cuT{sl}"], f"cuT{sl}")
                    DM(lambda e: e.dma_start(out=roTc[sl][:], in_=roT_s[g]), [("roT_s", g)], [f"croT{sl}"], f"croT{sl}")
                    DM(lambda e: e.dma_start(out=ydTc[sl][:], in_=ydT_s[g]), [("ydT_s", g)], [f"cydT{sl}"], f"cydT{sl}")
                    for q4 in range(4):
                        cs_ = slice(q4 * 512, (q4 + 1) * 512)
                        for k in range(8):
                            P(lambda e, k=k, cs_=cs_: e.matmul(pgA[:, cs_], lhsT=uTc[sl][:, k, :], rhs=wg_sb[:, k, cs_], start=(k == 0), stop=False),
                              [f"cuT{sl}", "wg_sb"], [("pgA", q4)])
                        P(lambda e, cs_=cs_: e.matmul(pgA[:, cs_], lhsT=ones1[:], rhs=bg_sb[:, cs_], start=False, stop=True),
                          ["ones1", "bg_sb"], [("pgA", q4)])
                    A(lambda e: e.activation(out=gab[:], in_=pgA[:], func=AF.Sigmoid), [("pgA", i) for i in range(4)], ["gab"])
                    for hf in range(2):
                        cs_ = slice(hf * 512, (hf + 1) * 512)
                        for k in range(8):
                            P(lambda e, k=k, cs_=cs_: e.matmul(pyr[:, cs_], lhsT=roTc[sl][:, k, :], rhs=wro_sb[:, k, cs_], start=(k == 0), stop=(k == 7)),
                              [f"croT{sl}", "wro_sb"], [("pyr", hf)])
                    V(lambda e: e.tensor_tensor(out=t1[:], in0=pyr[:], in1=gab[:, 0:D], op=ALU.mult), [("pyr", 0), ("pyr", 1), "gab"], ["t1"])
                    for hf in range(2):
                        cs_ = slice(hf * 512, (hf + 1) * 512)
                        for h in range(H):
                            P(lambda e, h=h, cs_=cs_: e.matmul(pyr[:, cs_], lhsT=ydTc[sl][:, h, :], rhs=wdo_sb[:, h, cs_], start=(h == 0), stop=(h == H - 1)),
                              [f"cydT{sl}", "wdo_sb"], [("pyr", hf)])
                    V(lambda e: e.tensor_tensor(out=t2[:], in0=pyr[:], in1=gab[:, D:2 * D], op=ALU.mult), [("pyr", 0), ("pyr", 1), "gab"], ["t2"])
                    V(lambda e: e.tensor_tensor(out=mm[:], in0=t1[:], in1=t2[:], op=ALU.add), ["t1", "t2"], ["mm"])
                    for k in range(8):
                        P(lambda e, k=k: e.transpose(out=pT[:, k, :], in_=mm[:, k * 128:(k + 1) * 128], identity=idb[:]), ["mm", "idb"], ["pT"])
                    A(lambda e: e.activation(out=mT[:], in_=pT[:], func=AF.Copy), ["pT"], ["mT"])
                    for hf in range(2):
                        cs_ = slice(hf * 512, (hf + 1) * 512)
                        for k in range(8):
                            P(lambda e, k=k, cs_=cs_: e.matmul(pyr[:, cs_], lhsT=mT[:, k, :], rhs=wo_sb[:, k, cs_], start=(k == 0), stop=(k == 7)),
                              ["mT", "wo_sb"], [("pyr", hf)])
                    h1 = h1b[sl]
                    V(lambda e: e.tensor_tensor(out=h1[:], in0=pyr[:], in1=xt[sl][:], op=ALU.add), [("pyr", 0), ("pyr", 1), f"cxt{sl}"], [f"h1_{sl}"])
                    DM(lambda e: e.dma_start(out=h1_s[g], in_=h1[:]), [f"h1_{sl}"], [("h1_s", g)], f"st_h{sl}")

                def route_tile(g):
                    sl = g % 2
                    h1 = h1b[sl]
                    norm_T(h1[:], f"h1_{sl}", junk[:], xs[:], pT, u2t, stat, G2T, sh2T, "c")
                    G(lambda e: e.tensor_copy(out=u2T[:, g, :, :], in_=u2t[:]), ["cuT"], [("u2T", g)])
                    for k in range(8):
                        P(lambda e, k=k: e.matmul(prt[:, 0:20], lhsT=u2t[:, k, :], rhs=wrt_sb[:, k, :], start=(k == 0), stop=(k == 7)),
                          ["cuT", "wrt_sb"], ["prt"])
                    V(lambda e: e.tensor_tensor(out=rl[:], in0=prt[:, 0:20], in1=brt[:], op=ALU.add), ["prt", "brt"], ["rl"])
                    R = lambda a, b_: rs[:, a:b_]
                    V(lambda e: e.tensor_reduce(out=R(0, 1), in_=rl[:, 0:4], axis=AX.X, op=ALU.max), ["rl"], ["rs"])
                    V(lambda e: e.tensor_scalar(out=R(1, 5), in0=rl[:, 0:4], scalar1=R(0, 1), scalar2=None, op0=ALU.is_ge), ["rl", "rs"], ["rs"])
                    V(lambda e: e.tensor_scalar(out=R(5, 6), in0=R(0, 1), scalar1=-1.0, scalar2=None, op0=ALU.mult), ["rs"], ["rs"])
                    A(lambda e: e.activation(out=R(6, 10), in_=rl[:, 0:4], func=AF.Exp, bias=R(5, 6), accum_out=R(10, 11)), ["rl", "rs"], ["rs2"])
                    V(lambda e: e.reciprocal(out=R(11, 12), in_=R(10, 11)), ["rs2"], ["rs3"])
                    V(lambda e: e.tensor_scalar(out=R(12, 16), in0=rl[:, 4:8], scalar1=R(1, 2), scalar2=None, op0=ALU.mult), ["rl", "rs"], ["rs4"])
                    for gi in range(1, 4):
                        V(lambda e, gi=gi: e.scalar_tensor_tensor(out=R(12, 16), in0=rl[:, 4 + 4 * gi:8 + 4 * gi], scalar=R(1 + gi, 2 + gi), in1=R(12, 16),
                                                                  op0=ALU.mult, op1=ALU.add), ["rl", "rs", "rs4"], ["rs4"])
                    V(lambda e: e.tensor_reduce(out=R(16, 17), in_=R(12, 16), axis=AX.X, op=ALU.max), ["rs4"], ["rs5"])
                    V(lambda e: e.tensor_scalar(out=R(17, 21), in0=R(12, 16), scalar1=R(16, 17), scalar2=None, op0=ALU.is_ge), ["rs4", "rs5"], ["rs6"])
                    V(lambda e: e.scalar_tensor_tensor(out=R(21, 25), in0=R(17, 21), scalar=NEG, in1=R(12, 16), op0=ALU.mult, op1=ALU.add),
                      ["rs6", "rs4"], ["rs7"])
                    V(lambda e: e.tensor_reduce(out=R(25, 26), in_=R(21, 25), axis=AX.X, op=ALU.max), ["rs7"], ["rs8"])
                    V(lambda e: e.tensor_scalar(out=R(26, 30), in0=R(21, 25), scalar1=R(25, 26), scalar2=None, op0=ALU.is_ge), ["rs7", "rs8"], ["rs9"])
                    V(lambda e: e.tensor_tensor(out=R(30, 31), in0=R(16, 17), in1=R(25, 26), op=ALU.subtract), ["rs5", "rs8"], ["rs10"])
                    A(lambda e: e.activation(out=R(31, 32), in_=R(30, 31), func=AF.Sigmoid), ["rs10"], ["rs11"])
                    V(lambda e: e.tensor_scalar(out=R(32, 33), in0=R(31, 32), scalar1=-1.0, scalar2=1.0, op0=ALU.mult, op1=ALU.add), ["rs11"], ["rs12"])
                    V(lambda e: e.tensor_scalar(out=R(33, 37), in0=R(17, 21), scalar1=R(31, 32), scalar2=None, op0=ALU.mult), ["rs6", "rs11"], ["rs13"])
                    V(lambda e: e.scalar_tensor_tensor(out=R(33, 37), in0=R(26, 30), scalar=R(32, 33), in1=R(33, 37), op0=ALU.mult, op1=ALU.add),
                      ["rs9", "rs12", "rs13"], ["rs13"])
                    V(lambda e: e.tensor_scalar(out=R(33, 37), in0=R(33, 37), scalar1=R(11, 12), scalar2=None, op0=ALU.mult), ["rs13", "rs3"], ["rs13"])
                    for gi in range(4):
                        V(lambda e, gi=gi: e.tensor_scalar(out=gates[:, g, 4 * gi:4 * gi + 4], in0=R(33, 37), scalar1=R(1 + gi, 2 + gi), scalar2=None, op0=ALU.mult),
                          ["rs13", "rs"], [("gates", g)])

                merge_tile(0)
                for g_ in range(NG):
                    if g_ + 1 < NG:
                        merge_tile(g_ + 1)
                    route_tile(g_)
                S.barrier()
                S.emit()

            with contextlib.ExitStack() as pd:
                NH = 2 if NG >= 2 else 1
                TPH = NG // NH
                hacc = sbt(pd, "hacc", [128, TPH, D], F32)
                w13 = [sbt(pd, f"w13_{i}", [128, 8, 512], BF16) for i in range(2)]
                w2s = [sbt(pd, f"w2s_{i}", [128, 2, D], BF16) for i in range(2)]
                gfin = sbt(pd, "gfin", [128, D], F32)
                sa = [sbt(pd, f"sa{i}", [128, 256], F32) for i in range(2)]
                hid = [sbt(pd, f"hid{i}", [128, 256], BF16) for i in range(2)]
                hidT = [sbt(pd, f"hidT{i}", [128, 2, 128], BF16) for i in range(2)]
                fjunk = sbt(pd, "fjunk", [128, D], BF16)
                fstat = sbt(pd, "fstat", [128, 4], F32)
                ot = [sbt(pd, f"ot{i}", [128, D], F32) for i in range(2)]
                pab = [pst(pd, f"pab{i}", [128, 512], F32) for i in range(2)]
                phT = [pst(pd, f"phT{i}", [128, 2, 128], BF16) for i in range(2)]
                py = [pst(pd, f"py{i}", [128, D], F32) for i in range(2)]
                DM(lambda e: e.dma_start(out=gfin[:], in_=gfin_rep), [], ["gfin"], "c6")

                def load_expert(ex, ws):
                    for k in range(8):
                        DG(lambda e, k=k: e.dma_start(out=w13[ws][:, k, 0:256], in_=w1[ex, k * 128:(k + 1) * 128, :]), [], [f"w13_{ws}"], f"w13_{ws}")
                        DG(lambda e, k=k: e.dma_start(out=w13[ws][:, k, 256:512], in_=w3[ex, k * 128:(k + 1) * 128, :]), [], [f"w13_{ws}"], f"w13_{ws}")
                    for f in range(2):
                        DG(lambda e, f=f: e.dma_start(out=w2s[ws][:, f, :], in_=w2[ex, f * 128:(f + 1) * 128, :]), [], [f"w2s_{ws}"], f"w2s_{ws}")
                        G(lambda e, f=f: e.tensor_tensor(out=w2s[ws][:, f, :], in0=w2s[ws][:, f, :], in1=g2rep[:], op=ALU.mult),
                          [f"w2s_{ws}", "mod"], [f"w2s_{ws}"])

                def u_ab(u, g, ex, ws):
                    b = u % 2
                    for k in range(8):
                        P(lambda e, k=k: e.matmul(pab[b][:], lhsT=u2T[:, g, k, :], rhs=w13[ws][:, k, :], start=(k == 0), stop=(k == 7)),
                          [("u2T", g), f"w13_{ws}"], [f"pab{b}"])

                def u_act_tr(u, g, ex, ws):
                    b = u % 2
                    A(lambda e: e.activation(out=sa[b][:], in_=pab[b][:, 0:256], func=AF.Silu), [f"pab{b}"], [f"sa{b}"])
                    V(lambda e: e.scalar_tensor_tensor(out=hid[b][:], in0=pab[b][:, 256:512], scalar=gates[:, g, ex:ex + 1], in1=sa[b][:],
                                                       op0=ALU.mult, op1=ALU.mult), [f"pab{b}", ("gates", g), f"sa{b}"], [f"hid{b}"])
                    for f in range(2):
                        P(lambda e, f=f: e.transpose(out=phT[b][:, f, :], in_=hid[b][:, f * 128:(f + 1) * 128], identity=idb[:]), [f"hid{b}", "idb"], [f"phT{b}"])
                    A(lambda e: e.activation(out=hidT[b][:], in_=phT[b][:], func=AF.Copy), [f"phT{b}"], [f"hidT{b}"])

                def u_y(u, g, ex, ws, tl):
                    b = u % 2
                    for hf in range(2):
                        cs_ = slice(hf * 512, (hf + 1) * 512)
                        for f in range(2):
                            P(lambda e, f=f, cs_=cs_: e.matmul(py[b][:, cs_], lhsT=hidT[b][:, f, :], rhs=w2s[ws][:, f, cs_], start=(f == 0), stop=(f == 1)),
                              [f"hidT{b}", f"w2s_{ws}"], [(f"py{b}", hf)])
                    V(lambda e: e.tensor_tensor(out=hacc[:, tl, :], in0=hacc[:, tl, :], in1=py[b][:], op=ALU.add),
                      [("hacc", tl), (f"py{b}", 0), (f"py{b}", 1)], [("hacc", tl)])

                for hh in range(NH):
                    for tl in range(TPH):
                        g = hh * TPH + tl
                        DM(lambda e, tl=tl, g=g: e.dma_start(out=hacc[:, tl, :], in_=h1_s[g]), [("h1_s", g)], [("hacc", tl)], f"ld_h{tl}")
                    units = []
                    for ex in range(16):
                        for tl in range(TPH):
                            units.append((hh * TPH + tl, ex, (hh * 16 + ex) % 2, tl))
                    NU = len(units)
                    load_expert(0, (hh * 16) % 2)
                    loaded = {0}
                    g0_, ex0_, ws0_, tl0_ = units[0]
                    u_ab(0, g0_, ex0_, ws0_)
                    for u in range(NU):
                        g, ex, ws, tl = units[u]
                        deferred = False
                        if u + 1 < NU:
                            g2_, ex2_, ws2_, tl2_ = units[u + 1]
                            if ex2_ in loaded:
                                u_ab(u + 1, g2_, ex2_, ws2_)
                            else:
                                deferred = True
                        u_act_tr(u, g, ex, ws)
                        if u >= 1:
                            g1_, ex1_, ws1_, tl1_ = units[u - 1]
                            u_y(u - 1, g1_, ex1_, ws1_, tl1_)
                        if tl == 0 and ex + 1 < 16:
                            load_expert(ex + 1, (hh * 16 + ex + 1) % 2)
                            loaded.add(ex + 1)
                        if deferred:
                            u_ab(u + 1, g2_, ex2_, ws2_)
                    g1_, ex1_, ws1_, tl1_ = units[NU - 1]
                    u_y(NU - 1, g1_, ex1_, ws1_, tl1_)
                    for tl in range(TPH):
                        g = hh * TPH + tl
                        b = tl % 2
                        A(lambda e, tl=tl: e.activation(out=fjunk[:], in_=hacc[:, tl, :], func=AF.Square, scale=1.0 / 32.0, accum_out=fstat[:, 0:1]),
                          [("hacc", tl)], ["fjunk", "fstat"])
                        A(lambda e: e.activation(out=fstat[:, 1:2], in_=fstat[:, 0:1], func=AF.Ln, bias=epsc[:, 0:1]), ["fstat", "epsc"], ["fstat"])
                        A(lambda e: e.activation(out=fstat[:, 2:3], in_=fstat[:, 1:2], func=AF.Exp, scale=-0.5), ["fstat"], ["fstat"])
                        V(lambda e, tl=tl, b=b: e.scalar_tensor_tensor(out=ot[b][:], in0=hacc[:, tl, :], scalar=fstat[:, 2:3], in1=gfin[:], op0=ALU.mult, op1=ALU.mult),
                          [("hacc", tl), "fstat", "gfin"], [f"ot{b}"])
                        DM(lambda e, g=g, b=b: e.dma_start(out=out[g * 128:(g + 1) * 128, :], in_=ot[b][:]), [f"ot{b}"], [("out", g)], f"st_o{b}")
                S.op("sp", None, reads=[("out", g) for g in range(NG)])
                S.barrier()
                S.emit()
    return nc


build.phases = 3


def _rel_bucket(rel):
    nb = 16
    ret = (rel > 0).astype(np.int64) * nb
    n = np.abs(rel)
    max_exact = nb // 2
    nf = np.maximum(n, 1).astype(np.float32)
    large = max_exact + (np.log(nf / max_exact) / math.log(128 / max_exact) * (nb - max_exact)).astype(np.int32)
    large = np.minimum(large, nb - 1)
    return ret + np.where(n < max_exact, n, large)


def make_inputs(NG, inp):
    NT = 4 * NG
    SB = NT * 128
    f = np.float32
    x = np.asarray(inp["x"], f)
    assert x.shape[1] == SB
    pos = np.arange(SB, dtype=np.float64)
    freqs = 10000.0 ** (-np.arange(0, 64, 2, dtype=np.float64) / 64)
    ang = (pos[:, None].astype(np.float32) * freqs[None, :].astype(np.float32)).astype(np.float32)
    rope_all = np.concatenate([np.cos(ang), np.sin(ang)], axis=1).astype(f)
    gam = np.array(GAM, dtype=np.float64)
    i_ = np.arange(128)
    ci = i_ // 64
    DT = np.zeros((128, H, 128), f)
    for h in range(H):
        same = ci[:, None] == ci[None, :]
        dm = np.where(same, gam[h] ** np.abs(i_[:, None] - i_[None, :]),
                      np.where((ci[:, None] == 1) & (ci[None, :] == 0), gam[h] ** (i_[:, None] - i_[None, :]).clip(0), 0.0))
        DT[:, h, :] = (dm.T * 0.125).astype(f)
    qdec = (gam[None, :] ** (i_[:, None] + 1)).astype(f)
    wtile = (gam[None, :] ** (127 - i_[:, None]) * 0.125).astype(f)
    ident = np.eye(128, dtype=f)
    selden = np.zeros((65, 64), f); selden[64, :] = 1.0
    rb = np.asarray(inp["rel_bias"], f)

    def rep(v, n=128):
        return np.ascontiguousarray(np.broadcast_to(np.asarray(v, f).reshape(1, -1), (n, np.asarray(v).size)))

    def fm(v):
        return np.ascontiguousarray(np.asarray(v, f).reshape(-1, 128).T)

    b_ada = np.asarray(inp["b_ada"], f)[0]
    common = dict(
        w_ada=np.ascontiguousarray(inp["w_ada"][0], dtype=f), b_adaT=fm(b_ada),
        b_g12=np.ascontiguousarray(np.concatenate([rep(b_ada[2 * D:3 * D]), rep(b_ada[5 * D:6 * D])], axis=1)),
        gmixT=fm(inp["norm_mix_g"][0]), gffnT=fm(inp["norm_ffn_g"][0]),
        gfin_rep=rep(inp["norm_final_g"]), gn_rep=rep(inp["ret_gn_g"][0]), kvg_rep=rep(inp["dsa_kv_norm_g"][0]),
        w_in=np.ascontiguousarray(inp["w_in"][0], dtype=f), w_kv=np.ascontiguousarray(inp["w_dsa_kv_up"][0], dtype=f),
        w_ret_out=np.ascontiguousarray(inp["w_ret_out"][0], dtype=f), w_dsa_out=np.ascontiguousarray(inp["w_dsa_out"][0], dtype=f),
        w_gate=np.ascontiguousarray(inp["w_gate"][0], dtype=f), b_gate=np.ascontiguousarray(inp["b_gate"], dtype=f).reshape(1, -1),
        w_o=np.ascontiguousarray(inp["w_o"][0], dtype=f),
        w_rt=np.ascontiguousarray(np.concatenate([inp["w_group_router"][0], inp["w_expert_router"][0]], axis=1), dtype=f),
        b_rt_rep=rep(np.concatenate([inp["b_group_router"][0], inp["b_expert_router"][0]])),
        w1=np.ascontiguousarray(inp["w_exp_gate"][0], dtype=f), w3=np.ascontiguousarray(inp["w_exp_up"][0], dtype=f),
        w2=np.ascontiguousarray(inp["w_exp_down"][0], dtype=f),
        rope_all=rope_all, ident=ident, DT=DT, qdec=qdec, wtile=wtile, selden=selden, c15_rep=rep(rb[15]),
    )
    maps = []
    for c in range(8):
        b, r = c // 4, c % 4
        own = np.array([4 * g + r for g in range(NG)])
        rows = (own[:, None] * 128 + np.arange(128)[None, :]).reshape(-1)
        sel = np.zeros((64, 4), f); sel[:, r] = 1.0
        madd = np.zeros((128, 512), f)
        kt = np.arange(512) // 128
        kc = (np.arange(512) % 128) // 64
        qc = np.arange(128) // 64
        inadm = (kt[None, :] > r) | ((kt[None, :] == r) & (kc[None, :] > qc[:, None]))
        madd[inadm] = NEG
        biasT = np.zeros((5, 128, H, 128), f)
        for s5 in range(5):
            rel = (s5 - 1 - r) * 128 + np.arange(128)[:, None] - np.arange(128)[None, :]
            bk = _rel_bucket(rel)
            biasT[s5] = np.transpose(rb[bk], (0, 2, 1))
        m = dict(common)
        m.update(xb=np.ascontiguousarray(x[b]), xo=np.ascontiguousarray(x[b][rows]),
                 ccol=fm(inp["c"][b]), rope_own=np.ascontiguousarray(rope_all[rows]),
                 sel=sel, madd=madd, biasT=biasT)
        maps.append(m)
    return maps


_NC_CACHE = {}


def kernel(**inputs):
    NG = 32
    if NG not in _NC_CACHE:
        _NC_CACHE[NG] = build(NG)
    nc = _NC_CACHE[NG]
    maps = make_inputs(NG, inputs)
    res = run_bass_kernel_spmd(nc, maps, core_ids=list(range(8)))
    B, SEQ = inputs["x"].shape[0], inputs["x"].shape[1]
    outp = np.zeros((B, SEQ, D), np.float32)
    for c in range(8):
        b, r = c // 4, c % 4
        o = np.asarray(res.results[c]["out"]).reshape(NG, 128, D)
        for g in range(NG):
            t = 4 * g + r
            outp[b, t * 128:(t + 1) * 128, :] = o[g]
    return outp
```

```python
import contextlib
import math
import numpy as np
import concourse.bass as bass
import concourse.mybir as mybir
from concourse.bass_utils import run_bass_kernel_spmd

F32 = mybir.dt.float32
BF16 = mybir.dt.bfloat16
AF = mybir.ActivationFunctionType
ALU = mybir.AluOpType
AX = mybir.AxisListType

D = 1024
H = 8
NEG = -1.0e30
NBIS = 10
GAM = [1.0 - 2.0 ** (-5.0 - h) for h in range(H)]


class Sched:
    ENGS = ("pe", "act", "dve", "pool", "sp")
    ROT = 3000

    def __init__(self, nc, stack):
        self.nc = nc
        self.stack = stack
        self.ops = {e: [] for e in self.ENGS}
        self.last_write = {}
        self.readers = {}
        self.nsem = 0
        self.cur = {}
        self.dma = {}
        self.seen = {e: {} for e in self.ENGS}
        self.pe_sems = set()
        self.latest = {}

    def _newsem(self, name):
        self.nsem += 1
        return self.stack.enter_context(self.nc.semaphore(f"s{self.nsem}_{name}"))

    def _tok_compute(self, eng):
        c = self.cur.get(eng)
        if c is None or c[1] >= self.ROT:
            c = [self._newsem(eng), 0]
            self.cur[eng] = c
            if eng == "pe":
                self.pe_sems.add(id(c[0]))
        c[1] += 1
        return (c[0], c[1])

    def _tok_dma(self, key):
        c = self.dma.get(key)
        if c is None or c[1] >= 16 * 1500:
            c = [self._newsem("d"), 0]
            self.dma[key] = c
        c[1] += 16
        return (c[0], c[1])

    def op(self, eng, fn, reads=(), writes=(), dma_key=None, extra=()):
        deps = {}

        def add(tok):
            if tok is None:
                return
            s, v = tok
            k = id(s)
            if k not in deps or deps[k][1] < v:
                deps[k] = (s, v)

        for r in reads:
            add(self.last_write.get(r))
        for w in writes:
            add(self.last_write.get(w))
            for t in self.readers.get(w, ()):
                add(t)
        for t in extra:
            add(t)
        if fn is None:
            tok = None
        elif dma_key is not None:
            tok = self._tok_dma(dma_key)
        else:
            tok = self._tok_compute(eng)
        waits = []
        seen = self.seen[eng]
        for k, (s, v) in deps.items():
            if eng == "pe" and dma_key is None and k in self.pe_sems:
                continue
            if seen.get(k, 0) >= v:
                continue
            seen[k] = v
            waits.append((s, v))
        if tok is not None:
            self.latest[id(tok[0])] = tok
            for r in reads:
                lst = self.readers.setdefault(r, [])
                lst.append(tok)
                if len(lst) > 16:
                    best = {}
                    for (s, v) in lst:
                        if id(s) not in best or best[id(s)][1] < v:
                            best[id(s)] = (s, v)
                    self.readers[r] = list(best.values())
            for w in writes:
                self.last_write[w] = tok
                self.readers[w] = []
        self.ops[eng].append((fn, waits, tok, dma_key is not None))
        return tok

    def barrier(self):
        toks = list(self.latest.values())
        for e in self.ENGS:
            self.op(e, None, extra=toks)

    def emit(self):
        nc = self.nc
        ops = self.ops
        self.ops = {e: [] for e in self.ENGS}
        with nc.Block() as block:
            def run(engobj, name):
                for fn, waits, tok, is_dma in ops[name]:
                    for s, v in waits:
                        engobj.wait_ge(s, v)
                    if fn is not None:
                        ins = fn(engobj)
                        ins.then_inc(tok[0], 16 if is_dma else 1)

            @block.tensor
            def _(e):
                run(e, "pe")

            @block.scalar
            def _(e):
                run(e, "act")

            @block.vector
            def _(e):
                run(e, "dve")

            @block.gpsimd
            def _(e):
                run(e, "pool")

            @block.sync
            def _(e):
                run(e, "sp")


C_RQ, C_RK, C_RV, C_RG, C_DQ, C_DKV, C_IQ, C_IK, C_IW = 0, 512, 1024, 2048, 3072, 3584, 3712, 4224, 4288
D_IN = 4296


def build(NG, dbg=False):
    NT = 4 * NG
    SB = NT * 128
    NO = NG * 128
    nc = bass.Bass("TRN2", target_bir_lowering=False)

    def din(name, shape, dt=F32):
        return nc.dram_tensor(name, list(shape), dt, kind="ExternalInput").ap()

    def dscr(name, shape, dt):
        if dbg:
            return nc.dram_tensor(name, list(shape), dt, kind="ExternalOutput").ap()
        return nc.dram_tensor(name, list(shape), dt).ap()

    xb = din("xb", [SB, D]); xo = din("xo", [NO, D]); ccol = din("ccol", [128, 8])
    w_ada = din("w_ada", [D, 6 * D]); b_adaT = din("b_adaT", [128, 48]); b_g12 = din("b_g12", [128, 2 * D])
    gmixT = din("gmixT", [128, 8]); gffnT = din("gffnT", [128, 8])
    gfin_rep = din("gfin_rep", [128, D]); gn_rep = din("gn_rep", [128, D]); kvg_rep = din("kvg_rep", [128, 128])
    w_in = din("w_in", [D, D_IN]); w_kv = din("w_kv", [128, 128])
    w_ret_out = din("w_ret_out", [D, D]); w_dsa_out = din("w_dsa_out", [512, D])
    w_gate = din("w_gate", [D, 2 * D]); b_gate = din("b_gate", [1, 2 * D]); w_o = din("w_o", [D, D])
    w_rt = din("w_rt", [D, 20]); b_rt_rep = din("b_rt_rep", [128, 20])
    w1 = din("w1", [16, D, 256]); w3 = din("w3", [16, D, 256]); w2 = din("w2", [16, 256, D])
    rope_all = din("rope_all", [SB, 64]); rope_own = din("rope_own", [NO, 64])
    ident = din("ident", [128, 128]); DTd = din("DT", [128, H, 128]); qdec = din("qdec", [128, H])
    wtile = din("wtile", [128, H]); seld = din("sel", [64, 4]); maddd = din("madd", [128, 512])
    biasTd = din("biasT", [5, 128, H, 128]); c15 = din("c15_rep", [128, H]); seldend = din("selden", [65, 64])
    out = nc.dram_tensor("out", [NO, D], F32, kind="ExternalOutput").ap()

    uT_s = dscr("uT_s", [NG, 128, 8, 128], BF16)
    roT_s = dscr("roT_s", [NG, 128, 8, 128], BF16)
    dqT_s = dscr("dqT_s", [NG, 64, H, 128], BF16)
    iqT_s = dscr("iqT_s", [NG, 64, H, 128], BF16)
    iw_s = dscr("iw_s", [NG, 128, H], F32)
    ydT_s = dscr("ydT_s", [NG, 64, H, 128], BF16)
    h1_s = dscr("h1_s", [NG, 128, D], F32)
    if dbg:
        kT_dbg = dscr("kT_dbg", [128, SB], BF16)
        dv_dbg = dscr("dv_dbg", [128, NT, 65], BF16)
        thr_dbg = dscr("thr_dbg", [NG, 128, 8], F32)
        sc_dbg = dscr("sc_dbg", [NG, 128, SB], BF16)
        mk_dbg = dscr("mk_dbg", [NG, 128, SB], BF16)

    with contextlib.ExitStack() as top:
        S = Sched(nc, top)

        def sbt(st, n, sh, dt):
            return st.enter_context(nc.sbuf_tensor("sb_" + n, list(sh), dt))

        def pst(st, n, sh, dt):
            return st.enter_context(nc.psum_tensor("ps_" + n, list(sh), dt))

        V = lambda fn, r, w: S.op("dve", fn, reads=r, writes=w)
        A = lambda fn, r, w: S.op("act", fn, reads=r, writes=w)
        P = lambda fn, r, w: S.op("pe", fn, reads=r, writes=w)
        G = lambda fn, r, w: S.op("pool", fn, reads=r, writes=w)
        DM = lambda fn, r, w, k: S.op("sp", fn, reads=r, writes=w, dma_key=k)
        DG = lambda fn, r, w, k: S.op("pool", fn, reads=r, writes=w, dma_key=k)

        idb = sbt(top, "idb", [128, 128], BF16)
        epsc = sbt(top, "epsc", [128, 2], F32)
        G1T = sbt(top, "G1T", [128, 8], F32); sh1T = sbt(top, "sh1T", [128, 8], F32)
        G2T = sbt(top, "G2T", [128, 8], F32); sh2T = sbt(top, "sh2T", [128, 8], F32)
        g1rep = sbt(top, "g1rep", [128, D], BF16); g2rep = sbt(top, "g2rep", [128, D], BF16)
        DG(lambda e: e.dma_start(out=idb[:], in_=ident), [], ["idb"], "c0")
        V(lambda e: e.memset(epsc[:, 0:1], 1e-6), [], ["epsc"])
        V(lambda e: e.memset(epsc[:, 1:2], 1e-5), [], ["epsc"])

        def norm_T(x_ap, xres, junk, xs, pT, uT, stat, GT, shT, tag, ures=None):
            ures = ures or (tag + "uT")
            A(lambda e: e.activation(out=junk, in_=x_ap, func=AF.Square, scale=1.0 / 32.0, accum_out=stat[:, 0:1]),
              [xres], [tag + "junk", tag + "st"])
            A(lambda e: e.activation(out=stat[:, 1:2], in_=stat[:, 0:1], func=AF.Ln, bias=epsc[:, 0:1]),
              [tag + "st", "epsc"], [tag + "st"])
            A(lambda e: e.activation(out=stat[:, 2:3], in_=stat[:, 1:2], func=AF.Exp, scale=-0.5),
              [tag + "st"], [tag + "st"])
            V(lambda e: e.tensor_scalar(out=xs, in0=x_ap, scalar1=stat[:, 2:3], scalar2=None, op0=ALU.mult),
              [xres, tag + "st"], [tag + "xs"])
            for k in range(8):
                P(lambda e, k=k: e.transpose(out=pT[:, k, :], in_=xs[:, k * 128:(k + 1) * 128], identity=idb[:]),
                  [tag + "xs", "idb"], ["pT"])
            for k in range(8):
                V(lambda e, k=k: e.tensor_scalar(out=uT[:, k, :], in0=pT[:, k, :], scalar1=GT[:, k:k + 1],
                                                 scalar2=shT[:, k:k + 1], op0=ALU.mult, op1=ALU.add),
                  ["pT", "mod"], [ures])

        def rope(src, cosb, sinb, dst, ta, tb, rres, wres, tag):
            s1, s2 = src[:, :, 0:32], src[:, :, 32:64]
            V(lambda e: e.tensor_tensor(out=ta, in0=s1, in1=cosb, op=ALU.mult), rres, [tag + "ta"])
            V(lambda e: e.tensor_tensor(out=tb, in0=s2, in1=sinb, op=ALU.mult), rres, [tag + "tb"])
            V(lambda e: e.tensor_tensor(out=dst[:, :, 0:32], in0=ta, in1=tb, op=ALU.subtract), [tag + "ta", tag + "tb"], wres)
            V(lambda e: e.tensor_tensor(out=ta, in0=s2, in1=cosb, op=ALU.mult), rres, [tag + "ta"])
            V(lambda e: e.tensor_tensor(out=tb, in0=s1, in1=sinb, op=ALU.mult), rres, [tag + "tb"])
            V(lambda e: e.tensor_tensor(out=dst[:, :, 32:64], in0=ta, in1=tb, op=ALU.add), [tag + "ta", tag + "tb"], wres)

        with contextlib.ExitStack() as ph:
            cs = sbt(ph, "cs", [128, 8], F32); csb = sbt(ph, "csb", [128, 8], BF16)
            csbc = sbt(ph, "csbc", [128, 8, 128], BF16)
            wa = [sbt(ph, f"wa{i}", [128, 8, D], BF16) for i in range(2)]
            badT = sbt(ph, "badT", [128, 48], F32); bg12 = sbt(ph, "bg12", [128, 2 * D], F32)
            gmx = sbt(ph, "gmx", [128, 8], F32); gff = sbt(ph, "gff", [128, 8], F32)
            modT = sbt(ph, "modT", [128, 32], F32)
            psm = pst(ph, "psm", [128, 32], F32)
            pg = pst(ph, "pg", [128, D], F32)
            DM(lambda e: e.dma_start(out=cs[:], in_=ccol), [], ["cs"], "c1_cs")
            DM(lambda e: e.dma_start(out=badT[:], in_=b_adaT), [], ["badT"], "c1_badT")
            DM(lambda e: e.dma_start(out=bg12[:], in_=b_g12), [], ["bg12"], "c1_bg12")
            DM(lambda e: e.dma_start(out=gmx[:], in_=gmixT), [], ["gmx"], "c1_gmx")
            DM(lambda e: e.dma_start(out=gff[:], in_=gffnT), [], ["gff"], "c1_gff")
            A(lambda e: e.activation(out=csb[:], in_=cs[:], func=AF.Silu), ["cs"], ["csb"])
            V(lambda e: e.tensor_copy(out=csbc[:], in_=csb[:].unsqueeze(2).broadcast_to([128, 8, 128])), ["csb"], ["csbc"])
            fm_i = 0
            for p in range(6):
                sl = p % 2
                for k in range(8):
                    DG(lambda e, p=p, k=k, sl=sl: e.dma_start(out=wa[sl][:, k, :], in_=w_ada[k * 128:(k + 1) * 128, p * D:(p + 1) * D]),
                       [], [f"wa{sl}"], f"wa{sl}")
                if p in (2, 5):
                    for hf in range(2):
                        for k in range(8):
                            P(lambda e, k=k, hf=hf, sl=sl: e.matmul(pg[:, hf * 512:(hf + 1) * 512], lhsT=csbc[:, k, :],
                                                                  rhs=wa[sl][:, k, hf * 512:(hf + 1) * 512], start=(k == 0), stop=(k == 7)),
                              ["csbc", f"wa{sl}"], ["pg"])
                    dst = g1rep if p == 2 else g2rep
                    off = 0 if p == 2 else D
                    V(lambda e, dst=dst, off=off: e.tensor_tensor(out=dst[:], in0=pg[:], in1=bg12[:, off:off + D], op=ALU.add),
                      ["pg", "bg12"], ["mod"])
                else:
                    for f in range(8):
                        for k in range(8):
                            P(lambda e, k=k, f=f, sl=sl, c=fm_i * 8 + f: e.matmul(psm[:, c:c + 1], lhsT=wa[sl][:, k, f * 128:(f + 1) * 128],
                                                                                 rhs=csb[:, k:k + 1], start=(k == 0), stop=(k == 7)),
                              ["csb", f"wa{sl}"], ["psm"])
                    fm_i += 1
            for i, p in enumerate((0, 1, 3, 4)):
                V(lambda e, i=i, p=p: e.tensor_tensor(out=modT[:, i * 8:(i + 1) * 8], in0=psm[:, i * 8:(i + 1) * 8],
                                                      in1=badT[:, p * 8:(p + 1) * 8], op=ALU.add), ["psm", "badT"], ["modT"])
            V(lambda e: e.tensor_copy(out=sh1T[:], in_=modT[:, 0:8]), ["modT"], ["mod"])
            V(lambda e: e.tensor_copy(out=sh2T[:], in_=modT[:, 16:24]), ["modT"], ["mod"])
            V(lambda e: e.scalar_tensor_tensor(out=G1T[:], in0=modT[:, 8:16], scalar=1.0, in1=gmx[:], op0=ALU.add, op1=ALU.mult),
              ["modT", "gmx"], ["mod"])
            V(lambda e: e.scalar_tensor_tensor(out=G2T[:], in0=modT[:, 24:32], scalar=1.0, in1=gff[:], op0=ALU.add, op1=ALU.mult),
              ["modT", "gff"], ["mod"])
            S.barrier()
            S.emit()

        kvs = contextlib.ExitStack()
        kT = sbt(kvs, "kT", [128, SB], BF16)
        dv1 = sbt(kvs, "dv1", [128, NT, 65], BF16)

        with contextlib.ExitStack() as ph:
            w_in_sb = sbt(ph, "w_in_sb", [128, 8, D_IN], BF16)
            wkv_sb = sbt(ph, "wkv_sb", [128, 192], BF16)
            kvg = sbt(ph, "kvg", [128, 128], F32); gnr = sbt(ph, "gnr", [128, D], F32)
            DTs = sbt(ph, "DTs", [128, H, 128], BF16); qdc = sbt(ph, "qdc", [128, H], F32)
            wtl = sbt(ph, "wtl", [128, H], F32); sels = sbt(ph, "sels", [64, 4], F32)
            xt = [sbt(ph, f"xt{i}", [128, D], F32) for i in range(2)]
            rp = [sbt(ph, f"rp{i}", [128, 64], F32) for i in range(2)]
            junk = sbt(ph, "junk", [128, D], BF16)
            xs = sbt(ph, "xs", [128, D], BF16)
            uT = [sbt(ph, f"uT{i}", [128, 8, 128], BF16) for i in range(2)]
            stat = sbt(ph, "stat", [128, 8], F32)
            ta = sbt(ph, "ta", [128, H, 32], F32); tb = sbt(ph, "tb", [128, H, 32], F32)
            krp = sbt(ph, "krp", [128, H, 64], F32); kw = sbt(ph, "kw", [128, H, 64], BF16)
            vb = sbt(ph, "vb", [128, D], BF16)
            Sst = sbt(ph, "Sst", [64, H, 128], F32); T0 = sbt(ph, "T0", [64, H, 128], F32)
            T0b = sbt(ph, "T0b", [64, H, 128], BF16)
            latn = sbt(ph, "latn", [128, 128], BF16); latnT = sbt(ph, "latnT", [128, 128], BF16)
            qr = sbt(ph, "qr", [128, H, 64], BF16); kro = sbt(ph, "kro", [128, H, 64], BF16)
            qT = sbt(ph, "qT", [64, H, 128], BF16); kTo = sbt(ph, "kTo", [64, H, 128], BF16)
            scm = sbt(ph, "scm", [128, H, 128], BF16)
            sg = sbt(ph, "sg", [128, D], BF16)
            oo = sbt(ph, "oo", [128, H, 128], F32); o2 = sbt(ph, "o2", [128, H, 128], F32)
            gst = sbt(ph, "gst", [128, 40], F32)
            ro = sbt(ph, "ro", [128, D], BF16); roT = sbt(ph, "roT", [128, 8, 128], BF16)
            dqb = sbt(ph, "dqb", [128, 512], BF16); iqb = sbt(ph, "iqb", [128, 512], BF16)
            dqT = sbt(ph, "dqT", [64, H, 128], BF16); iqT = dqT
            iwb = sbt(ph, "iwb", [128, H], F32)
            pT = pst(ph, "pT", [128, 8, 128], BF16)
            zA = pst(ph, "zA", [128, D], F32); zB = pst(ph, "zB", [128, D], F32)
            misc = pst(ph, "misc", [128, 512], F32)
            kvp = pst(ph, "kvp", [128, H, 128], F32)
            pTs = misc[:, 448:512].bitcast(BF16)

            for k in range(8):
                DG(lambda e, k=k: e.dma_start(out=w_in_sb[:, k, :], in_=w_in[k * 128:(k + 1) * 128, :]), [], ["w_in_sb"], "w_in")
            V(lambda e: e.memset(wkv_sb[:, 0:64], 0.0), [], ["wkv0"])
            DG(lambda e: e.dma_start(out=wkv_sb[:, 64:192], in_=w_kv), [], ["wkv1"], "c2_wkv")
            DG(lambda e: e.dma_start(out=DTs[:], in_=DTd), [], ["DTs"], "c2_DT")
            DM(lambda e: e.dma_start(out=kvg[:], in_=kvg_rep), [], ["kvg"], "c3_kvg")
            DM(lambda e: e.dma_start(out=gnr[:], in_=gn_rep), [], ["gnr"], "c3_gnr")
            DM(lambda e: e.dma_start(out=qdc[:], in_=qdec), [], ["qdc"], "c3_qdc")
            DM(lambda e: e.dma_start(out=wtl[:], in_=wtile), [], ["wtl"], "c3_wtl")
            DM(lambda e: e.dma_start(out=sels[:], in_=seld), [], ["sels"], "c3_sels")
            V(lambda e: e.memset(Sst[:], 0.0), [], ["Sst"])
            V(lambda e: e.memset(dv1[:, :, 64:65], 1.0), [], ["dv1one"])

            def kside_s1(pos, j):
                sl = pos % 2
                DM(lambda e: e.dma_start(out=xt[sl][:], in_=xb[j * 128:(j + 1) * 128, :]), [], [f"xt{sl}"], f"xt{sl}")
                DM(lambda e: e.dma_start(out=rp[sl][:], in_=rope_all[j * 128:(j + 1) * 128, :]), [], [f"rp{sl}"], f"rp{sl}")
                norm_T(xt[sl][:], f"xt{sl}", junk[:], xs[:], pT, uT[sl], stat, G1T, sh1T, "k", ures=f"kuT{sl}")

            def kside(pos, j):
                sl = pos % 2
                s = j % 4
                ures = [f"kuT{sl}", "w_in_sb"]
                for (dst, c0, n, res) in ((zA[:, 0:512], C_RK, 512, ("zA", 0)), (zB[:, 0:512], C_RV, 512, ("zB", 0)),
                                          (zB[:, 512:1024], C_RV + 512, 512, ("zB", 1)), (misc[:, 0:128], C_DKV, 128, "m_dkv")):
                    for k in range(8):
                        P(lambda e, k=k, dst=dst, c0=c0, n=n: e.matmul(dst, lhsT=uT[sl][:, k, :], rhs=w_in_sb[:, k, c0:c0 + n],
                                                                      start=(k == 0), stop=(k == 7)), ures, [res])
                for k in range(8):
                    P(lambda e, k=k: e.matmul(misc[0:64, 128:256], lhsT=w_in_sb[:, k, C_IK:C_IK + 64], rhs=uT[sl][:, k, :],
                                              start=(k == 0), stop=(k == 7)), ures, ["m_ik"])
                cosb = rp[sl][:, 0:32].unsqueeze(1).broadcast_to([128, H, 32])
                sinb = rp[sl][:, 32:64].unsqueeze(1).broadcast_to([128, H, 32])
                rope(zA[:, 0:512].rearrange("p (h d) -> p h d", h=H), cosb, sinb, krp, ta[:], tb[:], [("zA", 0), f"rp{sl}"], ["krp"], "r")
                V(lambda e: e.tensor_tensor(out=kw[:], in0=krp[:], in1=wtl[:].unsqueeze(2).broadcast_to([128, H, 64]), op=ALU.mult),
                  ["krp", "wtl"], ["kw"])
                A(lambda e: e.activation(out=vb[:], in_=zB[:], func=AF.Copy), [("zB", 0), ("zB", 1)], ["vb"])
                A(lambda e: e.activation(out=junk[:, 0:128], in_=misc[:, 0:128], func=AF.Square, scale=1.0 / math.sqrt(128.0),
                                         accum_out=stat[:, 4:5]), ["m_dkv"], ["kjunk", "st2"])
                A(lambda e: e.activation(out=stat[:, 5:6], in_=stat[:, 4:5], func=AF.Ln, bias=epsc[:, 0:1]), ["st2", "epsc"], ["st2"])
                A(lambda e: e.activation(out=stat[:, 6:7], in_=stat[:, 5:6], func=AF.Exp, scale=-0.5), ["st2"], ["st2"])
                V(lambda e: e.scalar_tensor_tensor(out=latn[:], in0=misc[:, 0:128], scalar=stat[:, 6:7], in1=kvg[:], op0=ALU.mult, op1=ALU.mult),
                  ["m_dkv", "st2", "kvg"], ["latn"])
                P(lambda e: e.transpose(out=pTs, in_=latn[:], identity=idb[:]), ["latn", "idb"], ["pTs"])
                A(lambda e: e.activation(out=latnT[:], in_=pTs, func=AF.Copy), ["pTs"], ["latnT"])
                P(lambda e: e.matmul(misc[:, 256:384], lhsT=wkv_sb[:, 0:128], rhs=latnT[:], start=True, stop=True),
                  ["latnT", "wkv0", "wkv1"], ["m_dk"])
                P(lambda e: e.matmul(misc[:, 384:448], lhsT=latnT[:], rhs=wkv_sb[:, 128:192], start=True, stop=True),
                  ["latnT", "wkv1"], ["m_dv"])
                A(lambda e: e.activation(out=kT[0:64, j * 128:(j + 1) * 128], in_=misc[0:64, 128:256], func=AF.Copy), ["m_ik"], [("kT", j)])
                A(lambda e: e.activation(out=kT[64:128, j * 128:(j + 1) * 128], in_=misc[64:128, 256:384], func=AF.Copy), ["m_dk"], [("kT", j)])
                V(lambda e: e.tensor_copy(out=dv1[:, j, 0:64], in_=misc[:, 384:448]), ["m_dv"], [("dv1", j)])
                if s == 0:
                    V(lambda e: e.tensor_scalar(out=T0[:], in0=Sst[:], scalar1=sels[:, 0:1], scalar2=None, op0=ALU.mult),
                      ["Sst", "sels"], ["T0"])
                else:
                    V(lambda e: e.scalar_tensor_tensor(out=T0[:], in0=Sst[:], scalar=sels[:, s:s + 1], in1=T0[:], op0=ALU.mult, op1=ALU.add),
                      ["Sst", "sels", "T0"], ["T0"])
                for h in range(H):
                    P(lambda e, h=h: e.matmul(kvp[0:64, h, :], lhsT=kw[:, h, :], rhs=vb[:, h * 128:(h + 1) * 128], start=True, stop=True),
                      ["kw", "vb"], [("kvp", h // 4)])
                for h in range(H):
                    V(lambda e, h=h: e.scalar_tensor_tensor(out=Sst[:, h, :], in0=Sst[:, h, :], scalar=float(GAM[h] ** 128),
                                                           in1=kvp[0:64, h, :], op0=ALU.mult, op1=ALU.add),
                      ["Sst", ("kvp", h // 4)], ["Sst"])

            def ownside_s1(pos, g):
                sl = pos % 2
                DM(lambda e: e.dma_start(out=xt[sl][:], in_=xo[g * 128:(g + 1) * 128, :]), [], [f"xt{sl}"], f"xt{sl}")
                DM(lambda e: e.dma_start(out=rp[sl][:], in_=rope_own[g * 128:(g + 1) * 128, :]), [], [f"rp{sl}"], f"rp{sl}")
                norm_T(xt[sl][:], f"xt{sl}", junk[:], xs[:], pT, uT[sl], stat, G1T, sh1T, "k", ures=f"kuT{sl}")
                DM(lambda e: e.dma_start(out=uT_s[g], in_=uT[sl][:]), [f"kuT{sl}"], [("uT_s", g)], "st_u")

            def ownside(pos, g):
                sl = pos % 2
                ures = [f"kuT{sl}", "w_in_sb"]
                u = uT[sl]

                def proj(dst, c0, n, res):
                    for k in range(8):
                        P(lambda e, k=k: e.matmul(dst, lhsT=u[:, k, :], rhs=w_in_sb[:, k, c0:c0 + n], start=(k == 0), stop=(k == 7)), ures, [res])

                cosb = rp[sl][:, 0:32].unsqueeze(1).broadcast_to([128, H, 32])
                sinb = rp[sl][:, 32:64].unsqueeze(1).broadcast_to([128, H, 32])
                proj(zA[:, 0:512], C_RQ, 512, ("zA", 0))
                proj(zA[:, 512:1024], C_RK, 512, ("zA", 1))
                proj(zB[:, 0:512], C_DQ, 512, ("zB", 0))
                proj(zB[:, 512:1024], C_IQ, 512, ("zB", 1))
                proj(misc[:, 0:8], C_IW, 8, "m_dkv")
                rope(zA[:, 0:512].rearrange("p (h d) -> p h d", h=H), cosb, sinb, qr, ta[:], tb[:], [("zA", 0), f"rp{sl}"], ["qr"], "r")
                rope(zA[:, 512:1024].rearrange("p (h d) -> p h d", h=H), cosb, sinb, kro, ta[:], tb[:], [("zA", 1), f"rp{sl}"], ["kro"], "r")
                pT64 = pT[0:64, :, :]
                for h in range(H):
                    P(lambda e, h=h: e.transpose(out=pT64[:, h, :], in_=qr[:, h, :], identity=idb[:]), ["qr", "idb"], ["pT"])
                V(lambda e: e.tensor_copy(out=qT[:], in_=pT64), ["pT"], ["qT"])
                for h in range(H):
                    P(lambda e, h=h: e.transpose(out=pT64[:, h, :], in_=kro[:, h, :], identity=idb[:]), ["kro", "idb"], ["pT"])
                A(lambda e: e.activation(out=kTo[:], in_=pT64, func=AF.Copy), ["pT"], ["kTo"])
                V(lambda e: e.tensor_copy(out=dqb[:], in_=zB[:, 0:512]), [("zB", 0)], ["dqb"])
                A(lambda e: e.activation(out=iqb[:], in_=zB[:, 512:1024], func=AF.Copy), [("zB", 1)], ["iqb"])
                V(lambda e: e.tensor_copy(out=iwb[:], in_=misc[:, 0:8]), ["m_dkv"], ["iwb"])
                DM(lambda e: e.dma_start(out=iw_s[g], in_=iwb[:]), ["iwb"], [("iw_s", g)], "st_w")
                for h in range(H):
                    P(lambda e, h=h: e.transpose(out=pT64[:, h, :], in_=dqb[:, h * 64:(h + 1) * 64], identity=idb[:]), ["dqb", "idb"], ["pT"])
                V(lambda e: e.tensor_copy(out=dqT[:], in_=pT64), ["pT"], ["dqT"])
                DM(lambda e: e.dma_start(out=dqT_s[g], in_=dqT[:]), ["dqT"], [("dqT_s", g)], "st_q")
                for h in range(H):
                    P(lambda e, h=h: e.transpose(out=pT64[:, h, :], in_=iqb[:, h * 64:(h + 1) * 64], identity=idb[:]), ["iqb", "idb"], ["pT"])
                A(lambda e: e.activation(out=iqT[:], in_=pT64, func=AF.Copy), ["pT"], ["dqT"])
                DM(lambda e: e.dma_start(out=iqT_s[g], in_=iqT[:]), ["dqT"], [("iqT_s", g)], "st_i")
                proj(zB[:, 0:512], C_RV, 512, ("zB", 0))
                proj(zB[:, 512:1024], C_RV + 512, 512, ("zB", 1))
                A(lambda e: e.activation(out=vb[:], in_=zB[:], func=AF.Copy), [("zB", 0), ("zB", 1)], ["vb"])
                zA3 = zA[:].rearrange("p (h d) -> p h d", h=H)
                for h in range(H):
                    P(lambda e, h=h: e.matmul(zA3[:, h, :], lhsT=kTo[:, h, :], rhs=qT[:, h, :], start=True, stop=True),
                      ["kTo", "qT"], [("zA", h // 4)])
                V(lambda e: e.tensor_tensor(out=scm[:], in0=zA3, in1=DTs[:], op=ALU.mult), [("zA", 0), ("zA", 1), "DTs"], ["scm"])
                zB3 = zB[:].rearrange("p (h d) -> p h d", h=H)
                for h in range(H):
                    P(lambda e, h=h: e.matmul(zB3[:, h, :], lhsT=scm[:, h, :], rhs=vb[:, h * 128:(h + 1) * 128], start=True, stop=True),
                      ["scm", "vb"], [("zB", h // 4)])
                V(lambda e: e.tensor_copy(out=T0b[:], in_=T0[:]), ["T0"], ["T0b"])
                for h in range(H):
                    P(lambda e, h=h: e.matmul(kvp[:, h, :], lhsT=qT[:, h, :], rhs=T0b[:, h, :], start=True, stop=True),
                      ["qT", "T0b"], [("kvp", h // 4)])
                V(lambda e: e.tensor_tensor(out=oo[:], in0=kvp[:], in1=qdc[:].unsqueeze(2).broadcast_to([128, H, 128]), op=ALU.mult),
                  [("kvp", 0), ("kvp", 1), "qdc"], ["oo"])
                V(lambda e: e.tensor_tensor(out=oo[:], in0=oo[:], in1=zB3, op=ALU.add), ["oo", ("zB", 0), ("zB", 1)], ["oo"])
                V(lambda e: e.tensor_reduce(out=gst[:, 0:8], in_=oo[:], axis=AX.X, op=ALU.add), ["oo"], ["gst"])
                V(lambda e: e.tensor_tensor(out=o2[:], in0=oo[:], in1=oo[:], op=ALU.mult), ["oo"], ["o2"])
                V(lambda e: e.tensor_reduce(out=gst[:, 8:16], in_=o2[:], axis=AX.X, op=ALU.add), ["o2"], ["gst"])
                V(lambda e: e.tensor_scalar(out=gst[:, 16:24], in0=gst[:, 0:8], scalar1=1.0 / 128.0, scalar2=None, op0=ALU.mult), ["gst"], ["gst"])
                V(lambda e: e.tensor_tensor(out=gst[:, 24:32], in0=gst[:, 16:24], in1=gst[:, 16:24], op=ALU.mult), ["gst"], ["gst"])
                V(lambda e: e.scalar_tensor_tensor(out=gst[:, 24:32], in0=gst[:, 8:16], scalar=1.0 / 128.0, in1=gst[:, 24:32],
                                                   op0=ALU.mult, op1=ALU.subtract), ["gst"], ["gst"])
                A(lambda e: e.activation(out=gst[:, 32:40], in_=gst[:, 24:32], func=AF.Ln, bias=epsc[:, 1:2]), ["gst", "epsc"], ["gst2"])
                A(lambda e: e.activation(out=gst[:, 32:40], in_=gst[:, 32:40], func=AF.Exp, scale=-0.5), ["gst2"], ["gst2"])
                for h in range(H):
                    V(lambda e, h=h: e.tensor_scalar(out=o2[:, h, :], in0=oo[:, h, :], scalar1=gst[:, 16 + h:17 + h], scalar2=gst[:, 32 + h:33 + h],
                                                     op0=ALU.subtract, op1=ALU.mult), ["oo", "gst", "gst2"], ["o2"])
                proj(zA[:, 0:512], C_RG, 512, ("zA", 0))
                proj(zA[:, 512:1024], C_RG + 512, 512, ("zA", 1))
                A(lambda e: e.activation(out=sg[:], in_=zA[:], func=AF.Silu), [("zA", 0), ("zA", 1)], ["sg"])
                o2f = o2[:].rearrange("p h d -> p (h d)")
                V(lambda e: e.tensor_tensor(out=o2f, in0=o2f, in1=gnr[:], op=ALU.mult), ["o2", "gnr"], ["o2"])
                V(lambda e: e.tensor_tensor(out=ro[:], in0=o2f, in1=sg[:], op=ALU.mult), ["o2", "sg"], ["ro"])
                for k in range(8):
                    P(lambda e, k=k: e.transpose(out=pT[:, k, :], in_=ro[:, k * 128:(k + 1) * 128], identity=idb[:]), ["ro", "idb"], ["pT"])
                A(lambda e: e.activation(out=roT[:], in_=pT[:], func=AF.Copy), ["pT"], ["roT"])
                DM(lambda e: e.dma_start(out=roT_s[g], in_=roT[:]), ["roT"], [("roT_s", g)], "st_r")

            items = []
            for g in range(NG):
                for s4 in range(4):
                    items.append(("k", 4 * g + s4))
                items.append(("o", g))

            def stage1(pos):
                kind, idx = items[pos]
                (kside_s1 if kind == "k" else ownside_s1)(pos, idx)

            stage1(0)
            for pos in range(len(items)):
                if pos + 1 < len(items):
                    stage1(pos + 1)
                kind, idx = items[pos]
                (kside if kind == "k" else ownside)(pos, idx)
            if dbg:
                DM(lambda e: e.dma_start(out=kT_dbg, in_=kT[:]), [("kT", j) for j in range(NT)], ["kT_dbg"], "dbg")
                DM(lambda e: e.dma_start(out=dv_dbg, in_=dv1[:]), [("dv1", j) for j in range(NT)] + ["dv1one"], ["dv_dbg"], "dbg")
            S.barrier()
            S.emit()

        PHASES = build.phases
        if PHASES >= 2:
          with contextlib.ExitStack() as ph:
            score2 = [sbt(ph, f"score{i}", [128, SB], BF16) for i in range(2)]
            msk = sbt(ph, "msk", [128, SB], BF16)
            mskT = [sbt(ph, f"mskT{i}", [128, 8, 128], BF16) for i in range(2)]
            iq_sb2 = [sbt(ph, f"iq_sb{i}", [128, H, 128], BF16) for i in range(2)]
            dq_sb2 = [sbt(ph, f"dq_sb{i}", [128, H, 128], BF16) for i in range(2)]
            iw_sb2 = [sbt(ph, f"iw_sb{i}", [128, H], F32) for i in range(2)]
            dg2 = [sbt(ph, f"dg{i}", [128, H, 128], BF16) for i in range(2)]
            maddb = sbt(ph, "maddb", [128, 512], BF16)
            rh = [sbt(ph, f"rh{i}", [128, 512], BF16) for i in range(4)]
            madd = sbt(ph, "madd", [128, 512], F32)
            bT = sbt(ph, "bT", [128, 5, H, 128], BF16)
            c15s = sbt(ph, "c15s", [128, H], F32)
            selden = sbt(ph, "selden", [65, 64], F32)
            bst = sbt(ph, "bst", [128, 8], F32)
            steps = sbt(ph, "steps", [128, NBIS], F32)
            pmx = [sbt(ph, f"pmx{i}", [128, 16], F32) for i in range(2)]
            pmn = [sbt(ph, f"pmn{i}", [128, 16], F32) for i in range(2)]
            bx = sbt(ph, "bx", [128, 2], F32)
            halves = sbt(ph, "halves", [128, NBIS], F32)
            tmpl = sbt(ph, "tmpl", [128, 512], F32)
            lg = sbt(ph, "lg", [128, H, 128], F32)
            pe_ = [sbt(ph, f"pe{i}", [128, H, 128], BF16) for i in range(2)]
            pm = [sbt(ph, f"pm{i}", [128, H, 128], BF16) for i in range(3)]
            OT = sbt(ph, "OT", [65, H * 128], F32)
            ydT = sbt(ph, "ydT", [64, H, 128], BF16)
            PP = [pst(ph, f"PP{i}", [128, 1024], F32) for i in range(4)]
            ps_s = [PP[k // 2][:, (k % 2) * 512:(k % 2 + 1) * 512] for k in range(4)]
            ps_s_res = [(f"PP{k // 2}", k % 2) for k in range(4)]
            ps_sc = [PP[2][:, 0:512], PP[2][:, 512:1024]]
            ps_sc_res = [("PP2", 0), ("PP2", 1)]
            ps_qk = [PP[0], PP[1]]
            ps_o = PP[2]
            ps_mT = PP[3][:, 0:512].bitcast(BF16).rearrange("p (a b) -> p a b", a=8)

            DM(lambda e: e.dma_start(out=madd[:], in_=maddd), [], ["madd"], "c4_madd")
            DG(lambda e: e.dma_start(out=bT[:], in_=biasTd.rearrange("s p h q -> p s h q")), [], ["bT"], "c4_bT")
            DM(lambda e: e.dma_start(out=c15s[:], in_=c15), [], ["c15s"], "c4_c15s")
            DM(lambda e: e.dma_start(out=selden[:], in_=seldend), [], ["selden"], "c4_selden")
            for s5 in range(5):
                V(lambda e, s5=s5: e.tensor_tensor(out=bT[:, s5, :, :], in0=bT[:, s5, :, :],
                                                   in1=c15s[:].unsqueeze(2).broadcast_to([128, H, 128]), op=ALU.subtract),
                  ["bT", "c15s"], ["bT"])
            for i in range(NBIS):
                V(lambda e, i=i: e.memset(halves[:, i:i + 1], 2.0 ** (-(i + 1))), [], ["halves"])
            for i in range(2):
                G(lambda e, i=i: e.memset(iq_sb2[i][64:128, :, :], 0.0), [], [f"iq_sb{i}"])
                G(lambda e, i=i: e.memset(dq_sb2[i][0:64, :, :], 0.0), [], [f"dq_sb{i}"])
            V(lambda e: e.tensor_copy(out=maddb[:], in_=madd[:]), ["madd"], ["maddb"])

            def load_q(g):
                sl = g % 2
                DM(lambda e: e.dma_start(out=iq_sb2[sl][0:64, :, :], in_=iqT_s[g]), [("iqT_s", g)], [f"iq_sb{sl}"], f"ld_i{sl}")
                DM(lambda e: e.dma_start(out=dq_sb2[sl][64:128, :, :], in_=dqT_s[g]), [("dqT_s", g)], [f"dq_sb{sl}"], f"ld_q{sl}")
                DM(lambda e: e.dma_start(out=iw_sb2[sl][:], in_=iw_s[g]), [("iw_s", g)], [f"iw_sb{sl}"], f"ld_w{sl}")
                for h in range(H):
                    V(lambda e, h=h: e.tensor_scalar(out=dg2[sl][:, h, :], in0=idb[:], scalar1=iw_sb2[sl][:, h:h + 1], scalar2=None, op0=ALU.mult),
                      ["idb", f"iw_sb{sl}"], [f"dg{sl}"])

            def indexer(g):
                sl = g % 2
                iq_sb = iq_sb2[sl]; dg = dg2[sl]; score = score2[sl]
                iqres = f"iq_sb{sl}"; dgres = f"dg{sl}"
                M = 8 * (g + 1)

                def smm(i):
                    c, h = i // 8, i % 8
                    kres = [("kT", 4 * c + t) for t in range(4)]
                    P(lambda e: e.matmul(ps_s[i % 4], lhsT=iq_sb[:, h, :], rhs=kT[:, c * 512:(c + 1) * 512], start=True, stop=True),
                      [iqres] + kres, [ps_s_res[i % 4]])

                def rl_dmm(i):
                    c, h = i // 8, i % 8
                    r4 = i % 4
                    A(lambda e: e.activation(out=rh[r4][:], in_=ps_s[r4], func=AF.Relu), [ps_s_res[r4]], [f"rh{r4}"])
                    last = (c == g)
                    P(lambda e: e.matmul(ps_sc[c % 2], lhsT=dg[:, h, :], rhs=rh[r4][:], start=(h == 0), stop=(h == H - 1 and not last)),
                      [dgres, f"rh{r4}"], [ps_sc_res[c % 2]])
                    if h == H - 1:
                        if last:
                            P(lambda e: e.matmul(ps_sc[c % 2], lhsT=idb[:], rhs=maddb[:], start=False, stop=True), ["idb", "maddb"], [ps_sc_res[c % 2]])
                        A(lambda e: e.activation(out=score[:, c * 512:(c + 1) * 512], in_=ps_sc[c % 2], func=AF.Copy), [ps_sc_res[c % 2]], [(f"score{sl}", c)])

                smm(0)
                smm(1)
                for i_ in range(M):
                    if i_ + 2 < M:
                        smm(i_ + 2)
                    rl_dmm(i_)

            def mm_ops(G):
                slg = G % 2
                sc = score2[slg]
                ops_ = []
                npc = (G + 3) // 4
                for p in range(npc):
                    a, b_ = p * 2048, min((p + 1) * 2048, G * 512)
                    rr = [(f"score{slg}", c) for c in range(4 * p, min(4 * p + 4, G))]
                    ops_.append(lambda p=p, a=a, b_=b_, rr=rr: V(lambda e: e.tensor_reduce(out=pmx[slg][:, p:p + 1], in_=sc[:, a:b_], axis=AX.X, op=ALU.max),
                                                                   rr, [(f"pmx{slg}", p)]))
                    ops_.append(lambda p=p, a=a, b_=b_, rr=rr: V(lambda e: e.tensor_reduce(out=pmn[slg][:, p:p + 1], in_=sc[:, a:b_], axis=AX.X, op=ALU.min),
                                                                   rr, [(f"pmn{slg}", p)]))
                return ops_

            def bisect(g):
                sl = g % 2
                score = score2[sl]
                nk = (g + 1) * 512
                sres = [(f"score{sl}", c) for c in range(g + 1)]
                V(lambda e: e.scalar_tensor_tensor(out=tmpl[:], in0=madd[:], scalar=-2.0, in1=score[:, nk - 512:nk], op0=ALU.mult, op1=ALU.add),
                  [(f"score{sl}", g), "madd"], ["tmpl"])
                V(lambda e: e.tensor_reduce(out=bst[:, 2:3], in_=tmpl[:], axis=AX.X, op=ALU.min), ["tmpl"], ["bst2"])
                V(lambda e: e.tensor_reduce(out=bst[:, 0:1], in_=score[:, nk - 512:nk], axis=AX.X, op=ALU.max), [(f"score{sl}", g)], ["bst0"])
                if g > 0:
                    npc = (g + 3) // 4
                    V(lambda e: e.tensor_reduce(out=bx[:, 0:1], in_=pmx[sl][:, 0:npc], axis=AX.X, op=ALU.max), [(f"pmx{sl}", p) for p in range(npc)], ["bx0"])
                    V(lambda e: e.tensor_tensor(out=bst[:, 0:1], in0=bst[:, 0:1], in1=bx[:, 0:1], op=ALU.max), ["bst0", "bx0"], ["bst0"])
                    V(lambda e: e.tensor_reduce(out=bst[:, 1:2], in_=pmn[sl][:, 0:npc], axis=AX.X, op=ALU.min), [(f"pmn{sl}", p) for p in range(npc)], ["bst1"])
                    V(lambda e: e.tensor_tensor(out=bst[:, 3:4], in0=bst[:, 1:2], in1=bst[:, 2:3], op=ALU.min), ["bst1", "bst2"], ["bt"])
                else:
                    V(lambda e: e.tensor_copy(out=bst[:, 3:4], in_=bst[:, 2:3]), ["bst2"], ["bt"])
                V(lambda e: e.tensor_tensor(out=bst[:, 4:5], in0=bst[:, 0:1], in1=bst[:, 3:4], op=ALU.subtract), ["bst0", "bt"], ["bR"])
                V(lambda e: e.tensor_scalar(out=steps[:], in0=halves[:], scalar1=bst[:, 4:5], scalar2=None, op0=ALU.mult), ["halves", "bR"], ["steps"])
                for i in range(NBIS):
                    V(lambda e, i=i: e.tensor_tensor(out=bst[:, 5:6], in0=bst[:, 3:4], in1=steps[:, i:i + 1], op=ALU.add), ["bt", "steps"], ["bcand"])
                    V(lambda e: e.tensor_scalar(out=msk[:, 0:nk], in0=score[:, 0:nk], scalar1=bst[:, 5:6], scalar2=0.0, op0=ALU.is_ge, op1=ALU.add,
                                                accum_out=bst[:, 6:7]), sres + ["bcand"], ["msk", "bcnt"])
                    V(lambda e, i=i: e.tensor_scalar(out=bst[:, 7:8], in0=bst[:, 6:7], scalar1=255.5, scalar2=steps[:, i:i + 1], op0=ALU.is_ge, op1=ALU.mult),
                      ["bcnt", "steps"], ["binc"])
                    V(lambda e: e.tensor_tensor(out=bst[:, 3:4], in0=bst[:, 3:4], in1=bst[:, 7:8], op=ALU.add), ["bt", "binc"], ["bt"])
                V(lambda e: e.tensor_scalar(out=msk[:, 0:nk], in0=score[:, 0:nk], scalar1=bst[:, 3:4], scalar2=0.0, op0=ALU.is_ge, op1=ALU.add,
                                            accum_out=bst[:, 6:7]), sres + ["bt"], ["msk", "bcnt"])
                if dbg:
                    DM(lambda e: e.dma_start(out=thr_dbg[g], in_=bst[:, 0:8]), ["bt", "bcnt", "bR", "bcand", "bst0", "bst1", "bst2", "binc"], [("thr_dbg", g)], "dbg2")
                    DM(lambda e: e.dma_start(out=sc_dbg[g, :, 0:nk], in_=score[:, 0:nk]), sres, [("sc_dbg", g)], "dbg2")
                    DM(lambda e: e.dma_start(out=mk_dbg[g, :, 0:nk], in_=msk[:, 0:nk]), ["msk"], [("mk_dbg", g)], "dbg2")

            def attention(g, pend):
                sl = g % 2
                dq_sb = dq_sb2[sl]
                dqres = f"dq_sb{sl}"
                ntile = (g + 1) * 4

                def mask_blk(j0):
                    nj = min(8, ntile - j0)
                    for jj in range(nj):
                        j = j0 + jj
                        P(lambda e, j=j, jj=jj: e.transpose(out=ps_mT[:, jj, :], in_=msk[:, j * 128:(j + 1) * 128], identity=idb[:]),
                          ["msk", "idb"], [("PP3", 0)])
                    mb = (j0 // 8) % 2
                    if mb == 0:
                        A(lambda e, mb=mb, nj=nj: e.activation(out=mskT[mb][:, 0:nj, :], in_=ps_mT[:, 0:nj, :], func=AF.Copy), [("PP3", 0)], [f"mskT{mb}"])
                    else:
                        V(lambda e, mb=mb, nj=nj: e.tensor_copy(out=mskT[mb][:, 0:nj, :], in_=ps_mT[:, 0:nj, :]), [("PP3", 0)], [f"mskT{mb}"])
                def att_qk(j):
                    qb = j % 2
                    for hf in range(2):
                        P(lambda e, hf=hf: e.matmul(ps_qk[qb][:, hf * 512:(hf + 1) * 512], lhsT=kT[:, j * 128:(j + 1) * 128],
                                                    rhs=dq_sb[:, 4 * hf:4 * hf + 4, :], start=True, stop=True),
                          [("kT", j), dqres], [(f"PP{qb}", hf)])

                def att_sm(j):
                    b = j % 2
                    if j % 8 == 0:
                        mask_blk(j)
                    mb = (j // 8) % 2
                    slot = j - (4 * g - 1)
                    qk3 = ps_qk[b][:].rearrange("p (h q) -> p h q", h=H)
                    qres = [(f"PP{b}", 0), (f"PP{b}", 1)]
                    if slot >= 0:
                        V(lambda e: e.scalar_tensor_tensor(out=lg[:], in0=qk3, scalar=0.125, in1=bT[:, slot, :, :], op0=ALU.mult, op1=ALU.add),
                          qres + ["bT"], ["lg"])
                        A(lambda e: e.activation(out=pe_[b][:], in_=lg[:], func=AF.Exp), ["lg"], [f"pe{b}"])
                    else:
                        A(lambda e: e.activation(out=pe_[b][:], in_=qk3, func=AF.Exp, scale=0.125), qres, [f"pe{b}"])
                    b3 = j % 3
                    V(lambda e: e.tensor_tensor(out=pm[b3][:], in0=pe_[b][:], in1=mskT[mb][:, j % 8, :].unsqueeze(1).broadcast_to([128, H, 128]), op=ALU.mult),
                      [f"pe{b}", f"mskT{mb}"], [f"pm{b3}"])

                def att_pv(j):
                    b = j % 3
                    for hf in range(2):
                        P(lambda e, hf=hf: e.matmul(ps_o[0:65, hf * 512:(hf + 1) * 512], lhsT=dv1[:, j, :], rhs=pm[b][:, 4 * hf:4 * hf + 4, :],
                                                    start=(j == 0), stop=(j == ntile - 1)),
                          [("dv1", j), "dv1one", f"pm{b}"], [("PP2", hf)])

                att_qk(0)
                for j_ in range(ntile):
                    if j_ + 1 < ntile:
                        att_qk(j_ + 1)
                    att_sm(j_)
                    if j_ % 4 == 1 and pend:
                        pend.pop(0)()
                    if j_ >= 1:
                        att_pv(j_ - 1)
                att_pv(ntile - 1)
                while pend:
                    pend.pop(0)()
                A(lambda e: e.activation(out=OT[:], in_=ps_o[0:65, :], func=AF.Copy), [("PP2", 0), ("PP2", 1)], ["OT"])
                for hf in range(2):
                    P(lambda e, hf=hf: e.matmul(PP[3][0:64, hf * 512:(hf + 1) * 512], lhsT=selden[:], rhs=OT[:, hf * 512:(hf + 1) * 512], start=True, stop=True),
                      ["selden", "OT"], [("PP3", hf)])
                rden = lg[0:64, :, :].rearrange("p h q -> p (h q)")
                V(lambda e: e.reciprocal(out=rden, in_=PP[3][0:64, :]), [("PP3", 0), ("PP3", 1)], ["lg"])
                V(lambda e: e.tensor_tensor(out=ydT[:].rearrange("p h q -> p (h q)"), in0=OT[0:64, :], in1=rden, op=ALU.mult), ["OT", "lg"], ["ydT"])
                DM(lambda e: e.dma_start(out=ydT_s[g], in_=ydT[:]), ["ydT"], [("ydT_s", g)], "st_y")

            load_q(0)
            indexer(0)
            for g_ in range(NG):
                pend_ = []
                if g_ + 1 < NG:
                    load_q(g_ + 1)
                    indexer(g_ + 1)
                    pend_ = mm_ops(g_ + 1)
                bisect(g_)
                attention(g_, pend_)
            S.barrier()
            S.emit()

        kvs.close()
        if PHASES >= 3:
          with contextlib.ExitStack() as ph:
            u2T = sbt(ph, "u2T", [128, NG, 8, 128], BF16)
            gates = sbt(ph, "gates", [128, NG, 16], F32)
            with contextlib.ExitStack() as pc:
                wg_sb = sbt(pc, "wg_sb", [128, 8, 2 * D], BF16)
                wro_sb = sbt(pc, "wro_sb", [128, 8, D], BF16)
                wdo_sb = sbt(pc, "wdo_sb", [64, H, D], BF16)
                wo_sb = sbt(pc, "wo_sb", [128, 8, D], BF16)
                wrt_sb = sbt(pc, "wrt_sb", [128, 8, 20], BF16)
                bg_sb = sbt(pc, "bg_sb", [1, 2 * D], BF16)
                ones1 = sbt(pc, "ones1", [1, 128], BF16)
                brt = sbt(pc, "brt", [128, 20], F32)
                xt = [sbt(pc, f"cxt{i}", [128, D], F32) for i in range(2)]
                uTc = [sbt(pc, f"cuT{i}", [128, 8, 128], BF16) for i in range(2)]
                roTc = [sbt(pc, f"croT{i}", [128, 8, 128], BF16) for i in range(2)]
                ydTc = [sbt(pc, f"cydT{i}", [64, H, 128], BF16) for i in range(2)]
                gab = sbt(pc, "gab", [128, 2 * D], BF16)
                t1 = sbt(pc, "t1", [128, D], F32); t2 = sbt(pc, "t2", [128, D], F32)
                mm = sbt(pc, "mm", [128, D], BF16); mT = sbt(pc, "mT", [128, 8, 128], BF16)
                h1b = [sbt(pc, f"h1_{i}", [128, D], F32) for i in range(2)]
                junk = sbt(pc, "cjunk", [128, D], BF16); xs = sbt(pc, "cxs", [128, D], BF16)
                stat = sbt(pc, "cstat", [128, 8], F32)
                rl = sbt(pc, "rl", [128, 20], F32)
                rs = sbt(pc, "rs", [128, 48], F32)
                u2t = sbt(pc, "u2t", [128, 8, 128], BF16)
                pT = pst(pc, "cpT", [128, 8, 128], BF16)
                pgA = pst(pc, "pgA", [128, 2 * D], F32)
                pyr = pst(pc, "pyr", [128, D], F32)
                prt = pst(pc, "prt", [128, 32], F32)

                for k in range(8):
                    DG(lambda e, k=k: e.dma_start(out=wg_sb[:, k, :], in_=w_gate[k * 128:(k + 1) * 128, :]), [], ["wg_sb"], "wC_wg_sb")
                    DG(lambda e, k=k: e.dma_start(out=wro_sb[:, k, :], in_=w_ret_out[k * 128:(k + 1) * 128, :]), [], ["wro_sb"], "wC_wro_sb")
                    DG(lambda e, k=k: e.dma_start(out=wo_sb[:, k, :], in_=w_o[k * 128:(k + 1) * 128, :]), [], ["wo_sb"], "wC_wo_sb")
                    DG(lambda e, k=k: e.dma_start(out=wdo_sb[:, k, :], in_=w_dsa_out[k * 64:(k + 1) * 64, :]), [], ["wdo_sb"], "wC_wdo_sb")
                DG(lambda e: e.dma_start(out=wrt_sb[:], in_=w_rt.rearrange("(k p) n -> p k n", p=128)), [], ["wrt_sb"], "wC_wrt_sb")
                DG(lambda e: e.dma_start(out=bg_sb[:], in_=b_gate), [], ["bg_sb"], "wC_bg_sb")
                DM(lambda e: e.dma_start(out=brt[:], in_=b_rt_rep), [], ["brt"], "c5")
                V(lambda e: e.memset(ones1[:], 1.0), [], ["ones1"])
                for k in range(8):
                    G(lambda e, k=k: e.tensor_tensor(out=wo_sb[:, k, :], in0=wo_sb[:, k, :], in1=g1rep[:], op=ALU.mult), ["wo_sb", "mod"], ["wo_sb"])

                def merge_tile(g):
                    sl = g % 2
                    DM(lambda e: e.dma_start(out=xt[sl][:], in_=xo[g * 128:(g + 1) * 128, :]), [], [f"cxt{sl}"], f"cxt{sl}")
                    DM(lambda e: e.dma_start(out=uTc[sl][:], in_=uT_s[g]), [("uT_s", g)], [f"cuT{sl}"], f"cuT{sl}")
                    DM(lambda e: e.dma_start(out=roTc[sl][:], in_=roT_s[g]), [("roT_s", g)], [f"croT{sl}"], f"croT{sl}")
                    DM(lambda e: e.dma_start(out=ydTc[sl][:], in_=ydT_s[g]), [("ydT_s", g)], [f"cydT{sl}"], f"cydT{sl}")
                    for q4 in range(4):
                        cs_ = slice(q4 * 512, (q4 + 1) * 512)
                        for k in range(8):
                            P(lambda e, k=k, cs_=cs_: e.matmul(pgA[:, cs_], lhsT=uTc[sl][:, k, :], rhs=wg_sb[:, k, cs_], start=(k == 0), stop=False),
                              [f"cuT{sl}", "wg_sb"], [("pgA", q4)])
                        P(lambda e, cs_=cs_: e.matmul(pgA[:, cs_], lhsT=ones1[:], rhs=bg_sb[:, cs_], start=False, stop=True),
                          ["ones1", "bg_sb"], [("pgA", q4)])
                    A(lambda e: e.activation(out=gab[:], in_=pgA[:], func=AF.Sigmoid), [("pgA", i) for i in range(4)], ["gab"])
                    for hf in range(2):
                        cs_ = slice(hf * 512, (hf + 1) * 512)
                        for k in range(8):
                            P(lambda e, k=k, cs_=cs_: e.matmul(pyr[:, cs_], lhsT=roTc[sl][:, k, :], rhs=wro_sb[:, k, cs_], start=(k == 0), stop=(k == 7)),
                              [f"croT{sl}", "wro_sb"], [("pyr", hf)])
                    V(lambda e: e.tensor_tensor(out=t1[:], in0=pyr[:], in1=gab[:, 0:D], op=ALU.mult), [("pyr", 0), ("pyr", 1), "gab"], ["t1"])
                    for hf in range(2):
                        cs_ = slice(hf * 512, (hf + 1) * 512)
                        for h in range(H):
                            P(lambda e, h=h, cs_=cs_: e.matmul(pyr[:, cs_], lhsT=ydTc[sl][:, h, :], rhs=wdo_sb[:, h, cs_], start=(h == 0), stop=(h == H - 1)),
                              [f"cydT{sl}", "wdo_sb"], [("pyr", hf)])
                    V(lambda e: e.tensor_tensor(out=t2[:], in0=pyr[:], in1=gab[:, D:2 * D], op=ALU.mult), [("pyr", 0), ("pyr", 1), "gab"], ["t2"])
                    V(lambda e: e.tensor_tensor(out=mm[:], in0=t1[:], in1=t2[:], op=ALU.add), ["t1", "t2"], ["mm"])
                    for k in range(8):
                        P(lambda e, k=k: e.transpose(out=pT[:, k, :], in_=mm[:, k * 128:(k + 1) * 128], identity=idb[:]), ["mm", "idb"], ["pT"])
                    A(lambda e: e.activation(out=mT[:], in_=pT[:], func=AF.Copy), ["pT"], ["mT"])
                    for hf in range(2):
                        cs_ = slice(hf * 512, (hf + 1) * 512)
                        for k in range(8):
                            P(lambda e, k=k, cs_=cs_: e.matmul(pyr[:, cs_], lhsT=mT[:, k, :], rhs=wo_sb[:, k, cs_], start=(k == 0), stop=(k == 7)),
                              ["mT", "wo_sb"], [("pyr", hf)])
                    h1 = h1b[sl]
                    V(lambda e: e.tensor_tensor(out=h1[:], in0=pyr[:], in1=xt[sl][:], op=ALU.add), [("pyr", 0), ("pyr", 1), f"cxt{sl}"], [f"h1_{sl}"])
                    DM(lambda e: e.dma_start(out=h1_s[g], in_=h1[:]), [f"h1_{sl}"], [("h1_s", g)], f"st_h{sl}")

                def route_tile(g):
                    sl = g % 2
                    h1 = h1b[sl]
                    norm_T(h1[:], f"h1_{sl}", junk[:], xs[:], pT, u2t, stat, G2T, sh2T, "c")
                    G(lambda e: e.tensor_copy(out=u2T[:, g, :, :], in_=u2t[:]), ["cuT"], [("u2T", g)])
                    for k in range(8):
                        P(lambda e, k=k: e.matmul(prt[:, 0:20], lhsT=u2t[:, k, :], rhs=wrt_sb[:, k, :], start=(k == 0), stop=(k == 7)),
                          ["cuT", "wrt_sb"], ["prt"])
                    V(lambda e: e.tensor_tensor(out=rl[:], in0=prt[:, 0:20], in1=brt[:], op=ALU.add), ["prt", "brt"], ["rl"])
                    R = lambda a, b_: rs[:, a:b_]
                    V(lambda e: e.tensor_reduce(out=R(0, 1), in_=rl[:, 0:4], axis=AX.X, op=ALU.max), ["rl"], ["rs"])
                    V(lambda e: e.tensor_scalar(out=R(1, 5), in0=rl[:, 0:4], scalar1=R(0, 1), scalar2=None, op0=ALU.is_ge), ["rl", "rs"], ["rs"])
                    V(lambda e: e.tensor_scalar(out=R(5, 6), in0=R(0, 1), scalar1=-1.0, scalar2=None, op0=ALU.mult), ["rs"], ["rs"])
                    A(lambda e: e.activation(out=R(6, 10), in_=rl[:, 0:4], func=AF.Exp, bias=R(5, 6), accum_out=R(10, 11)), ["rl", "rs"], ["rs2"])
                    V(lambda e: e.reciprocal(out=R(11, 12), in_=R(10, 11)), ["rs2"], ["rs3"])
                    V(lambda e: e.tensor_scalar(out=R(12, 16), in0=rl[:, 4:8], scalar1=R(1, 2), scalar2=None, op0=ALU.mult), ["rl", "rs"], ["rs4"])
                    for gi in range(1, 4):
                        V(lambda e, gi=gi: e.scalar_tensor_tensor(out=R(12, 16), in0=rl[:, 4 + 4 * gi:8 + 4 * gi], scalar=R(1 + gi, 2 + gi), in1=R(12, 16),
                                                                  op0=ALU.mult, op1=ALU.add), ["rl", "rs", "rs4"], ["rs4"])
                    V(lambda e: e.tensor_reduce(out=R(16, 17), in_=R(12, 16), axis=AX.X, op=ALU.max), ["rs4"], ["rs5"])
                    V(lambda e: e.tensor_scalar(out=R(17, 21), in0=R(12, 16), scalar1=R(16, 17), scalar2=None, op0=ALU.is_ge), ["rs4", "rs5"], ["rs6"])
                    V(lambda e: e.scalar_tensor_tensor(out=R(21, 25), in0=R(17, 21), scalar=NEG, in1=R(12, 16), op0=ALU.mult, op1=ALU.add),
                      ["rs6", "rs4"], ["rs7"])
                    V(lambda e: e.tensor_reduce(out=R(25, 26), in_=R(21, 25), axis=AX.X, op=ALU.max), ["rs7"], ["rs8"])
                    V(lambda e: e.tensor_scalar(out=R(26, 30), in0=R(21, 25), scalar1=R(25, 26), scalar2=None, op0=ALU.is_ge), ["rs7", "rs8"], ["rs9"])
                    V(lambda e: e.tensor_tensor(out=R(30, 31), in0=R(16, 17), in1=R(25, 26), op=ALU.subtract), ["rs5", "rs8"], ["rs10"])
                    A(lambda e: e.activation(out=R(31, 32), in_=R(30, 31), func=AF.Sigmoid), ["rs10"], ["rs11"])
                    V(lambda e: e.tensor_scalar(out=R(32, 33), in0=R(31, 32), scalar1=-1.0, scalar2=1.0, op0=ALU.mult, op1=ALU.add), ["rs11"], ["rs12"])
                    V(lambda e: e.tensor_scalar(out=R(33, 37), in0=R(17, 21), scalar1=R(31, 32), scalar2=None, op0=ALU.mult), ["rs6", "rs11"], ["rs13"])
                    V(lambda e: e.scalar_tensor_tensor(out=R(33, 37), in0=R(26, 30), scalar=R(32, 33), in1=R(33, 37), op0=ALU.mult, op1=ALU.add),
                      ["rs9", "rs12", "rs13"], ["rs13"])
                    V(lambda e: e.tensor_scalar(out=R(33, 37), in0=R(33, 37), scalar1=R(11, 12), scalar2=None, op0=ALU.mult), ["rs13", "rs3"], ["rs13"])
                    for gi in range(4):
                        V(lambda e, gi=gi: e.tensor_scalar(out=gates[:, g, 4 * gi:4 * gi + 4], in0=R(33, 37), scalar1=R(1 + gi, 2 + gi), scalar2=None, op0=ALU.mult),
                          ["rs13", "rs"], [("gates", g)])

                merge_tile(0)
                for g_ in range(NG):
                    if g_ + 1 < NG:
                        merge_tile(g_ + 1)
                    route_tile(g_)
                S.barrier()
                S.emit()

            with contextlib.ExitStack() as pd:
                NH = 2 if NG >= 2 else 1
                TPH = NG // NH
                hacc = sbt(pd, "hacc", [128, TPH, D], F32)
                w13 = [sbt(pd, f"w13_{i}", [128, 8, 512], BF16) for i in range(2)]
                w2s = [sbt(pd, f"w2s_{i}", [128, 2, D], BF16) for i in range(2)]
                gfin = sbt(pd, "gfin", [128, D], F32)
                sa = [sbt(pd, f"sa{i}", [128, 256], F32) for i in range(2)]
                hid = [sbt(pd, f"hid{i}", [128, 256], BF16) for i in range(2)]
                hidT = [sbt(pd, f"hidT{i}", [128, 2, 128], BF16) for i in range(2)]
                fjunk = sbt(pd, "fjunk", [128, D], BF16)
                fstat = sbt(pd, "fstat", [128, 4], F32)
                ot = [sbt(pd, f"ot{i}", [128, D], F32) for i in range(2)]
                pab = [pst(pd, f"pab{i}", [128, 512], F32) for i in range(2)]
                phT = [pst(pd, f"phT{i}", [128, 2, 128], BF16) for i in range(2)]
                py = [pst(pd, f"py{i}", [128, D], F32) for i in range(2)]
                DM(lambda e: e.dma_start(out=gfin[:], in_=gfin_rep), [], ["gfin"], "c6")

                def load_expert(ex, ws):
                    for k in range(8):
                        DG(lambda e, k=k: e.dma_start(out=w13[ws][:, k, 0:256], in_=w1[ex, k * 128:(k + 1) * 128, :]), [], [f"w13_{ws}"], f"w13_{ws}")
                        DG(lambda e, k=k: e.dma_start(out=w13[ws][:, k, 256:512], in_=w3[ex, k * 128:(k + 1) * 128, :]), [], [f"w13_{ws}"], f"w13_{ws}")
                    for f in range(2):
                        DG(lambda e, f=f: e.dma_start(out=w2s[ws][:, f, :], in_=w2[ex, f * 128:(f + 1) * 128, :]), [], [f"w2s_{ws}"], f"w2s_{ws}")
                        G(lambda e, f=f: e.tensor_tensor(out=w2s[ws][:, f, :], in0=w2s[ws][:, f, :], in1=g2rep[:], op=ALU.mult),
                          [f"w2s_{ws}", "mod"], [f"w2s_{ws}"])

                def u_ab(u, g, ex, ws):
                    b = u % 2
                    for k in range(8):
                        P(lambda e, k=k: e.matmul(pab[b][:], lhsT=u2T[:, g, k, :], rhs=w13[ws][:, k, :], start=(k == 0), stop=(k == 7)),
                          [("u2T", g), f"w13_{ws}"], [f"pab{b}"])

                def u_act_tr(u, g, ex, ws):
                    b = u % 2
                    A(lambda e: e.activation(out=sa[b][:], in_=pab[b][:, 0:256], func=AF.Silu), [f"pab{b}"], [f"sa{b}"])
                    V(lambda e: e.scalar_tensor_tensor(out=hid[b][:], in0=pab[b][:, 256:512], scalar=gates[:, g, ex:ex + 1], in1=sa[b][:],
                                                       op0=ALU.mult, op1=ALU.mult), [f"pab{b}", ("gates", g), f"sa{b}"], [f"hid{b}"])
                    for f in range(2):
                        P(lambda e, f=f: e.transpose(out=phT[b][:, f, :], in_=hid[b][:, f * 128:(f + 1) * 128], identity=idb[:]), [f"hid{b}", "idb"], [f"phT{b}"])
                    A(lambda e: e.activation(out=hidT[b][:], in_=phT[b][:], func=AF.Copy), [f"phT{b}"], [f"hidT{b}"])

                def u_y(u, g, ex, ws, tl):
                    b = u % 2
                    for hf in range(2):
                        cs_ = slice(hf * 512, (hf + 1) * 512)
                        for f in range(2):
                            P(lambda e, f=f, cs_=cs_: e.matmul(py[b][:, cs_], lhsT=hidT[b][:, f, :], rhs=w2s[ws][:, f, cs_], start=(f == 0), stop=(f == 1)),
                              [f"hidT{b}", f"w2s_{ws}"], [(f"py{b}", hf)])
                    V(lambda e: e.tensor_tensor(out=hacc[:, tl, :], in0=hacc[:, tl, :], in1=py[b][:], op=ALU.add),
                      [("hacc", tl), (f"py{b}", 0), (f"py{b}", 1)], [("hacc", tl)])

                for hh in range(NH):
                    for tl in range(TPH):
                        g = hh * TPH + tl
                        DM(lambda e, tl=tl, g=g: e.dma_start(out=hacc[:, tl, :], in_=h1_s[g]), [("h1_s", g)], [("hacc", tl)], f"ld_h{tl}")
                    units = []
                    for ex in range(16):
                        for tl in range(TPH):
                            units.append((hh * TPH + tl, ex, (hh * 16 + ex) % 2, tl))
                    NU = len(units)
                    load_expert(0, (hh * 16) % 2)
                    loaded = {0}
                    g0_, ex0_, ws0_, tl0_ = units[0]
                    u_ab(0, g0_, ex0_, ws0_)
                    for u in range(NU):
                        g, ex, ws, tl = units[u]
                        deferred = False
                        if u + 1 < NU:
                            g2_, ex2_, ws2_, tl2_ = units[u + 1]
                            if ex2_ in loaded:
                                u_ab(u + 1, g2_, ex2_, ws2_)
                            else:
                                deferred = True
                        u_act_tr(u, g, ex, ws)
                        if u >= 1:
                            g1_, ex1_, ws1_, tl1_ = units[u - 1]
                            u_y(u - 1, g1_, ex1_, ws1_, tl1_)
                        if tl == 0 and ex + 1 < 16:
                            load_expert(ex + 1, (hh * 16 + ex + 1) % 2)
                            loaded.add(ex + 1)
                        if deferred:
                            u_ab(u + 1, g2_, ex2_, ws2_)
                    g1_, ex1_, ws1_, tl1_ = units[NU - 1]
                    u_y(NU - 1, g1_, ex1_, ws1_, tl1_)
                    for tl in range(TPH):
                        g = hh * TPH + tl
                        b = tl % 2
                        A(lambda e, tl=tl: e.activation(out=fjunk[:], in_=hacc[:, tl, :], func=AF.Square, scale=1.0 / 32.0, accum_out=fstat[:, 0:1]),
                          [("hacc", tl)], ["fjunk", "fstat"])
                        A(lambda e: e.activation(out=fstat[:, 1:2], in_=fstat[:, 0:1], func=AF.Ln, bias=epsc[:, 0:1]), ["fstat", "epsc"], ["fstat"])
                        A(lambda e: e.activation(out=fstat[:, 2:3], in_=fstat[:, 1:2], func=AF.Exp, scale=-0.5), ["fstat"], ["fstat"])
                        V(lambda e, tl=tl, b=b: e.scalar_tensor_tensor(out=ot[b][:], in0=hacc[:, tl, :], scalar=fstat[:, 2:3], in1=gfin[:], op0=ALU.mult, op1=ALU.mult),
                          [("hacc", tl), "fstat", "gfin"], [f"ot{b}"])
                        DM(lambda e, g=g, b=b: e.dma_start(out=out[g * 128:(g + 1) * 128, :], in_=ot[b][:]), [f"ot{b}"], [("out", g)], f"st_o{b}")
                S.op("sp", None, reads=[("out", g) for g in range(NG)])
                S.barrier()
                S.emit()
    return nc


build.phases = 3


def _rel_bucket(rel):
    nb = 16
    ret = (rel > 0).astype(np.int64) * nb
    n = np.abs(rel)
    max_exact = nb // 2
    nf = np.maximum(n, 1).astype(np.float32)
    large = max_exact + (np.log(nf / max_exact) / math.log(128 / max_exact) * (nb - max_exact)).astype(np.int32)
    large = np.minimum(large, nb - 1)
    return ret + np.where(n < max_exact, n, large)


def make_inputs(NG, inp):
    NT = 4 * NG
    SB = NT * 128
    f = np.float32
    x = np.asarray(inp["x"], f)
    assert x.shape[1] == SB
    pos = np.arange(SB, dtype=np.float64)
    freqs = 10000.0 ** (-np.arange(0, 64, 2, dtype=np.float64) / 64)
    ang = (pos[:, None].astype(np.float32) * freqs[None, :].astype(np.float32)).astype(np.float32)
    rope_all = np.concatenate([np.cos(ang), np.sin(ang)], axis=1).astype(f)
    gam = np.array(GAM, dtype=np.float64)
    i_ = np.arange(128)
    ci = i_ // 64
    DT = np.zeros((128, H, 128), f)
    for h in range(H):
        same = ci[:, None] == ci[None, :]
        dm = np.where(same, gam[h] ** np.abs(i_[:, None] - i_[None, :]),
                      np.where((ci[:, None] == 1) & (ci[None, :] == 0), gam[h] ** (i_[:, None] - i_[None, :]).clip(0), 0.0))
        DT[:, h, :] = (dm.T * 0.125).astype(f)
    qdec = (gam[None, :] ** (i_[:, None] + 1)).astype(f)
    wtile = (gam[None, :] ** (127 - i_[:, None]) * 0.125).astype(f)
    ident = np.eye(128, dtype=f)
    selden = np.zeros((65, 64), f); selden[64, :] = 1.0
    rb = np.asarray(inp["rel_bias"], f)

    def rep(v, n=128):
        return np.ascontiguousarray(np.broadcast_to(np.asarray(v, f).reshape(1, -1), (n, np.asarray(v).size)))

    def fm(v):
        return np.ascontiguousarray(np.asarray(v, f).reshape(-1, 128).T)

    b_ada = np.asarray(inp["b_ada"], f)[0]
    common = dict(
        w_ada=np.ascontiguousarray(inp["w_ada"][0], dtype=f), b_adaT=fm(b_ada),
        b_g12=np.ascontiguousarray(np.concatenate([rep(b_ada[2 * D:3 * D]), rep(b_ada[5 * D:6 * D])], axis=1)),
        gmixT=fm(inp["norm_mix_g"][0]), gffnT=fm(inp["norm_ffn_g"][0]),
        gfin_rep=rep(inp["norm_final_g"]), gn_rep=rep(inp["ret_gn_g"][0]), kvg_rep=rep(inp["dsa_kv_norm_g"][0]),
        w_in=np.ascontiguousarray(inp["w_in"][0], dtype=f), w_kv=np.ascontiguousarray(inp["w_dsa_kv_up"][0], dtype=f),
        w_ret_out=np.ascontiguousarray(inp["w_ret_out"][0], dtype=f), w_dsa_out=np.ascontiguousarray(inp["w_dsa_out"][0], dtype=f),
        w_gate=np.ascontiguousarray(inp["w_gate"][0], dtype=f), b_gate=np.ascontiguousarray(inp["b_gate"], dtype=f).reshape(1, -1),
        w_o=np.ascontiguousarray(inp["w_o"][0], dtype=f),
        w_rt=np.ascontiguousarray(np.concatenate([inp["w_group_router"][0], inp["w_expert_router"][0]], axis=1), dtype=f),
        b_rt_rep=rep(np.concatenate([inp["b_group_router"][0], inp["b_expert_router"][0]])),
        w1=np.ascontiguousarray(inp["w_exp_gate"][0], dtype=f), w3=np.ascontiguousarray(inp["w_exp_up"][0], dtype=f),
        w2=np.ascontiguousarray(inp["w_exp_down"][0], dtype=f),
        rope_all=rope_all, ident=ident, DT=DT, qdec=qdec, wtile=wtile, selden=selden, c15_rep=rep(rb[15]),
    )
    maps = []
    for c in range(8):
        b, r = c // 4, c % 4
        own = np.array([4 * g + r for g in range(NG)])
        rows = (own[:, None] * 128 + np.arange(128)[None, :]).reshape(-1)
        sel = np.zeros((64, 4), f); sel[:, r] = 1.0
        madd = np.zeros((128, 512), f)
        kt = np.arange(512) // 128
        kc = (np.arange(512) % 128) // 64
        qc = np.arange(128) // 64
        inadm = (kt[None, :] > r) | ((kt[None, :] == r) & (kc[None, :] > qc[:, None]))
        madd[inadm] = NEG
        biasT = np.zeros((5, 128, H, 128), f)
        for s5 in range(5):
            rel = (s5 - 1 - r) * 128 + np.arange(128)[:, None] - np.arange(128)[None, :]
            bk = _rel_bucket(rel)
            biasT[s5] = np.transpose(rb[bk], (0, 2, 1))
        m = dict(common)
        m.update(xb=np.ascontiguousarray(x[b]), xo=np.ascontiguousarray(x[b][rows]),
                 ccol=fm(inp["c"][b]), rope_own=np.ascontiguousarray(rope_all[rows]),
                 sel=sel, madd=madd, biasT=biasT)
        maps.append(m)
    return maps


_NC_CACHE = {}


def kernel(**inputs):
    NG = 32
    if NG not in _NC_CACHE:
        _NC_CACHE[NG] = build(NG)
    nc = _NC_CACHE[NG]
    maps = make_inputs(NG, inputs)
    res = run_bass_kernel_spmd(nc, maps, core_ids=list(range(8)))
    B, SEQ = inputs["x"].shape[0], inputs["x"].shape[1]
    outp = np.zeros((B, SEQ, D), np.float32)
    for c in range(8):
        b, r = c // 4, c % 4
        o = np.asarray(res.results[c]["out"]).reshape(NG, 128, D)
        for g in range(NG):
            t = 4 * g + r
            outp[b, t * 128:(t + 1) * 128, :] = o[g]
    return outp
```
